# Optimizing a Trainium2 kernel written in Bass

```python
import jax, jax.numpy as jnp
from jax import lax
import numpy as np

D_MODEL = 1024
BATCH = 8
SEQ = 4096
DEPTH = 4

HEAD_DIM = 64
GROUP_WIDTH = D_MODEL // 4
GROUP_HEADS = GROUP_WIDTH // HEAD_DIM
MIX_WIDTH = 4 * GROUP_WIDTH
CMP_BLOCK = 32
CMP_STRIDE = 16
CMP_HIDDEN = 128
SLC_BLOCK = 64
N_SEL_BLOCKS = 16
NSA_WINDOW = 512
DILATED_PAIRS = ((128, 1), (512, 4), (2048, 16))
BAND_BLOCK = 128
Q_CHUNK = 128
RWKV_W_LORA = 64
RWKV_A_LORA = 64
RWKV_G_LORA = 128
RWKV_GN_EPS = 64e-5
CONV_K = 31
FFN_CONV_K = 3
D_FF = 11 * D_MODEL // 4
RMS_EPS = 1e-6
LN_EPS = 1e-5
NEG = -1e30
FORCE_SCORE = 1e9
NSA_COLS = GROUP_WIDTH + 6 * HEAD_DIM + 3 * GROUP_HEADS
DIL_COLS = 3 * GROUP_WIDTH
RWKV_COLS = 3 * GROUP_WIDTH + RWKV_W_LORA + RWKV_A_LORA + RWKV_G_LORA
CONV_COLS = 2 * GROUP_WIDTH
IN_COLS = NSA_COLS + DIL_COLS + RWKV_COLS + CONV_COLS

kernel_name = 'hymba_nsa_dilated_rwkv7_conformer_trunk'


def split_cols(x, sizes):
    return jnp.split(x, [int(s) for s in np.cumsum(sizes)[:-1]], axis=-1)


def rmsnorm(x, g):
    xf = x.astype(jnp.float32)
    y = xf * lax.rsqrt(jnp.mean(xf * xf, axis=-1, keepdims=True) + RMS_EPS)
    return (y * g).astype(x.dtype)


def layernorm(x, g, b, eps):
    xf = x.astype(jnp.float32)
    mu = jnp.mean(xf, axis=-1, keepdims=True)
    var = jnp.mean((xf - mu) ** 2, axis=-1, keepdims=True)
    return ((xf - mu) * lax.rsqrt(var + eps) * g + b).astype(x.dtype)


def masked_softmax(s, mask):
    p = jax.nn.softmax(jnp.where(mask, s.astype(jnp.float32), NEG), axis=-1)
    return jnp.where(mask, p, 0.0)


def causal_dwconv(x, w, b):
    K, C = w.shape
    y = lax.conv_general_dilated(x, w[:, None, :].astype(x.dtype), window_strides=(1,),
                                 padding=[(K - 1, 0)], dimension_numbers=('NWC', 'WIO', 'NWC'),
                                 feature_group_count=C)
    return y + b


def banded_causal_attention(q, k, v, window, block):
    lead = q.shape[:-2]
    L, dh = q.shape[-2:]
    k = jnp.broadcast_to(k, lead + k.shape[-2:])
    v = jnp.broadcast_to(v, lead + v.shape[-2:])
    n_blk = -(-L // block)
    Lp = n_blk * block
    n_prev = -(-window // block)
    nz = [(0, 0)] * len(lead)
    qb = jnp.pad(q, nz + [(0, Lp - L), (0, 0)]).reshape(lead + (n_blk, block, dh))
    kp = jnp.pad(k, nz + [(n_prev * block, Lp - L), (0, 0)])
    vp = jnp.pad(v, nz + [(n_prev * block, Lp - L), (0, 0)])

    def windows(a):
        return jnp.concatenate([a[..., o * block:o * block + Lp, :].reshape(lead + (n_blk, block, dh))
                                for o in range(n_prev + 1)], axis=-2)

    kb, vb = windows(kp), windows(vp)
    s = jnp.einsum('...nqd,...nkd->...nqk', qb, kb).astype(jnp.float32) * (dh ** -0.5)
    qpos = jnp.arange(Lp).reshape(n_blk, block, 1)
    kpos = (jnp.arange(n_blk)[:, None, None] * block
            + jnp.arange((n_prev + 1) * block)[None, None, :] - n_prev * block)
    dist = qpos - kpos
    mask = (dist >= 0) & (dist <= window) & (kpos >= 0)
    s = jnp.where(mask, s, NEG)
    m = jnp.max(s, axis=-1, keepdims=True)
    e = jnp.exp(s - m)
    den = jnp.sum(e, axis=-1, keepdims=True)
    lse = (m + jnp.log(den))[..., 0]
    o = jnp.einsum('...nqk,...nkd->...nqd', (e / den).astype(v.dtype), vb)
    return (o.reshape(lead + (Lp, dh))[..., :L, :], lse.reshape(lead + (Lp,))[..., :L])


def compress_blocks(k, pos, w1, w2):
    B, S, dh = k.shape
    half = k.reshape(B, S // CMP_STRIDE, CMP_STRIDE, dh)
    blocks = jnp.concatenate([half[:, :-1], half[:, 1:]], axis=2)
    flat = (blocks + pos).reshape(B, blocks.shape[1], CMP_BLOCK * dh)
    return jax.nn.silu(flat @ w1) @ w2


def selected_block_attention(q, ks, vs, idx):
    B, S, H, dh = q.shape
    n_sel = idx.shape[-1]
    nq = S // Q_CHUNK
    k_blocks = ks.reshape(B, S // SLC_BLOCK, SLC_BLOCK, dh)
    v_blocks = vs.reshape(B, S // SLC_BLOCK, SLC_BLOCK, dh)
    b_idx = jnp.arange(B)[:, None, None]
    offs = jnp.arange(SLC_BLOCK)

    def chunk_fn(args):
        q_c, idx_c, t_c = args
        k_sel = k_blocks[b_idx, idx_c].reshape(B, Q_CHUNK, n_sel * SLC_BLOCK, dh)
        v_sel = v_blocks[b_idx, idx_c].reshape(B, Q_CHUNK, n_sel * SLC_BLOCK, dh)
        kpos = (idx_c[..., None] * SLC_BLOCK + offs).reshape(B, Q_CHUNK, n_sel * SLC_BLOCK)
        mask = (kpos <= t_c[None, :, None])[:, :, None, :]
        s = jnp.einsum('bqhd,bqnd->bqhn', q_c, k_sel) * (dh ** -0.5)
        p = masked_softmax(s, mask)
        return jnp.einsum('bqhn,bqnd->bqhd', p.astype(v_sel.dtype), v_sel)

    xs = (q.reshape(B, nq, Q_CHUNK, H, dh).swapaxes(0, 1),
          idx.reshape(B, nq, Q_CHUNK, n_sel).swapaxes(0, 1),
          jnp.arange(S).reshape(nq, Q_CHUNK))
    o = lax.map(chunk_fn, xs)
    return o.swapaxes(0, 1).reshape(B, S, H, dh)


def nsa_mixer(pa, cmp_pos, ck_w1, ck_w2, cv_w1, cv_w2):
    B, S, _ = pa.shape
    q, kc, vc, ks, vs, kw, vw, gates = split_cols(pa, [GROUP_WIDTH] + [HEAD_DIM] * 6 + [3 * GROUP_HEADS])
    q = q.reshape(B, S, GROUP_HEADS, HEAD_DIM)
    gates = jax.nn.sigmoid(gates.reshape(B, S, GROUP_HEADS, 3))
    t = jnp.arange(S)
    k_cmp = compress_blocks(kc, cmp_pos, ck_w1, ck_w2)
    v_cmp = compress_blocks(vc, cmp_pos, cv_w1, cv_w2)
    nC = k_cmp.shape[1]
    c_end = jnp.arange(nC) * CMP_STRIDE + CMP_BLOCK - 1
    s_cmp = jnp.einsum('bshd,bcd->bhsc', q, k_cmp) * (HEAD_DIM ** -0.5)
    p_cmp = masked_softmax(s_cmp, c_end[None, :] <= t[:, None])
    o_cmp = jnp.einsum('bhsc,bcd->bshd', p_cmp.astype(v_cmp.dtype), v_cmp)
    imp = jnp.pad(jnp.sum(p_cmp, axis=1), ((0, 0), (0, 0), (1, 1)))
    chunk = imp[..., :-1] + imp[..., 1:]
    nS = S // SLC_BLOCK
    blk = chunk.reshape(B, S, nS, SLC_BLOCK // CMP_STRIDE).sum(-1)
    j = jnp.arange(nS)[None, :]
    cur = (t // SLC_BLOCK)[:, None]
    forced = (j == 0) | (j == cur) | (j == cur - 1)
    valid = j * SLC_BLOCK <= t[:, None]
    score = jnp.where(valid, jnp.where(forced, FORCE_SCORE, blk), NEG)
    _, idx = lax.top_k(score, min(N_SEL_BLOCKS, nS))
    o_slc = selected_block_attention(q, ks, vs, idx)
    o_win, _ = banded_causal_attention(q.transpose(0, 2, 1, 3), kw[:, None], vw[:, None], NSA_WINDOW, BAND_BLOCK)
    o_win = o_win.transpose(0, 2, 1, 3)
    out = gates[..., 0:1] * o_cmp + gates[..., 1:2] * o_slc + gates[..., 2:3] * o_win
    return out.reshape(B, S, GROUP_WIDTH)


def dilated_mixer(pb):
    B, S, _ = pb.shape
    H, dh = GROUP_HEADS, HEAD_DIM
    q, k, v = [a.reshape(B, S, H, dh).transpose(0, 2, 1, 3) for a in split_cols(pb, [GROUP_WIDTH] * 3)]
    outs, lses = [], []
    for window, dil in DILATED_PAIRS:
        def by_residue(a):
            return a.reshape(B, H, S // dil, dil, dh).swapaxes(2, 3)
        o, lse = banded_causal_attention(by_residue(q), by_residue(k), by_residue(v), window // dil, BAND_BLOCK)
        outs.append(o.swapaxes(2, 3).reshape(B, H, S, dh))
        lses.append(lse.swapaxes(2, 3).reshape(B, H, S))
    wts = jax.nn.softmax(jnp.stack(lses), axis=0)
    out = jnp.sum(wts[..., None].astype(outs[0].dtype) * jnp.stack(outs), axis=0)
    return out.transpose(0, 2, 1, 3).reshape(B, S, GROUP_WIDTH)


def rwkv7_scan(r, w, k, v, kk, a):
    def step(state, inp):
        r_t, w_t, k_t, v_t, kk_t, a_t = inp
        sa = jnp.einsum('bhvk,bhk->bhv', state, -kk_t)
        state = (state * w_t[:, :, None, :] + sa[..., None] * (kk_t * a_t)[:, :, None, :]
                 + v_t[..., None] * k_t[:, :, None, :])
        return state, jnp.einsum('bhvk,bhk->bhv', state, r_t)

    B, S, H, dh = r.shape
    xs = tuple(jnp.moveaxis(t, 1, 0) for t in (r, w, k, v, kk, a))
    _, y = lax.scan(step, jnp.zeros((B, H, dh, dh), jnp.float32), xs)
    return jnp.moveaxis(y, 0, 1)


def rwkv7_mixer(pc, mu, w0, w_up, a0, a_up, g_up, k_k, k_a, r_k, ln_g, ln_b):
    B, S, _ = pc.shape
    H, dh = GROUP_HEADS, HEAD_DIM
    prev = jnp.pad(pc, ((0, 0), (1, 0), (0, 0)))[:, :-1]
    pc = pc + (prev - pc) * mu
    r, k, v, wd, ad, gd = split_cols(pc, [GROUP_WIDTH] * 3 + [RWKV_W_LORA, RWKV_A_LORA, RWKV_G_LORA])
    log_w = -jax.nn.softplus(-(w0 + jnp.tanh(wd) @ w_up)) - 0.5
    decay = jnp.exp(-jnp.exp(log_w.astype(jnp.float32)))
    a = jax.nn.sigmoid(a0 + ad @ a_up)
    g = jax.nn.sigmoid(gd) @ g_up

    def heads(t):
        return t.reshape(B, S, H, dh).astype(jnp.float32)

    kk = heads(k * k_k)
    kk = kk * lax.rsqrt(jnp.sum(kk * kk, axis=-1, keepdims=True) + 1e-12)
    k = k * (1 + (a - 1) * k_a)
    r_h, k_h, v_h, a_h, w_h = heads(r), heads(k), heads(v), heads(a), heads(decay)
    y = rwkv7_scan(r_h, w_h, k_h, v_h, kk, a_h)
    y = y + jnp.sum(r_h * k_h * r_k, axis=-1, keepdims=True) * v_h
    mu_y = jnp.mean(y, axis=-1, keepdims=True)
    var_y = jnp.mean((y - mu_y) ** 2, axis=-1, keepdims=True)
    y = ((y - mu_y) * lax.rsqrt(var_y + RWKV_GN_EPS)).reshape(B, S, GROUP_WIDTH) * ln_g + ln_b
    return y.astype(pc.dtype) * g


def conformer_conv(pd, dw, dw_b, ln_g, ln_b):
    a, b = split_cols(pd, [GROUP_WIDTH, GROUP_WIDTH])
    h = causal_dwconv(a * jax.nn.sigmoid(b), dw, dw_b)
    return jax.nn.silu(layernorm(h, ln_g, ln_b, LN_EPS))


def conv_ffn(h, up, dw, dw_b, down):
    u = causal_dwconv(h @ up, dw, dw_b)
    gate, val = split_cols(u, [D_FF, D_FF])
    return (jax.nn.silu(gate) * val) @ down


def setup_inputs(seed: int = 0) -> dict:
    key = jax.random.key(seed)
    ks = jax.random.split(key, 32)
    f32 = jnp.float32
    L = DEPTH

    def nrm(k, shape, scale):
        return jax.random.normal(k, shape, f32) * scale

    return {
        'x': nrm(ks[0], (BATCH, SEQ, D_MODEL), 1.0),
        'norm_mix': 1 + nrm(ks[1], (L, D_MODEL), 0.02),
        'w_in': nrm(ks[2], (L, D_MODEL, IN_COLS), D_MODEL ** -0.5),
        'cmp_pos': nrm(ks[3], (L, CMP_BLOCK, HEAD_DIM), 0.1),
        'cmp_k_w1': nrm(ks[4], (L, CMP_BLOCK * HEAD_DIM, CMP_HIDDEN), (CMP_BLOCK * HEAD_DIM) ** -0.5),
        'cmp_k_w2': nrm(ks[5], (L, CMP_HIDDEN, HEAD_DIM), CMP_HIDDEN ** -0.5),
        'cmp_v_w1': nrm(ks[6], (L, CMP_BLOCK * HEAD_DIM, CMP_HIDDEN), (CMP_BLOCK * HEAD_DIM) ** -0.5),
        'cmp_v_w2': nrm(ks[7], (L, CMP_HIDDEN, HEAD_DIM), CMP_HIDDEN ** -0.5),
        'beta_nsa': 1 + nrm(ks[8], (L, GROUP_WIDTH), 0.02),
        'beta_dil': 1 + nrm(ks[9], (L, GROUP_WIDTH), 0.02),
        'rwkv_mu': jax.random.uniform(ks[10], (L, RWKV_COLS), f32),
        'rwkv_w0': nrm(ks[11], (L, GROUP_WIDTH), 0.5),
        'rwkv_w_up': nrm(ks[12], (L, RWKV_W_LORA, GROUP_WIDTH), 0.5 * RWKV_W_LORA ** -0.5),
        'rwkv_a0': nrm(ks[13], (L, GROUP_WIDTH), 0.1),
        'rwkv_a_up': nrm(ks[14], (L, RWKV_A_LORA, GROUP_WIDTH), RWKV_A_LORA ** -0.5),
        'rwkv_g_up': nrm(ks[15], (L, RWKV_G_LORA, GROUP_WIDTH), RWKV_G_LORA ** -0.5),
        'rwkv_k_k': 0.85 + nrm(ks[16], (L, GROUP_WIDTH), 0.02),
        'rwkv_k_a': 1 + nrm(ks[17], (L, GROUP_WIDTH), 0.02),
        'rwkv_r_k': nrm(ks[18], (L, GROUP_HEADS, HEAD_DIM), 0.1),
        'rwkv_ln_g': 1 + nrm(ks[19], (L, GROUP_WIDTH), 0.02),
        'rwkv_ln_b': nrm(ks[20], (L, GROUP_WIDTH), 0.02),
        'conv_dw': nrm(ks[21], (L, CONV_K, GROUP_WIDTH), CONV_K ** -0.5),
        'conv_dw_b': nrm(ks[22], (L, GROUP_WIDTH), 0.02),
        'conv_ln_g': 1 + nrm(ks[23], (L, GROUP_WIDTH), 0.02),
        'conv_ln_b': nrm(ks[24], (L, GROUP_WIDTH), 0.02),
        'w_out': nrm(ks[25], (L, MIX_WIDTH, D_MODEL), MIX_WIDTH ** -0.5),
        'norm_ffn': 1 + nrm(ks[26], (L, D_MODEL), 0.02),
        'ffn_up': nrm(ks[27], (L, D_MODEL, 2 * D_FF), D_MODEL ** -0.5),
        'ffn_dw': nrm(ks[28], (L, FFN_CONV_K, 2 * D_FF), FFN_CONV_K ** -0.5),
        'ffn_dw_b': nrm(ks[29], (L, 2 * D_FF), 0.02),
        'ffn_down': nrm(ks[30], (L, D_FF, D_MODEL), D_FF ** -0.5),
        'norm_final': 1 + nrm(ks[31], (D_MODEL,), 0.02),
    }


def reference(x, norm_mix, w_in, cmp_pos, cmp_k_w1, cmp_k_w2, cmp_v_w1, cmp_v_w2, beta_nsa, beta_dil,
              rwkv_mu, rwkv_w0, rwkv_w_up, rwkv_a0, rwkv_a_up, rwkv_g_up, rwkv_k_k, rwkv_k_a, rwkv_r_k,
              rwkv_ln_g, rwkv_ln_b, conv_dw, conv_dw_b, conv_ln_g, conv_ln_b, w_out, norm_ffn,
              ffn_up, ffn_dw, ffn_dw_b, ffn_down, norm_final):
    for l in range(DEPTH):
        h = rmsnorm(x, norm_mix[l])
        proj = h @ w_in[l]
        pa, pb, pc, pd = split_cols(proj, [NSA_COLS, DIL_COLS, RWKV_COLS, CONV_COLS])
        y_a = rmsnorm(nsa_mixer(pa, cmp_pos[l], cmp_k_w1[l], cmp_k_w2[l], cmp_v_w1[l], cmp_v_w2[l]), beta_nsa[l])
        y_b = rmsnorm(dilated_mixer(pb), beta_dil[l])
        y_c = rwkv7_mixer(pc, rwkv_mu[l], rwkv_w0[l], rwkv_w_up[l], rwkv_a0[l], rwkv_a_up[l], rwkv_g_up[l],
                          rwkv_k_k[l], rwkv_k_a[l], rwkv_r_k[l], rwkv_ln_g[l], rwkv_ln_b[l])
        y_d = conformer_conv(pd, conv_dw[l], conv_dw_b[l], conv_ln_g[l], conv_ln_b[l])
        x = x + jnp.concatenate([y_a, y_b, y_c, y_d], axis=-1) @ w_out[l]
        h = rmsnorm(x, norm_ffn[l])
        x = x + conv_ffn(h, ffn_up[l], ffn_dw[l], ffn_dw_b[l], ffn_down[l])
    return rmsnorm(x, norm_final)
```

```python
import numpy as np
import ml_dtypes
from contextlib import ExitStack
import concourse.bass as bass
import concourse.mybir as mybir
from concourse.bass_utils import run_bass_kernel_spmd

F32 = mybir.dt.float32
BF16 = mybir.dt.bfloat16
I32 = mybir.dt.int32
AF = mybir.ActivationFunctionType
ALU = mybir.AluOpType
AX = mybir.AxisListType

S = 4096
D = 1024
NT = S // 128
NBLK = S // 512
DEPTH = 4
DFF = 2816
IN_COLS = 2956
WRITE_NAMES = ("out", "accum_out", "ap")


class SemT:
    def __init__(self, handle):
        self.h = handle
        self.count = 0


class Rec:
    __slots__ = ("lw", "rd")

    def __init__(self):
        self.lw = None
        self.rd = []


class Tile:
    def __init__(self, t, name):
        self.t = t
        self.name = name
        self.regs = {None: Rec()}
        self.excl = False

    def recs_dep(self, key):
        if key is None:
            return list(self.regs.values())
        if key not in self.regs:
            self.regs[key] = Rec()
        return [self.regs[key], self.regs[None]]

    def recs_upd(self, key, is_write):
        if key is None:
            return list(self.regs.values()) if is_write else [self.regs[None]]
        if key not in self.regs:
            self.regs[key] = Rec()
        return [self.regs[key]]

    def __getitem__(self, idx):
        return Ref(self, None, self.t[idx])

    def k(self, key):
        return KeyView(self, key)


class KeyView:
    def __init__(self, tile, key):
        self.tile = tile
        self.key = key

    def __getitem__(self, idx):
        return Ref(self.tile, self.key, self.tile.t[idx])


class Ref:
    def __init__(self, tile, key, ap):
        self.tile = tile
        self.key = key
        self.ap = ap

    def with_ap(self, ap):
        return Ref(self.tile, self.key, ap)


class Eng:
    def __init__(self, kb, name, raw, sem, is_pe=False):
        self.kb = kb
        self.name = name
        self.raw = raw
        self.sem = sem
        self.waited = {}
        self.is_pe = is_pe

    def __getattr__(self, opname):
        def call(**kw):
            reads, writes = [], []
            kw2 = {}
            for n, v in kw.items():
                if isinstance(v, Ref):
                    (writes if n in WRITE_NAMES else reads).append(v)
                    kw2[n] = v.ap
                else:
                    kw2[n] = v
            return self.kb.emit(self, lambda: getattr(self.raw, opname)(**kw2), reads, writes)
        return call


class KB:
    def __init__(self):
        self.nc = bass.Bass("TRN2", target_bir_lowering=False)
        nc = self.nc
        self.es = ExitStack()
        mk = lambda n: SemT(self.es.enter_context(nc.semaphore(n)))
        self.pe = Eng(self, "pe", nc.tensor, mk("s_pe"), is_pe=True)
        self.act = Eng(self, "act", nc.scalar, mk("s_act"))
        self.dve = Eng(self, "dve", nc.vector, mk("s_dve"))
        self.pool = Eng(self, "pool", nc.gpsimd, mk("s_pool"))
        self.sp = Eng(self, "sp", nc.sync, mk("s_sp"))
        self.ring = [mk(f"s_dma{i}") for i in range(24)]
        self.ring_i = 0
        self.pring = [mk(f"s_pdma{i}") for i in range(8)]
        self.pring_i = 0
        self.n_inst = 0
        self.out_deps = []

    def dram(self, name, shape, dtype, kind="Internal"):
        return Tile(self.nc.dram_tensor(name, list(shape), dtype, kind=kind).ap(), name)

    def sb(self, stack, name, shape, dtype):
        self.n_alloc = getattr(self, "n_alloc", 0) + 1
        name = f"{name}_{self.n_alloc}"
        return Tile(stack.enter_context(self.nc.sbuf_tensor(name, list(shape), dtype)), name)

    def psum(self, stack, name, shape, dtype):
        self.n_alloc = getattr(self, "n_alloc", 0) + 1
        name = f"{name}_{self.n_alloc}"
        t = Tile(stack.enter_context(self.nc.psum_tensor(name, list(shape), dtype)), name)
        t.excl = True
        return t

    def _wait(self, eng, deps):
        best = {}
        for (st, v) in deps:
            if v is None:
                continue
            if id(st) not in best or best[id(st)][1] < v:
                best[id(st)] = (st, v)
        for st, v in best.values():
            if st is eng.sem and eng.is_pe:
                continue
            if eng.waited.get(id(st), 0) >= v:
                continue
            eng.raw.wait_ge(st.h, v)
            eng.waited[id(st)] = v

    def _collect(self, reads, writes):
        deps = []
        for r in reads:
            for rec in r.tile.recs_dep(r.key):
                if rec.lw is not None:
                    deps.append(rec.lw)
                if r.tile.excl:
                    deps.extend(rec.rd)
        for w in writes:
            for rec in w.tile.recs_dep(w.key):
                if rec.lw is not None:
                    deps.append(rec.lw)
                deps.extend(rec.rd)
        return deps

    def _update(self, reads, writes, tag):
        for r in reads:
            for rec in r.tile.recs_upd(r.key, False):
                rec.rd.append(tag)
                if len(rec.rd) > 48:
                    best = {}
                    for st, v in rec.rd:
                        if id(st) not in best or best[id(st)][1] < v:
                            best[id(st)] = (st, v)
                    rec.rd = list(best.values())
        for w in writes:
            for rec in w.tile.recs_upd(w.key, True):
                rec.lw = tag
                rec.rd = []

    def emit(self, eng, fn, reads, writes):
        deps = self._collect(reads, writes)
        self._wait(eng, deps)
        inst = fn()
        eng.sem.count += 1
        inst.then_inc(eng.sem.h, 1)
        self._update(reads, writes, (eng.sem, eng.sem.count))
        self.n_inst += 1
        return inst

    def dma(self, out, in_, via_pool=False, **kw):
        eng = self.pool if via_pool else self.sp
        if via_pool:
            st = self.pring[self.pring_i % len(self.pring)]
            self.pring_i += 1
        else:
            st = self.ring[self.ring_i % len(self.ring)]
            self.ring_i += 1
        deps = self._collect([in_], [out])
        deps.append((st, st.count))
        self._wait(eng, deps)
        inst = eng.raw.dma_start(out=out.ap, in_=in_.ap, **kw)
        st.count += 16
        inst.then_inc(st.h, 16)
        tag = (st, st.count)
        self._update([in_], [out], tag)
        self.n_inst += 1
        return tag

    def barrier(self):
        deps = [(st, st.count) for st in self.ring + self.pring]
        deps += [(e.sem, e.sem.count) for e in (self.pe, self.act, self.dve, self.pool)]
        deps = [d for d in deps if d[1] > 0]
        for e in (self.pe, self.act, self.dve, self.pool, self.sp):
            self._wait(e, [d for d in deps if not (d[0] is e.sem)])

    def finish(self, out_tiles):
        deps = [(st, st.count) for st in self.ring + self.pring]
        deps += [(e.sem, e.sem.count) for e in (self.pe, self.act, self.dve, self.pool)]
        self._wait(self.sp, [d for d in deps if d[1] > 0])
        self.es.close()


def _bf(a):
    return np.ascontiguousarray(a.astype(np.float32)).astype(ml_dtypes.bfloat16)


def make_consts():
    c = {}
    c["ident_bf"] = _bf(np.eye(128))
    c["ident_f"] = np.eye(128, dtype=np.float32)
    sp = np.arange(128)[:, None]
    tq = np.arange(512)[None, :]
    c["cms"] = _bf(np.stack([(128 * j + sp <= tq) for j in range(4)], axis=1))
    tq1 = np.arange(128)[None, :]
    c["cmw"] = _bf(np.stack([(sp <= tq1), (sp >= tq1)], axis=1))
    dm = []
    for delta in range(-3, 17):
        d = tq - sp + 128 * delta
        cnt = ((d >= 0) & (d <= 128)).astype(np.float32)
        cnt += ((d >= 0) & (d <= 512) & (d % 4 == 0))
        cnt += ((d >= 0) & (d <= 2048) & (d % 16 == 0))
        dm.append(cnt)
    c["dm"] = _bf(np.stack(dm, axis=1))
    j = np.arange(64)[:, None, None]
    kt = np.arange(32)[None, :, None]
    s = np.arange(128)[None, None, :]
    c["ek"] = _bf(((128 * kt + s) // 64 == j))
    mc = np.zeros((128, 16, 512), np.float32)
    for b in range(8):
        for ct in range(2):
            mc[:, b * 2 + ct, :] = (16 * (128 * ct + sp) + 31 <= 512 * b + tq)
    c["mc"] = _bf(mc)
    c["gc"] = (16 * np.arange(256)[None, :] + 31 - np.arange(128)[:, None]).astype(np.float32)
    c["d0"] = (64 * np.arange(64)[None, :] - np.arange(128)[:, None]).astype(np.float32)
    i = np.arange(128)[:, None]
    t = np.arange(128)[None, :]
    c["tri"] = ((i // 64 == t // 64) & (i <= t)).astype(np.float32)
    i6 = np.arange(64)[:, None]
    t6 = np.arange(64)[None, :]
    c["mstrict"] = np.tile((i6 < t6).astype(np.float32)[:, None, :], (1, 8, 1))
    c["mincl"] = np.tile((i6 <= t6).astype(np.float32)[:, None, :], (1, 8, 1))
    c["mstrictT"] = np.tile((i6 > t6).astype(np.float32)[:, None, :], (1, 8, 1))
    return c


CONST_DT = {"ident_bf": BF16, "cms": BF16, "cmw": BF16, "dm": BF16, "ek": BF16, "mc": BF16}

PARAM_SHAPES = {
    'norm_mix': (DEPTH, D), 'w_in': (DEPTH, D, IN_COLS), 'cmp_pos': (DEPTH, 32, 64),
    'cmp_k_w1': (DEPTH, 2048, 128), 'cmp_k_w2': (DEPTH, 128, 64), 'cmp_v_w1': (DEPTH, 2048, 128),
    'cmp_v_w2': (DEPTH, 128, 64), 'beta_nsa': (DEPTH, 256), 'beta_dil': (DEPTH, 256),
    'rwkv_mu': (DEPTH, 1024), 'rwkv_w0': (DEPTH, 256), 'rwkv_w_up': (DEPTH, 64, 256),
    'rwkv_a0': (DEPTH, 256), 'rwkv_a_up': (DEPTH, 64, 256), 'rwkv_g_up': (DEPTH, 128, 256),
    'rwkv_k_k': (DEPTH, 256), 'rwkv_k_a': (DEPTH, 256), 'rwkv_r_k': (DEPTH, 4, 64),
    'rwkv_ln_g': (DEPTH, 256), 'rwkv_ln_b': (DEPTH, 256), 'conv_dw': (DEPTH, 31, 256),
    'conv_dw_b': (DEPTH, 256), 'conv_ln_g': (DEPTH, 256), 'conv_ln_b': (DEPTH, 256),
    'w_out': (DEPTH, D, D), 'norm_ffn': (DEPTH, D), 'ffn_up': (DEPTH, D, 2 * DFF),
    'ffn_dw': (DEPTH, 3, 2 * DFF), 'ffn_dw_b': (DEPTH, 2 * DFF), 'ffn_down': (DEPTH, DFF, D),
    'norm_final': (D,),
}


class Ctx:
    pass


def bcast_rows(ap_1d_row, nparts):
    return ap_1d_row.partition_broadcast(nparts)


def load_bcast(kb, dst_tile, src_dram_tile, row_ap, n):
    kb.dma(out=dst_tile[:, 0:n], in_=Ref(src_dram_tile, None, row_ap.partition_broadcast(128)))


FM_CHUNKS = [
    (0, 128, "qT", 0), (128, 128, "qT", 128), (256, 128, "kcvcT", 0), (384, 64, "ksT", 0), (512, 64, "kwT", 0),
    (652, 128, "dqT", 0), (780, 128, "dqT", 128), (908, 128, "dkT", 0), (1036, 128, "dkT", 128),
    (2188, 128, "loraT", 0), (2316, 128, "loraT", 128),
    (2444, 128, "convT", 0), (2572, 128, "convT", 128), (2700, 128, "convT", 256), (2828, 128, "convT", 384),
]
TM_GROUPS = [(448, 204, "tmA", 0), (1164, 512, "tmB", 0), (1676, 512, "tmB", 512)]


def load_weight_bf16(kb, dst, dram_w, rows0, nk, cols0, ncols):
    for k in range(nk):
        c = 0
        while c < ncols:
            w = min(1024, ncols - c)
            kb.dma(out=dst[:, k, c:c + w],
                   in_=dram_w[rows0 + k * 128: rows0 + (k + 1) * 128, cols0 + c: cols0 + c + w], via_pool=True)
            c += w


def rms_rstd(kb, ssq_ref, out_ref, n, eps, tmp_ref):
    kb.dve.tensor_scalar(out=tmp_ref, in0=ssq_ref, scalar1=1.0 / n, scalar2=eps, op0=ALU.mult, op1=ALU.add)
    kb.act.activation(out=tmp_ref, in_=tmp_ref, func=AF.Sqrt)
    kb.dve.reciprocal(out=out_ref, in_=tmp_ref)


def phase_a(kb, cx, l):
    P = cx.P
    scr = cx.scr
    with ExitStack() as st:
        w_sb = kb.sb(st, "wA", [128, 8, IN_COLS], BF16)
        for k in range(8):
            c = 0
            while c < IN_COLS:
                w = min(1024, IN_COLS - c)
                kb.dma(out=w_sb[:, k, c:c + w], in_=P["w_in"][l, k * 128:(k + 1) * 128, c:c + w], via_pool=True)
                c += w
        gbc = kb.sb(st, "gbcA", [128, D], F32)
        kb.dma(out=gbc[:, :], in_=Ref(P["norm_mix"], None, P["norm_mix"].t[l:l + 1, :].partition_broadcast(128)))
        xt = [kb.sb(st, f"xtA{i}", [128, 4, D], F32) for i in range(2)]
        hbf = [kb.sb(st, f"hbfA{i}", [128, D], BF16) for i in range(2)]
        junk = kb.sb(st, "junkA", [128, D], BF16)
        hT = [kb.sb(st, f"hTA{i}", [128, 8, 512], BF16) for i in range(2)]
        small = kb.sb(st, "smallA", [128, 16], F32)
        stg_bf = [kb.sb(st, f"stgbA{i}", [128, 512], BF16) for i in range(3)]
        stg_f = [kb.sb(st, f"stgfA{i}", [128, 512], F32) for i in range(3)]
        tp = [kb.psum(st, f"tpA{i}", [128, 8, 128], BF16) for i in range(2)]
        acc = [kb.psum(st, f"accA{i}", [128, 512], F32) for i in range(4)]
        n_acc = 0
        n_stg = 0
        for b in range(NBLK):
            x_t = xt[b % 2]
            kb.dma(out=x_t[:, :, :], in_=cx.xres[b * 512:(b + 1) * 512, :].with_ap(
                cx.xres.t[b * 512:(b + 1) * 512, :].rearrange("(j p) d -> p j d", p=128)))
            h_T = hT[b % 2]
            for j in range(4):
                hb = hbf[j % 2]
                ssq = small[:, j:j + 1]
                kb.act.activation(out=junk[:, :], in_=x_t[:, j, :], func=AF.Square, accum_out=ssq)
                rms_rstd(kb, ssq, small[:, 4 + j:5 + j], D, 1e-6, small[:, 8 + j:9 + j])
                kb.dve.scalar_tensor_tensor(out=hb[:, :], in0=x_t[:, j, :], scalar=small[:, 4 + j:5 + j], in1=gbc[:, :],
                                            op0=ALU.mult, op1=ALU.mult)
                t_p = tp[j % 2]
                for kc in range(8):
                    kb.pe.transpose(out=t_p[:, kc, :], in_=hb[:, kc * 128:(kc + 1) * 128], identity=cx.ident_bf[:, :])
                kb.act.copy(out=h_T[:, :, j * 128:(j + 1) * 128], in_=t_p[:, :, :])
            for (c0, cw, dst, r0) in FM_CHUNKS:
                a = acc[n_acc % 4]
                n_acc += 1
                for kc in range(8):
                    kb.pe.matmul(out=a[0:cw, :], lhsT=w_sb[:, kc, c0:c0 + cw], rhs=h_T[:, kc, :], start=(kc == 0), stop=(kc == 7))
                dt_f32 = dst in ("loraT", "convT")
                sg = (stg_f if dt_f32 else stg_bf)[n_stg % 3]
                if n_stg % 2 == 0:
                    kb.dve.tensor_copy(out=sg[0:cw, :], in_=a[0:cw, :])
                else:
                    kb.act.copy(out=sg[0:cw, :], in_=a[0:cw, :])
                n_stg += 1
                kb.dma(out=getattr(scr, dst).k(b)[r0:r0 + cw, b * 512:(b + 1) * 512], in_=sg[0:cw, :])
            for j in range(4):
                for (c0, cw, dst, d0) in TM_GROUPS:
                    a = acc[n_acc % 4]
                    n_acc += 1
                    for kc in range(8):
                        kb.pe.matmul(out=a[:, 0:cw], lhsT=h_T[:, kc, j * 128:(j + 1) * 128], rhs=w_sb[:, kc, c0:c0 + cw],
                                     start=(kc == 0), stop=(kc == 7))
                    sg = stg_f[n_stg % 3]
                    if n_stg % 2 == 0:
                        kb.dve.tensor_copy(out=sg[:, 0:cw], in_=a[:, 0:cw])
                    else:
                        kb.act.copy(out=sg[:, 0:cw], in_=a[:, 0:cw])
                    n_stg += 1
                    r = b * 512 + j * 128
                    kb.dma(out=getattr(scr, dst).k(b)[r:r + 128, d0:d0 + cw], in_=sg[:, 0:cw])
        kb.barrier()


def phase_conv(kb, cx, l):
    P = cx.P
    scr = cx.scr
    with ExitStack() as st:
        glu = [kb.sb(st, f"gluD{i}", [128, 30 + S], F32) for i in range(2)]
        accs = [kb.sb(st, f"accD{i}", [128, S], F32) for i in range(2)]
        bt = kb.sb(st, "btD", [128, S], F32)
        wdw = kb.sb(st, "wdwD", [128, 2, 32], F32)
        lng = kb.sb(st, "lngD", [128, 256], F32)
        lnb = kb.sb(st, "lnbD", [128, 256], F32)
        small = kb.sb(st, "smallD", [128, 16], F32)
        stats = kb.sb(st, "statsD", [128, 8], F32)
        xn = [kb.sb(st, f"xnD{i}", [128, 256], F32) for i in range(2)]
        tps = [kb.psum(st, f"tpD{i}", [128, 512], F32) for i in range(2)]
        kb.dma(out=lng[:, :], in_=Ref(P["conv_ln_g"], None, P["conv_ln_g"].t[l:l + 1, :].partition_broadcast(128)))
        kb.dma(out=lnb[:, :], in_=Ref(P["conv_ln_b"], None, P["conv_ln_b"].t[l:l + 1, :].partition_broadcast(128)))
        for ci in range(2):
            with kb.nc.allow_non_contiguous_dma(reason="tiny transposed conv weights"):
                kb.dma(out=wdw[:, ci, 0:31], in_=Ref(P["conv_dw"], None,
                       P["conv_dw"].t[l, :, ci * 128:(ci + 1) * 128].rearrange("k c -> c k")))
                kb.dma(out=wdw[:, ci, 31:32], in_=Ref(P["conv_dw_b"], None,
                       P["conv_dw_b"].t[l:l + 1, ci * 128:(ci + 1) * 128].rearrange("o c -> c o")))
            g = glu[ci]
            kb.pool.memset(ap=g[:, 0:30], constant=0.0)
            kb.dma(out=g[:, 30:30 + S], in_=scr.convT[ci * 128:(ci + 1) * 128, :])
            kb.dma(out=bt[:, :], in_=scr.convT[256 + ci * 128:256 + (ci + 1) * 128, :])
            kb.act.activation(out=bt[:, :], in_=bt[:, :], func=AF.Sigmoid)
            kb.pool.tensor_tensor(out=g[:, 30:30 + S], in0=g[:, 30:30 + S], in1=bt[:, :], op=ALU.mult)
            a = accs[ci]
            for h0 in range(0, S, 2048):
                kb.dve.tensor_scalar(out=a[:, h0:h0 + 2048], in0=g[:, 30 + h0:30 + h0 + 2048], scalar1=wdw[:, ci, 30:31],
                                     scalar2=wdw[:, ci, 31:32], op0=ALU.mult, op1=ALU.add)
                for j in range(30):
                    kb.dve.scalar_tensor_tensor(out=a[:, h0:h0 + 2048], in0=g[:, j + h0:j + h0 + 2048], scalar=wdw[:, ci, j:j + 1],
                                                in1=a[:, h0:h0 + 2048], op0=ALU.mult, op1=ALU.add)
        for i in range(NT):
            tp = tps[i % 2]
            for ci in range(2):
                kb.pe.transpose(out=tp[:, ci * 128:(ci + 1) * 128], in_=accs[ci][:, i * 128:(i + 1) * 128], identity=cx.ident_f[:, :])
            x_n = xn[i % 2]
            kb.dve.bn_stats(out=stats[:, 0:6], in_=tp[:, 0:256])
            kb.dve.bn_aggr(out=small[:, 0:2], in_=stats[:, 0:6])
            kb.dve.tensor_scalar(out=small[:, 2:3], in0=small[:, 1:2], scalar1=1e-5, scalar2=None, op0=ALU.add)
            kb.act.activation(out=small[:, 2:3], in_=small[:, 2:3], func=AF.Sqrt)
            kb.dve.reciprocal(out=small[:, 3:4], in_=small[:, 2:3])
            kb.dve.tensor_scalar(out=x_n[:, :], in0=tp[:, 0:256], scalar1=small[:, 0:1], scalar2=small[:, 3:4],
                                 op0=ALU.subtract, op1=ALU.mult)
            kb.pool.tensor_tensor(out=x_n[:, :], in0=x_n[:, :], in1=lng[:, :], op=ALU.mult)
            kb.pool.tensor_tensor(out=x_n[:, :], in0=x_n[:, :], in1=lnb[:, :], op=ALU.add)
            kb.act.activation(out=x_n[:, :], in_=x_n[:, :], func=AF.Silu)
            kb.dma(out=scr.ymix.k(("d", i))[i * 128:(i + 1) * 128, 768:1024], in_=x_n[:, :])
        kb.barrier()


def load_w_bf16(kb, dst, wtile, l, nk, ncols):
    for k in range(nk):
        c = 0
        while c < ncols:
            w = min(1024, ncols - c)
            kb.dma(out=dst[:, k, c:c + w], in_=wtile[l, k * 128:(k + 1) * 128, c:c + w], via_pool=True)
            c += w


def phase_b(kb, cx, l):
    P = cx.P
    scr = cx.scr
    with ExitStack() as st:
        wo = kb.sb(st, "woB", [128, 8, D], BF16)
        load_w_bf16(kb, wo, P["w_out"], l, 8, D)
        yt = [kb.sb(st, f"ytB{i}", [128, D], F32) for i in range(2)]
        xt = [kb.sb(st, f"xtB{i}", [128, D], F32) for i in range(2)]
        ybf = [kb.sb(st, f"ybfB{i}", [128, D], BF16) for i in range(2)]
        yT = [kb.sb(st, f"yTB{i}", [128, 8, 128], BF16) for i in range(2)]
        tp = [kb.psum(st, f"tpB{i}", [128, 8, 128], BF16) for i in range(2)]
        acc = [kb.psum(st, f"accB{i}", [128, 512], F32) for i in range(4)]
        na = 0
        for i in range(NT):
            y_t, x_t, y_b, y_T, t_p = yt[i % 2], xt[i % 2], ybf[i % 2], yT[i % 2], tp[i % 2]
            kb.dma(out=y_t[:, :], in_=scr.ymix[i * 128:(i + 1) * 128, :])
            kb.dma(out=x_t[:, :], in_=cx.xres[i * 128:(i + 1) * 128, :])
            kb.pool.tensor_copy(out=y_b[:, :], in_=y_t[:, :])
            for kc in range(8):
                kb.pe.transpose(out=t_p[:, kc, :], in_=y_b[:, kc * 128:(kc + 1) * 128], identity=cx.ident_bf[:, :])
            kb.act.copy(out=y_T[:, :, :], in_=t_p[:, :, :])
            for c0 in (0, 512):
                a = acc[na % 4]
                na += 1
                for kc in range(8):
                    kb.pe.matmul(out=a[:, :], lhsT=y_T[:, kc, :], rhs=wo[:, kc, c0:c0 + 512], start=(kc == 0), stop=(kc == 7))
                kb.dve.tensor_tensor(out=x_t[:, c0:c0 + 512], in0=x_t[:, c0:c0 + 512], in1=a[:, :], op=ALU.add)
            kb.dma(out=cx.xres[i * 128:(i + 1) * 128, :], in_=x_t[:, :])
        kb.barrier()


def phase_c(kb, cx, l):
    P = cx.P
    TB = 256
    with ExitStack() as st:
        wu = kb.sb(st, "wuC", [128, 8, 2 * DFF], BF16)
        wd = kb.sb(st, "wdC", [128, 22, D], BF16)
        load_w_bf16(kb, wu, P["ffn_up"], l, 8, 2 * DFF)
        load_w_bf16(kb, wd, P["ffn_down"], l, 22, D)
        gbc = kb.sb(st, "gbcC", [128, D], F32)
        kb.dma(out=gbc[:, :], in_=Ref(P["norm_ffn"], None, P["norm_ffn"].t[l:l + 1, :].partition_broadcast(128)))
        cw = kb.sb(st, "cwC", [128, 44, 4], F32)
        with kb.nc.allow_non_contiguous_dma(reason="tiny transposed conv weights"):
            for j in range(3):
                kb.dma(out=cw[:, :, j:j + 1], in_=Ref(P["ffn_dw"], None,
                       P["ffn_dw"].t[l, j:j + 1, :].rearrange("o (c p) -> p c o", p=128)))
            kb.dma(out=cw[:, :, 3:4], in_=Ref(P["ffn_dw_b"], None,
                   P["ffn_dw_b"].t[l:l + 1, :].rearrange("o (c p) -> p c o", p=128)))
        carry = kb.sb(st, "carryC", [128, 44, 2], F32)
        kb.pool.memset(ap=carry[:, :, :], constant=0.0)
        xt = [kb.sb(st, f"xtC{i}", [128, 2, D], F32) for i in range(2)]
        hbf = [kb.sb(st, f"hbfC{i}", [128, D], BF16) for i in range(2)]
        junk = kb.sb(st, "junkC", [128, D], BF16)
        hT = [kb.sb(st, f"hTC{i}", [128, 8, TB], BF16) for i in range(2)]
        small = kb.sb(st, "smallC", [128, 16], F32)
        G = [kb.sb(st, f"GC{i}", [128, 22, TB], BF16) for i in range(2)]
        ub = [kb.sb(st, f"ubC{i}", [128, TB + 2], F32) for i in range(4)]
        ac = [kb.sb(st, f"acC{i}", [128, TB], F32) for i in range(4)]
        tp = [kb.psum(st, f"tpC{i}", [128, 8, 128], BF16) for i in range(2)]
        ups = [kb.psum(st, f"upC{i}", [128, 512], F32) for i in range(4)]
        dps = [kb.psum(st, f"dpC{i}", [128, 512], F32) for i in range(2)]
        nu = 0
        nd = 0
        for b in range(S // TB):
            x_t, h_T, Gb = xt[b % 2], hT[b % 2], G[b % 2]
            kb.dma(out=x_t[:, :, :], in_=cx.xres[b * TB:(b + 1) * TB, :].with_ap(
                cx.xres.t[b * TB:(b + 1) * TB, :].rearrange("(j p) d -> p j d", p=128)))
            for j in range(2):
                hb = hbf[j]
                kb.act.activation(out=junk[:, :], in_=x_t[:, j, :], func=AF.Square, accum_out=small[:, j:j + 1])
                rms_rstd(kb, small[:, j:j + 1], small[:, 4 + j:5 + j], D, 1e-6, small[:, 8 + j:9 + j])
                kb.dve.scalar_tensor_tensor(out=hb[:, :], in0=x_t[:, j, :], scalar=small[:, 4 + j:5 + j], in1=gbc[:, :],
                                            op0=ALU.mult, op1=ALU.mult)
                t_p = tp[j]
                for kc in range(8):
                    kb.pe.transpose(out=t_p[:, kc, :], in_=hb[:, kc * 128:(kc + 1) * 128], identity=cx.ident_bf[:, :])
                kb.act.copy(out=h_T[:, :, j * 128:(j + 1) * 128], in_=t_p[:, :, :])
            for ci in range(22):
                res = []
                for half in range(2):
                    c = ci + 22 * half
                    u = ups[nu % 4]
                    u_b = ub[nu % 4]
                    a = ac[nu % 4]
                    nu += 1
                    for kc in range(8):
                        kb.pe.matmul(out=u[:, 0:TB], lhsT=wu[:, kc, c * 128:(c + 1) * 128], rhs=h_T[:, kc, :],
                                     start=(kc == 0), stop=(kc == 7))
                    kb.act.copy(out=u_b[:, 2:TB + 2], in_=u[:, 0:TB])
                    kb.pool.tensor_copy(out=u_b[:, 0:2], in_=carry[:, c, :])
                    kb.dve.tensor_scalar(out=a[:, :], in0=u[:, 0:TB], scalar1=cw[:, c, 2:3], scalar2=cw[:, c, 3:4],
                                         op0=ALU.mult, op1=ALU.add)
                    kb.dve.scalar_tensor_tensor(out=a[:, :], in0=u_b[:, 1:TB + 1], scalar=cw[:, c, 1:2], in1=a[:, :],
                                                op0=ALU.mult, op1=ALU.add)
                    kb.dve.scalar_tensor_tensor(out=a[:, :], in0=u_b[:, 0:TB], scalar=cw[:, c, 0:1], in1=a[:, :],
                                                op0=ALU.mult, op1=ALU.add)
                    kb.pool.tensor_copy(out=carry[:, c, :], in_=u_b[:, TB:TB + 2])
                    res.append(a)
                kb.act.activation(out=res[0][:, :], in_=res[0][:, :], func=AF.Silu)
                kb.pool.tensor_tensor(out=Gb[:, ci, :], in0=res[0][:, :], in1=res[1][:, :], op=ALU.mult)
            for j in range(2):
                for c0 in (0, 512):
                    d = dps[nd % 2]
                    nd += 1
                    for ci in range(22):
                        kb.pe.matmul(out=d[:, :], lhsT=Gb[:, ci, j * 128:(j + 1) * 128], rhs=wd[:, ci, c0:c0 + 512],
                                     start=(ci == 0), stop=(ci == 21))
                    kb.dve.tensor_tensor(out=x_t[:, j, c0:c0 + 512], in0=x_t[:, j, c0:c0 + 512], in1=d[:, :], op=ALU.add)
            kb.dma(out=cx.xres[b * TB:(b + 1) * TB, :].with_ap(
                cx.xres.t[b * TB:(b + 1) * TB, :].rearrange("(j p) d -> p j d", p=128)), in_=x_t[:, :, :])
        kb.barrier()


def phase_final(kb, cx, y_out):
    P = cx.P
    with ExitStack() as st:
        gbc = kb.sb(st, "gbcF", [128, D], F32)
        kb.dma(out=gbc[:, :], in_=Ref(P["norm_final"], None, P["norm_final"].t.rearrange("(o d) -> o d", o=1).partition_broadcast(128)))
        xt = [kb.sb(st, f"xtF{i}", [128, D], F32) for i in range(2)]
        ot = [kb.sb(st, f"otF{i}", [128, D], F32) for i in range(2)]
        junk = kb.sb(st, "junkF", [128, D], BF16)
        small = kb.sb(st, "smallF", [128, 16], F32)
        for i in range(NT):
            x_t, o_t = xt[i % 2], ot[i % 2]
            kb.dma(out=x_t[:, :], in_=cx.xres[i * 128:(i + 1) * 128, :])
            kb.act.activation(out=junk[:, :], in_=x_t[:, :], func=AF.Square, accum_out=small[:, 0:1])
            rms_rstd(kb, small[:, 0:1], small[:, 1:2], D, 1e-6, small[:, 2:3])
            kb.dve.scalar_tensor_tensor(out=o_t[:, :], in0=x_t[:, :], scalar=small[:, 1:2], in1=gbc[:, :],
                                        op0=ALU.mult, op1=ALU.mult)
            kb.dma(out=y_out[i * 128:(i + 1) * 128, :], in_=o_t[:, :])
        kb.barrier()


class AttnBufs:
    def __init__(self, kb, st, tag):
        self.sps = [kb.psum(st, f"sps{tag}{i}", [128, 512], F32) for i in range(2)]
        self.acc = kb.psum(st, f"acc{tag}", [128, 4, 512], F32)
        self.pts = [kb.sb(st, f"pts{tag}{i}", [128, 512], BF16) for i in range(3)]
        self.ns = 0
        self.nm = 0


def attn_core(kb, A, q_rhs, kv_list, heads_view=False):
    n = len(kv_list)
    for idx, (kT, Vp, mask) in enumerate(kv_list):
        sp = A.sps[A.ns % 2]
        pt = A.pts[A.ns % 3]
        A.ns += 1
        if heads_view:
            spv = sp[:, :].with_ap(sp.t[:, :].rearrange("p (h q) -> p h q", h=4))
            ptv = pt[:, :].with_ap(pt.t[:, :].rearrange("p (h q) -> p h q", h=4))
        else:
            spv, ptv = sp[:, :], pt[:, :]
        kb.pe.matmul(out=spv, lhsT=kT, rhs=q_rhs, start=True, stop=True)
        kb.act.activation(out=pt[:, :], in_=sp[:, :], func=AF.Exp, scale=0.125)
        if mask is not None:
            eng = kb.dve if (A.nm % 3 != 2) else kb.pool
            A.nm += 1
            eng.tensor_tensor(out=ptv, in0=ptv, in1=mask, op=ALU.mult)
        for j in range(4):
            kb.pe.matmul(out=A.acc[:, j, 0:65], lhsT=pt[:, j * 128:(j + 1) * 128], rhs=Vp, start=(idx == 0), stop=(idx == n - 1))


def attn_evac(kb, A, small, dst, gate=None, first=True, tmp=None):
    kb.dve.tensor_scalar(out=small[:, 0:4], in0=A.acc[:, :, 64], scalar1=1e-30, scalar2=None, op0=ALU.max)
    kb.dve.reciprocal(out=small[:, 4:8], in_=small[:, 0:4])
    if gate is not None:
        kb.dve.tensor_tensor(out=small[:, 4:8], in0=small[:, 4:8], in1=gate, op=ALU.mult)
    sc = small[:, 4:8].with_ap(small.t[:, 4:8].unsqueeze(2).to_broadcast([128, 4, 64]))
    if first:
        kb.dve.tensor_tensor(out=dst, in0=A.acc[:, :, 0:64], in1=sc, op=ALU.mult)
    else:
        kb.dve.tensor_tensor(out=tmp, in0=A.acc[:, :, 0:64], in1=sc, op=ALU.mult)
        kb.pool.tensor_tensor(out=dst, in0=dst, in1=tmp, op=ALU.add)


def group_rmsnorm_store(kb, ob_ref, beta_bc, small, junk, stage_ref, dram_ref):
    kb.act.activation(out=junk, in_=ob_ref, func=AF.Square, accum_out=small[:, 8:9])
    rms_rstd(kb, small[:, 8:9], small[:, 9:10], 256, 1e-6, small[:, 10:11])
    kb.dve.scalar_tensor_tensor(out=stage_ref, in0=ob_ref, scalar=small[:, 9:10], in1=beta_bc, op0=ALU.mult, op1=ALU.mult)
    kb.dma(out=dram_ref, in_=stage_ref)


def build_vprime(kb, vp, src_dram_cols, ld, nh):
    kb.dma(out=ld[:, :, 0:nh * 64], in_=src_dram_cols.with_ap(src_dram_cols.ap.rearrange("(i p) c -> p i c", p=128)))
    kb.pool.memset(ap=vp[:, :, :, 64:65], constant=1.0)
    for h in range(nh):
        kb.dve.tensor_copy(out=vp[:, :, h, 0:64], in_=ld[:, :, h * 64:(h + 1) * 64])


def phase_dil(kb, cx, l):
    P, scr, C = cx.P, cx.scr, cx.C
    with ExitStack() as st:
        qT = kb.sb(st, "qTd", [64, 4, S], BF16)
        kT = kb.sb(st, "kTd", [64, 4, S], BF16)
        kb.dma(out=qT[:, :, :], in_=scr.dqT[:, :].with_ap(scr.dqT.t.rearrange("(h d) s -> d h s", d=64)))
        kb.dma(out=kT[:, :, :], in_=scr.dkT[:, :].with_ap(scr.dkT.t.rearrange("(h d) s -> d h s", d=64)))
        vp = kb.sb(st, "vpd", [128, NT, 4, 65], BF16)
        with ExitStack() as st2:
            ld = kb.sb(st2, "ldd", [128, NT, 256], F32)
            build_vprime(kb, vp, scr.tmB[:, 0:256], ld, 4)
            kb.barrier()
        dm = kb.sb(st, "dmd", [128, 20, 512], BF16)
        kb.dma(out=dm[:, :, :], in_=C["dm"][:, :, :])
        beta = kb.sb(st, "betad", [128, 256], F32)
        kb.dma(out=beta[:, :], in_=Ref(P["beta_dil"], None, P["beta_dil"].t[l:l + 1, :].partition_broadcast(128)))
        ob = [kb.sb(st, f"obd{i}", [128, 4, 256], F32) for i in range(2)]
        stage = [kb.sb(st, f"stgd{i}", [128, 256], F32) for i in range(2)]
        junk = kb.sb(st, "junkd", [128, 256], BF16)
        small = kb.sb(st, "smalld", [128, 16], F32)
        A = AttnBufs(kb, st, "d")
        for b in range(NBLK):
            o_b = ob[b % 2]
            for h in range(4):
                kv = []
                for kt in range(max(0, 4 * b - 16), 4 * b + 4):
                    delta = 4 * b - kt
                    kv.append((kT[:, h, kt * 128:(kt + 1) * 128], vp[:, kt, h, :], dm[:, delta + 3, :]))
                attn_core(kb, A, qT[:, h, b * 512:(b + 1) * 512], kv)
                attn_evac(kb, A, small, o_b[:, :, h * 64:(h + 1) * 64])
            for qt in range(4):
                i = b * 4 + qt
                group_rmsnorm_store(kb, o_b[:, qt, :], beta[:, :], small, junk[:, :], stage[qt % 2][:, :],
                                    scr.ymix.k(("b", i))[i * 128:(i + 1) * 128, 256:512])
        kb.barrier()
def phase_nsa(kb, cx, l):
    P, scr, C = cx.P, cx.scr, cx.C
    with ExitStack() as st:
        qT = kb.sb(st, "qTn", [64, 4, S], BF16)
        kb.dma(out=qT[:, :, :], in_=scr.qT[:, :].with_ap(scr.qT.t.rearrange("(h d) s -> d h s", d=64)))
        ksT = kb.sb(st, "ksTn", [64, S], BF16)
        kwT = kb.sb(st, "kwTn", [64, S], BF16)
        kb.dma(out=ksT[:, :], in_=scr.ksT[:, :])
        kb.dma(out=kwT[:, :], in_=scr.kwT[:, :])
        vps = kb.sb(st, "vpsn", [128, NT, 1, 65], BF16)
        vpw = kb.sb(st, "vpwn", [128, NT, 1, 65], BF16)
        gts = kb.sb(st, "gtsn", [128, NT, 12], F32)
        kcmpT = kb.sb(st, "kcmpTn", [64, 256], BF16)
        vpc = kb.sb(st, "vpcn", [128, 2, 65], BF16)
        selT = kb.sb(st, "selTn", [64, S], BF16)
        small = kb.sb(st, "smalln", [128, 16], F32)
        A = AttnBufs(kb, st, "n")
        x1 = kb.psum(st, "x1n", [128, 512], F32)
        x2 = kb.psum(st, "x2n", [128, 512], F32)
        with ExitStack() as st2:
            ld = kb.sb(st2, "ldn", [128, NT, 204], F32)
            kb.dma(out=ld[:, :, :], in_=scr.tmA[:, :].with_ap(scr.tmA.t.rearrange("(i p) c -> p i c", p=128)))
            kb.pool.memset(ap=vps[:, :, :, 64:65], constant=1.0)
            kb.pool.memset(ap=vpw[:, :, :, 64:65], constant=1.0)
            kb.dve.tensor_copy(out=vps[:, :, 0, 0:64], in_=ld[:, :, 0:64])
            kb.dve.tensor_copy(out=vpw[:, :, 0, 0:64], in_=ld[:, :, 128:192])
            kb.act.activation(out=gts[:, :, :], in_=ld[:, :, 192:204], func=AF.Sigmoid)
            x2t = kb.sb(st2, "x2n_", [128, S], BF16)
            pos2 = kb.sb(st2, "pos2n", [128, 16], F32)
            w1 = kb.sb(st2, "w1n", [128, 16, 128], BF16)
            w2 = kb.sb(st2, "w2n", [128, 64], BF16)
            am = kb.sb(st2, "amn", [128, 16, 256], BF16)
            hidT = kb.sb(st2, "hidTn", [128, 256], BF16)
            with kb.nc.allow_non_contiguous_dma(reason="tiny pos table"):
                kb.dma(out=pos2[:, :], in_=Ref(P["cmp_pos"], None, P["cmp_pos"].t[l].rearrange("(m i) d -> (i d) m", i=2)))
            for kind in ("k", "v"):
                r0 = 0 if kind == "k" else 64
                kb.pool.memset(ap=x2t[:, S - 1:S], constant=0.0)
                kb.dma(out=x2t[0:64, :], in_=scr.kcvcT[r0:r0 + 64, :])
                kb.dma(out=x2t[64:128, 0:S - 1], in_=scr.kcvcT[r0:r0 + 64, 1:S])
                wn1 = P["cmp_k_w1" if kind == "k" else "cmp_v_w1"]
                wn2 = P["cmp_k_w2" if kind == "k" else "cmp_v_w2"]
                kb.dma(out=w1[:, :, :], in_=wn1[l].with_ap(wn1.t[l].rearrange("(m p) h -> p m h", p=128)), via_pool=True)
                kb.dma(out=w2[:, :], in_=wn2[l, :, :], via_pool=True)
                kb.pool.memset(ap=am[:, :, 255:256], constant=0.0)
                xv = x2t.t[:, :].rearrange("p (c r) -> p c r", r=16)
                for m in range(16):
                    src = xv[:, 0:255, 2 * m] if m < 8 else xv[:, 1:256, 2 * m - 16]
                    kb.dve.tensor_scalar(out=am[:, m, 0:255], in0=Ref(x2t, None, src), scalar1=pos2[:, m:m + 1], scalar2=None, op0=ALU.add)
                for m in range(16):
                    kb.pe.matmul(out=x1[:, 0:256], lhsT=w1[:, m, :], rhs=am[:, m, :], start=(m == 0), stop=(m == 15))
                kb.act.activation(out=hidT[:, :], in_=x1[:, 0:256], func=AF.Silu)
                if kind == "k":
                    kb.pe.matmul(out=x2[0:64, 0:256], lhsT=w2[:, :], rhs=hidT[:, :], start=True, stop=True)
                    kb.dve.tensor_copy(out=kcmpT[:, :], in_=x2[0:64, 0:256])
                else:
                    kb.pool.memset(ap=vpc[:, :, 64:65], constant=1.0)
                    for ct in range(2):
                        kb.pe.matmul(out=x2[:, ct * 64:(ct + 1) * 64], lhsT=hidT[:, ct * 128:(ct + 1) * 128], rhs=w2[:, :], start=True, stop=True)
                    kb.dve.tensor_copy(out=vpc[:, :, 0:64], in_=x2[:, 0:128].with_ap(x2.t[:, 0:128].rearrange("p (c d) -> p c d", c=2)))
            kb.barrier()
        with ExitStack() as st3:
            gc = kb.sb(st3, "gcn", [128, 256], F32)
            d0 = kb.sb(st3, "d0n", [128, 64], F32)
            kb.dma(out=gc[:, :], in_=C["gc"][:, :])
            kb.dma(out=d0[:, :], in_=C["d0"][:, :])
            pex = [kb.sb(st3, f"pexn{i}", [128, 4, 256], F32) for i in range(2)]
            imp = [kb.sb(st3, f"impn{i}", [128, 258], F32) for i in range(2)]
            chk = kb.sb(st3, "chkn", [128, 256], F32)
            blk = kb.sb(st3, "blkn", [128, 64], F32)
            sc = kb.sb(st3, "scn", [128, 64], F32)
            sc2 = kb.sb(st3, "sc2n", [128, 64], F32)
            vm = kb.sb(st3, "vmn", [128, 64], F32)
            m8 = kb.sb(st3, "m8n", [128, 16], F32)
            sel = [kb.sb(st3, f"seln{i}", [128, 64], BF16) for i in range(2)]
            for i in range(2):
                kb.pool.memset(ap=imp[i][:, :], constant=0.0)
            scps = [A.acc.k(0), A.acc.k(1)]
            for i in range(NT):
                pe_, im = pex[i % 2], imp[i % 2]
                base = (i % 2) * 2
                scv = A.acc.k(i % 2)[:, base:base + 2, :].with_ap(
                    A.acc.t[:, base:base + 2, :].rearrange("p b (h c) -> p (b h) c", h=2))
                for h in range(4):
                    kb.pe.matmul(out=A.acc.k(i % 2)[:, base + h // 2, (h % 2) * 256:(h % 2) * 256 + 256],
                                 lhsT=qT[:, h, i * 128:(i + 1) * 128], rhs=kcmpT[:, :], start=True, stop=True)
                kb.act.activation(out=pe_[:, :, :], in_=scv, func=AF.Exp, scale=0.125)
                gcb = Ref(gc, None, gc.t[:, :].unsqueeze(1).to_broadcast([128, 4, 256]))
                kb.dve.scalar_tensor_tensor(out=pe_[:, :, :], in0=gcb, scalar=float(128 * i), in1=pe_[:, :, :],
                                            op0=ALU.is_le, op1=ALU.mult)
                kb.dve.tensor_reduce(out=small[:, 0:4], in_=pe_[:, :, :], axis=AX.X, op=ALU.add)
                kb.dve.tensor_scalar(out=small[:, 0:4], in0=small[:, 0:4], scalar1=1e-30, scalar2=None, op0=ALU.max)
                kb.dve.reciprocal(out=small[:, 4:8], in_=small[:, 0:4])
                kb.dve.tensor_scalar(out=im[:, 1:257], in0=pe_[:, 0, :], scalar1=small[:, 4:5], scalar2=None, op0=ALU.mult)
                for h in range(1, 4):
                    kb.dve.scalar_tensor_tensor(out=im[:, 1:257], in0=pe_[:, h, :], scalar=small[:, 4 + h:5 + h], in1=im[:, 1:257],
                                                op0=ALU.mult, op1=ALU.add)
                kb.dve.tensor_tensor(out=chk[:, :], in0=im[:, 0:256], in1=im[:, 1:257], op=ALU.add)
                kb.dve.tensor_reduce(out=blk[:, :], in_=chk[:, :].with_ap(chk.t[:, :].rearrange("p (b r) -> p b r", r=4)),
                                     axis=AX.X, op=ALU.add)
                kb.dve.tensor_scalar(out=sc[:, :], in0=d0[:, :], scalar1=float(128 * i - 127), scalar2=1e9, op0=ALU.is_ge, op1=ALU.mult)
                kb.dve.tensor_tensor(out=sc[:, :], in0=sc[:, :], in1=blk[:, :], op=ALU.max)
                kb.dve.memset(ap=sc[:, 0:1], constant=1e9)
                kb.dve.tensor_single_scalar(out=vm[:, :], in_=d0[:, :], scalar=float(128 * i), op=ALU.is_le)
                kb.dve.tensor_tensor(out=sc[:, :], in0=sc[:, :], in1=vm[:, :], op=ALU.mult)
                kb.dve.scalar_tensor_tensor(out=sc[:, :], in0=vm[:, :], scalar=-1.0, in1=sc[:, :], op0=ALU.add, op1=ALU.add)
                kb.dve.max(out=m8[:, 0:8], in_=sc[:, :])
                kb.dve.match_replace(out=sc2[:, :], in_to_replace=m8[:, 0:8], in_values=sc[:, :], imm_value=-3e38)
                kb.dve.max(out=m8[:, 8:16], in_=sc2[:, :])
                kb.dve.tensor_scalar(out=sel[i % 2][:, :], in0=sc[:, :], scalar1=m8[:, 15:16], scalar2=None, op0=ALU.is_ge)
                tpv = x1[0:64, 0:64].with_ap(x1.t[0:64, 0:64].bitcast(BF16))
                kb.pe.transpose(out=tpv, in_=sel[i % 2][:, :], identity=cx.ident_bf[:, :])
                kb.act.copy(out=selT[:, i * 128:(i + 1) * 128], in_=tpv)
            kb.barrier()
        with ExitStack() as st4:
            maskS = kb.sb(st4, "maskSn", [128, NT, 512], BF16)
            mc = kb.sb(st4, "mcn", [128, 16, 512], BF16)
            cms = kb.sb(st4, "cmsn", [128, 4, 512], BF16)
            cmw = kb.sb(st4, "cmwn", [128, 2, 128], BF16)
            ek = kb.sb(st4, "ekn", [64, 32, 128], BF16)
            kb.dma(out=mc[:, :, :], in_=C["mc"][:, :, :])
            kb.dma(out=cms[:, :, :], in_=C["cms"][:, :, :])
            kb.dma(out=cmw[:, :, :], in_=C["cmw"][:, :, :])
            kb.dma(out=ek[:, :, :], in_=C["ek"][:, :, :])
            beta = kb.sb(st4, "betan", [128, 256], F32)
            kb.dma(out=beta[:, :], in_=Ref(P["beta_nsa"], None, P["beta_nsa"].t[l:l + 1, :].partition_broadcast(128)))
            ob = [kb.sb(st4, f"obn{i}", [128, 4, 256], F32) for i in range(2)]
            tmp = kb.sb(st4, "tmpn", [128, 4, 64], F32)
            stage = [kb.sb(st4, f"stgn{i}", [128, 256], F32) for i in range(2)]
            junk = kb.sb(st4, "junkn", [128, 256], BF16)
            xs = [x1, x2]
            nx = 0
            for b in range(NBLK):
                o_b = ob[b % 2]
                for kt in range(4 * b + 4):
                    xp = xs[nx % 2]
                    nx += 1
                    kb.pe.matmul(out=xp[:, :], lhsT=ek[:, kt, :], rhs=selT[:, b * 512:(b + 1) * 512], start=True, stop=True)
                    if kt >= 4 * b:
                        kb.dve.tensor_tensor(out=maskS[:, kt, :], in0=xp[:, :], in1=cms[:, kt - 4 * b, :], op=ALU.mult)
                    else:
                        kb.act.copy(out=maskS[:, kt, :], in_=xp[:, :])
                for h in range(4):
                    dst = o_b[:, :, h * 64:(h + 1) * 64]
                    kv = [(kcmpT[:, 0:128], vpc[:, 0, :], None if b >= 5 else mc[:, 2 * b, :])]
                    if b >= 4:
                        kv.append((kcmpT[:, 128:256], vpc[:, 1, :], mc[:, 2 * b + 1, :]))
                    attn_core(kb, A, qT[:, h, b * 512:(b + 1) * 512], kv)
                    attn_evac(kb, A, small, dst, gate=gts[:, 4 * b:4 * b + 4, 3 * h], first=True)
                    kv = [(ksT[:, kt * 128:(kt + 1) * 128], vps[:, kt, 0, :], maskS[:, kt, :]) for kt in range(4 * b + 4)]
                    attn_core(kb, A, qT[:, h, b * 512:(b + 1) * 512], kv)
                    attn_evac(kb, A, small, dst, gate=gts[:, 4 * b:4 * b + 4, 3 * h + 1], first=False, tmp=tmp[:, :, :])
                for qt in range(4):
                    i = 4 * b + qt
                    kv = []
                    for kt in range(max(0, i - 4), i + 1):
                        mk = None
                        if kt == i:
                            mk = Ref(cmw, None, cmw.t[:, 0:1, :].to_broadcast([128, 4, 128]))
                        elif kt == i - 4:
                            mk = Ref(cmw, None, cmw.t[:, 1:2, :].to_broadcast([128, 4, 128]))
                        kv.append((kwT[:, kt * 128:(kt + 1) * 128], vpw[:, kt, 0, :], mk))
                    attn_core(kb, A, qT[:, :, i * 128:(i + 1) * 128], kv, heads_view=True)
                    dstw = o_b[:, qt, :].with_ap(o_b.t[:, qt, :].rearrange("p (h d) -> p h d", h=4))
                    gw = gts[:, i, :].with_ap(gts.t[:, i, :].rearrange("p (h r) -> p h r", r=3)[:, :, 2])
                    attn_evac(kb, A, small, dstw, gate=gw, first=False, tmp=tmp[:, :, :])
                for qt in range(4):
                    i = b * 4 + qt
                    group_rmsnorm_store(kb, o_b[:, qt, :], beta[:, :], small, junk[:, :], stage[qt % 2][:, :],
                                        scr.ymix.k(("a", i))[i * 128:(i + 1) * 128, 0:256])
            kb.barrier()
C0 = 0.6065306597126334
RWKV_STAGE = [99]


def phase_rwkv(kb, cx, l):
    P, scr, C = cx.P, cx.scr, cx.C
    CH = 64
    with ExitStack() as st:
        def bc(name, src, c0, n, rows=64):
            t = kb.sb(st, name, [rows, n], F32)
            kb.dma(out=t[:, :], in_=Ref(P[src], None, P[src].t[l:l + 1, c0:c0 + n].partition_broadcast(rows)))
            return t
        mu_bc = bc("mu_r", "rwkv_mu", 0, 768)
        w0_bc = bc("w0_r", "rwkv_w0", 0, 256)
        a0_bc = bc("a0_r", "rwkv_a0", 0, 256)
        kk_bc = bc("kk_r", "rwkv_k_k", 0, 256)
        ka_bc = bc("ka_r", "rwkv_k_a", 0, 256)
        lg_bc = bc("lg_r", "rwkv_ln_g", 0, 256)
        lb_bc = bc("lb_r", "rwkv_ln_b", 0, 256)
        rk_bc = kb.sb(st, "rk_r", [64, 256], F32)
        kb.dma(out=rk_bc[:, :], in_=Ref(P["rwkv_r_k"], None,
               P["rwkv_r_k"].t[l:l + 1].rearrange("o h d -> o (h d)").partition_broadcast(64)))
        oka = kb.sb(st, "oka_r", [64, 256], F32)
        kb.dve.tensor_scalar(out=oka[:, :], in0=ka_bc[:, :], scalar1=-1.0, scalar2=1.0, op0=ALU.mult, op1=ALU.add)
        mu_lo = kb.sb(st, "mulo_r", [128, 2], F32)
        with kb.nc.allow_non_contiguous_dma(reason="tiny mu columns"):
            kb.dma(out=mu_lo[:, 0:1], in_=Ref(P["rwkv_mu"], None, P["rwkv_mu"].t[l:l + 1, 768:896].rearrange("o c -> c o")))
            kb.dma(out=mu_lo[:, 1:2], in_=Ref(P["rwkv_mu"], None, P["rwkv_mu"].t[l:l + 1, 896:1024].rearrange("o c -> c o")))
        wa_up = kb.sb(st, "waup_r", [64, 256], F32)
        a_up = kb.sb(st, "aup_r", [64, 256], F32)
        g_up = kb.sb(st, "gup_r", [128, 256], F32)
        mu_a = kb.sb(st, "mua_r", [64, 1], F32)
        with kb.nc.allow_non_contiguous_dma(reason="tiny mu columns"):
            kb.dma(out=mu_a[:, 0:1], in_=Ref(P["rwkv_mu"], None, P["rwkv_mu"].t[l:l + 1, 832:896].rearrange("o c -> c o")))
        kb.dma(out=wa_up[0:64, :], in_=P["rwkv_w_up"][l, :, :])
        kb.dma(out=a_up[0:64, :], in_=P["rwkv_a_up"][l, :, :])
        kb.dma(out=g_up[:, :], in_=P["rwkv_g_up"][l, :, :])
        tri = kb.sb(st, "tri_r", [64, 2, 64], F32)
        kb.dma(out=tri[:, 0, :], in_=C["tri"][0:64, 0:64])
        kb.dma(out=tri[:, 1, :], in_=C["mstrictT"][:, 0, :])
        mst = kb.sb(st, "mst_r", [64, 4, 64], F32)
        mstT = kb.sb(st, "mstT_r", [64, 4, 64], F32)
        minc = kb.sb(st, "minc_r", [64, 4, 64], F32)
        kb.dma(out=mst[:, :, :], in_=C["mstrict"][:, 0:4, :])
        kb.dma(out=mstT[:, :, :], in_=C["mstrictT"][:, 0:4, :])
        kb.dma(out=minc[:, :, :], in_=C["mincl"][:, 0:4, :])
        ST = kb.sb(st, "ST_r", [64, 4, 64], F32)
        kb.pool.memset(ap=ST[:, :, :], constant=0.0)
        idb = Ref(cx.ident_f, None, cx.ident_f.t[0:64, 0:64].unsqueeze(1).to_broadcast([64, 4, 64]))

        def T2(name, shape, n=2):
            return [kb.sb(st, f"{name}{i}_r", shape, F32) for i in range(n)]
        rkv, prv, lo, gd = T2("rkv", [64, 768]), T2("prv", [64, 768]), T2("lo", [64, 65]), T2("gd", [128, 65])
        los, gds = T2("los", [64, 64]), T2("gds", [128, 64])
        loa, loas = T2("loa", [64, 65]), T2("loas", [64, 64])
        sgt, a_t, g_t = T2("sgt", [64, 256]), T2("at", [64, 256]), T2("gt", [64, 256])
        kk, k2, bb = T2("kkt", [64, 256]), T2("k2t", [64, 256]), T2("bbt", [64, 256])
        tmp, tmp2 = T2("tmp", [64, 256]), T2("tmp2", [64, 256])
        Pm, iP, Pp, Pr = T2("Pm", [64, 256]), T2("iP", [64, 256]), T2("Pp", [64, 256]), T2("Pr", [64, 256])
        Kt, Bt, KKt, Rt, Kh, Bh = (T2("Kt", [64, 256]), T2("Bt", [64, 256]), T2("KKt", [64, 256]), T2("Rt", [64, 256]),
                                    T2("Kh", [64, 256]), T2("Bh", [64, 256]))
        FMq = T2("FMq", [64, 16, 64])
        N, NT_, Mak, Abr, Akr = (T2("N", [64, 4, 64]), T2("NT", [64, 4, 64]), T2("Mak", [64, 4, 64]), T2("Abr", [64, 4, 64]),
                                 T2("Akr", [64, 4, 64]))
        Tm = T2("Tm", [64, 4, 64])
        Wsb, Xsb, U0T, UT = T2("Wsb", [64, 4, 64]), T2("Xsb", [64, 4, 64]), T2("U0T", [64, 4, 64]), T2("UT", [64, 4, 64])
        pcs = T2("pcs", [64, 4])
        yv = T2("yv", [64, 256])
        small = T2("small", [64, 16])
        B = [kb.psum(st, f"B{i}_r", [64, 512], F32) for i in range(8)]

        def v3(ref_tile, c0):
            return ref_tile[:, c0:c0 + 256].with_ap(ref_tile.t[:, c0:c0 + 256].rearrange("p (h d) -> p h d", h=4))

        def hb(t_small, c0):
            return t_small[:, c0:c0 + 4].with_ap(t_small.t[:, c0:c0 + 4].unsqueeze(2).to_broadcast([64, 4, 64]))

        import os as _os
        for c in range(int(_os.environ.get('RWKV_NCH', S // CH))):
            p = c % 2
            t0 = c * CH
            kb.dma(out=rkv[p][:, :], in_=scr.tmB[t0:t0 + CH, 256:1024])
            if c == 0:
                kb.pool.memset(ap=prv[p][0:1, :], constant=0.0)
                kb.dma(out=prv[p][1:CH, :], in_=scr.tmB[0:CH - 1, 256:1024])
                kb.pool.memset(ap=lo[p][:, 0:1], constant=0.0)
                kb.pool.memset(ap=gd[p][:, 0:1], constant=0.0)
                kb.dma(out=lo[p][:, 1:65], in_=scr.loraT[0:64, 0:CH])
                kb.pool.memset(ap=loa[p][:, 0:1], constant=0.0)
                kb.dma(out=loa[p][:, 1:65], in_=scr.loraT[64:128, 0:CH])
                kb.dma(out=gd[p][:, 1:65], in_=scr.loraT[128:256, 0:CH])
            else:
                kb.dma(out=prv[p][:, :], in_=scr.tmB[t0 - 1:t0 + CH - 1, 256:1024])
                kb.dma(out=lo[p][:, :], in_=scr.loraT[0:64, t0 - 1:t0 + CH])
                kb.dma(out=loa[p][:, :], in_=scr.loraT[64:128, t0 - 1:t0 + CH])
                kb.dma(out=gd[p][:, :], in_=scr.loraT[128:256, t0 - 1:t0 + CH])
            kb.dve.tensor_tensor(out=los[p][:, :], in0=lo[p][:, 0:64], in1=lo[p][:, 1:65], op=ALU.subtract)
            kb.dve.scalar_tensor_tensor(out=los[p][:, :], in0=los[p][:, :], scalar=mu_lo[0:64, 0:1], in1=lo[p][:, 1:65], op0=ALU.mult, op1=ALU.add)
            kb.dve.tensor_tensor(out=loas[p][:, :], in0=loa[p][:, 0:64], in1=loa[p][:, 1:65], op=ALU.subtract)
            kb.dve.scalar_tensor_tensor(out=loas[p][:, :], in0=loas[p][:, :], scalar=mu_a[:, 0:1], in1=loa[p][:, 1:65], op0=ALU.mult, op1=ALU.add)
            kb.dve.tensor_tensor(out=gds[p][:, :], in0=gd[p][:, 0:64], in1=gd[p][:, 1:65], op=ALU.subtract)
            kb.dve.scalar_tensor_tensor(out=gds[p][:, :], in0=gds[p][:, :], scalar=mu_lo[:, 1:2], in1=gd[p][:, 1:65], op0=ALU.mult, op1=ALU.add)
            kb.act.activation(out=los[p][0:64, :], in_=los[p][0:64, :], func=AF.Tanh)
            kb.act.activation(out=gds[p][:, :], in_=gds[p][:, :], func=AF.Sigmoid)
            kb.pe.matmul(out=B[0][:, 0:256], lhsT=los[p][0:64, :], rhs=wa_up[0:64, :], start=True, stop=True)
            kb.pe.matmul(out=B[0][:, 256:512], lhsT=loas[p][:, :], rhs=a_up[:, :], start=True, stop=True)
            kb.pe.matmul(out=B[1][:, 0:256], lhsT=gds[p][:, :], rhs=g_up[:, :], start=True, stop=True)
            kb.dve.tensor_tensor(out=sgt[p][:, :], in0=B[0][:, 0:256], in1=w0_bc[:, :], op=ALU.add)
            kb.act.activation(out=sgt[p][:, :], in_=sgt[p][:, :], func=AF.Sigmoid)
            kb.dve.tensor_tensor(out=a_t[p][:, :], in0=B[0][:, 256:512], in1=a0_bc[:, :], op=ALU.add)
            kb.act.activation(out=a_t[p][:, :], in_=a_t[p][:, :], func=AF.Sigmoid)
            kb.act.copy(out=g_t[p][:, :], in_=B[1][:, 0:256])
            kb.pool.tensor_tensor(out=prv[p][:, :], in0=prv[p][:, :], in1=rkv[p][:, :], op=ALU.subtract)
            kb.pool.tensor_tensor(out=prv[p][:, :], in0=prv[p][:, :], in1=mu_bc[:, :], op=ALU.mult)
            kb.dve.tensor_tensor(out=rkv[p][:, :], in0=rkv[p][:, :], in1=prv[p][:, :], op=ALU.add)
            if RWKV_STAGE[0] <= 1:
                break
            r_, k_, v_ = rkv[p][:, 0:256], rkv[p][:, 256:512], rkv[p][:, 512:768]
            kb.dve.tensor_tensor(out=kk[p][:, :], in0=k_, in1=kk_bc[:, :], op=ALU.mult)
            kb.pool.tensor_tensor(out=tmp[p][:, :], in0=kk[p][:, :], in1=kk[p][:, :], op=ALU.mult)
            kb.dve.tensor_reduce(out=small[p][:, 0:4], in_=v3(tmp[p], 0), axis=AX.X, op=ALU.add)
            kb.dve.tensor_scalar(out=small[p][:, 0:4], in0=small[p][:, 0:4], scalar1=1e-12, scalar2=None, op0=ALU.add)
            kb.act.activation(out=small[p][:, 0:4], in_=small[p][:, 0:4], func=AF.Sqrt)
            kb.dve.reciprocal(out=small[p][:, 4:8], in_=small[p][:, 0:4])
            kb.dve.tensor_tensor(out=v3(kk[p], 0), in0=v3(kk[p], 0), in1=hb(small[p], 4), op=ALU.mult)
            kb.pool.tensor_tensor(out=tmp[p][:, :], in0=a_t[p][:, :], in1=ka_bc[:, :], op=ALU.mult)
            kb.pool.tensor_tensor(out=tmp[p][:, :], in0=tmp[p][:, :], in1=oka[:, :], op=ALU.add)
            kb.dve.tensor_tensor(out=k2[p][:, :], in0=k_, in1=tmp[p][:, :], op=ALU.mult)
            kb.pool.tensor_tensor(out=bb[p][:, :], in0=kk[p][:, :], in1=a_t[p][:, :], op=ALU.mult)
            if RWKV_STAGE[0] <= 2:
                break
            kb.pe.matmul(out=B[2][:, 0:256], lhsT=tri[:, 0, :], rhs=sgt[p][:, :], start=True, stop=True)
            kb.pe.matmul(out=B[2][:, 256:512], lhsT=tri[:, 1, :], rhs=sgt[p][:, :], start=True, stop=True)
            kb.act.activation(out=Pm[p][:, :], in_=B[2][:, 0:256], func=AF.Exp, scale=-C0)
            kb.act.activation(out=iP[p][:, :], in_=B[2][:, 0:256], func=AF.Exp, scale=C0)
            kb.dve.tensor_tensor(out=tmp2[p][:, :], in0=B[2][:, 0:256], in1=sgt[p][:, :], op=ALU.subtract)
            kb.act.activation(out=Pp[p][:, :], in_=tmp2[p][:, :], func=AF.Exp, scale=-C0)
            kb.act.activation(out=Pr[p][:, :], in_=B[2][:, 256:512], func=AF.Exp, scale=-C0)
            kb.dve.tensor_tensor(out=Kt[p][:, :], in0=k2[p][:, :], in1=iP[p][:, :], op=ALU.mult)
            kb.pool.tensor_tensor(out=Bt[p][:, :], in0=bb[p][:, :], in1=iP[p][:, :], op=ALU.mult)
            kb.dve.tensor_tensor(out=KKt[p][:, :], in0=kk[p][:, :], in1=Pp[p][:, :], op=ALU.mult)
            kb.pool.tensor_tensor(out=Rt[p][:, :], in0=r_, in1=Pm[p][:, :], op=ALU.mult)
            kb.dve.tensor_tensor(out=Kh[p][:, :], in0=k2[p][:, :], in1=Pr[p][:, :], op=ALU.mult)
            kb.pool.tensor_tensor(out=Bh[p][:, :], in0=bb[p][:, :], in1=Pr[p][:, :], op=ALU.mult)
            if RWKV_STAGE[0] <= 3:
                break
            for h in range(4):
                kb.pe.matmul(out=B[1][:, 256 + 2 * h:258 + 2 * h], lhsT=Pm[p][:, h * 64:(h + 1) * 64], rhs=cx.ident_f[0:64, 62:64], start=True, stop=True)
            kb.act.copy(out=pcs[p][:, :], in_=B[1][:, 256:264].with_ap(B[1].t[:, 256:264].rearrange("p (h two) -> p h two", two=2)[:, :, 1]))
            if RWKV_STAGE[0] <= 4:
                break
            for qi, q in enumerate((Bt, Kt, KKt, Rt)):
                for h in range(4):
                    idx = qi * 4 + h
                    bk = B[3] if idx < 8 else B[4]
                    kb.pe.matmul(out=bk[:, (idx % 8) * 64:(idx % 8 + 1) * 64], lhsT=q[p][:, h * 64:(h + 1) * 64], rhs=cx.ident_f[0:64, 0:64], start=True, stop=True)
            fm = FMq[p]
            kb.act.copy(out=fm[:, 0:8, :], in_=B[3][:, :].with_ap(B[3].t[:, :].rearrange("p (a b) -> p a b", b=64)))
            kb.dve.tensor_copy(out=fm[:, 8:16, :], in_=B[4][:, :].with_ap(B[4].t[:, :].rearrange("p (a b) -> p a b", b=64)))
            BT = lambda h: fm[:, 0 + h, :]
            KT = lambda h: fm[:, 4 + h, :]
            KKT = lambda h: fm[:, 8 + h, :]
            RT = lambda h: fm[:, 12 + h, :]
            for h in range(4):
                kb.pe.matmul(out=B[5][:, h * 64:(h + 1) * 64], lhsT=BT(h), rhs=KKT(h), start=True, stop=True)
                kb.pe.matmul(out=B[5][:, 256 + h * 64:256 + (h + 1) * 64], lhsT=KKT(h), rhs=BT(h), start=True, stop=True)
                kb.pe.matmul(out=B[6][:, h * 64:(h + 1) * 64], lhsT=KT(h), rhs=KKT(h), start=True, stop=True)
                kb.pe.matmul(out=B[6][:, 256 + h * 64:256 + (h + 1) * 64], lhsT=BT(h), rhs=RT(h), start=True, stop=True)
                kb.pe.matmul(out=B[7][:, h * 64:(h + 1) * 64], lhsT=KT(h), rhs=RT(h), start=True, stop=True)
            b3 = lambda bk, c0: bk[:, c0:c0 + 256].with_ap(bk.t[:, c0:c0 + 256].rearrange("p (h d) -> p h d", h=4))
            kb.dve.scalar_tensor_tensor(out=N[p][:, :, :], in0=b3(B[5], 0), scalar=-1.0, in1=mst[:, :, :], op0=ALU.mult, op1=ALU.mult)
            kb.dve.scalar_tensor_tensor(out=NT_[p][:, :, :], in0=b3(B[5], 256), scalar=-1.0, in1=mstT[:, :, :], op0=ALU.mult, op1=ALU.mult)
            kb.dve.tensor_tensor(out=Mak[p][:, :, :], in0=b3(B[6], 0), in1=mst[:, :, :], op=ALU.mult)
            kb.dve.tensor_tensor(out=Abr[p][:, :, :], in0=b3(B[6], 256), in1=minc[:, :, :], op=ALU.mult)
            kb.dve.tensor_tensor(out=Akr[p][:, :, :], in0=b3(B[7], 0), in1=minc[:, :, :], op=ALU.mult)
            if RWKV_STAGE[0] <= 5:
                break
            kb.pool.tensor_tensor(out=Tm[p][:, :, :], in0=N[p][:, :, :], in1=idb, op=ALU.add)
            A_, AT_ = N[p], NT_[p]
            for j in range(1, 6):
                for h in range(4):
                    kb.pe.matmul(out=B[5][:, h * 64:(h + 1) * 64], lhsT=AT_[:, h, :], rhs=A_[:, h, :], start=True, stop=True)
                    kb.pe.matmul(out=B[5][:, 256 + h * 64:256 + (h + 1) * 64], lhsT=A_[:, h, :], rhs=AT_[:, h, :], start=True, stop=True)
                kb.act.copy(out=A_[:, :, :], in_=b3(B[5], 0))
                kb.dve.tensor_copy(out=AT_[:, :, :], in_=b3(B[5], 256))
                for h in range(4):
                    kb.pe.matmul(out=B[6][:, h * 64:(h + 1) * 64], lhsT=AT_[:, h, :], rhs=Tm[p][:, h, :], start=True, stop=True)
                kb.dve.tensor_tensor(out=Tm[p][:, :, :], in0=Tm[p][:, :, :], in1=b3(B[6], 0), op=ALU.add)
            if RWKV_STAGE[0] <= 6:
                break
            for h in range(4):
                kb.pe.matmul(out=B[7][:, 256 + h * 64:256 + (h + 1) * 64], lhsT=KKt[p][:, h * 64:(h + 1) * 64], rhs=Tm[p][:, h, :], start=True, stop=True)
                kb.pe.matmul(out=B[3][:, h * 64:(h + 1) * 64], lhsT=Mak[p][:, h, :], rhs=rkv[p][:, 512 + h * 64:512 + (h + 1) * 64], start=True, stop=True)
            kb.act.copy(out=Wsb[p][:, :, :], in_=b3(B[7], 256))
            kb.dve.tensor_copy(out=Xsb[p][:, :, :], in_=b3(B[3], 0))
            for h in range(4):
                kb.pe.matmul(out=B[3][:, 256 + h * 64:256 + (h + 1) * 64], lhsT=Tm[p][:, h, :], rhs=Xsb[p][:, h, :], start=True, stop=True)
            kb.act.activation(out=U0T[p][:, :, :], in_=b3(B[3], 256), func=AF.Copy, scale=-1.0)
            if RWKV_STAGE[0] <= 7:
                break
            for h in range(4):
                vh = rkv[p][:, 512 + h * 64:512 + (h + 1) * 64]
                kb.pe.matmul(out=B[4][:, h * 64:(h + 1) * 64], lhsT=Wsb[p][:, h, :], rhs=ST[:, h, :], start=True, stop=True)
                kb.dve.tensor_tensor(out=UT[p][:, h, :], in0=U0T[p][:, h, :], in1=B[4][:, h * 64:(h + 1) * 64], op=ALU.subtract)
                yo = B[4][:, 256 + h * 64:256 + (h + 1) * 64]
                kb.pe.matmul(out=yo, lhsT=RT(h), rhs=ST[:, h, :], start=True, stop=False)
                kb.pe.matmul(out=yo, lhsT=Abr[p][:, h, :], rhs=UT[p][:, h, :], start=False, stop=False)
                kb.pe.matmul(out=yo, lhsT=Akr[p][:, h, :], rhs=vh, start=False, stop=True)
                so = B[2][:, h * 64:(h + 1) * 64]
                kb.pe.matmul(out=so, lhsT=Bh[p][:, h * 64:(h + 1) * 64], rhs=UT[p][:, h, :], start=True, stop=False)
                kb.pe.matmul(out=so, lhsT=Kh[p][:, h * 64:(h + 1) * 64], rhs=vh, start=False, stop=True)
                kb.dve.scalar_tensor_tensor(out=ST[:, h, :], in0=ST[:, h, :], scalar=pcs[p][:, h:h + 1], in1=so, op0=ALU.mult, op1=ALU.add)
            if RWKV_STAGE[0] <= 8:
                break
            y = yv[p]
            kb.pool.tensor_tensor(out=tmp[p][:, :], in0=r_, in1=k2[p][:, :], op=ALU.mult)
            kb.pool.tensor_tensor(out=tmp[p][:, :], in0=tmp[p][:, :], in1=rk_bc[:, :], op=ALU.mult)
            kb.dve.tensor_reduce(out=small[p][:, 8:12], in_=v3(tmp[p], 0), axis=AX.X, op=ALU.add)
            kb.dve.tensor_tensor(out=v3(tmp2[p], 0), in0=v3(rkv[p], 512), in1=hb(small[p], 8), op=ALU.mult)
            kb.dve.tensor_tensor(out=y[:, :], in0=tmp2[p][:, :], in1=B[4][:, 256:512], op=ALU.add)
            kb.dve.tensor_reduce(out=small[p][:, 0:4], in_=v3(y, 0), axis=AX.X, op=ALU.add)
            kb.dve.tensor_scalar(out=small[p][:, 0:4], in0=small[p][:, 0:4], scalar1=1.0 / 64, scalar2=None, op0=ALU.mult)
            kb.dve.tensor_tensor(out=v3(y, 0), in0=v3(y, 0), in1=hb(small[p], 0), op=ALU.subtract)
            kb.pool.tensor_tensor(out=tmp[p][:, :], in0=y[:, :], in1=y[:, :], op=ALU.mult)
            kb.dve.tensor_reduce(out=small[p][:, 4:8], in_=v3(tmp[p], 0), axis=AX.X, op=ALU.add)
            kb.dve.tensor_scalar(out=small[p][:, 4:8], in0=small[p][:, 4:8], scalar1=1.0 / 64, scalar2=64e-5, op0=ALU.mult, op1=ALU.add)
            kb.act.activation(out=small[p][:, 4:8], in_=small[p][:, 4:8], func=AF.Sqrt)
            kb.dve.reciprocal(out=small[p][:, 12:16], in_=small[p][:, 4:8])
            kb.dve.tensor_tensor(out=v3(y, 0), in0=v3(y, 0), in1=hb(small[p], 12), op=ALU.mult)
            kb.pool.tensor_tensor(out=y[:, :], in0=y[:, :], in1=lg_bc[:, :], op=ALU.mult)
            kb.pool.tensor_tensor(out=y[:, :], in0=y[:, :], in1=lb_bc[:, :], op=ALU.add)
            kb.dve.tensor_tensor(out=y[:, :], in0=y[:, :], in1=g_t[p][:, :], op=ALU.mult)
            kb.dma(out=scr.ymix.k(("c", c))[t0:t0 + CH, 512:768], in_=y[:, :])
        kb.barrier()
def build(depth=DEPTH, debug=None, stop_after=None, only=None):
    kb = KB()
    nc = kb.nc
    cx = Ctx()
    cx.P = {}
    x_in = kb.dram("x", [S, D], F32, kind="ExternalInput")
    for n, shp in PARAM_SHAPES.items():
        cx.P[n] = kb.dram(n, list(shp), F32, kind="ExternalInput")
    consts = make_consts()
    cx.C = {}
    for n, a in consts.items():
        cx.C[n] = kb.dram("c_" + n, list(a.shape), CONST_DT.get(n, F32), kind="ExternalInput")
    y_out = kb.dram("y", [S, D], F32, kind="ExternalOutput")
    scr = Ctx()
    cx.scr = scr
    dbg = debug or []

    def scratch(name, shape, dt):
        kind = "ExternalOutput" if name in dbg else "Internal"
        return kb.dram("scr_" + name, shape, dt, kind=kind)
    scr.qT = scratch("qT", [256, S], BF16)
    scr.kcvcT = scratch("kcvcT", [128, S], BF16)
    scr.ksT = scratch("ksT", [64, S], BF16)
    scr.kwT = scratch("kwT", [64, S], BF16)
    scr.dqT = scratch("dqT", [256, S], BF16)
    scr.dkT = scratch("dkT", [256, S], BF16)
    scr.loraT = scratch("loraT", [256, S], F32)
    scr.convT = scratch("convT", [512, S], F32)
    scr.tmA = scratch("tmA", [S, 204], F32)
    scr.tmB = scratch("tmB", [S, 1024], F32)
    scr.ymix = scratch("ymix", [S, D], F32)
    cx.xres = scratch("xres", [S, D], F32)
    outs = [y_out] + [getattr(scr, n) if hasattr(scr, n) else cx.xres for n in dbg]

    gst = ExitStack()
    cx.ident_bf = kb.sb(gst, "ident_bf", [128, 128], BF16)
    cx.ident_f = kb.sb(gst, "ident_f", [128, 128], F32)
    kb.dma(out=cx.ident_bf[:, :], in_=cx.C["ident_bf"][:, :])
    kb.dma(out=cx.ident_f[:, :], in_=cx.C["ident_f"][:, :])
    kb.dma(out=cx.xres[:, :], in_=x_in[:, :])

    for l in range(depth):
        phase_a(kb, cx, l)
        if stop_after == "a":
            break
        if only in (None, "conv"):
            phase_conv(kb, cx, l)
        if only in (None, "dil"):
            phase_dil(kb, cx, l)
        if only in (None, "nsa"):
            phase_nsa(kb, cx, l)
        if only in (None, "rwkv"):
            phase_rwkv(kb, cx, l)
        if stop_after == "mix":
            break
        phase_b(kb, cx, l)
        if stop_after == "b":
            break
        phase_c(kb, cx, l)
    if stop_after is None:
        phase_final(kb, cx, y_out)
    gst.close()
    kb.finish(outs)
    return kb, consts


_CACHE = {}


def kernel(**inputs):
    if "prog" not in _CACHE:
        _CACHE["prog"] = build()
    kb, consts = _CACHE["prog"]
    x = np.ascontiguousarray(inputs["x"], dtype=np.float32)
    in_maps = []
    for c in range(8):
        m = {"x": x[c]}
        for n in PARAM_SHAPES:
            m[n] = np.ascontiguousarray(inputs[n], dtype=np.float32)
        for n, a in consts.items():
            m["c_" + n] = a
        in_maps.append(m)
    res = run_bass_kernel_spmd(kb.nc, in_maps, core_ids=list(range(8)))
    return np.stack([res.results[c]["y"] for c in range(8)], axis=0)
```

```python
import numpy as np
import ml_dtypes
from contextlib import ExitStack
import concourse.bass as bass
import concourse.mybir as mybir
from concourse.bass_utils import run_bass_kernel_spmd

F32 = mybir.dt.float32
BF16 = mybir.dt.bfloat16
I32 = mybir.dt.int32
AF = mybir.ActivationFunctionType
ALU = mybir.AluOpType
AX = mybir.AxisListType

S = 4096
D = 1024
NT = S // 128
NBLK = S // 512
DEPTH = 4
DFF = 2816
IN_COLS = 2956
WRITE_NAMES = ("out", "accum_out", "ap")


class SemT:
    def __init__(self, handle):
        self.h = handle
        self.count = 0


class Rec:
    __slots__ = ("lw", "rd")

    def __init__(self):
        self.lw = None
        self.rd = []


class Tile:
    def __init__(self, t, name):
        self.t = t
        self.name = name
        self.regs = {None: Rec()}
        self.excl = False

    def recs_dep(self, key):
        if key is None:
            return list(self.regs.values())
        if key not in self.regs:
            self.regs[key] = Rec()
        return [self.regs[key], self.regs[None]]

    def recs_upd(self, key, is_write):
        if key is None:
            return list(self.regs.values()) if is_write else [self.regs[None]]
        if key not in self.regs:
            self.regs[key] = Rec()
        return [self.regs[key]]

    def __getitem__(self, idx):
        return Ref(self, None, self.t[idx])

    def k(self, key):
        return KeyView(self, key)


class KeyView:
    def __init__(self, tile, key):
        self.tile = tile
        self.key = key

    def __getitem__(self, idx):
        return Ref(self.tile, self.key, self.tile.t[idx])


class Ref:
    def __init__(self, tile, key, ap):
        self.tile = tile
        self.key = key
        self.ap = ap

    def with_ap(self, ap):
        return Ref(self.tile, self.key, ap)


class Eng:
    def __init__(self, kb, name, raw, sem, is_pe=False):
        self.kb = kb
        self.name = name
        self.raw = raw
        self.sem = sem
        self.waited = {}
        self.is_pe = is_pe

    def __getattr__(self, opname):
        def call(**kw):
            reads, writes = [], []
            kw2 = {}
            for n, v in kw.items():
                if isinstance(v, Ref):
                    (writes if n in WRITE_NAMES else reads).append(v)
                    kw2[n] = v.ap
                else:
                    kw2[n] = v
            return self.kb.emit(self, lambda: getattr(self.raw, opname)(**kw2), reads, writes)
        return call


class KB:
    def __init__(self):
        self.nc = bass.Bass("TRN2", target_bir_lowering=False)
        nc = self.nc
        self.es = ExitStack()
        mk = lambda n: SemT(self.es.enter_context(nc.semaphore(n)))
        self.pe = Eng(self, "pe", nc.tensor, mk("s_pe"), is_pe=True)
        self.act = Eng(self, "act", nc.scalar, mk("s_act"))
        self.dve = Eng(self, "dve", nc.vector, mk("s_dve"))
        self.pool = Eng(self, "pool", nc.gpsimd, mk("s_pool"))
        self.sp = Eng(self, "sp", nc.sync, mk("s_sp"))
        self.ring = [mk(f"s_dma{i}") for i in range(24)]
        self.ring_i = 0
        self.pring = [mk(f"s_pdma{i}") for i in range(8)]
        self.pring_i = 0
        self.n_inst = 0
        self.out_deps = []

    def dram(self, name, shape, dtype, kind="Internal"):
        return Tile(self.nc.dram_tensor(name, list(shape), dtype, kind=kind).ap(), name)

    def sb(self, stack, name, shape, dtype):
        self.n_alloc = getattr(self, "n_alloc", 0) + 1
        name = f"{name}_{self.n_alloc}"
        return Tile(stack.enter_context(self.nc.sbuf_tensor(name, list(shape), dtype)), name)

    def psum(self, stack, name, shape, dtype):
        self.n_alloc = getattr(self, "n_alloc", 0) + 1
        name = f"{name}_{self.n_alloc}"
        t = Tile(stack.enter_context(self.nc.psum_tensor(name, list(shape), dtype)), name)
        t.excl = True
        return t

    def _wait(self, eng, deps):
        best = {}
        for (st, v) in deps:
            if v is None:
                continue
            if id(st) not in best or best[id(st)][1] < v:
                best[id(st)] = (st, v)
        for st, v in best.values():
            if st is eng.sem and eng.is_pe:
                continue
            if eng.waited.get(id(st), 0) >= v:
                continue
            eng.raw.wait_ge(st.h, v)
            eng.waited[id(st)] = v

    def _collect(self, reads, writes):
        deps = []
        for r in reads:
            for rec in r.tile.recs_dep(r.key):
                if rec.lw is not None:
                    deps.append(rec.lw)
                if r.tile.excl:
                    deps.extend(rec.rd)
        for w in writes:
            for rec in w.tile.recs_dep(w.key):
                if rec.lw is not None:
                    deps.append(rec.lw)
                deps.extend(rec.rd)
        return deps

    def _update(self, reads, writes, tag):
        for r in reads:
            for rec in r.tile.recs_upd(r.key, False):
                rec.rd.append(tag)
                if len(rec.rd) > 48:
                    best = {}
                    for st, v in rec.rd:
                        if id(st) not in best or best[id(st)][1] < v:
                            best[id(st)] = (st, v)
                    rec.rd = list(best.values())
        for w in writes:
            for rec in w.tile.recs_upd(w.key, True):
                rec.lw = tag
                rec.rd = []

    def emit(self, eng, fn, reads, writes):
        deps = self._collect(reads, writes)
        self._wait(eng, deps)
        inst = fn()
        eng.sem.count += 1
        inst.then_inc(eng.sem.h, 1)
        self._update(reads, writes, (eng.sem, eng.sem.count))
        self.n_inst += 1
        return inst

    def dma(self, out, in_, via_pool=False, **kw):
        eng = self.pool if via_pool else self.sp
        if via_pool:
            st = self.pring[self.pring_i % len(self.pring)]
            self.pring_i += 1
        else:
            st = self.ring[self.ring_i % len(self.ring)]
            self.ring_i += 1
        deps = self._collect([in_], [out])
        deps.append((st, st.count))
        self._wait(eng, deps)
        inst = eng.raw.dma_start(out=out.ap, in_=in_.ap, **kw)
        st.count += 16
        inst.then_inc(st.h, 16)
        tag = (st, st.count)
        self._update([in_], [out], tag)
        self.n_inst += 1
        return tag

    def barrier(self):
        deps = [(st, st.count) for st in self.ring + self.pring]
        deps += [(e.sem, e.sem.count) for e in (self.pe, self.act, self.dve, self.pool)]
        deps = [d for d in deps if d[1] > 0]
        for e in (self.pe, self.act, self.dve, self.pool, self.sp):
            self._wait(e, [d for d in deps if not (d[0] is e.sem)])

    def finish(self, out_tiles):
        deps = [(st, st.count) for st in self.ring + self.pring]
        deps += [(e.sem, e.sem.count) for e in (self.pe, self.act, self.dve, self.pool)]
        self._wait(self.sp, [d for d in deps if d[1] > 0])
        self.es.close()


def _bf(a):
    return np.ascontiguousarray(a.astype(np.float32)).astype(ml_dtypes.bfloat16)


def make_consts():
    c = {}
    c["ident_bf"] = _bf(np.eye(128))
    c["ident_f"] = np.eye(128, dtype=np.float32)
    sp = np.arange(128)[:, None]
    tq = np.arange(512)[None, :]
    c["cms"] = _bf(np.stack([(128 * j + sp <= tq) for j in range(4)], axis=1))
    tq1 = np.arange(128)[None, :]
    c["cmw"] = _bf(np.stack([(sp <= tq1), (sp >= tq1)], axis=1))
    dm = []
    for delta in range(-3, 17):
        d = tq - sp + 128 * delta
        cnt = ((d >= 0) & (d <= 128)).astype(np.float32)
        cnt += ((d >= 0) & (d <= 512) & (d % 4 == 0))
        cnt += ((d >= 0) & (d <= 2048) & (d % 16 == 0))
        dm.append(cnt)
    c["dm"] = _bf(np.stack(dm, axis=1))
    j = np.arange(64)[:, None, None]
    kt = np.arange(32)[None, :, None]
    s = np.arange(128)[None, None, :]
    c["ek"] = _bf(((128 * kt + s) // 64 == j))
    mc = np.zeros((128, 16, 512), np.float32)
    for b in range(8):
        for ct in range(2):
            mc[:, b * 2 + ct, :] = (16 * (128 * ct + sp) + 31 <= 512 * b + tq)
    c["mc"] = _bf(mc)
    c["gc"] = (16 * np.arange(256)[None, :] + 31 - np.arange(128)[:, None]).astype(np.float32)
    c["d0"] = (64 * np.arange(64)[None, :] - np.arange(128)[:, None]).astype(np.float32)
    i = np.arange(128)[:, None]
    t = np.arange(128)[None, :]
    c["tri"] = ((i // 64 == t // 64) & (i <= t)).astype(np.float32)
    i6 = np.arange(64)[:, None]
    t6 = np.arange(64)[None, :]
    c["mstrict"] = np.tile((i6 < t6).astype(np.float32)[:, None, :], (1, 8, 1))
    c["mincl"] = np.tile((i6 <= t6).astype(np.float32)[:, None, :], (1, 8, 1))
    c["mstrictT"] = np.tile((i6 > t6).astype(np.float32)[:, None, :], (1, 8, 1))
    return c


CONST_DT = {"ident_bf": BF16, "cms": BF16, "cmw": BF16, "dm": BF16, "ek": BF16, "mc": BF16}

PARAM_SHAPES = {
    'norm_mix': (DEPTH, D), 'w_in': (DEPTH, D, IN_COLS), 'cmp_pos': (DEPTH, 32, 64),
    'cmp_k_w1': (DEPTH, 2048, 128), 'cmp_k_w2': (DEPTH, 128, 64), 'cmp_v_w1': (DEPTH, 2048, 128),
    'cmp_v_w2': (DEPTH, 128, 64), 'beta_nsa': (DEPTH, 256), 'beta_dil': (DEPTH, 256),
    'rwkv_mu': (DEPTH, 1024), 'rwkv_w0': (DEPTH, 256), 'rwkv_w_up': (DEPTH, 64, 256),
    'rwkv_a0': (DEPTH, 256), 'rwkv_a_up': (DEPTH, 64, 256), 'rwkv_g_up': (DEPTH, 128, 256),
    'rwkv_k_k': (DEPTH, 256), 'rwkv_k_a': (DEPTH, 256), 'rwkv_r_k': (DEPTH, 4, 64),
    'rwkv_ln_g': (DEPTH, 256), 'rwkv_ln_b': (DEPTH, 256), 'conv_dw': (DEPTH, 31, 256),
    'conv_dw_b': (DEPTH, 256), 'conv_ln_g': (DEPTH, 256), 'conv_ln_b': (DEPTH, 256),
    'w_out': (DEPTH, D, D), 'norm_ffn': (DEPTH, D), 'ffn_up': (DEPTH, D, 2 * DFF),
    'ffn_dw': (DEPTH, 3, 2 * DFF), 'ffn_dw_b': (DEPTH, 2 * DFF), 'ffn_down': (DEPTH, DFF, D),
    'norm_final': (D,),
}


class Ctx:
    pass


def bcast_rows(ap_1d_row, nparts):
    return ap_1d_row.partition_broadcast(nparts)


def load_bcast(kb, dst_tile, src_dram_tile, row_ap, n):
    kb.dma(out=dst_tile[:, 0:n], in_=Ref(src_dram_tile, None, row_ap.partition_broadcast(128)))


FM_CHUNKS = [
    (0, 128, "qT", 0), (128, 128, "qT", 128), (256, 128, "kcvcT", 0), (384, 64, "ksT", 0), (512, 64, "kwT", 0),
    (652, 128, "dqT", 0), (780, 128, "dqT", 128), (908, 128, "dkT", 0), (1036, 128, "dkT", 128),
    (2188, 128, "loraT", 0), (2316, 128, "loraT", 128),
    (2444, 128, "convT", 0), (2572, 128, "convT", 128), (2700, 128, "convT", 256), (2828, 128, "convT", 384),
]
TM_GROUPS = [(448, 204, "tmA", 0), (1164, 512, "tmB", 0), (1676, 512, "tmB", 512)]


def load_weight_bf16(kb, dst, dram_w, rows0, nk, cols0, ncols):
    for k in range(nk):
        c = 0
        while c < ncols:
            w = min(1024, ncols - c)
            kb.dma(out=dst[:, k, c:c + w],
                   in_=dram_w[rows0 + k * 128: rows0 + (k + 1) * 128, cols0 + c: cols0 + c + w], via_pool=True)
            c += w


def rms_rstd(kb, ssq_ref, out_ref, n, eps, tmp_ref):
    kb.dve.tensor_scalar(out=tmp_ref, in0=ssq_ref, scalar1=1.0 / n, scalar2=eps, op0=ALU.mult, op1=ALU.add)
    kb.act.activation(out=tmp_ref, in_=tmp_ref, func=AF.Sqrt)
    kb.dve.reciprocal(out=out_ref, in_=tmp_ref)


def phase_a(kb, cx, l):
    P = cx.P
    scr = cx.scr
    with ExitStack() as st:
        w_sb = kb.sb(st, "wA", [128, 8, IN_COLS], BF16)
        wblocks = [(b, b * 512, min(512, IN_COLS - b * 512)) for b in range(6)]
        for (key, c0, cw) in wblocks:
            src = P["w_in"].t[l, :, c0:c0 + cw].rearrange("(k p) c -> p k c", p=128)
            kb.dma(out=w_sb.k(key)[:, :, c0:c0 + cw], in_=Ref(P["w_in"], None, src), via_pool=True)

        def wkey(c0, cw):
            return w_sb.k(c0 // 512) if c0 // 512 == (c0 + cw - 1) // 512 else w_sb
        gbc = kb.sb(st, "gbcA", [128, D], F32)
        kb.dma(out=gbc[:, :], in_=Ref(P["norm_mix"], None, P["norm_mix"].t[l:l + 1, :].partition_broadcast(128)))
        xt = [kb.sb(st, f"xtA{i}", [128, 4, D], F32) for i in range(2)]
        hbf = [kb.sb(st, f"hbfA{i}", [128, D], BF16) for i in range(2)]
        junk = kb.sb(st, "junkA", [128, D], BF16)
        hT = [kb.sb(st, f"hTA{i}", [128, 8, 512], BF16) for i in range(2)]
        small = kb.sb(st, "smallA", [128, 16], F32)
        stg_bf = [kb.sb(st, f"stgbA{i}", [128, 512], BF16) for i in range(3)]
        stg_f = [kb.sb(st, f"stgfA{i}", [128, 512], F32) for i in range(3)]
        tp = [kb.psum(st, f"tpA{i}", [128, 8, 128], BF16) for i in range(2)]
        acc = [kb.psum(st, f"accA{i}", [128, 512], F32) for i in range(4)]
        n_acc = 0
        n_stg = 0
        for b in range(NBLK):
            x_t = xt[b % 2]
            kb.dma(out=x_t[:, :, :], in_=cx.xres[b * 512:(b + 1) * 512, :].with_ap(
                cx.xres.t[b * 512:(b + 1) * 512, :].rearrange("(j p) d -> p j d", p=128)))
            h_T = hT[b % 2]
            for j in range(4):
                hb = hbf[j % 2]
                ssq = small[:, j:j + 1]
                kb.act.activation(out=junk[:, :], in_=x_t[:, j, :], func=AF.Square, accum_out=ssq)
                rms_rstd(kb, ssq, small[:, 4 + j:5 + j], D, 1e-6, small[:, 8 + j:9 + j])
                kb.dve.scalar_tensor_tensor(out=hb[:, :], in0=x_t[:, j, :], scalar=small[:, 4 + j:5 + j], in1=gbc[:, :],
                                            op0=ALU.mult, op1=ALU.mult)
                t_p = tp[j % 2]
                for kc in range(8):
                    kb.pe.transpose(out=t_p[:, kc, :], in_=hb[:, kc * 128:(kc + 1) * 128], identity=cx.ident_bf[:, :])
                kb.act.copy(out=h_T[:, :, j * 128:(j + 1) * 128], in_=t_p[:, :, :])
            for (c0, cw, dst, r0) in FM_CHUNKS:
                a = acc[n_acc % 4]
                n_acc += 1
                for kc in range(8):
                    kb.pe.matmul(out=a[0:cw, :], lhsT=wkey(c0, cw)[:, kc, c0:c0 + cw], rhs=h_T[:, kc, :], start=(kc == 0), stop=(kc == 7))
                dt_f32 = dst in ("loraT", "convT")
                sg = (stg_f if dt_f32 else stg_bf)[n_stg % 3]
                if n_stg % 2 == 0:
                    kb.dve.tensor_copy(out=sg[0:cw, :], in_=a[0:cw, :])
                else:
                    kb.act.copy(out=sg[0:cw, :], in_=a[0:cw, :])
                n_stg += 1
                kb.dma(out=getattr(scr, dst).k(b)[r0:r0 + cw, b * 512:(b + 1) * 512], in_=sg[0:cw, :])
            for j in range(4):
                for (c0, cw, dst, d0) in TM_GROUPS:
                    a = acc[n_acc % 4]
                    n_acc += 1
                    for kc in range(8):
                        kb.pe.matmul(out=a[:, 0:cw], lhsT=h_T[:, kc, j * 128:(j + 1) * 128], rhs=wkey(c0, cw)[:, kc, c0:c0 + cw],
                                     start=(kc == 0), stop=(kc == 7))
                    sg = stg_f[n_stg % 3]
                    if n_stg % 2 == 0:
                        kb.dve.tensor_copy(out=sg[:, 0:cw], in_=a[:, 0:cw])
                    else:
                        kb.act.copy(out=sg[:, 0:cw], in_=a[:, 0:cw])
                    n_stg += 1
                    r = b * 512 + j * 128
                    kb.dma(out=getattr(scr, dst).k(b)[r:r + 128, d0:d0 + cw], in_=sg[:, 0:cw])
        kb.barrier()


def phase_conv(kb, cx, l):
    P = cx.P
    scr = cx.scr
    with ExitStack() as st:
        glu = [kb.sb(st, f"gluD{i}", [128, 30 + S], F32) for i in range(2)]
        accs = [kb.sb(st, f"accD{i}", [128, S], F32) for i in range(2)]
        bt = kb.sb(st, "btD", [128, S], F32)
        wdw = kb.sb(st, "wdwD", [128, 2, 32], F32)
        lng = kb.sb(st, "lngD", [128, 256], F32)
        lnb = kb.sb(st, "lnbD", [128, 256], F32)
        small = kb.sb(st, "smallD", [128, 16], F32)
        stats = kb.sb(st, "statsD", [128, 8], F32)
        xn = [kb.sb(st, f"xnD{i}", [128, 256], F32) for i in range(2)]
        tps = [kb.psum(st, f"tpD{i}", [128, 512], F32) for i in range(2)]
        kb.dma(out=lng[:, :], in_=Ref(P["conv_ln_g"], None, P["conv_ln_g"].t[l:l + 1, :].partition_broadcast(128)))
        kb.dma(out=lnb[:, :], in_=Ref(P["conv_ln_b"], None, P["conv_ln_b"].t[l:l + 1, :].partition_broadcast(128)))
        for ci in range(2):
            with kb.nc.allow_non_contiguous_dma(reason="tiny transposed conv weights"):
                kb.dma(out=wdw[:, ci, 0:31], in_=Ref(P["conv_dw"], None,
                       P["conv_dw"].t[l, :, ci * 128:(ci + 1) * 128].rearrange("k c -> c k")))
                kb.dma(out=wdw[:, ci, 31:32], in_=Ref(P["conv_dw_b"], None,
                       P["conv_dw_b"].t[l:l + 1, ci * 128:(ci + 1) * 128].rearrange("o c -> c o")))
            g = glu[ci]
            kb.pool.memset(ap=g[:, 0:30], constant=0.0)
            kb.dma(out=g[:, 30:30 + S], in_=scr.convT[ci * 128:(ci + 1) * 128, :])
            kb.dma(out=bt[:, :], in_=scr.convT[256 + ci * 128:256 + (ci + 1) * 128, :])
            kb.act.activation(out=bt[:, :], in_=bt[:, :], func=AF.Sigmoid)
            kb.pool.tensor_tensor(out=g[:, 30:30 + S], in0=g[:, 30:30 + S], in1=bt[:, :], op=ALU.mult)
            a = accs[ci]
            for h0 in range(0, S, 2048):
                kb.dve.tensor_scalar(out=a[:, h0:h0 + 2048], in0=g[:, 30 + h0:30 + h0 + 2048], scalar1=wdw[:, ci, 30:31],
                                     scalar2=wdw[:, ci, 31:32], op0=ALU.mult, op1=ALU.add)
                for j in range(30):
                    kb.dve.scalar_tensor_tensor(out=a[:, h0:h0 + 2048], in0=g[:, j + h0:j + h0 + 2048], scalar=wdw[:, ci, j:j + 1],
                                                in1=a[:, h0:h0 + 2048], op0=ALU.mult, op1=ALU.add)
        for i in range(NT):
            tp = tps[i % 2]
            for ci in range(2):
                kb.pe.transpose(out=tp[:, ci * 128:(ci + 1) * 128], in_=accs[ci][:, i * 128:(i + 1) * 128], identity=cx.ident_f[:, :])
            x_n = xn[i % 2]
            kb.dve.bn_stats(out=stats[:, 0:6], in_=tp[:, 0:256])
            kb.dve.bn_aggr(out=small[:, 0:2], in_=stats[:, 0:6])
            kb.dve.tensor_scalar(out=small[:, 2:3], in0=small[:, 1:2], scalar1=1e-5, scalar2=None, op0=ALU.add)
            kb.act.activation(out=small[:, 2:3], in_=small[:, 2:3], func=AF.Sqrt)
            kb.dve.reciprocal(out=small[:, 3:4], in_=small[:, 2:3])
            kb.dve.tensor_scalar(out=x_n[:, :], in0=tp[:, 0:256], scalar1=small[:, 0:1], scalar2=small[:, 3:4],
                                 op0=ALU.subtract, op1=ALU.mult)
            kb.pool.tensor_tensor(out=x_n[:, :], in0=x_n[:, :], in1=lng[:, :], op=ALU.mult)
            kb.pool.tensor_tensor(out=x_n[:, :], in0=x_n[:, :], in1=lnb[:, :], op=ALU.add)
            kb.act.activation(out=x_n[:, :], in_=x_n[:, :], func=AF.Silu)
            kb.dma(out=scr.ymix.k(("d", i))[i * 128:(i + 1) * 128, 768:1024], in_=x_n[:, :])
        kb.barrier()


def load_w_bf16(kb, dst, wtile, l, nk, ncols):
    for k in range(nk):
        c = 0
        while c < ncols:
            w = min(1024, ncols - c)
            kb.dma(out=dst[:, k, c:c + w], in_=wtile[l, k * 128:(k + 1) * 128, c:c + w], via_pool=True)
            c += w


def load_w_blocks(kb, dst, wtile, l, nk, blocks):
    for (key, c0, cw) in blocks:
        src = wtile.t[l, 0:nk * 128, c0:c0 + cw].rearrange("(k p) c -> p k c", p=128)
        kb.dma(out=dst.k(key)[:, 0:nk, c0:c0 + cw], in_=Ref(wtile, None, src), via_pool=True)


def phase_b(kb, cx, l):
    P = cx.P
    scr = cx.scr
    with ExitStack() as st:
        wo = kb.sb(st, "woB", [128, 8, D], BF16)
        load_w_bf16(kb, wo, P["w_out"], l, 8, D)
        yt = [kb.sb(st, f"ytB{i}", [128, D], F32) for i in range(2)]
        xt = [kb.sb(st, f"xtB{i}", [128, D], F32) for i in range(2)]
        ybf = [kb.sb(st, f"ybfB{i}", [128, D], BF16) for i in range(2)]
        yT = [kb.sb(st, f"yTB{i}", [128, 8, 128], BF16) for i in range(2)]
        tp = [kb.psum(st, f"tpB{i}", [128, 8, 128], BF16) for i in range(2)]
        acc = [kb.psum(st, f"accB{i}", [128, 512], F32) for i in range(4)]
        na = 0
        for i in range(NT):
            y_t, x_t, y_b, y_T, t_p = yt[i % 2], xt[i % 2], ybf[i % 2], yT[i % 2], tp[i % 2]
            kb.dma(out=y_t[:, :], in_=scr.ymix[i * 128:(i + 1) * 128, :])
            kb.dma(out=x_t[:, :], in_=cx.xres[i * 128:(i + 1) * 128, :])
            kb.pool.tensor_copy(out=y_b[:, :], in_=y_t[:, :])
            for kc in range(8):
                kb.pe.transpose(out=t_p[:, kc, :], in_=y_b[:, kc * 128:(kc + 1) * 128], identity=cx.ident_bf[:, :])
            kb.act.copy(out=y_T[:, :, :], in_=t_p[:, :, :])
            for c0 in (0, 512):
                a = acc[na % 4]
                na += 1
                for kc in range(8):
                    kb.pe.matmul(out=a[:, :], lhsT=y_T[:, kc, :], rhs=wo[:, kc, c0:c0 + 512], start=(kc == 0), stop=(kc == 7))
                kb.dve.tensor_tensor(out=x_t[:, c0:c0 + 512], in0=x_t[:, c0:c0 + 512], in1=a[:, :], op=ALU.add)
            kb.dma(out=cx.xres[i * 128:(i + 1) * 128, :], in_=x_t[:, :])
        kb.barrier()


def phase_c(kb, cx, l):
    P = cx.P
    TB = 256
    with ExitStack() as st:
        wu = kb.sb(st, "wuC", [128, 8, 2 * DFF], BF16)
        wd = kb.sb(st, "wdC", [128, 22, D], BF16)
        order = []
        for i in range(6):
            order += [i, i + 5] if i + 5 < 11 else [i]
        order = [b for b in dict.fromkeys(order) if b < 11]
        load_w_blocks(kb, wu, P["ffn_up"], l, 8, [(b, b * 512, 512) for b in order])
        for k in range(22):
            kb.dma(out=wd.k(k)[:, k, :], in_=P["ffn_down"][l, k * 128:(k + 1) * 128, :], via_pool=True)
        gbc = kb.sb(st, "gbcC", [128, D], F32)
        kb.dma(out=gbc[:, :], in_=Ref(P["norm_ffn"], None, P["norm_ffn"].t[l:l + 1, :].partition_broadcast(128)))
        cw = kb.sb(st, "cwC", [128, 44, 4], F32)
        with kb.nc.allow_non_contiguous_dma(reason="tiny transposed conv weights"):
            for j in range(3):
                kb.dma(out=cw[:, :, j:j + 1], in_=Ref(P["ffn_dw"], None,
                       P["ffn_dw"].t[l, j:j + 1, :].rearrange("o (c p) -> p c o", p=128)))
            kb.dma(out=cw[:, :, 3:4], in_=Ref(P["ffn_dw_b"], None,
                   P["ffn_dw_b"].t[l:l + 1, :].rearrange("o (c p) -> p c o", p=128)))
        carry = kb.sb(st, "carryC", [128, 44, 2], F32)
        kb.pool.memset(ap=carry[:, :, :], constant=0.0)
        xt = [kb.sb(st, f"xtC{i}", [128, 2, D], F32) for i in range(2)]
        hbf = [kb.sb(st, f"hbfC{i}", [128, D], BF16) for i in range(2)]
        junk = kb.sb(st, "junkC", [128, D], BF16)
        hT = [kb.sb(st, f"hTC{i}", [128, 8, TB], BF16) for i in range(2)]
        small = kb.sb(st, "smallC", [128, 16], F32)
        G = [kb.sb(st, f"GC{i}", [128, 22, TB], BF16) for i in range(2)]
        ub = [kb.sb(st, f"ubC{i}", [128, TB + 2], F32) for i in range(4)]
        ac = [kb.sb(st, f"acC{i}", [128, TB], F32) for i in range(4)]
        tp = [kb.psum(st, f"tpC{i}", [128, 8, 128], BF16) for i in range(2)]
        ups = [kb.psum(st, f"upC{i}", [128, 512], F32) for i in range(4)]
        dps = [kb.psum(st, f"dpC{i}", [128, 512], F32) for i in range(2)]
        nu = 0
        nd = 0
        for b in range(S // TB):
            x_t, h_T, Gb = xt[b % 2], hT[b % 2], G[b % 2]
            kb.dma(out=x_t[:, :, :], in_=cx.xres[b * TB:(b + 1) * TB, :].with_ap(
                cx.xres.t[b * TB:(b + 1) * TB, :].rearrange("(j p) d -> p j d", p=128)))
            for j in range(2):
                hb = hbf[j]
                kb.act.activation(out=junk[:, :], in_=x_t[:, j, :], func=AF.Square, accum_out=small[:, j:j + 1])
                rms_rstd(kb, small[:, j:j + 1], small[:, 4 + j:5 + j], D, 1e-6, small[:, 8 + j:9 + j])
                kb.dve.scalar_tensor_tensor(out=hb[:, :], in0=x_t[:, j, :], scalar=small[:, 4 + j:5 + j], in1=gbc[:, :],
                                            op0=ALU.mult, op1=ALU.mult)
                t_p = tp[j]
                for kc in range(8):
                    kb.pe.transpose(out=t_p[:, kc, :], in_=hb[:, kc * 128:(kc + 1) * 128], identity=cx.ident_bf[:, :])
                kb.act.copy(out=h_T[:, :, j * 128:(j + 1) * 128], in_=t_p[:, :, :])
            for ci in range(22):
                res = []
                for half in range(2):
                    c = ci + 22 * half
                    u = ups[nu % 4]
                    u_b = ub[nu % 4]
                    a = ac[nu % 4]
                    nu += 1
                    for kc in range(8):
                        kb.pe.matmul(out=u[:, 0:TB], lhsT=wu.k(c // 4)[:, kc, c * 128:(c + 1) * 128], rhs=h_T[:, kc, :],
                                     start=(kc == 0), stop=(kc == 7))
                    kb.act.copy(out=u_b[:, 2:TB + 2], in_=u[:, 0:TB])
                    kb.pool.tensor_copy(out=u_b[:, 0:2], in_=carry[:, c, :])
                    kb.dve.tensor_scalar(out=a[:, :], in0=u[:, 0:TB], scalar1=cw[:, c, 2:3], scalar2=cw[:, c, 3:4],
                                         op0=ALU.mult, op1=ALU.add)
                    kb.dve.scalar_tensor_tensor(out=a[:, :], in0=u_b[:, 1:TB + 1], scalar=cw[:, c, 1:2], in1=a[:, :],
                                                op0=ALU.mult, op1=ALU.add)
                    kb.dve.scalar_tensor_tensor(out=a[:, :], in0=u_b[:, 0:TB], scalar=cw[:, c, 0:1], in1=a[:, :],
                                                op0=ALU.mult, op1=ALU.add)
                    kb.pool.tensor_copy(out=carry[:, c, :], in_=u_b[:, TB:TB + 2])
                    res.append(a)
                kb.act.activation(out=res[0][:, :], in_=res[0][:, :], func=AF.Silu)
                kb.pool.tensor_tensor(out=Gb[:, ci, :], in0=res[0][:, :], in1=res[1][:, :], op=ALU.mult)
            for j in range(2):
                for c0 in (0, 512):
                    d = dps[nd % 2]
                    nd += 1
                    for ci in range(22):
                        kb.pe.matmul(out=d[:, :], lhsT=Gb[:, ci, j * 128:(j + 1) * 128], rhs=wd.k(ci)[:, ci, c0:c0 + 512],
                                     start=(ci == 0), stop=(ci == 21))
                    kb.dve.tensor_tensor(out=x_t[:, j, c0:c0 + 512], in0=x_t[:, j, c0:c0 + 512], in1=d[:, :], op=ALU.add)
            kb.dma(out=cx.xres[b * TB:(b + 1) * TB, :].with_ap(
                cx.xres.t[b * TB:(b + 1) * TB, :].rearrange("(j p) d -> p j d", p=128)), in_=x_t[:, :, :])
        kb.barrier()


def phase_final(kb, cx, y_out):
    P = cx.P
    with ExitStack() as st:
        gbc = kb.sb(st, "gbcF", [128, D], F32)
        kb.dma(out=gbc[:, :], in_=Ref(P["norm_final"], None, P["norm_final"].t.rearrange("(o d) -> o d", o=1).partition_broadcast(128)))
        xt = [kb.sb(st, f"xtF{i}", [128, D], F32) for i in range(2)]
        ot = [kb.sb(st, f"otF{i}", [128, D], F32) for i in range(2)]
        junk = kb.sb(st, "junkF", [128, D], BF16)
        small = kb.sb(st, "smallF", [128, 16], F32)
        for i in range(NT):
            x_t, o_t = xt[i % 2], ot[i % 2]
            kb.dma(out=x_t[:, :], in_=cx.xres[i * 128:(i + 1) * 128, :])
            kb.act.activation(out=junk[:, :], in_=x_t[:, :], func=AF.Square, accum_out=small[:, 0:1])
            rms_rstd(kb, small[:, 0:1], small[:, 1:2], D, 1e-6, small[:, 2:3])
            kb.dve.scalar_tensor_tensor(out=o_t[:, :], in0=x_t[:, :], scalar=small[:, 1:2], in1=gbc[:, :],
                                        op0=ALU.mult, op1=ALU.mult)
            kb.dma(out=y_out[i * 128:(i + 1) * 128, :], in_=o_t[:, :])
        kb.barrier()


class AttnBufs:
    def __init__(self, kb, st, tag):
        self.sps = [kb.psum(st, f"sps{tag}{i}", [128, 512], F32) for i in range(2)]
        self.acc = kb.psum(st, f"acc{tag}", [128, 4, 512], F32)
        self.pts = [kb.sb(st, f"pts{tag}{i}", [128, 512], BF16) for i in range(3)]
        self.ns = 0
        self.nm = 0


def attn_core(kb, A, q_rhs, kv_list, heads_view=False):
    n = len(kv_list)
    for idx, (kT, Vp, mask) in enumerate(kv_list):
        sp = A.sps[A.ns % 2]
        pt = A.pts[A.ns % 3]
        A.ns += 1
        if heads_view:
            spv = sp[:, :].with_ap(sp.t[:, :].rearrange("p (h q) -> p h q", h=4))
            ptv = pt[:, :].with_ap(pt.t[:, :].rearrange("p (h q) -> p h q", h=4))
        else:
            spv, ptv = sp[:, :], pt[:, :]
        kb.pe.matmul(out=sp[:, :], lhsT=kT, rhs=q_rhs, start=True, stop=True)
        kb.act.activation(out=pt[:, :], in_=sp[:, :], func=AF.Exp, scale=0.125)
        if mask is not None:
            eng = kb.dve if (A.nm % 3 != 2) else kb.pool
            A.nm += 1
            eng.tensor_tensor(out=ptv, in0=ptv, in1=mask, op=ALU.mult)
        for j in range(4):
            kb.pe.matmul(out=A.acc[:, j, 0:65], lhsT=pt[:, j * 128:(j + 1) * 128], rhs=Vp, start=(idx == 0), stop=(idx == n - 1))


def attn_evac(kb, A, small, dst, gate=None, first=True, tmp=None):
    kb.dve.tensor_scalar(out=small[:, 0:4], in0=A.acc[:, :, 64], scalar1=1e-30, scalar2=None, op0=ALU.max)
    kb.dve.reciprocal(out=small[:, 4:8], in_=small[:, 0:4])
    if gate is not None:
        kb.dve.tensor_tensor(out=small[:, 4:8], in0=small[:, 4:8], in1=gate, op=ALU.mult)
    sc = small[:, 4:8].with_ap(small.t[:, 4:8].unsqueeze(2).to_broadcast([128, 4, 64]))
    if first:
        kb.dve.tensor_tensor(out=dst, in0=A.acc[:, :, 0:64], in1=sc, op=ALU.mult)
    else:
        kb.dve.tensor_tensor(out=tmp, in0=A.acc[:, :, 0:64], in1=sc, op=ALU.mult)
        kb.pool.tensor_tensor(out=dst, in0=dst, in1=tmp, op=ALU.add)


def group_rmsnorm_store(kb, ob_ref, beta_bc, small, junk, stage_ref, dram_ref):
    kb.act.activation(out=junk, in_=ob_ref, func=AF.Square, accum_out=small[:, 8:9])
    rms_rstd(kb, small[:, 8:9], small[:, 9:10], 256, 1e-6, small[:, 10:11])
    kb.dve.scalar_tensor_tensor(out=stage_ref, in0=ob_ref, scalar=small[:, 9:10], in1=beta_bc, op0=ALU.mult, op1=ALU.mult)
    kb.dma(out=dram_ref, in_=stage_ref)


def build_vprime(kb, vp, src_dram_cols, ld, nh):
    kb.dma(out=ld[:, :, 0:nh * 64], in_=src_dram_cols.with_ap(src_dram_cols.ap.rearrange("(i p) c -> p i c", p=128)))
    kb.pool.memset(ap=vp[:, :, :, 64:65], constant=1.0)
    for h in range(nh):
        kb.dve.tensor_copy(out=vp[:, :, h, 0:64], in_=ld[:, :, h * 64:(h + 1) * 64])


def phase_dil(kb, cx, l):
    P, scr, C = cx.P, cx.scr, cx.C
    with ExitStack() as st:
        qT = kb.sb(st, "qTd", [64, 4, S], BF16)
        kT = kb.sb(st, "kTd", [64, 4, S], BF16)
        kb.dma(out=qT[:, :, :], in_=scr.dqT[:, :].with_ap(scr.dqT.t.rearrange("(h d) s -> d h s", d=64)))
        kb.dma(out=kT[:, :, :], in_=scr.dkT[:, :].with_ap(scr.dkT.t.rearrange("(h d) s -> d h s", d=64)))
        vp = kb.sb(st, "vpd", [128, NT, 4, 65], BF16)
        with ExitStack() as st2:
            ld = kb.sb(st2, "ldd", [128, NT, 256], F32)
            build_vprime(kb, vp, scr.tmB[:, 0:256], ld, 4)
            kb.barrier()
        dm = kb.sb(st, "dmd", [128, 20, 512], BF16)
        kb.dma(out=dm[:, :, :], in_=C["dm"][:, :, :])
        beta = kb.sb(st, "betad", [128, 256], F32)
        kb.dma(out=beta[:, :], in_=Ref(P["beta_dil"], None, P["beta_dil"].t[l:l + 1, :].partition_broadcast(128)))
        ob = [kb.sb(st, f"obd{i}", [128, 4, 256], F32) for i in range(2)]
        stage = [kb.sb(st, f"stgd{i}", [128, 256], F32) for i in range(2)]
        junk = kb.sb(st, "junkd", [128, 256], BF16)
        small = kb.sb(st, "smalld", [128, 16], F32)
        A = AttnBufs(kb, st, "d")
        for b in range(NBLK):
            o_b = ob[b % 2]
            for h in range(4):
                kv = []
                for kt in range(max(0, 4 * b - 16), 4 * b + 4):
                    delta = 4 * b - kt
                    kv.append((kT[:, h, kt * 128:(kt + 1) * 128], vp[:, kt, h, :], dm[:, delta + 3, :]))
                attn_core(kb, A, qT[:, h, b * 512:(b + 1) * 512], kv)
                attn_evac(kb, A, small, o_b[:, :, h * 64:(h + 1) * 64])
            for qt in range(4):
                i = b * 4 + qt
                group_rmsnorm_store(kb, o_b[:, qt, :], beta[:, :], small, junk[:, :], stage[qt % 2][:, :],
                                    scr.ymix.k(("b", i))[i * 128:(i + 1) * 128, 256:512])
        kb.barrier()
def phase_nsa(kb, cx, l):
    P, scr, C = cx.P, cx.scr, cx.C
    with ExitStack() as st:
        qT = kb.sb(st, "qTn", [64, 4, S], BF16)
        kb.dma(out=qT[:, :, :], in_=scr.qT[:, :].with_ap(scr.qT.t.rearrange("(h d) s -> d h s", d=64)))
        ksT = kb.sb(st, "ksTn", [64, S], BF16)
        kwT = kb.sb(st, "kwTn", [64, S], BF16)
        kb.dma(out=ksT[:, :], in_=scr.ksT[:, :])
        kb.dma(out=kwT[:, :], in_=scr.kwT[:, :])
        vps = kb.sb(st, "vpsn", [128, NT, 1, 65], BF16)
        vpw = kb.sb(st, "vpwn", [128, NT, 1, 65], BF16)
        gts = kb.sb(st, "gtsn", [128, NT, 12], F32)
        kcmpT = kb.sb(st, "kcmpTn", [64, 256], BF16)
        vpc = kb.sb(st, "vpcn", [128, 2, 65], BF16)
        selT = kb.sb(st, "selTn", [64, S], BF16)
        small = kb.sb(st, "smalln", [128, 16], F32)
        A = AttnBufs(kb, st, "n")
        x1 = kb.psum(st, "x1n", [128, 512], F32)
        x2 = kb.psum(st, "x2n", [128, 512], F32)
        with ExitStack() as st2:
            ld = kb.sb(st2, "ldn", [128, NT, 204], F32)
            kb.dma(out=ld[:, :, :], in_=scr.tmA[:, :].with_ap(scr.tmA.t.rearrange("(i p) c -> p i c", p=128)))
            kb.pool.memset(ap=vps[:, :, :, 64:65], constant=1.0)
            kb.pool.memset(ap=vpw[:, :, :, 64:65], constant=1.0)
            kb.dve.tensor_copy(out=vps[:, :, 0, 0:64], in_=ld[:, :, 0:64])
            kb.dve.tensor_copy(out=vpw[:, :, 0, 0:64], in_=ld[:, :, 128:192])
            kb.act.activation(out=gts[:, :, :], in_=ld[:, :, 192:204], func=AF.Sigmoid)
            x2t = kb.sb(st2, "x2n_", [128, S], BF16)
            pos2 = kb.sb(st2, "pos2n", [128, 16], F32)
            w1 = kb.sb(st2, "w1n", [128, 16, 128], BF16)
            w2 = kb.sb(st2, "w2n", [128, 64], BF16)
            am = kb.sb(st2, "amn", [128, 16, 256], BF16)
            hidT = kb.sb(st2, "hidTn", [128, 256], BF16)
            with kb.nc.allow_non_contiguous_dma(reason="tiny pos table"):
                kb.dma(out=pos2[:, :], in_=Ref(P["cmp_pos"], None, P["cmp_pos"].t[l].rearrange("(m i) d -> (i d) m", i=2)))
            for kind in ("k", "v"):
                r0 = 0 if kind == "k" else 64
                kb.pool.memset(ap=x2t[:, S - 1:S], constant=0.0)
                kb.dma(out=x2t[0:64, :], in_=scr.kcvcT[r0:r0 + 64, :])
                kb.dma(out=x2t[64:128, 0:S - 1], in_=scr.kcvcT[r0:r0 + 64, 1:S])
                wn1 = P["cmp_k_w1" if kind == "k" else "cmp_v_w1"]
                wn2 = P["cmp_k_w2" if kind == "k" else "cmp_v_w2"]
                kb.dma(out=w1[:, :, :], in_=wn1[l].with_ap(wn1.t[l].rearrange("(m p) h -> p m h", p=128)), via_pool=True)
                kb.dma(out=w2[:, :], in_=wn2[l, :, :], via_pool=True)
                kb.pool.memset(ap=am[:, :, 255:256], constant=0.0)
                xv = x2t.t[:, :].rearrange("p (c r) -> p c r", r=16)
                for m in range(16):
                    src = xv[:, 0:255, 2 * m] if m < 8 else xv[:, 1:256, 2 * m - 16]
                    kb.dve.tensor_scalar(out=am[:, m, 0:255], in0=Ref(x2t, None, src), scalar1=pos2[:, m:m + 1], scalar2=None, op0=ALU.add)
                for m in range(16):
                    kb.pe.matmul(out=x1[:, 0:256], lhsT=w1[:, m, :], rhs=am[:, m, :], start=(m == 0), stop=(m == 15))
                kb.act.activation(out=hidT[:, :], in_=x1[:, 0:256], func=AF.Silu)
                if kind == "k":
                    kb.pe.matmul(out=x2[0:64, 0:256], lhsT=w2[:, :], rhs=hidT[:, :], start=True, stop=True)
                    kb.dve.tensor_copy(out=kcmpT[:, :], in_=x2[0:64, 0:256])
                else:
                    kb.pool.memset(ap=vpc[:, :, 64:65], constant=1.0)
                    for ct in range(2):
                        kb.pe.matmul(out=x2[:, ct * 64:(ct + 1) * 64], lhsT=hidT[:, ct * 128:(ct + 1) * 128], rhs=w2[:, :], start=True, stop=True)
                    kb.dve.tensor_copy(out=vpc[:, :, 0:64], in_=x2[:, 0:128].with_ap(x2.t[:, 0:128].rearrange("p (c d) -> p c d", c=2)))
            kb.barrier()
        with ExitStack() as st3:
            gc = kb.sb(st3, "gcn", [128, 256], F32)
            d0 = kb.sb(st3, "d0n", [128, 64], F32)
            kb.dma(out=gc[:, :], in_=C["gc"][:, :])
            kb.dma(out=d0[:, :], in_=C["d0"][:, :])
            pex = [kb.sb(st3, f"pexn{i}", [128, 4, 256], F32) for i in range(2)]
            imp = [kb.sb(st3, f"impn{i}", [128, 258], F32) for i in range(2)]
            chk = kb.sb(st3, "chkn", [128, 256], F32)
            blk = kb.sb(st3, "blkn", [128, 64], F32)
            sc = kb.sb(st3, "scn", [128, 64], F32)
            sc2 = kb.sb(st3, "sc2n", [128, 64], F32)
            vm = kb.sb(st3, "vmn", [128, 64], F32)
            m8 = kb.sb(st3, "m8n", [128, 16], F32)
            sel = [kb.sb(st3, f"seln{i}", [128, 64], BF16) for i in range(2)]
            for i in range(2):
                kb.pool.memset(ap=imp[i][:, :], constant=0.0)
            scps = [A.acc.k(0), A.acc.k(1)]
            for i in range(NT):
                pe_, im = pex[i % 2], imp[i % 2]
                base = (i % 2) * 2
                scv = A.acc.k(i % 2)[:, base:base + 2, :].with_ap(
                    A.acc.t[:, base:base + 2, :].rearrange("p b (h c) -> p (b h) c", h=2))
                for h in range(4):
                    kb.pe.matmul(out=A.acc.k(i % 2)[:, base + h // 2, (h % 2) * 256:(h % 2) * 256 + 256],
                                 lhsT=qT[:, h, i * 128:(i + 1) * 128], rhs=kcmpT[:, :], start=True, stop=True)
                kb.act.activation(out=pe_[:, :, :], in_=scv, func=AF.Exp, scale=0.125)
                gcb = Ref(gc, None, gc.t[:, :].unsqueeze(1).to_broadcast([128, 4, 256]))
                kb.dve.scalar_tensor_tensor(out=pe_[:, :, :], in0=gcb, scalar=float(128 * i), in1=pe_[:, :, :],
                                            op0=ALU.is_le, op1=ALU.mult)
                kb.dve.tensor_reduce(out=small[:, 0:4], in_=pe_[:, :, :], axis=AX.X, op=ALU.add)
                kb.dve.tensor_scalar(out=small[:, 0:4], in0=small[:, 0:4], scalar1=1e-30, scalar2=None, op0=ALU.max)
                kb.dve.reciprocal(out=small[:, 4:8], in_=small[:, 0:4])
                kb.dve.tensor_scalar(out=im[:, 1:257], in0=pe_[:, 0, :], scalar1=small[:, 4:5], scalar2=None, op0=ALU.mult)
                for h in range(1, 4):
                    kb.dve.scalar_tensor_tensor(out=im[:, 1:257], in0=pe_[:, h, :], scalar=small[:, 4 + h:5 + h], in1=im[:, 1:257],
                                                op0=ALU.mult, op1=ALU.add)
                kb.dve.tensor_tensor(out=chk[:, :], in0=im[:, 0:256], in1=im[:, 1:257], op=ALU.add)
                kb.dve.tensor_reduce(out=blk[:, :], in_=chk[:, :].with_ap(chk.t[:, :].rearrange("p (b r) -> p b r", r=4)),
                                     axis=AX.X, op=ALU.add)
                kb.dve.tensor_scalar(out=sc[:, :], in0=d0[:, :], scalar1=float(128 * i - 127), scalar2=1e9, op0=ALU.is_ge, op1=ALU.mult)
                kb.dve.tensor_tensor(out=sc[:, :], in0=sc[:, :], in1=blk[:, :], op=ALU.max)
                kb.dve.memset(ap=sc[:, 0:1], constant=1e9)
                kb.dve.tensor_single_scalar(out=vm[:, :], in_=d0[:, :], scalar=float(128 * i), op=ALU.is_le)
                kb.dve.tensor_tensor(out=sc[:, :], in0=sc[:, :], in1=vm[:, :], op=ALU.mult)
                kb.dve.scalar_tensor_tensor(out=sc[:, :], in0=vm[:, :], scalar=-1.0, in1=sc[:, :], op0=ALU.add, op1=ALU.add)
                kb.dve.max(out=m8[:, 0:8], in_=sc[:, :])
                kb.dve.match_replace(out=sc2[:, :], in_to_replace=m8[:, 0:8], in_values=sc[:, :], imm_value=-3e38)
                kb.dve.max(out=m8[:, 8:16], in_=sc2[:, :])
                kb.dve.tensor_scalar(out=sel[i % 2][:, :], in0=sc[:, :], scalar1=m8[:, 15:16], scalar2=None, op0=ALU.is_ge)
                tpv = x1[0:64, 0:64].with_ap(x1.t[0:64, 0:64].bitcast(BF16))
                kb.pe.transpose(out=tpv, in_=sel[i % 2][:, :], identity=cx.ident_bf[:, :])
                kb.act.copy(out=selT[:, i * 128:(i + 1) * 128], in_=tpv)
            kb.barrier()
        with ExitStack() as st4:
            maskS = kb.sb(st4, "maskSn", [128, NT, 512], BF16)
            mc = kb.sb(st4, "mcn", [128, 16, 512], BF16)
            cms = kb.sb(st4, "cmsn", [128, 4, 512], BF16)
            cmw = kb.sb(st4, "cmwn", [128, 2, 128], BF16)
            ek = kb.sb(st4, "ekn", [64, 32, 128], BF16)
            kb.dma(out=mc[:, :, :], in_=C["mc"][:, :, :])
            kb.dma(out=cms[:, :, :], in_=C["cms"][:, :, :])
            kb.dma(out=cmw[:, :, :], in_=C["cmw"][:, :, :])
            kb.dma(out=ek[:, :, :], in_=C["ek"][:, :, :])
            beta = kb.sb(st4, "betan", [128, 256], F32)
            kb.dma(out=beta[:, :], in_=Ref(P["beta_nsa"], None, P["beta_nsa"].t[l:l + 1, :].partition_broadcast(128)))
            ob = [kb.sb(st4, f"obn{i}", [128, 4, 256], F32) for i in range(2)]
            tmp = kb.sb(st4, "tmpn", [128, 4, 64], F32)
            stage = [kb.sb(st4, f"stgn{i}", [128, 256], F32) for i in range(2)]
            junk = kb.sb(st4, "junkn", [128, 256], BF16)
            xs = [x1, x2]
            nx = 0
            for b in range(NBLK):
                o_b = ob[b % 2]
                for kt in range(4 * b + 4):
                    xp = xs[nx % 2]
                    nx += 1
                    kb.pe.matmul(out=xp[:, :], lhsT=ek[:, kt, :], rhs=selT[:, b * 512:(b + 1) * 512], start=True, stop=True)
                    if kt >= 4 * b:
                        kb.dve.tensor_tensor(out=maskS[:, kt, :], in0=xp[:, :], in1=cms[:, kt - 4 * b, :], op=ALU.mult)
                    else:
                        kb.act.copy(out=maskS[:, kt, :], in_=xp[:, :])
                for h in range(4):
                    dst = o_b[:, :, h * 64:(h + 1) * 64]
                    kv = [(kcmpT[:, 0:128], vpc[:, 0, :], None if b >= 5 else mc[:, 2 * b, :])]
                    if b >= 4:
                        kv.append((kcmpT[:, 128:256], vpc[:, 1, :], mc[:, 2 * b + 1, :]))
                    attn_core(kb, A, qT[:, h, b * 512:(b + 1) * 512], kv)
                    attn_evac(kb, A, small, dst, gate=gts[:, 4 * b:4 * b + 4, 3 * h], first=True)
                    kv = [(ksT[:, kt * 128:(kt + 1) * 128], vps[:, kt, 0, :], maskS[:, kt, :]) for kt in range(4 * b + 4)]
                    attn_core(kb, A, qT[:, h, b * 512:(b + 1) * 512], kv)
                    attn_evac(kb, A, small, dst, gate=gts[:, 4 * b:4 * b + 4, 3 * h + 1], first=False, tmp=tmp[:, :, :])
                for qt in range(4):
                    i = 4 * b + qt
                    kv = []
                    for kt in range(max(0, i - 4), i + 1):
                        mk = None
                        if kt == i:
                            mk = Ref(cmw, None, cmw.t[:, 0:1, :].to_broadcast([128, 4, 128]))
                        elif kt == i - 4:
                            mk = Ref(cmw, None, cmw.t[:, 1:2, :].to_broadcast([128, 4, 128]))
                        kv.append((kwT[:, kt * 128:(kt + 1) * 128], vpw[:, kt, 0, :], mk))
                    attn_core(kb, A, qT[:, :, i * 128:(i + 1) * 128], kv, heads_view=True)
                    dstw = o_b[:, qt, :].with_ap(o_b.t[:, qt, :].rearrange("p (h d) -> p h d", h=4))
                    gw = gts[:, i, :].with_ap(gts.t[:, i, :].rearrange("p (h r) -> p h r", r=3)[:, :, 2])
                    attn_evac(kb, A, small, dstw, gate=gw, first=False, tmp=tmp[:, :, :])
                for qt in range(4):
                    i = b * 4 + qt
                    group_rmsnorm_store(kb, o_b[:, qt, :], beta[:, :], small, junk[:, :], stage[qt % 2][:, :],
                                        scr.ymix.k(("a", i))[i * 128:(i + 1) * 128, 0:256])
            kb.barrier()
C0 = 0.6065306597126334
RWKV_STAGE = [99]


def phase_rwkv(kb, cx, l):
    P, scr, C = cx.P, cx.scr, cx.C
    CH = 64
    NBUF = 3
    with ExitStack() as st:
        def bc(name, src, c0, n, rows=64):
            t = kb.sb(st, name, [rows, n], F32)
            kb.dma(out=t[:, :], in_=Ref(P[src], None, P[src].t[l:l + 1, c0:c0 + n].partition_broadcast(rows)))
            return t
        mu_bc = bc("mu_r", "rwkv_mu", 0, 768)
        w0_bc = bc("w0_r", "rwkv_w0", 0, 256)
        a0_bc = bc("a0_r", "rwkv_a0", 0, 256)
        kk_bc = bc("kk_r", "rwkv_k_k", 0, 256)
        ka_bc = bc("ka_r", "rwkv_k_a", 0, 256)
        lg_bc = bc("lg_r", "rwkv_ln_g", 0, 256)
        lb_bc = bc("lb_r", "rwkv_ln_b", 0, 256)
        rk_bc = kb.sb(st, "rk_r", [64, 256], F32)
        kb.dma(out=rk_bc[:, :], in_=Ref(P["rwkv_r_k"], None,
               P["rwkv_r_k"].t[l:l + 1].rearrange("o h d -> o (h d)").partition_broadcast(64)))
        oka = kb.sb(st, "oka_r", [64, 256], F32)
        kb.dve.tensor_scalar(out=oka[:, :], in0=ka_bc[:, :], scalar1=-1.0, scalar2=1.0, op0=ALU.mult, op1=ALU.add)
        mu_lo = kb.sb(st, "mulo_r", [128, 2], F32)
        with kb.nc.allow_non_contiguous_dma(reason="tiny mu columns"):
            kb.dma(out=mu_lo[:, 0:1], in_=Ref(P["rwkv_mu"], None, P["rwkv_mu"].t[l:l + 1, 768:896].rearrange("o c -> c o")))
            kb.dma(out=mu_lo[:, 1:2], in_=Ref(P["rwkv_mu"], None, P["rwkv_mu"].t[l:l + 1, 896:1024].rearrange("o c -> c o")))
        wa_up = kb.sb(st, "waup_r", [64, 256], F32)
        a_up = kb.sb(st, "aup_r", [64, 256], F32)
        g_up = kb.sb(st, "gup_r", [128, 256], F32)
        mu_a = kb.sb(st, "mua_r", [64, 1], F32)
        with kb.nc.allow_non_contiguous_dma(reason="tiny mu columns"):
            kb.dma(out=mu_a[:, 0:1], in_=Ref(P["rwkv_mu"], None, P["rwkv_mu"].t[l:l + 1, 832:896].rearrange("o c -> c o")))
        kb.dma(out=wa_up[0:64, :], in_=P["rwkv_w_up"][l, :, :])
        kb.dma(out=a_up[0:64, :], in_=P["rwkv_a_up"][l, :, :])
        kb.dma(out=g_up[:, :], in_=P["rwkv_g_up"][l, :, :])
        tri = kb.sb(st, "tri_r", [64, 2, 64], F32)
        kb.dma(out=tri[:, 0, :], in_=C["tri"][0:64, 0:64])
        kb.dma(out=tri[:, 1, :], in_=C["mstrictT"][:, 0, :])
        mst = kb.sb(st, "mst_r", [64, 4, 64], F32)
        mstT = kb.sb(st, "mstT_r", [64, 4, 64], F32)
        minc = kb.sb(st, "minc_r", [64, 4, 64], F32)
        kb.dma(out=mst[:, :, :], in_=C["mstrict"][:, 0:4, :])
        kb.dma(out=mstT[:, :, :], in_=C["mstrictT"][:, 0:4, :])
        kb.dma(out=minc[:, :, :], in_=C["mincl"][:, 0:4, :])
        ST = kb.sb(st, "ST_r", [64, 4, 64], F32)
        kb.pool.memset(ap=ST[:, :, :], constant=0.0)
        idb = Ref(cx.ident_f, None, cx.ident_f.t[0:64, 0:64].unsqueeze(1).to_broadcast([64, 4, 64]))

        def T2(name, shape, n=3):
            return [kb.sb(st, f"{name}{i}_r", shape, F32) for i in range(n)]
        rkv, prv, lo, gd = T2("rkv", [64, 768]), T2("prv", [64, 768]), T2("lo", [64, 65]), T2("gd", [128, 65])
        los, gds = T2("los", [64, 64]), T2("gds", [128, 64])
        loa, loas = T2("loa", [64, 65]), T2("loas", [64, 64])
        sgt, a_t, g_t = T2("sgt", [64, 256]), T2("at", [64, 256]), T2("gt", [64, 256])
        kk, k2, bb = T2("kkt", [64, 256]), T2("k2t", [64, 256]), T2("bbt", [64, 256])
        tmp, tmp2 = T2("tmp", [64, 256]), T2("tmp2", [64, 256])
        Pm, iP, Pp, Pr = T2("Pm", [64, 256]), T2("iP", [64, 256]), T2("Pp", [64, 256]), T2("Pr", [64, 256])
        Kt, Bt, KKt, Rt, Kh, Bh = (T2("Kt", [64, 256]), T2("Bt", [64, 256]), T2("KKt", [64, 256]), T2("Rt", [64, 256]),
                                    T2("Kh", [64, 256]), T2("Bh", [64, 256]))
        FMq = T2("FMq", [64, 16, 64])
        N, NT_, Mak, Abr, Akr = (T2("N", [64, 4, 64]), T2("NT", [64, 4, 64]), T2("Mak", [64, 4, 64]), T2("Abr", [64, 4, 64]),
                                 T2("Akr", [64, 4, 64]))
        TA_ = T2("TA", [64, 8, 64])
        Wsb, Xsb, U0T, UT = T2("Wsb", [64, 4, 64]), T2("Xsb", [64, 4, 64]), T2("U0T", [64, 4, 64]), T2("UT", [64, 4, 64])
        pcs = T2("pcs", [64, 4])
        yv = T2("yv", [64, 256])
        small = T2("small", [64, 16])
        B = [kb.psum(st, f"B{i}_r", [64, 512], F32) for i in range(8)]

        def v3(ref_tile, c0):
            return ref_tile[:, c0:c0 + 256].with_ap(ref_tile.t[:, c0:c0 + 256].rearrange("p (h d) -> p h d", h=4))

        def hb(t_small, c0):
            return t_small[:, c0:c0 + 4].with_ap(t_small.t[:, c0:c0 + 4].unsqueeze(2).to_broadcast([64, 4, 64]))

        def chunk(c):
            p = c % NBUF
            t0 = c * CH
            kb.dma(out=rkv[p][:, :], in_=scr.tmB[t0:t0 + CH, 256:1024])
            if c == 0:
                kb.pool.memset(ap=prv[p][0:1, :], constant=0.0)
                kb.dma(out=prv[p][1:CH, :], in_=scr.tmB[0:CH - 1, 256:1024])
                kb.pool.memset(ap=lo[p][:, 0:1], constant=0.0)
                kb.pool.memset(ap=gd[p][:, 0:1], constant=0.0)
                kb.dma(out=lo[p][:, 1:65], in_=scr.loraT[0:64, 0:CH])
                kb.pool.memset(ap=loa[p][:, 0:1], constant=0.0)
                kb.dma(out=loa[p][:, 1:65], in_=scr.loraT[64:128, 0:CH])
                kb.dma(out=gd[p][:, 1:65], in_=scr.loraT[128:256, 0:CH])
            else:
                kb.dma(out=prv[p][:, :], in_=scr.tmB[t0 - 1:t0 + CH - 1, 256:1024])
                kb.dma(out=lo[p][:, :], in_=scr.loraT[0:64, t0 - 1:t0 + CH])
                kb.dma(out=loa[p][:, :], in_=scr.loraT[64:128, t0 - 1:t0 + CH])
                kb.dma(out=gd[p][:, :], in_=scr.loraT[128:256, t0 - 1:t0 + CH])
            kb.dve.tensor_tensor(out=los[p][:, :], in0=lo[p][:, 0:64], in1=lo[p][:, 1:65], op=ALU.subtract)
            kb.dve.scalar_tensor_tensor(out=los[p][:, :], in0=los[p][:, :], scalar=mu_lo[0:64, 0:1], in1=lo[p][:, 1:65], op0=ALU.mult, op1=ALU.add)
            kb.dve.tensor_tensor(out=loas[p][:, :], in0=loa[p][:, 0:64], in1=loa[p][:, 1:65], op=ALU.subtract)
            kb.dve.scalar_tensor_tensor(out=loas[p][:, :], in0=loas[p][:, :], scalar=mu_a[:, 0:1], in1=loa[p][:, 1:65], op0=ALU.mult, op1=ALU.add)
            kb.dve.tensor_tensor(out=gds[p][:, :], in0=gd[p][:, 0:64], in1=gd[p][:, 1:65], op=ALU.subtract)
            kb.dve.scalar_tensor_tensor(out=gds[p][:, :], in0=gds[p][:, :], scalar=mu_lo[:, 1:2], in1=gd[p][:, 1:65], op0=ALU.mult, op1=ALU.add)
            kb.act.activation(out=los[p][0:64, :], in_=los[p][0:64, :], func=AF.Tanh)
            kb.act.activation(out=gds[p][:, :], in_=gds[p][:, :], func=AF.Sigmoid)
            kb.pe.matmul(out=B[0][:, 0:256], lhsT=los[p][0:64, :], rhs=wa_up[0:64, :], start=True, stop=True)
            kb.pe.matmul(out=B[0][:, 256:512], lhsT=loas[p][:, :], rhs=a_up[:, :], start=True, stop=True)
            kb.pe.matmul(out=B[1][:, 0:256], lhsT=gds[p][:, :], rhs=g_up[:, :], start=True, stop=True)
            kb.dve.tensor_tensor(out=sgt[p][:, :], in0=B[0][:, 0:256], in1=w0_bc[:, :], op=ALU.add)
            kb.act.activation(out=sgt[p][:, :], in_=sgt[p][:, :], func=AF.Sigmoid)
            kb.dve.tensor_tensor(out=a_t[p][:, :], in0=B[0][:, 256:512], in1=a0_bc[:, :], op=ALU.add)
            kb.act.activation(out=a_t[p][:, :], in_=a_t[p][:, :], func=AF.Sigmoid)
            kb.act.copy(out=g_t[p][:, :], in_=B[1][:, 0:256])
            kb.pool.tensor_tensor(out=prv[p][:, :], in0=prv[p][:, :], in1=rkv[p][:, :], op=ALU.subtract)
            kb.pool.tensor_tensor(out=prv[p][:, :], in0=prv[p][:, :], in1=mu_bc[:, :], op=ALU.mult)
            kb.dve.tensor_tensor(out=rkv[p][:, :], in0=rkv[p][:, :], in1=prv[p][:, :], op=ALU.add)
            yield
            r_, k_, v_ = rkv[p][:, 0:256], rkv[p][:, 256:512], rkv[p][:, 512:768]
            kb.dve.tensor_tensor(out=kk[p][:, :], in0=k_, in1=kk_bc[:, :], op=ALU.mult)
            kb.pool.tensor_tensor(out=tmp[p][:, :], in0=kk[p][:, :], in1=kk[p][:, :], op=ALU.mult)
            kb.dve.tensor_reduce(out=small[p][:, 0:4], in_=v3(tmp[p], 0), axis=AX.X, op=ALU.add)
            kb.dve.tensor_scalar(out=small[p][:, 0:4], in0=small[p][:, 0:4], scalar1=1e-12, scalar2=None, op0=ALU.add)
            kb.act.activation(out=small[p][:, 0:4], in_=small[p][:, 0:4], func=AF.Sqrt)
            kb.dve.reciprocal(out=small[p][:, 4:8], in_=small[p][:, 0:4])
            kb.dve.tensor_tensor(out=v3(kk[p], 0), in0=v3(kk[p], 0), in1=hb(small[p], 4), op=ALU.mult)
            kb.pool.tensor_tensor(out=tmp[p][:, :], in0=a_t[p][:, :], in1=ka_bc[:, :], op=ALU.mult)
            kb.pool.tensor_tensor(out=tmp[p][:, :], in0=tmp[p][:, :], in1=oka[:, :], op=ALU.add)
            kb.dve.tensor_tensor(out=k2[p][:, :], in0=k_, in1=tmp[p][:, :], op=ALU.mult)
            kb.pool.tensor_tensor(out=bb[p][:, :], in0=kk[p][:, :], in1=a_t[p][:, :], op=ALU.mult)
            yield
            kb.pe.matmul(out=B[2][:, 0:256], lhsT=tri[:, 0, :], rhs=sgt[p][:, :], start=True, stop=True)
            kb.pe.matmul(out=B[2][:, 256:512], lhsT=tri[:, 1, :], rhs=sgt[p][:, :], start=True, stop=True)
            kb.act.activation(out=Pm[p][:, :], in_=B[2][:, 0:256], func=AF.Exp, scale=-C0)
            kb.act.activation(out=iP[p][:, :], in_=B[2][:, 0:256], func=AF.Exp, scale=C0)
            kb.dve.tensor_tensor(out=tmp2[p][:, :], in0=B[2][:, 0:256], in1=sgt[p][:, :], op=ALU.subtract)
            kb.act.activation(out=Pp[p][:, :], in_=tmp2[p][:, :], func=AF.Exp, scale=-C0)
            kb.act.activation(out=Pr[p][:, :], in_=B[2][:, 256:512], func=AF.Exp, scale=-C0)
            kb.dve.tensor_tensor(out=Kt[p][:, :], in0=k2[p][:, :], in1=iP[p][:, :], op=ALU.mult)
            kb.pool.tensor_tensor(out=Bt[p][:, :], in0=bb[p][:, :], in1=iP[p][:, :], op=ALU.mult)
            kb.dve.tensor_tensor(out=KKt[p][:, :], in0=kk[p][:, :], in1=Pp[p][:, :], op=ALU.mult)
            kb.pool.tensor_tensor(out=Rt[p][:, :], in0=r_, in1=Pm[p][:, :], op=ALU.mult)
            kb.dve.tensor_tensor(out=Kh[p][:, :], in0=k2[p][:, :], in1=Pr[p][:, :], op=ALU.mult)
            kb.pool.tensor_tensor(out=Bh[p][:, :], in0=bb[p][:, :], in1=Pr[p][:, :], op=ALU.mult)
            yield
            for h in range(4):
                kb.pe.matmul(out=B[1][:, 256 + 2 * h:258 + 2 * h], lhsT=Pm[p][:, h * 64:(h + 1) * 64], rhs=cx.ident_f[0:64, 62:64], start=True, stop=True)
            kb.act.copy(out=pcs[p][:, :], in_=B[1][:, 256:264].with_ap(B[1].t[:, 256:264].rearrange("p (h two) -> p h two", two=2)[:, :, 1]))
            yield
            for qi, q in enumerate((Bt, Kt, KKt, Rt)):
                for h in range(4):
                    idx = qi * 4 + h
                    bk = B[3] if idx < 8 else B[4]
                    kb.pe.transpose(out=bk[:, (idx % 8) * 64:(idx % 8 + 1) * 64], in_=q[p][:, h * 64:(h + 1) * 64], identity=cx.ident_f[0:64, 0:64])
            fm = FMq[p]
            kb.act.copy(out=fm[:, 0:8, :], in_=B[3][:, :].with_ap(B[3].t[:, :].rearrange("p (a b) -> p a b", b=64)))
            kb.dve.tensor_copy(out=fm[:, 8:16, :], in_=B[4][:, :].with_ap(B[4].t[:, :].rearrange("p (a b) -> p a b", b=64)))
            BT = lambda h: fm[:, 0 + h, :]
            KT = lambda h: fm[:, 4 + h, :]
            KKT = lambda h: fm[:, 8 + h, :]
            RT = lambda h: fm[:, 12 + h, :]
            fm4 = fm.t.rearrange("p (q h) t -> p q h t", q=4)
            for h in range(4):
                kkr = Ref(fm, None, fm4[:, 2:4, h, :])
                o5 = B[5][:, h * 128:(h + 1) * 128]
                o6 = B[6][:, h * 128:(h + 1) * 128]
                kb.pe.matmul(out=o5, lhsT=BT(h), rhs=kkr, start=True, stop=True)
                kb.pe.matmul(out=o6, lhsT=KT(h), rhs=kkr, start=True, stop=True)
                kb.pe.matmul(out=B[7][:, h * 64:(h + 1) * 64], lhsT=KKT(h), rhs=BT(h), start=True, stop=True)
            hw = lambda bk, w: Ref(bk, None, bk.t.rearrange("p (h w t) -> p h w t", h=4, w=2)[:, :, w, :])
            b3 = lambda bk, c0: bk[:, c0:c0 + 256].with_ap(bk.t[:, c0:c0 + 256].rearrange("p (h d) -> p h d", h=4))
            TAv = TA_[p].t.rearrange("p (h w) t -> p h w t", w=2)
            TA2 = TA_[p].t.rearrange("p a t -> p (a t)")
            Tv = Ref(TA_[p], None, TAv[:, :, 0, :])
            Av = Ref(TA_[p], None, TAv[:, :, 1, :])
            kb.dve.scalar_tensor_tensor(out=Av, in0=hw(B[5], 0), scalar=-1.0, in1=mst[:, :, :], op0=ALU.mult, op1=ALU.mult)
            kb.dve.scalar_tensor_tensor(out=NT_[p][:, :, :], in0=b3(B[7], 0), scalar=-1.0, in1=mstT[:, :, :], op0=ALU.mult, op1=ALU.mult)
            kb.dve.tensor_tensor(out=Mak[p][:, :, :], in0=hw(B[6], 0), in1=mst[:, :, :], op=ALU.mult)
            kb.dve.tensor_tensor(out=Abr[p][:, :, :], in0=hw(B[5], 1), in1=minc[:, :, :], op=ALU.mult)
            kb.dve.tensor_tensor(out=Akr[p][:, :, :], in0=hw(B[6], 1), in1=minc[:, :, :], op=ALU.mult)
            yield
            kb.pool.tensor_copy(out=Tv, in_=idb)
            AT_ = NT_[p]
            B5v = B[5].t.rearrange("p (h w t) -> p h w t", h=4, w=2)
            for j in range(6):
                last = (j == 5)
                for h in range(4):
                    if last:
                        kb.pe.matmul(out=B[5][:, h * 128:h * 128 + 64], lhsT=AT_[:, h, :], rhs=Ref(TA_[p], None, TAv[:, h, 0, :]), start=True, stop=True)
                    else:
                        kb.pe.matmul(out=B[5][:, h * 128:(h + 1) * 128], lhsT=AT_[:, h, :], rhs=Ref(TA_[p], None, TA2[:, h * 128:(h + 1) * 128]), start=True, stop=True)
                        kb.pe.matmul(out=B[6][:, h * 64:(h + 1) * 64], lhsT=Ref(TA_[p], None, TAv[:, h, 1, :]), rhs=AT_[:, h, :], start=True, stop=True)
                kb.dve.tensor_tensor(out=Tv, in0=Tv, in1=Ref(B[5], None, B5v[:, :, 0, :]), op=ALU.add)
                if not last:
                    kb.act.copy(out=Av, in_=Ref(B[5], None, B5v[:, :, 1, :]))
                    kb.dve.tensor_copy(out=AT_[:, :, :], in_=b3(B[6], 0))
                yield
            yield
            for h in range(4):
                kb.pe.matmul(out=B[7][:, 256 + h * 64:256 + (h + 1) * 64], lhsT=KKt[p][:, h * 64:(h + 1) * 64], rhs=Ref(TA_[p], None, TAv[:, h, 0, :]), start=True, stop=True)
                kb.pe.matmul(out=B[3][:, h * 64:(h + 1) * 64], lhsT=Mak[p][:, h, :], rhs=rkv[p][:, 512 + h * 64:512 + (h + 1) * 64], start=True, stop=True)
            kb.act.copy(out=Wsb[p][:, :, :], in_=b3(B[7], 256))
            kb.dve.tensor_copy(out=Xsb[p][:, :, :], in_=b3(B[3], 0))
            for h in range(4):
                kb.pe.matmul(out=B[3][:, 256 + h * 64:256 + (h + 1) * 64], lhsT=Ref(TA_[p], None, TAv[:, h, 0, :]), rhs=Xsb[p][:, h, :], start=True, stop=True)
            kb.act.activation(out=U0T[p][:, :, :], in_=b3(B[3], 256), func=AF.Copy, scale=-1.0)
            yield
            for h in range(4):
                vh = rkv[p][:, 512 + h * 64:512 + (h + 1) * 64]
                kb.pe.matmul(out=B[4][:, h * 64:(h + 1) * 64], lhsT=Wsb[p][:, h, :], rhs=ST[:, h, :], start=True, stop=True)
                kb.dve.tensor_tensor(out=UT[p][:, h, :], in0=U0T[p][:, h, :], in1=B[4][:, h * 64:(h + 1) * 64], op=ALU.subtract)
                yo = B[4][:, 256 + h * 64:256 + (h + 1) * 64]
                kb.pe.matmul(out=yo, lhsT=RT(h), rhs=ST[:, h, :], start=True, stop=False)
                kb.pe.matmul(out=yo, lhsT=Abr[p][:, h, :], rhs=UT[p][:, h, :], start=False, stop=False)
                kb.pe.matmul(out=yo, lhsT=Akr[p][:, h, :], rhs=vh, start=False, stop=True)
                kb.act.copy(out=yv[p][:, h * 64:(h + 1) * 64], in_=yo)
                so = B[2][:, h * 64:(h + 1) * 64]
                kb.pe.matmul(out=so, lhsT=Bh[p][:, h * 64:(h + 1) * 64], rhs=UT[p][:, h, :], start=True, stop=False)
                kb.pe.matmul(out=so, lhsT=Kh[p][:, h * 64:(h + 1) * 64], rhs=vh, start=False, stop=True)
                kb.dve.scalar_tensor_tensor(out=ST[:, h, :], in0=ST[:, h, :], scalar=pcs[p][:, h:h + 1], in1=so, op0=ALU.mult, op1=ALU.add)
                yield
            yield
            y = yv[p]
            kb.pool.tensor_tensor(out=tmp[p][:, :], in0=r_, in1=k2[p][:, :], op=ALU.mult)
            kb.pool.tensor_tensor(out=tmp[p][:, :], in0=tmp[p][:, :], in1=rk_bc[:, :], op=ALU.mult)
            kb.dve.tensor_reduce(out=small[p][:, 8:12], in_=v3(tmp[p], 0), axis=AX.X, op=ALU.add)
            kb.dve.tensor_tensor(out=v3(tmp2[p], 0), in0=v3(rkv[p], 512), in1=hb(small[p], 8), op=ALU.mult)
            kb.dve.tensor_tensor(out=y[:, :], in0=tmp2[p][:, :], in1=y[:, :], op=ALU.add)
            kb.dve.tensor_reduce(out=small[p][:, 0:4], in_=v3(y, 0), axis=AX.X, op=ALU.add)
            kb.dve.tensor_scalar(out=small[p][:, 0:4], in0=small[p][:, 0:4], scalar1=1.0 / 64, scalar2=None, op0=ALU.mult)
            kb.dve.tensor_tensor(out=v3(y, 0), in0=v3(y, 0), in1=hb(small[p], 0), op=ALU.subtract)
            kb.pool.tensor_tensor(out=tmp[p][:, :], in0=y[:, :], in1=y[:, :], op=ALU.mult)
            kb.dve.tensor_reduce(out=small[p][:, 4:8], in_=v3(tmp[p], 0), axis=AX.X, op=ALU.add)
            kb.dve.tensor_scalar(out=small[p][:, 4:8], in0=small[p][:, 4:8], scalar1=1.0 / 64, scalar2=64e-5, op0=ALU.mult, op1=ALU.add)
            kb.act.activation(out=small[p][:, 4:8], in_=small[p][:, 4:8], func=AF.Sqrt)
            kb.dve.reciprocal(out=small[p][:, 12:16], in_=small[p][:, 4:8])
            kb.dve.tensor_tensor(out=v3(y, 0), in0=v3(y, 0), in1=hb(small[p], 12), op=ALU.mult)
            kb.pool.tensor_tensor(out=y[:, :], in0=y[:, :], in1=lg_bc[:, :], op=ALU.mult)
            kb.pool.tensor_tensor(out=y[:, :], in0=y[:, :], in1=lb_bc[:, :], op=ALU.add)
            kb.dve.tensor_tensor(out=y[:, :], in0=y[:, :], in1=g_t[p][:, :], op=ALU.mult)
            kb.dma(out=scr.ymix.k(("c", c))[t0:t0 + CH, 512:768], in_=y[:, :])

        import os as _os
        nch = int(_os.environ.get('RWKV_NCH', S // CH))
        active = []
        nxt = 0
        while nxt < nch or active:
            if nxt < nch and len(active) < NBUF:
                active.append(chunk(nxt))
                nxt += 1
            for g in list(active):
                try:
                    next(g)
                except StopIteration:
                    active.remove(g)
        kb.barrier()
def build(depth=DEPTH, debug=None, stop_after=None, only=None):
    kb = KB()
    nc = kb.nc
    cx = Ctx()
    cx.P = {}
    x_in = kb.dram("x", [S, D], F32, kind="ExternalInput")
    for n, shp in PARAM_SHAPES.items():
        cx.P[n] = kb.dram(n, list(shp), F32, kind="ExternalInput")
    consts = make_consts()
    cx.C = {}
    for n, a in consts.items():
        cx.C[n] = kb.dram("c_" + n, list(a.shape), CONST_DT.get(n, F32), kind="ExternalInput")
    y_out = kb.dram("y", [S, D], F32, kind="ExternalOutput")
    scr = Ctx()
    cx.scr = scr
    dbg = debug or []

    def scratch(name, shape, dt):
        kind = "ExternalOutput" if name in dbg else "Internal"
        return kb.dram("scr_" + name, shape, dt, kind=kind)
    scr.qT = scratch("qT", [256, S], BF16)
    scr.kcvcT = scratch("kcvcT", [128, S], BF16)
    scr.ksT = scratch("ksT", [64, S], BF16)
    scr.kwT = scratch("kwT", [64, S], BF16)
    scr.dqT = scratch("dqT", [256, S], BF16)
    scr.dkT = scratch("dkT", [256, S], BF16)
    scr.loraT = scratch("loraT", [256, S], F32)
    scr.convT = scratch("convT", [512, S], F32)
    scr.tmA = scratch("tmA", [S, 204], F32)
    scr.tmB = scratch("tmB", [S, 1024], F32)
    scr.ymix = scratch("ymix", [S, D], F32)
    cx.xres = scratch("xres", [S, D], F32)
    outs = [y_out] + [getattr(scr, n) if hasattr(scr, n) else cx.xres for n in dbg]

    gst = ExitStack()
    cx.ident_bf = kb.sb(gst, "ident_bf", [128, 128], BF16)
    cx.ident_f = kb.sb(gst, "ident_f", [128, 128], F32)
    kb.dma(out=cx.ident_bf[:, :], in_=cx.C["ident_bf"][:, :])
    kb.dma(out=cx.ident_f[:, :], in_=cx.C["ident_f"][:, :])
    kb.dma(out=cx.xres[:, :], in_=x_in[:, :])

    for l in range(depth):
        phase_a(kb, cx, l)
        if stop_after == "a":
            break
        if only in (None, "conv"):
            phase_conv(kb, cx, l)
        if only in (None, "dil"):
            phase_dil(kb, cx, l)
        if only in (None, "nsa"):
            phase_nsa(kb, cx, l)
        if only in (None, "rwkv"):
            phase_rwkv(kb, cx, l)
        if stop_after == "mix":
            break
        phase_b(kb, cx, l)
        if stop_after == "b":
            break
        phase_c(kb, cx, l)
    if stop_after is None:
        phase_final(kb, cx, y_out)
    gst.close()
    kb.finish(outs)
    return kb, consts


_CACHE = {}


def kernel(**inputs):
    if "prog" not in _CACHE:
        _CACHE["prog"] = build()
    kb, consts = _CACHE["prog"]
    x = np.ascontiguousarray(inputs["x"], dtype=np.float32)
    in_maps = []
    for c in range(8):
        m = {"x": x[c]}
        for n in PARAM_SHAPES:
            m[n] = np.ascontiguousarray(inputs[n], dtype=np.float32)
        for n, a in consts.items():
            m["c_" + n] = a
        in_maps.append(m)
    res = run_bass_kernel_spmd(kb.nc, in_maps, core_ids=list(range(8)))
    return np.stack([res.results[c]["y"] for c in range(8)], axis=0)
```

```python
import numpy as np
import ml_dtypes
from contextlib import ExitStack
import concourse.bass as bass
import concourse.mybir as mybir
from concourse.bass_utils import run_bass_kernel_spmd

F32 = mybir.dt.float32
BF16 = mybir.dt.bfloat16
I32 = mybir.dt.int32
AF = mybir.ActivationFunctionType
ALU = mybir.AluOpType
AX = mybir.AxisListType

S = 4096
D = 1024
NT = S // 128
NBLK = S // 512
DEPTH = 4
DFF = 2816
IN_COLS = 2956
WRITE_NAMES = ("out", "accum_out", "ap")


class SemT:
    def __init__(self, handle):
        self.h = handle
        self.count = 0


class Rec:
    __slots__ = ("lw", "rd")

    def __init__(self):
        self.lw = None
        self.rd = []


class Tile:
    def __init__(self, t, name):
        self.t = t
        self.name = name
        self.regs = {None: Rec()}
        self.excl = False

    def recs_dep(self, key):
        if key is None:
            return list(self.regs.values())
        if key not in self.regs:
            self.regs[key] = Rec()
        return [self.regs[key], self.regs[None]]

    def recs_upd(self, key, is_write):
        if key is None:
            return list(self.regs.values()) if is_write else [self.regs[None]]
        if key not in self.regs:
            self.regs[key] = Rec()
        return [self.regs[key]]

    def __getitem__(self, idx):
        return Ref(self, None, self.t[idx])

    def k(self, key):
        return KeyView(self, key)


class KeyView:
    def __init__(self, tile, key):
        self.tile = tile
        self.key = key

    def __getitem__(self, idx):
        return Ref(self.tile, self.key, self.tile.t[idx])


class Ref:
    def __init__(self, tile, key, ap):
        self.tile = tile
        self.key = key
        self.ap = ap

    def with_ap(self, ap):
        return Ref(self.tile, self.key, ap)


class Eng:
    def __init__(self, kb, name, raw, sem, is_pe=False):
        self.kb = kb
        self.name = name
        self.raw = raw
        self.sem = sem
        self.waited = {}
        self.is_pe = is_pe

    def __getattr__(self, opname):
        def call(**kw):
            reads, writes = [], []
            kw2 = {}
            for n, v in kw.items():
                if isinstance(v, Ref):
                    (writes if n in WRITE_NAMES else reads).append(v)
                    kw2[n] = v.ap
                else:
                    kw2[n] = v
            return self.kb.emit(self, lambda: getattr(self.raw, opname)(**kw2), reads, writes)
        return call


class KB:
    def __init__(self):
        self.nc = bass.Bass("TRN2", target_bir_lowering=False)
        nc = self.nc
        self.es = ExitStack()
        mk = lambda n: SemT(self.es.enter_context(nc.semaphore(n)))
        self.pe = Eng(self, "pe", nc.tensor, mk("s_pe"), is_pe=True)
        self.act = Eng(self, "act", nc.scalar, mk("s_act"))
        self.dve = Eng(self, "dve", nc.vector, mk("s_dve"))
        self.pool = Eng(self, "pool", nc.gpsimd, mk("s_pool"))
        self.sp = Eng(self, "sp", nc.sync, mk("s_sp"))
        self.ring = [mk(f"s_dma{i}") for i in range(24)]
        self.ring_i = 0
        self.pring = [mk(f"s_pdma{i}") for i in range(8)]
        self.pring_i = 0
        self.n_inst = 0
        self.out_deps = []

    def dram(self, name, shape, dtype, kind="Internal"):
        return Tile(self.nc.dram_tensor(name, list(shape), dtype, kind=kind).ap(), name)

    def sb(self, stack, name, shape, dtype):
        self.n_alloc = getattr(self, "n_alloc", 0) + 1
        name = f"{name}_{self.n_alloc}"
        return Tile(stack.enter_context(self.nc.sbuf_tensor(name, list(shape), dtype)), name)

    def psum(self, stack, name, shape, dtype):
        self.n_alloc = getattr(self, "n_alloc", 0) + 1
        name = f"{name}_{self.n_alloc}"
        t = Tile(stack.enter_context(self.nc.psum_tensor(name, list(shape), dtype)), name)
        t.excl = True
        return t

    def _wait(self, eng, deps):
        best = {}
        for (st, v) in deps:
            if v is None:
                continue
            if id(st) not in best or best[id(st)][1] < v:
                best[id(st)] = (st, v)
        for st, v in best.values():
            if st is eng.sem and eng.is_pe:
                continue
            if eng.waited.get(id(st), 0) >= v:
                continue
            eng.raw.wait_ge(st.h, v)
            eng.waited[id(st)] = v

    def _collect(self, reads, writes):
        deps = []
        for r in reads:
            for rec in r.tile.recs_dep(r.key):
                if rec.lw is not None:
                    deps.append(rec.lw)
                if r.tile.excl:
                    deps.extend(rec.rd)
        for w in writes:
            for rec in w.tile.recs_dep(w.key):
                if rec.lw is not None:
                    deps.append(rec.lw)
                deps.extend(rec.rd)
        return deps

    def _update(self, reads, writes, tag):
        for r in reads:
            for rec in r.tile.recs_upd(r.key, False):
                rec.rd.append(tag)
                if len(rec.rd) > 48:
                    best = {}
                    for st, v in rec.rd:
                        if id(st) not in best or best[id(st)][1] < v:
                            best[id(st)] = (st, v)
                    rec.rd = list(best.values())
        for w in writes:
            for rec in w.tile.recs_upd(w.key, True):
                rec.lw = tag
                rec.rd = []

    def emit(self, eng, fn, reads, writes):
        deps = self._collect(reads, writes)
        self._wait(eng, deps)
        inst = fn()
        eng.sem.count += 1
        inst.then_inc(eng.sem.h, 1)
        self._update(reads, writes, (eng.sem, eng.sem.count))
        self.n_inst += 1
        return inst

    def dma(self, out, in_, via_pool=False, **kw):
        eng = self.pool if via_pool else self.sp
        if via_pool:
            st = self.pring[self.pring_i % len(self.pring)]
            self.pring_i += 1
        else:
            st = self.ring[self.ring_i % len(self.ring)]
            self.ring_i += 1
        deps = self._collect([in_], [out])
        deps.append((st, st.count))
        self._wait(eng, deps)
        inst = eng.raw.dma_start(out=out.ap, in_=in_.ap, **kw)
        st.count += 16
        inst.then_inc(st.h, 16)
        tag = (st, st.count)
        self._update([in_], [out], tag)
        self.n_inst += 1
        return tag

    def barrier(self):
        deps = [(st, st.count) for st in self.ring + self.pring]
        deps += [(e.sem, e.sem.count) for e in (self.pe, self.act, self.dve, self.pool)]
        deps = [d for d in deps if d[1] > 0]
        for e in (self.pe, self.act, self.dve, self.pool, self.sp):
            self._wait(e, [d for d in deps if not (d[0] is e.sem)])

    def finish(self, out_tiles):
        deps = [(st, st.count) for st in self.ring + self.pring]
        deps += [(e.sem, e.sem.count) for e in (self.pe, self.act, self.dve, self.pool)]
        self._wait(self.sp, [d for d in deps if d[1] > 0])
        self.es.close()


def _bf(a):
    return np.ascontiguousarray(a.astype(np.float32)).astype(ml_dtypes.bfloat16)


def make_consts():
    c = {}
    c["ident_bf"] = _bf(np.eye(128))
    c["ident_f"] = np.eye(128, dtype=np.float32)
    sp = np.arange(128)[:, None]
    tq = np.arange(512)[None, :]
    c["cms"] = _bf(np.stack([(128 * j + sp <= tq) for j in range(4)], axis=1))
    tq1 = np.arange(128)[None, :]
    c["cmw"] = _bf(np.stack([(sp <= tq1), (sp >= tq1)], axis=1))
    dm = []
    for delta in range(-3, 17):
        d = tq - sp + 128 * delta
        cnt = ((d >= 0) & (d <= 128)).astype(np.float32)
        cnt += ((d >= 0) & (d <= 512) & (d % 4 == 0))
        cnt += ((d >= 0) & (d <= 2048) & (d % 16 == 0))
        dm.append(cnt)
    c["dm"] = _bf(np.stack(dm, axis=1))
    j = np.arange(64)[:, None, None]
    kt = np.arange(32)[None, :, None]
    s = np.arange(128)[None, None, :]
    c["ek"] = _bf(((128 * kt + s) // 64 == j))
    mc = np.zeros((128, 16, 512), np.float32)
    for b in range(8):
        for ct in range(2):
            mc[:, b * 2 + ct, :] = (16 * (128 * ct + sp) + 31 <= 512 * b + tq)
    c["mc"] = _bf(mc)
    c["gc"] = (16 * np.arange(256)[None, :] + 31 - np.arange(128)[:, None]).astype(np.float32)
    c["d0"] = (64 * np.arange(64)[None, :] - np.arange(128)[:, None]).astype(np.float32)
    i = np.arange(128)[:, None]
    t = np.arange(128)[None, :]
    c["tri"] = ((i // 64 == t // 64) & (i <= t)).astype(np.float32)
    i6 = np.arange(64)[:, None]
    t6 = np.arange(64)[None, :]
    c["mstrict"] = np.tile((i6 < t6).astype(np.float32)[:, None, :], (1, 8, 1))
    c["mincl"] = np.tile((i6 <= t6).astype(np.float32)[:, None, :], (1, 8, 1))
    c["mstrictT"] = np.tile((i6 > t6).astype(np.float32)[:, None, :], (1, 8, 1))
    return c


CONST_DT = {"ident_bf": BF16, "cms": BF16, "cmw": BF16, "dm": BF16, "ek": BF16, "mc": BF16}

PARAM_SHAPES = {
    'norm_mix': (DEPTH, D), 'w_in': (DEPTH, D, IN_COLS), 'cmp_pos': (DEPTH, 32, 64),
    'cmp_k_w1': (DEPTH, 2048, 128), 'cmp_k_w2': (DEPTH, 128, 64), 'cmp_v_w1': (DEPTH, 2048, 128),
    'cmp_v_w2': (DEPTH, 128, 64), 'beta_nsa': (DEPTH, 256), 'beta_dil': (DEPTH, 256),
    'rwkv_mu': (DEPTH, 1024), 'rwkv_w0': (DEPTH, 256), 'rwkv_w_up': (DEPTH, 64, 256),
    'rwkv_a0': (DEPTH, 256), 'rwkv_a_up': (DEPTH, 64, 256), 'rwkv_g_up': (DEPTH, 128, 256),
    'rwkv_k_k': (DEPTH, 256), 'rwkv_k_a': (DEPTH, 256), 'rwkv_r_k': (DEPTH, 4, 64),
    'rwkv_ln_g': (DEPTH, 256), 'rwkv_ln_b': (DEPTH, 256), 'conv_dw': (DEPTH, 31, 256),
    'conv_dw_b': (DEPTH, 256), 'conv_ln_g': (DEPTH, 256), 'conv_ln_b': (DEPTH, 256),
    'w_out': (DEPTH, D, D), 'norm_ffn': (DEPTH, D), 'ffn_up': (DEPTH, D, 2 * DFF),
    'ffn_dw': (DEPTH, 3, 2 * DFF), 'ffn_dw_b': (DEPTH, 2 * DFF), 'ffn_down': (DEPTH, DFF, D),
    'norm_final': (D,),
}


class Ctx:
    pass


def bcast_rows(ap_1d_row, nparts):
    return ap_1d_row.partition_broadcast(nparts)


def load_bcast(kb, dst_tile, src_dram_tile, row_ap, n):
    kb.dma(out=dst_tile[:, 0:n], in_=Ref(src_dram_tile, None, row_ap.partition_broadcast(128)))


FM_CHUNKS = [
    (0, 128, "qT", 0), (128, 128, "qT", 128), (256, 128, "kcvcT", 0), (384, 64, "ksT", 0), (512, 64, "kwT", 0),
    (652, 128, "dqT", 0), (780, 128, "dqT", 128), (908, 128, "dkT", 0), (1036, 128, "dkT", 128),
    (2188, 128, "loraT", 0), (2316, 128, "loraT", 128),
    (2444, 128, "convT", 0), (2572, 128, "convT", 128), (2700, 128, "convT", 256), (2828, 128, "convT", 384),
]
TM_GROUPS = [(448, 204, "tmA", 0), (1164, 512, "tmB", 0), (1676, 512, "tmB", 512)]


def load_weight_bf16(kb, dst, dram_w, rows0, nk, cols0, ncols):
    for k in range(nk):
        c = 0
        while c < ncols:
            w = min(1024, ncols - c)
            kb.dma(out=dst[:, k, c:c + w],
                   in_=dram_w[rows0 + k * 128: rows0 + (k + 1) * 128, cols0 + c: cols0 + c + w], via_pool=True)
            c += w


def rms_rstd(kb, ssq_ref, out_ref, n, eps, tmp_ref):
    kb.dve.tensor_scalar(out=tmp_ref, in0=ssq_ref, scalar1=1.0 / n, scalar2=eps, op0=ALU.mult, op1=ALU.add)
    kb.act.activation(out=tmp_ref, in_=tmp_ref, func=AF.Sqrt)
    kb.dve.reciprocal(out=out_ref, in_=tmp_ref)


def phase_a(kb, cx, l):
    P = cx.P
    scr = cx.scr
    with ExitStack() as st:
        w_sb = kb.sb(st, "wA", [128, 8, IN_COLS], BF16)
        wblocks = [(b, b * 512, min(512, IN_COLS - b * 512)) for b in range(6)]
        for (key, c0, cw) in wblocks:
            src = P["w_in"].t[l, :, c0:c0 + cw].rearrange("(k p) c -> p k c", p=128)
            kb.dma(out=w_sb.k(key)[:, :, c0:c0 + cw], in_=Ref(P["w_in"], None, src), via_pool=True)

        def wkey(c0, cw):
            return w_sb.k(c0 // 512) if c0 // 512 == (c0 + cw - 1) // 512 else w_sb
        gbc = kb.sb(st, "gbcA", [128, D], F32)
        kb.dma(out=gbc[:, :], in_=Ref(P["norm_mix"], None, P["norm_mix"].t[l:l + 1, :].partition_broadcast(128)))
        xt = [kb.sb(st, f"xtA{i}", [128, 4, D], F32) for i in range(2)]
        hbf = [kb.sb(st, f"hbfA{i}", [128, D], BF16) for i in range(2)]
        junk = kb.sb(st, "junkA", [128, D], BF16)
        hT = [kb.sb(st, f"hTA{i}", [128, 8, 512], BF16) for i in range(2)]
        small = kb.sb(st, "smallA", [128, 16], F32)
        stg_bf = [kb.sb(st, f"stgbA{i}", [128, 512], BF16) for i in range(3)]
        stg_f = [kb.sb(st, f"stgfA{i}", [128, 512], F32) for i in range(3)]
        tp = [kb.psum(st, f"tpA{i}", [128, 8, 128], BF16) for i in range(2)]
        acc = [kb.psum(st, f"accA{i}", [128, 512], F32) for i in range(4)]
        n_acc = 0
        n_stg = 0
        def load_x(b):
            kb.dma(out=xt[b % 2][:, :, :], in_=cx.xres[b * 512:(b + 1) * 512, :].with_ap(
                cx.xres.t[b * 512:(b + 1) * 512, :].rearrange("(j p) d -> p j d", p=128)))
        load_x(0)
        for b in range(NBLK):
            x_t = xt[b % 2]
            if b + 1 < NBLK:
                load_x(b + 1)
            h_T = hT[b % 2]
            for j in range(4):
                hb = hbf[j % 2]
                ssq = small[:, j:j + 1]
                kb.act.activation(out=junk[:, :], in_=x_t[:, j, :], func=AF.Square, accum_out=ssq)
                rms_rstd(kb, ssq, small[:, 4 + j:5 + j], D, 1e-6, small[:, 8 + j:9 + j])
                kb.dve.scalar_tensor_tensor(out=hb[:, :], in0=x_t[:, j, :], scalar=small[:, 4 + j:5 + j], in1=gbc[:, :],
                                            op0=ALU.mult, op1=ALU.mult)
                t_p = tp[j % 2]
                for kc in range(8):
                    kb.pe.transpose(out=t_p[:, kc, :], in_=hb[:, kc * 128:(kc + 1) * 128], identity=cx.ident_bf[:, :])
                kb.act.copy(out=h_T[:, :, j * 128:(j + 1) * 128], in_=t_p[:, :, :])
            for (c0, cw, dst, r0) in FM_CHUNKS:
                a = acc[n_acc % 4]
                n_acc += 1
                for kc in range(8):
                    kb.pe.matmul(out=a[0:cw, :], lhsT=wkey(c0, cw)[:, kc, c0:c0 + cw], rhs=h_T[:, kc, :], start=(kc == 0), stop=(kc == 7))
                dt_f32 = dst in ("loraT", "convT")
                sg = (stg_f if dt_f32 else stg_bf)[n_stg % 3]
                if n_stg % 2 == 0:
                    kb.dve.tensor_copy(out=sg[0:cw, :], in_=a[0:cw, :])
                else:
                    kb.act.copy(out=sg[0:cw, :], in_=a[0:cw, :])
                n_stg += 1
                kb.dma(out=getattr(scr, dst).k(b)[r0:r0 + cw, b * 512:(b + 1) * 512], in_=sg[0:cw, :])
            for j in range(4):
                for (c0, cw, dst, d0) in TM_GROUPS:
                    a = acc[n_acc % 4]
                    n_acc += 1
                    for kc in range(8):
                        kb.pe.matmul(out=a[:, 0:cw], lhsT=h_T[:, kc, j * 128:(j + 1) * 128], rhs=wkey(c0, cw)[:, kc, c0:c0 + cw],
                                     start=(kc == 0), stop=(kc == 7))
                    sg = stg_f[n_stg % 3]
                    if n_stg % 2 == 0:
                        kb.dve.tensor_copy(out=sg[:, 0:cw], in_=a[:, 0:cw])
                    else:
                        kb.act.copy(out=sg[:, 0:cw], in_=a[:, 0:cw])
                    n_stg += 1
                    r = b * 512 + j * 128
                    kb.dma(out=getattr(scr, dst).k(b)[r:r + 128, d0:d0 + cw], in_=sg[:, 0:cw])
        kb.barrier()


def phase_conv(kb, cx, l):
    P = cx.P
    scr = cx.scr
    with ExitStack() as st:
        glu = [kb.sb(st, f"gluD{i}", [128, 30 + S], F32) for i in range(2)]
        accs = [kb.sb(st, f"accD{i}", [128, S], F32) for i in range(2)]
        bt = kb.sb(st, "btD", [128, S], F32)
        wdw = kb.sb(st, "wdwD", [128, 2, 32], F32)
        lng = kb.sb(st, "lngD", [128, 256], F32)
        lnb = kb.sb(st, "lnbD", [128, 256], F32)
        small = kb.sb(st, "smallD", [128, 16], F32)
        stats = kb.sb(st, "statsD", [128, 8], F32)
        xn = [kb.sb(st, f"xnD{i}", [128, 256], F32) for i in range(2)]
        tps = [kb.psum(st, f"tpD{i}", [128, 512], F32) for i in range(2)]
        kb.dma(out=lng[:, :], in_=Ref(P["conv_ln_g"], None, P["conv_ln_g"].t[l:l + 1, :].partition_broadcast(128)))
        kb.dma(out=lnb[:, :], in_=Ref(P["conv_ln_b"], None, P["conv_ln_b"].t[l:l + 1, :].partition_broadcast(128)))
        for ci in range(2):
            with kb.nc.allow_non_contiguous_dma(reason="tiny transposed conv weights"):
                kb.dma(out=wdw[:, ci, 0:31], in_=Ref(P["conv_dw"], None,
                       P["conv_dw"].t[l, :, ci * 128:(ci + 1) * 128].rearrange("k c -> c k")))
                kb.dma(out=wdw[:, ci, 31:32], in_=Ref(P["conv_dw_b"], None,
                       P["conv_dw_b"].t[l:l + 1, ci * 128:(ci + 1) * 128].rearrange("o c -> c o")))
            g = glu[ci]
            kb.pool.memset(ap=g[:, 0:30], constant=0.0)
            kb.dma(out=g[:, 30:30 + S], in_=scr.convT[ci * 128:(ci + 1) * 128, :])
            kb.dma(out=bt[:, :], in_=scr.convT[256 + ci * 128:256 + (ci + 1) * 128, :])
            kb.act.activation(out=bt[:, :], in_=bt[:, :], func=AF.Sigmoid)
            kb.pool.tensor_tensor(out=g[:, 30:30 + S], in0=g[:, 30:30 + S], in1=bt[:, :], op=ALU.mult)
            a = accs[ci]
            for h0 in range(0, S, 2048):
                kb.dve.tensor_scalar(out=a[:, h0:h0 + 2048], in0=g[:, 30 + h0:30 + h0 + 2048], scalar1=wdw[:, ci, 30:31],
                                     scalar2=wdw[:, ci, 31:32], op0=ALU.mult, op1=ALU.add)
                for j in range(30):
                    kb.dve.scalar_tensor_tensor(out=a[:, h0:h0 + 2048], in0=g[:, j + h0:j + h0 + 2048], scalar=wdw[:, ci, j:j + 1],
                                                in1=a[:, h0:h0 + 2048], op0=ALU.mult, op1=ALU.add)
        for i in range(NT):
            tp = tps[i % 2]
            for ci in range(2):
                kb.pe.transpose(out=tp[:, ci * 128:(ci + 1) * 128], in_=accs[ci][:, i * 128:(i + 1) * 128], identity=cx.ident_f[:, :])
            x_n = xn[i % 2]
            kb.dve.bn_stats(out=stats[:, 0:6], in_=tp[:, 0:256])
            kb.dve.bn_aggr(out=small[:, 0:2], in_=stats[:, 0:6])
            kb.dve.tensor_scalar(out=small[:, 2:3], in0=small[:, 1:2], scalar1=1e-5, scalar2=None, op0=ALU.add)
            kb.act.activation(out=small[:, 2:3], in_=small[:, 2:3], func=AF.Sqrt)
            kb.dve.reciprocal(out=small[:, 3:4], in_=small[:, 2:3])
            kb.dve.tensor_scalar(out=x_n[:, :], in0=tp[:, 0:256], scalar1=small[:, 0:1], scalar2=small[:, 3:4],
                                 op0=ALU.subtract, op1=ALU.mult)
            kb.pool.tensor_tensor(out=x_n[:, :], in0=x_n[:, :], in1=lng[:, :], op=ALU.mult)
            kb.pool.tensor_tensor(out=x_n[:, :], in0=x_n[:, :], in1=lnb[:, :], op=ALU.add)
            kb.act.activation(out=x_n[:, :], in_=x_n[:, :], func=AF.Silu)
            kb.dma(out=scr.ymix.k(("d", i))[i * 128:(i + 1) * 128, 768:1024], in_=x_n[:, :])
        kb.barrier()


def load_w_bf16(kb, dst, wtile, l, nk, ncols):
    for k in range(nk):
        c = 0
        while c < ncols:
            w = min(1024, ncols - c)
            kb.dma(out=dst[:, k, c:c + w], in_=wtile[l, k * 128:(k + 1) * 128, c:c + w], via_pool=True)
            c += w


def load_w_blocks(kb, dst, wtile, l, nk, blocks):
    for (key, c0, cw) in blocks:
        src = wtile.t[l, 0:nk * 128, c0:c0 + cw].rearrange("(k p) c -> p k c", p=128)
        kb.dma(out=dst.k(key)[:, 0:nk, c0:c0 + cw], in_=Ref(wtile, None, src), via_pool=True)


def phase_b(kb, cx, l):
    P = cx.P
    scr = cx.scr
    with ExitStack() as st:
        wo = kb.sb(st, "woB", [128, 8, D], BF16)
        load_w_bf16(kb, wo, P["w_out"], l, 8, D)
        yt = [kb.sb(st, f"ytB{i}", [128, D], F32) for i in range(2)]
        xt = [kb.sb(st, f"xtB{i}", [128, D], F32) for i in range(2)]
        ybf = [kb.sb(st, f"ybfB{i}", [128, D], BF16) for i in range(2)]
        yT = [kb.sb(st, f"yTB{i}", [128, 8, 128], BF16) for i in range(2)]
        tp = [kb.psum(st, f"tpB{i}", [128, 8, 128], BF16) for i in range(2)]
        acc = [kb.psum(st, f"accB{i}", [128, 512], F32) for i in range(4)]
        na = 0
        def load_b(i):
            kb.dma(out=yt[i % 2][:, :], in_=scr.ymix[i * 128:(i + 1) * 128, :])
            kb.dma(out=xt[i % 2][:, :], in_=cx.xres.k(i)[i * 128:(i + 1) * 128, :])
        load_b(0)
        for i in range(NT):
            y_t, x_t, y_b, y_T, t_p = yt[i % 2], xt[i % 2], ybf[i % 2], yT[i % 2], tp[i % 2]
            if i + 1 < NT:
                load_b(i + 1)
            kb.pool.tensor_copy(out=y_b[:, :], in_=y_t[:, :])
            for kc in range(8):
                kb.pe.transpose(out=t_p[:, kc, :], in_=y_b[:, kc * 128:(kc + 1) * 128], identity=cx.ident_bf[:, :])
            kb.act.copy(out=y_T[:, :, :], in_=t_p[:, :, :])
            for c0 in (0, 512):
                a = acc[na % 4]
                na += 1
                for kc in range(8):
                    kb.pe.matmul(out=a[:, :], lhsT=y_T[:, kc, :], rhs=wo[:, kc, c0:c0 + 512], start=(kc == 0), stop=(kc == 7))
                kb.dve.tensor_tensor(out=x_t[:, c0:c0 + 512], in0=x_t[:, c0:c0 + 512], in1=a[:, :], op=ALU.add)
            kb.dma(out=cx.xres.k(i)[i * 128:(i + 1) * 128, :], in_=x_t[:, :])
        kb.barrier()


def phase_c(kb, cx, l):
    P = cx.P
    TB = 256
    with ExitStack() as st:
        wu = kb.sb(st, "wuC", [128, 8, 2 * DFF], BF16)
        wd = kb.sb(st, "wdC", [128, 22, D], BF16)
        order = []
        for i in range(6):
            order += [i, i + 5] if i + 5 < 11 else [i]
        order = [b for b in dict.fromkeys(order) if b < 11]
        load_w_blocks(kb, wu, P["ffn_up"], l, 8, [(b, b * 512, 512) for b in order])
        for k in range(22):
            kb.dma(out=wd.k(k)[:, k, :], in_=P["ffn_down"][l, k * 128:(k + 1) * 128, :], via_pool=True)
        gbc = kb.sb(st, "gbcC", [128, D], F32)
        kb.dma(out=gbc[:, :], in_=Ref(P["norm_ffn"], None, P["norm_ffn"].t[l:l + 1, :].partition_broadcast(128)))
        cw = kb.sb(st, "cwC", [128, 44, 4], F32)
        with kb.nc.allow_non_contiguous_dma(reason="tiny transposed conv weights"):
            for j in range(3):
                kb.dma(out=cw[:, :, j:j + 1], in_=Ref(P["ffn_dw"], None,
                       P["ffn_dw"].t[l, j:j + 1, :].rearrange("o (c p) -> p c o", p=128)))
            kb.dma(out=cw[:, :, 3:4], in_=Ref(P["ffn_dw_b"], None,
                   P["ffn_dw_b"].t[l:l + 1, :].rearrange("o (c p) -> p c o", p=128)))
        carry = kb.sb(st, "carryC", [128, 44, 2], F32)
        kb.pool.memset(ap=carry[:, :, :], constant=0.0)
        xt = [kb.sb(st, f"xtC{i}", [128, 2, D], F32) for i in range(2)]
        hbf = [kb.sb(st, f"hbfC{i}", [128, D], BF16) for i in range(2)]
        junk = kb.sb(st, "junkC", [128, D], BF16)
        hT = [kb.sb(st, f"hTC{i}", [128, 8, TB], BF16) for i in range(2)]
        small = kb.sb(st, "smallC", [128, 16], F32)
        G = [kb.sb(st, f"GC{i}", [128, 22, TB], BF16) for i in range(2)]
        ub = [kb.sb(st, f"ubC{i}", [128, TB + 2], F32) for i in range(4)]
        ac = [kb.sb(st, f"acC{i}", [128, TB], F32) for i in range(4)]
        tp = [kb.psum(st, f"tpC{i}", [128, 8, 128], BF16) for i in range(2)]
        ups = [kb.psum(st, f"upC{i}", [128, 512], F32) for i in range(4)]
        dps = [kb.psum(st, f"dpC{i}", [128, 512], F32) for i in range(2)]
        nu = 0
        nd = 0
        def load_c(b):
            kb.dma(out=xt[b % 2][:, :, :], in_=cx.xres.k(b)[b * TB:(b + 1) * TB, :].with_ap(
                cx.xres.t[b * TB:(b + 1) * TB, :].rearrange("(j p) d -> p j d", p=128)))
        load_c(0)
        for b in range(S // TB):
            x_t, h_T, Gb = xt[b % 2], hT[b % 2], G[b % 2]
            if b + 1 < S // TB:
                load_c(b + 1)
            for j in range(2):
                hb = hbf[j]
                kb.act.activation(out=junk[:, :], in_=x_t[:, j, :], func=AF.Square, accum_out=small[:, j:j + 1])
                rms_rstd(kb, small[:, j:j + 1], small[:, 4 + j:5 + j], D, 1e-6, small[:, 8 + j:9 + j])
                kb.dve.scalar_tensor_tensor(out=hb[:, :], in0=x_t[:, j, :], scalar=small[:, 4 + j:5 + j], in1=gbc[:, :],
                                            op0=ALU.mult, op1=ALU.mult)
                t_p = tp[j]
                for kc in range(8):
                    kb.pe.transpose(out=t_p[:, kc, :], in_=hb[:, kc * 128:(kc + 1) * 128], identity=cx.ident_bf[:, :])
                kb.act.copy(out=h_T[:, :, j * 128:(j + 1) * 128], in_=t_p[:, :, :])
            for ci in range(22):
                res = []
                for half in range(2):
                    c = ci + 22 * half
                    u = ups[nu % 4]
                    u_b = ub[nu % 4]
                    a = ac[nu % 4]
                    nu += 1
                    for kc in range(8):
                        kb.pe.matmul(out=u[:, 0:TB], lhsT=wu.k(c // 4)[:, kc, c * 128:(c + 1) * 128], rhs=h_T[:, kc, :],
                                     start=(kc == 0), stop=(kc == 7))
                    kb.act.copy(out=u_b[:, 2:TB + 2], in_=u[:, 0:TB])
                    kb.pool.tensor_copy(out=u_b[:, 0:2], in_=carry[:, c, :])
                    kb.dve.tensor_scalar(out=a[:, :], in0=u[:, 0:TB], scalar1=cw[:, c, 2:3], scalar2=cw[:, c, 3:4],
                                         op0=ALU.mult, op1=ALU.add)
                    kb.dve.scalar_tensor_tensor(out=a[:, :], in0=u_b[:, 1:TB + 1], scalar=cw[:, c, 1:2], in1=a[:, :],
                                                op0=ALU.mult, op1=ALU.add)
                    kb.dve.scalar_tensor_tensor(out=a[:, :], in0=u_b[:, 0:TB], scalar=cw[:, c, 0:1], in1=a[:, :],
                                                op0=ALU.mult, op1=ALU.add)
                    kb.pool.tensor_copy(out=carry[:, c, :], in_=u_b[:, TB:TB + 2])
                    res.append(a)
                kb.act.activation(out=res[0][:, :], in_=res[0][:, :], func=AF.Silu)
                kb.pool.tensor_tensor(out=Gb[:, ci, :], in0=res[0][:, :], in1=res[1][:, :], op=ALU.mult)
            for j in range(2):
                for c0 in (0, 512):
                    d = dps[nd % 2]
                    nd += 1
                    for ci in range(22):
                        kb.pe.matmul(out=d[:, :], lhsT=Gb[:, ci, j * 128:(j + 1) * 128], rhs=wd.k(ci)[:, ci, c0:c0 + 512],
                                     start=(ci == 0), stop=(ci == 21))
                    kb.dve.tensor_tensor(out=x_t[:, j, c0:c0 + 512], in0=x_t[:, j, c0:c0 + 512], in1=d[:, :], op=ALU.add)
            kb.dma(out=cx.xres.k(b)[b * TB:(b + 1) * TB, :].with_ap(
                cx.xres.t[b * TB:(b + 1) * TB, :].rearrange("(j p) d -> p j d", p=128)), in_=x_t[:, :, :])
        kb.barrier()


def phase_final(kb, cx, y_out):
    P = cx.P
    with ExitStack() as st:
        gbc = kb.sb(st, "gbcF", [128, D], F32)
        kb.dma(out=gbc[:, :], in_=Ref(P["norm_final"], None, P["norm_final"].t.rearrange("(o d) -> o d", o=1).partition_broadcast(128)))
        xt = [kb.sb(st, f"xtF{i}", [128, D], F32) for i in range(2)]
        ot = [kb.sb(st, f"otF{i}", [128, D], F32) for i in range(2)]
        junk = kb.sb(st, "junkF", [128, D], BF16)
        small = kb.sb(st, "smallF", [128, 16], F32)
        kb.dma(out=xt[0][:, :], in_=cx.xres[0:128, :])
        for i in range(NT):
            x_t, o_t = xt[i % 2], ot[i % 2]
            if i + 1 < NT:
                kb.dma(out=xt[(i + 1) % 2][:, :], in_=cx.xres[(i + 1) * 128:(i + 2) * 128, :])
            kb.act.activation(out=junk[:, :], in_=x_t[:, :], func=AF.Square, accum_out=small[:, 0:1])
            rms_rstd(kb, small[:, 0:1], small[:, 1:2], D, 1e-6, small[:, 2:3])
            kb.dve.scalar_tensor_tensor(out=o_t[:, :], in0=x_t[:, :], scalar=small[:, 1:2], in1=gbc[:, :],
                                        op0=ALU.mult, op1=ALU.mult)
            kb.dma(out=y_out[i * 128:(i + 1) * 128, :], in_=o_t[:, :])
        kb.barrier()


class AttnBufs:
    def __init__(self, kb, st, tag):
        self.sps = [kb.psum(st, f"sps{tag}{i}", [128, 512], F32) for i in range(2)]
        self.acc = kb.psum(st, f"acc{tag}", [128, 4, 512], F32)
        self.pts = [kb.sb(st, f"pts{tag}{i}", [128, 512], BF16) for i in range(3)]
        self.ns = 0
        self.nm = 0


def attn_core(kb, A, q_rhs, kv_list, heads_view=False):
    n = len(kv_list)
    for idx, (kT, Vp, mask) in enumerate(kv_list):
        sp = A.sps[A.ns % 2]
        pt = A.pts[A.ns % 3]
        A.ns += 1
        if heads_view:
            spv = sp[:, :].with_ap(sp.t[:, :].rearrange("p (h q) -> p h q", h=4))
            ptv = pt[:, :].with_ap(pt.t[:, :].rearrange("p (h q) -> p h q", h=4))
        else:
            spv, ptv = sp[:, :], pt[:, :]
        kb.pe.matmul(out=sp[:, :], lhsT=kT, rhs=q_rhs, start=True, stop=True)
        kb.act.activation(out=pt[:, :], in_=sp[:, :], func=AF.Exp, scale=0.125)
        if mask is not None:
            eng = kb.dve if (A.nm % 3 != 2) else kb.pool
            A.nm += 1
            eng.tensor_tensor(out=ptv, in0=ptv, in1=mask, op=ALU.mult)
        for j in range(4):
            kb.pe.matmul(out=A.acc[:, j, 0:65], lhsT=pt[:, j * 128:(j + 1) * 128], rhs=Vp, start=(idx == 0), stop=(idx == n - 1))


def attn_evac(kb, A, small, dst, gate=None, first=True, tmp=None):
    kb.dve.tensor_scalar(out=small[:, 0:4], in0=A.acc[:, :, 64], scalar1=1e-30, scalar2=None, op0=ALU.max)
    kb.dve.reciprocal(out=small[:, 4:8], in_=small[:, 0:4])
    if gate is not None:
        kb.dve.tensor_tensor(out=small[:, 4:8], in0=small[:, 4:8], in1=gate, op=ALU.mult)
    sc = small[:, 4:8].with_ap(small.t[:, 4:8].unsqueeze(2).to_broadcast([128, 4, 64]))
    if first:
        kb.dve.tensor_tensor(out=dst, in0=A.acc[:, :, 0:64], in1=sc, op=ALU.mult)
    else:
        kb.dve.tensor_tensor(out=tmp, in0=A.acc[:, :, 0:64], in1=sc, op=ALU.mult)
        kb.pool.tensor_tensor(out=dst, in0=dst, in1=tmp, op=ALU.add)


def group_rmsnorm_store(kb, ob_ref, beta_bc, small, junk, stage_ref, dram_ref):
    kb.act.activation(out=junk, in_=ob_ref, func=AF.Square, accum_out=small[:, 8:9])
    rms_rstd(kb, small[:, 8:9], small[:, 9:10], 256, 1e-6, small[:, 10:11])
    kb.dve.scalar_tensor_tensor(out=stage_ref, in0=ob_ref, scalar=small[:, 9:10], in1=beta_bc, op0=ALU.mult, op1=ALU.mult)
    kb.dma(out=dram_ref, in_=stage_ref)


def build_vprime(kb, vp, src_dram_cols, ld, nh):
    kb.dma(out=ld[:, :, 0:nh * 64], in_=src_dram_cols.with_ap(src_dram_cols.ap.rearrange("(i p) c -> p i c", p=128)))
    kb.pool.memset(ap=vp[:, :, :, 64:65], constant=1.0)
    for h in range(nh):
        kb.dve.tensor_copy(out=vp[:, :, h, 0:64], in_=ld[:, :, h * 64:(h + 1) * 64])


def phase_dil(kb, cx, l):
    P, scr, C = cx.P, cx.scr, cx.C
    with ExitStack() as st:
        qT = kb.sb(st, "qTd", [64, 4, S], BF16)
        kT = kb.sb(st, "kTd", [64, 4, S], BF16)
        kb.dma(out=qT[:, :, :], in_=scr.dqT[:, :].with_ap(scr.dqT.t.rearrange("(h d) s -> d h s", d=64)))
        kb.dma(out=kT[:, :, :], in_=scr.dkT[:, :].with_ap(scr.dkT.t.rearrange("(h d) s -> d h s", d=64)))
        vp = kb.sb(st, "vpd", [128, NT, 4, 65], BF16)
        with ExitStack() as st2:
            ld = kb.sb(st2, "ldd", [128, NT, 256], F32)
            build_vprime(kb, vp, scr.tmB[:, 0:256], ld, 4)
            kb.barrier()
        dm = kb.sb(st, "dmd", [128, 20, 512], BF16)
        kb.dma(out=dm[:, :, :], in_=C["dm"][:, :, :])
        beta = kb.sb(st, "betad", [128, 256], F32)
        kb.dma(out=beta[:, :], in_=Ref(P["beta_dil"], None, P["beta_dil"].t[l:l + 1, :].partition_broadcast(128)))
        ob = [kb.sb(st, f"obd{i}", [128, 4, 256], F32) for i in range(2)]
        stage = [kb.sb(st, f"stgd{i}", [128, 256], F32) for i in range(2)]
        junk = kb.sb(st, "junkd", [128, 256], BF16)
        small = kb.sb(st, "smalld", [128, 16], F32)
        A = AttnBufs(kb, st, "d")
        for b in range(NBLK):
            o_b = ob[b % 2]
            for h in range(4):
                kv = []
                for kt in range(max(0, 4 * b - 16), 4 * b + 4):
                    delta = 4 * b - kt
                    kv.append((kT[:, h, kt * 128:(kt + 1) * 128], vp[:, kt, h, :], dm[:, delta + 3, :]))
                attn_core(kb, A, qT[:, h, b * 512:(b + 1) * 512], kv)
                attn_evac(kb, A, small, o_b[:, :, h * 64:(h + 1) * 64])
            for qt in range(4):
                i = b * 4 + qt
                group_rmsnorm_store(kb, o_b[:, qt, :], beta[:, :], small, junk[:, :], stage[qt % 2][:, :],
                                    scr.ymix.k(("b", i))[i * 128:(i + 1) * 128, 256:512])
        kb.barrier()
def phase_nsa(kb, cx, l):
    P, scr, C = cx.P, cx.scr, cx.C
    with ExitStack() as st:
        qT = kb.sb(st, "qTn", [64, 4, S], BF16)
        kb.dma(out=qT[:, :, :], in_=scr.qT[:, :].with_ap(scr.qT.t.rearrange("(h d) s -> d h s", d=64)))
        ksT = kb.sb(st, "ksTn", [64, S], BF16)
        kwT = kb.sb(st, "kwTn", [64, S], BF16)
        kb.dma(out=ksT[:, :], in_=scr.ksT[:, :])
        kb.dma(out=kwT[:, :], in_=scr.kwT[:, :])
        vps = kb.sb(st, "vpsn", [128, NT, 1, 65], BF16)
        vpw = kb.sb(st, "vpwn", [128, NT, 1, 65], BF16)
        gts = kb.sb(st, "gtsn", [128, NT, 12], F32)
        kcmpT = kb.sb(st, "kcmpTn", [64, 256], BF16)
        vpc = kb.sb(st, "vpcn", [128, 2, 65], BF16)
        selT = kb.sb(st, "selTn", [64, S], BF16)
        small = kb.sb(st, "smalln", [128, 16], F32)
        A = AttnBufs(kb, st, "n")
        x1 = kb.psum(st, "x1n", [128, 512], F32)
        x2 = kb.psum(st, "x2n", [128, 512], F32)
        with ExitStack() as st2:
            ld = kb.sb(st2, "ldn", [128, NT, 204], F32)
            kb.dma(out=ld[:, :, :], in_=scr.tmA[:, :].with_ap(scr.tmA.t.rearrange("(i p) c -> p i c", p=128)))
            kb.pool.memset(ap=vps[:, :, :, 64:65], constant=1.0)
            kb.pool.memset(ap=vpw[:, :, :, 64:65], constant=1.0)
            kb.dve.tensor_copy(out=vps[:, :, 0, 0:64], in_=ld[:, :, 0:64])
            kb.dve.tensor_copy(out=vpw[:, :, 0, 0:64], in_=ld[:, :, 128:192])
            kb.act.activation(out=gts[:, :, :], in_=ld[:, :, 192:204], func=AF.Sigmoid)
            x2t = kb.sb(st2, "x2n_", [128, S], BF16)
            pos2 = kb.sb(st2, "pos2n", [128, 16], F32)
            w1 = kb.sb(st2, "w1n", [128, 16, 128], BF16)
            w2 = kb.sb(st2, "w2n", [128, 64], BF16)
            am = kb.sb(st2, "amn", [128, 16, 256], BF16)
            hidT = kb.sb(st2, "hidTn", [128, 256], BF16)
            with kb.nc.allow_non_contiguous_dma(reason="tiny pos table"):
                kb.dma(out=pos2[:, :], in_=Ref(P["cmp_pos"], None, P["cmp_pos"].t[l].rearrange("(m i) d -> (i d) m", i=2)))
            for kind in ("k", "v"):
                r0 = 0 if kind == "k" else 64
                kb.pool.memset(ap=x2t[:, S - 1:S], constant=0.0)
                kb.dma(out=x2t[0:64, :], in_=scr.kcvcT[r0:r0 + 64, :])
                kb.dma(out=x2t[64:128, 0:S - 1], in_=scr.kcvcT[r0:r0 + 64, 1:S])
                wn1 = P["cmp_k_w1" if kind == "k" else "cmp_v_w1"]
                wn2 = P["cmp_k_w2" if kind == "k" else "cmp_v_w2"]
                kb.dma(out=w1[:, :, :], in_=wn1[l].with_ap(wn1.t[l].rearrange("(m p) h -> p m h", p=128)), via_pool=True)
                kb.dma(out=w2[:, :], in_=wn2[l, :, :], via_pool=True)
                kb.pool.memset(ap=am[:, :, 255:256], constant=0.0)
                xv = x2t.t[:, :].rearrange("p (c r) -> p c r", r=16)
                for m in range(16):
                    src = xv[:, 0:255, 2 * m] if m < 8 else xv[:, 1:256, 2 * m - 16]
                    kb.dve.tensor_scalar(out=am[:, m, 0:255], in0=Ref(x2t, None, src), scalar1=pos2[:, m:m + 1], scalar2=None, op0=ALU.add)
                for m in range(16):
                    kb.pe.matmul(out=x1[:, 0:256], lhsT=w1[:, m, :], rhs=am[:, m, :], start=(m == 0), stop=(m == 15))
                kb.act.activation(out=hidT[:, :], in_=x1[:, 0:256], func=AF.Silu)
                if kind == "k":
                    kb.pe.matmul(out=x2[0:64, 0:256], lhsT=w2[:, :], rhs=hidT[:, :], start=True, stop=True)
                    kb.dve.tensor_copy(out=kcmpT[:, :], in_=x2[0:64, 0:256])
                else:
                    kb.pool.memset(ap=vpc[:, :, 64:65], constant=1.0)
                    for ct in range(2):
                        kb.pe.matmul(out=x2[:, ct * 64:(ct + 1) * 64], lhsT=hidT[:, ct * 128:(ct + 1) * 128], rhs=w2[:, :], start=True, stop=True)
                    kb.dve.tensor_copy(out=vpc[:, :, 0:64], in_=x2[:, 0:128].with_ap(x2.t[:, 0:128].rearrange("p (c d) -> p c d", c=2)))
            kb.barrier()
        with ExitStack() as st3:
            gc = kb.sb(st3, "gcn", [128, 256], F32)
            d0 = kb.sb(st3, "d0n", [128, 64], F32)
            kb.dma(out=gc[:, :], in_=C["gc"][:, :])
            kb.dma(out=d0[:, :], in_=C["d0"][:, :])
            pex = [kb.sb(st3, f"pexn{i}", [128, 4, 256], F32) for i in range(2)]
            imp = [kb.sb(st3, f"impn{i}", [128, 258], F32) for i in range(2)]
            chk = kb.sb(st3, "chkn", [128, 256], F32)
            blk = kb.sb(st3, "blkn", [128, 64], F32)
            sc = kb.sb(st3, "scn", [128, 64], F32)
            sc2 = kb.sb(st3, "sc2n", [128, 64], F32)
            vm = kb.sb(st3, "vmn", [128, 64], F32)
            m8 = kb.sb(st3, "m8n", [128, 16], F32)
            sel = [kb.sb(st3, f"seln{i}", [128, 64], BF16) for i in range(2)]
            for i in range(2):
                kb.pool.memset(ap=imp[i][:, :], constant=0.0)
            scps = [A.acc.k(0), A.acc.k(1)]
            for i in range(NT):
                pe_, im = pex[i % 2], imp[i % 2]
                base = (i % 2) * 2
                scv = A.acc.k(i % 2)[:, base:base + 2, :].with_ap(
                    A.acc.t[:, base:base + 2, :].rearrange("p b (h c) -> p (b h) c", h=2))
                for h in range(4):
                    kb.pe.matmul(out=A.acc.k(i % 2)[:, base + h // 2, (h % 2) * 256:(h % 2) * 256 + 256],
                                 lhsT=qT[:, h, i * 128:(i + 1) * 128], rhs=kcmpT[:, :], start=True, stop=True)
                kb.act.activation(out=pe_[:, :, :], in_=scv, func=AF.Exp, scale=0.125)
                gcb = Ref(gc, None, gc.t[:, :].unsqueeze(1).to_broadcast([128, 4, 256]))
                kb.dve.scalar_tensor_tensor(out=pe_[:, :, :], in0=gcb, scalar=float(128 * i), in1=pe_[:, :, :],
                                            op0=ALU.is_le, op1=ALU.mult)
                kb.dve.tensor_reduce(out=small[:, 0:4], in_=pe_[:, :, :], axis=AX.X, op=ALU.add)
                kb.dve.tensor_scalar(out=small[:, 0:4], in0=small[:, 0:4], scalar1=1e-30, scalar2=None, op0=ALU.max)
                kb.dve.reciprocal(out=small[:, 4:8], in_=small[:, 0:4])
                kb.dve.tensor_scalar(out=im[:, 1:257], in0=pe_[:, 0, :], scalar1=small[:, 4:5], scalar2=None, op0=ALU.mult)
                for h in range(1, 4):
                    kb.dve.scalar_tensor_tensor(out=im[:, 1:257], in0=pe_[:, h, :], scalar=small[:, 4 + h:5 + h], in1=im[:, 1:257],
                                                op0=ALU.mult, op1=ALU.add)
                kb.dve.tensor_tensor(out=chk[:, :], in0=im[:, 0:256], in1=im[:, 1:257], op=ALU.add)
                kb.dve.tensor_reduce(out=blk[:, :], in_=chk[:, :].with_ap(chk.t[:, :].rearrange("p (b r) -> p b r", r=4)),
                                     axis=AX.X, op=ALU.add)
                kb.dve.tensor_scalar(out=sc[:, :], in0=d0[:, :], scalar1=float(128 * i - 127), scalar2=1e9, op0=ALU.is_ge, op1=ALU.mult)
                kb.dve.tensor_tensor(out=sc[:, :], in0=sc[:, :], in1=blk[:, :], op=ALU.max)
                kb.dve.memset(ap=sc[:, 0:1], constant=1e9)
                kb.dve.tensor_single_scalar(out=vm[:, :], in_=d0[:, :], scalar=float(128 * i), op=ALU.is_le)
                kb.dve.tensor_tensor(out=sc[:, :], in0=sc[:, :], in1=vm[:, :], op=ALU.mult)
                kb.dve.scalar_tensor_tensor(out=sc[:, :], in0=vm[:, :], scalar=-1.0, in1=sc[:, :], op0=ALU.add, op1=ALU.add)
                kb.dve.max(out=m8[:, 0:8], in_=sc[:, :])
                kb.dve.match_replace(out=sc2[:, :], in_to_replace=m8[:, 0:8], in_values=sc[:, :], imm_value=-3e38)
                kb.dve.max(out=m8[:, 8:16], in_=sc2[:, :])
                kb.dve.tensor_scalar(out=sel[i % 2][:, :], in0=sc[:, :], scalar1=m8[:, 15:16], scalar2=None, op0=ALU.is_ge)
                tpv = x1[0:64, 0:64].with_ap(x1.t[0:64, 0:64].bitcast(BF16))
                kb.pe.transpose(out=tpv, in_=sel[i % 2][:, :], identity=cx.ident_bf[:, :])
                kb.act.copy(out=selT[:, i * 128:(i + 1) * 128], in_=tpv)
            kb.barrier()
        with ExitStack() as st4:
            maskS = kb.sb(st4, "maskSn", [128, NT, 512], BF16)
            mc = kb.sb(st4, "mcn", [128, 16, 512], BF16)
            cms = kb.sb(st4, "cmsn", [128, 4, 512], BF16)
            cmw = kb.sb(st4, "cmwn", [128, 2, 128], BF16)
            ek = kb.sb(st4, "ekn", [64, 32, 128], BF16)
            kb.dma(out=mc[:, :, :], in_=C["mc"][:, :, :])
            kb.dma(out=cms[:, :, :], in_=C["cms"][:, :, :])
            kb.dma(out=cmw[:, :, :], in_=C["cmw"][:, :, :])
            kb.dma(out=ek[:, :, :], in_=C["ek"][:, :, :])
            beta = kb.sb(st4, "betan", [128, 256], F32)
            kb.dma(out=beta[:, :], in_=Ref(P["beta_nsa"], None, P["beta_nsa"].t[l:l + 1, :].partition_broadcast(128)))
            ob = [kb.sb(st4, f"obn{i}", [128, 4, 256], F32) for i in range(2)]
            tmp = kb.sb(st4, "tmpn", [128, 4, 64], F32)
            stage = [kb.sb(st4, f"stgn{i}", [128, 256], F32) for i in range(2)]
            junk = kb.sb(st4, "junkn", [128, 256], BF16)
            xs = [x1, x2]
            nx = 0
            for b in range(NBLK):
                o_b = ob[b % 2]
                for kt in range(4 * b + 4):
                    xp = xs[nx % 2]
                    nx += 1
                    kb.pe.matmul(out=xp[:, :], lhsT=ek[:, kt, :], rhs=selT[:, b * 512:(b + 1) * 512], start=True, stop=True)
                    if kt >= 4 * b:
                        kb.dve.tensor_tensor(out=maskS[:, kt, :], in0=xp[:, :], in1=cms[:, kt - 4 * b, :], op=ALU.mult)
                    else:
                        kb.act.copy(out=maskS[:, kt, :], in_=xp[:, :])
                for h in range(4):
                    dst = o_b[:, :, h * 64:(h + 1) * 64]
                    kv = [(kcmpT[:, 0:128], vpc[:, 0, :], None if b >= 5 else mc[:, 2 * b, :])]
                    if b >= 4:
                        kv.append((kcmpT[:, 128:256], vpc[:, 1, :], mc[:, 2 * b + 1, :]))
                    attn_core(kb, A, qT[:, h, b * 512:(b + 1) * 512], kv)
                    attn_evac(kb, A, small, dst, gate=gts[:, 4 * b:4 * b + 4, 3 * h], first=True)
                    kv = [(ksT[:, kt * 128:(kt + 1) * 128], vps[:, kt, 0, :], maskS[:, kt, :]) for kt in range(4 * b + 4)]
                    attn_core(kb, A, qT[:, h, b * 512:(b + 1) * 512], kv)
                    attn_evac(kb, A, small, dst, gate=gts[:, 4 * b:4 * b + 4, 3 * h + 1], first=False, tmp=tmp[:, :, :])
                for qt in range(4):
                    i = 4 * b + qt
                    kv = []
                    for kt in range(max(0, i - 4), i + 1):
                        mk = None
                        if kt == i:
                            mk = Ref(cmw, None, cmw.t[:, 0:1, :].to_broadcast([128, 4, 128]))
                        elif kt == i - 4:
                            mk = Ref(cmw, None, cmw.t[:, 1:2, :].to_broadcast([128, 4, 128]))
                        kv.append((kwT[:, kt * 128:(kt + 1) * 128], vpw[:, kt, 0, :], mk))
                    attn_core(kb, A, qT[:, :, i * 128:(i + 1) * 128], kv, heads_view=True)
                    dstw = o_b[:, qt, :].with_ap(o_b.t[:, qt, :].rearrange("p (h d) -> p h d", h=4))
                    gw = gts[:, i, :].with_ap(gts.t[:, i, :].rearrange("p (h r) -> p h r", r=3)[:, :, 2])
                    attn_evac(kb, A, small, dstw, gate=gw, first=False, tmp=tmp[:, :, :])
                for qt in range(4):
                    i = b * 4 + qt
                    group_rmsnorm_store(kb, o_b[:, qt, :], beta[:, :], small, junk[:, :], stage[qt % 2][:, :],
                                        scr.ymix.k(("a", i))[i * 128:(i + 1) * 128, 0:256])
            kb.barrier()
C0 = 0.6065306597126334
RWKV_STAGE = [99]


def phase_rwkv(kb, cx, l):
    P, scr, C = cx.P, cx.scr, cx.C
    CH = 64
    NBUF = 3
    with ExitStack() as st:
        def bc(name, src, c0, n, rows=64):
            t = kb.sb(st, name, [rows, n], F32)
            kb.dma(out=t[:, :], in_=Ref(P[src], None, P[src].t[l:l + 1, c0:c0 + n].partition_broadcast(rows)))
            return t
        mu_bc = bc("mu_r", "rwkv_mu", 0, 768)
        w0_bc = bc("w0_r", "rwkv_w0", 0, 256)
        a0_bc = bc("a0_r", "rwkv_a0", 0, 256)
        kk_bc = bc("kk_r", "rwkv_k_k", 0, 256)
        ka_bc = bc("ka_r", "rwkv_k_a", 0, 256)
        lg_bc = bc("lg_r", "rwkv_ln_g", 0, 256)
        lb_bc = bc("lb_r", "rwkv_ln_b", 0, 256)
        rk_bc = kb.sb(st, "rk_r", [64, 256], F32)
        kb.dma(out=rk_bc[:, :], in_=Ref(P["rwkv_r_k"], None,
               P["rwkv_r_k"].t[l:l + 1].rearrange("o h d -> o (h d)").partition_broadcast(64)))
        oka = kb.sb(st, "oka_r", [64, 256], F32)
        kb.dve.tensor_scalar(out=oka[:, :], in0=ka_bc[:, :], scalar1=-1.0, scalar2=1.0, op0=ALU.mult, op1=ALU.add)
        mu_lo = kb.sb(st, "mulo_r", [128, 2], F32)
        with kb.nc.allow_non_contiguous_dma(reason="tiny mu columns"):
            kb.dma(out=mu_lo[:, 0:1], in_=Ref(P["rwkv_mu"], None, P["rwkv_mu"].t[l:l + 1, 768:896].rearrange("o c -> c o")))
            kb.dma(out=mu_lo[:, 1:2], in_=Ref(P["rwkv_mu"], None, P["rwkv_mu"].t[l:l + 1, 896:1024].rearrange("o c -> c o")))
        wa_up = kb.sb(st, "waup_r", [64, 256], F32)
        a_up = kb.sb(st, "aup_r", [64, 256], F32)
        g_up = kb.sb(st, "gup_r", [128, 256], F32)
        mu_a = kb.sb(st, "mua_r", [64, 1], F32)
        with kb.nc.allow_non_contiguous_dma(reason="tiny mu columns"):
            kb.dma(out=mu_a[:, 0:1], in_=Ref(P["rwkv_mu"], None, P["rwkv_mu"].t[l:l + 1, 832:896].rearrange("o c -> c o")))
        kb.dma(out=wa_up[0:64, :], in_=P["rwkv_w_up"][l, :, :])
        kb.dma(out=a_up[0:64, :], in_=P["rwkv_a_up"][l, :, :])
        kb.dma(out=g_up[:, :], in_=P["rwkv_g_up"][l, :, :])
        tri = kb.sb(st, "tri_r", [64, 2, 64], F32)
        kb.dma(out=tri[:, 0, :], in_=C["tri"][0:64, 0:64])
        kb.dma(out=tri[:, 1, :], in_=C["mstrictT"][:, 0, :])
        mst = kb.sb(st, "mst_r", [64, 4, 64], F32)
        mstT = kb.sb(st, "mstT_r", [64, 4, 64], F32)
        minc = kb.sb(st, "minc_r", [64, 4, 64], F32)
        kb.dma(out=mst[:, :, :], in_=C["mstrict"][:, 0:4, :])
        kb.dma(out=mstT[:, :, :], in_=C["mstrictT"][:, 0:4, :])
        kb.dma(out=minc[:, :, :], in_=C["mincl"][:, 0:4, :])
        ST = kb.sb(st, "ST_r", [64, 4, 64], F32)
        kb.pool.memset(ap=ST[:, :, :], constant=0.0)
        idb = Ref(cx.ident_f, None, cx.ident_f.t[0:64, 0:64].unsqueeze(1).to_broadcast([64, 4, 64]))

        def T2(name, shape, n=3):
            return [kb.sb(st, f"{name}{i}_r", shape, F32) for i in range(n)]
        rkv, prv, lo, gd = T2("rkv", [64, 768]), T2("prv", [64, 768]), T2("lo", [64, 65]), T2("gd", [128, 65])
        los, gds = T2("los", [64, 64]), T2("gds", [128, 64])
        loa, loas = T2("loa", [64, 65]), T2("loas", [64, 64])
        sgt, a_t, g_t = T2("sgt", [64, 256]), T2("at", [64, 256]), T2("gt", [64, 256])
        kk, k2, bb = T2("kkt", [64, 256]), T2("k2t", [64, 256]), T2("bbt", [64, 256])
        tmp, tmp2 = T2("tmp", [64, 256]), T2("tmp2", [64, 256])
        Pm, iP, Pp, Pr = T2("Pm", [64, 256]), T2("iP", [64, 256]), T2("Pp", [64, 256]), T2("Pr", [64, 256])
        Kt, Bt, KKt, Rt, Kh, Bh = (T2("Kt", [64, 256]), T2("Bt", [64, 256]), T2("KKt", [64, 256]), T2("Rt", [64, 256]),
                                    T2("Kh", [64, 256]), T2("Bh", [64, 256]))
        FMq = T2("FMq", [64, 16, 64])
        N, NT_, Mak, Abr, Akr = (T2("N", [64, 4, 64]), T2("NT", [64, 4, 64]), T2("Mak", [64, 4, 64]), T2("Abr", [64, 4, 64]),
                                 T2("Akr", [64, 4, 64]))
        TA_ = T2("TA", [64, 8, 64])
        Wsb, Xsb, U0T, UT = T2("Wsb", [64, 4, 64]), T2("Xsb", [64, 4, 64]), T2("U0T", [64, 4, 64]), T2("UT", [64, 4, 64])
        pcs = T2("pcs", [64, 4])
        yv = T2("yv", [64, 256])
        small = T2("small", [64, 16])
        B = [kb.psum(st, f"B{i}_r", [64, 512], F32) for i in range(8)]

        def v3(ref_tile, c0):
            return ref_tile[:, c0:c0 + 256].with_ap(ref_tile.t[:, c0:c0 + 256].rearrange("p (h d) -> p h d", h=4))

        def hb(t_small, c0):
            return t_small[:, c0:c0 + 4].with_ap(t_small.t[:, c0:c0 + 4].unsqueeze(2).to_broadcast([64, 4, 64]))

        def chunk(c):
            p = c % NBUF
            t0 = c * CH
            kb.dma(out=rkv[p][:, :], in_=scr.tmB[t0:t0 + CH, 256:1024])
            if c == 0:
                kb.pool.memset(ap=prv[p][0:1, :], constant=0.0)
                kb.dma(out=prv[p][1:CH, :], in_=scr.tmB[0:CH - 1, 256:1024])
                kb.pool.memset(ap=lo[p][:, 0:1], constant=0.0)
                kb.pool.memset(ap=gd[p][:, 0:1], constant=0.0)
                kb.dma(out=lo[p][:, 1:65], in_=scr.loraT[0:64, 0:CH])
                kb.pool.memset(ap=loa[p][:, 0:1], constant=0.0)
                kb.dma(out=loa[p][:, 1:65], in_=scr.loraT[64:128, 0:CH])
                kb.dma(out=gd[p][:, 1:65], in_=scr.loraT[128:256, 0:CH])
            else:
                kb.dma(out=prv[p][:, :], in_=scr.tmB[t0 - 1:t0 + CH - 1, 256:1024])
                kb.dma(out=lo[p][:, :], in_=scr.loraT[0:64, t0 - 1:t0 + CH])
                kb.dma(out=loa[p][:, :], in_=scr.loraT[64:128, t0 - 1:t0 + CH])
                kb.dma(out=gd[p][:, :], in_=scr.loraT[128:256, t0 - 1:t0 + CH])
            kb.dve.tensor_tensor(out=los[p][:, :], in0=lo[p][:, 0:64], in1=lo[p][:, 1:65], op=ALU.subtract)
            kb.dve.scalar_tensor_tensor(out=los[p][:, :], in0=los[p][:, :], scalar=mu_lo[0:64, 0:1], in1=lo[p][:, 1:65], op0=ALU.mult, op1=ALU.add)
            kb.dve.tensor_tensor(out=loas[p][:, :], in0=loa[p][:, 0:64], in1=loa[p][:, 1:65], op=ALU.subtract)
            kb.dve.scalar_tensor_tensor(out=loas[p][:, :], in0=loas[p][:, :], scalar=mu_a[:, 0:1], in1=loa[p][:, 1:65], op0=ALU.mult, op1=ALU.add)
            kb.dve.tensor_tensor(out=gds[p][:, :], in0=gd[p][:, 0:64], in1=gd[p][:, 1:65], op=ALU.subtract)
            kb.dve.scalar_tensor_tensor(out=gds[p][:, :], in0=gds[p][:, :], scalar=mu_lo[:, 1:2], in1=gd[p][:, 1:65], op0=ALU.mult, op1=ALU.add)
            kb.act.activation(out=los[p][0:64, :], in_=los[p][0:64, :], func=AF.Tanh)
            kb.act.activation(out=gds[p][:, :], in_=gds[p][:, :], func=AF.Sigmoid)
            kb.pe.matmul(out=B[0][:, 0:256], lhsT=los[p][0:64, :], rhs=wa_up[0:64, :], start=True, stop=True)
            kb.pe.matmul(out=B[0][:, 256:512], lhsT=loas[p][:, :], rhs=a_up[:, :], start=True, stop=True)
            kb.pe.matmul(out=B[1][:, 0:256], lhsT=gds[p][:, :], rhs=g_up[:, :], start=True, stop=True)
            kb.dve.tensor_tensor(out=sgt[p][:, :], in0=B[0][:, 0:256], in1=w0_bc[:, :], op=ALU.add)
            kb.act.activation(out=sgt[p][:, :], in_=sgt[p][:, :], func=AF.Sigmoid)
            kb.dve.tensor_tensor(out=a_t[p][:, :], in0=B[0][:, 256:512], in1=a0_bc[:, :], op=ALU.add)
            kb.act.activation(out=a_t[p][:, :], in_=a_t[p][:, :], func=AF.Sigmoid)
            kb.act.copy(out=g_t[p][:, :], in_=B[1][:, 0:256])
            kb.pool.tensor_tensor(out=prv[p][:, :], in0=prv[p][:, :], in1=rkv[p][:, :], op=ALU.subtract)
            kb.pool.tensor_tensor(out=prv[p][:, :], in0=prv[p][:, :], in1=mu_bc[:, :], op=ALU.mult)
            kb.dve.tensor_tensor(out=rkv[p][:, :], in0=rkv[p][:, :], in1=prv[p][:, :], op=ALU.add)
            yield
            r_, k_, v_ = rkv[p][:, 0:256], rkv[p][:, 256:512], rkv[p][:, 512:768]
            kb.dve.tensor_tensor(out=kk[p][:, :], in0=k_, in1=kk_bc[:, :], op=ALU.mult)
            kb.pool.tensor_tensor(out=tmp[p][:, :], in0=kk[p][:, :], in1=kk[p][:, :], op=ALU.mult)
            kb.dve.tensor_reduce(out=small[p][:, 0:4], in_=v3(tmp[p], 0), axis=AX.X, op=ALU.add)
            kb.dve.tensor_scalar(out=small[p][:, 0:4], in0=small[p][:, 0:4], scalar1=1e-12, scalar2=None, op0=ALU.add)
            kb.act.activation(out=small[p][:, 0:4], in_=small[p][:, 0:4], func=AF.Sqrt)
            kb.dve.reciprocal(out=small[p][:, 4:8], in_=small[p][:, 0:4])
            kb.dve.tensor_tensor(out=v3(kk[p], 0), in0=v3(kk[p], 0), in1=hb(small[p], 4), op=ALU.mult)
            kb.pool.tensor_tensor(out=tmp[p][:, :], in0=a_t[p][:, :], in1=ka_bc[:, :], op=ALU.mult)
            kb.pool.tensor_tensor(out=tmp[p][:, :], in0=tmp[p][:, :], in1=oka[:, :], op=ALU.add)
            kb.dve.tensor_tensor(out=k2[p][:, :], in0=k_, in1=tmp[p][:, :], op=ALU.mult)
            kb.pool.tensor_tensor(out=bb[p][:, :], in0=kk[p][:, :], in1=a_t[p][:, :], op=ALU.mult)
            yield
            kb.pe.matmul(out=B[2][:, 0:256], lhsT=tri[:, 0, :], rhs=sgt[p][:, :], start=True, stop=True)
            kb.pe.matmul(out=B[2][:, 256:512], lhsT=tri[:, 1, :], rhs=sgt[p][:, :], start=True, stop=True)
            kb.act.activation(out=Pm[p][:, :], in_=B[2][:, 0:256], func=AF.Exp, scale=-C0)
            kb.act.activation(out=iP[p][:, :], in_=B[2][:, 0:256], func=AF.Exp, scale=C0)
            kb.dve.tensor_tensor(out=tmp2[p][:, :], in0=B[2][:, 0:256], in1=sgt[p][:, :], op=ALU.subtract)
            kb.act.activation(out=Pp[p][:, :], in_=tmp2[p][:, :], func=AF.Exp, scale=-C0)
            kb.act.activation(out=Pr[p][:, :], in_=B[2][:, 256:512], func=AF.Exp, scale=-C0)
            kb.dve.tensor_tensor(out=Kt[p][:, :], in0=k2[p][:, :], in1=iP[p][:, :], op=ALU.mult)
            kb.pool.tensor_tensor(out=Bt[p][:, :], in0=bb[p][:, :], in1=iP[p][:, :], op=ALU.mult)
            kb.dve.tensor_tensor(out=KKt[p][:, :], in0=kk[p][:, :], in1=Pp[p][:, :], op=ALU.mult)
            kb.pool.tensor_tensor(out=Rt[p][:, :], in0=r_, in1=Pm[p][:, :], op=ALU.mult)
            kb.dve.tensor_tensor(out=Kh[p][:, :], in0=k2[p][:, :], in1=Pr[p][:, :], op=ALU.mult)
            kb.pool.tensor_tensor(out=Bh[p][:, :], in0=bb[p][:, :], in1=Pr[p][:, :], op=ALU.mult)
            yield
            for h in range(4):
                kb.pe.matmul(out=B[1][:, 256 + 2 * h:258 + 2 * h], lhsT=Pm[p][:, h * 64:(h + 1) * 64], rhs=cx.ident_f[0:64, 62:64], start=True, stop=True)
            kb.act.copy(out=pcs[p][:, :], in_=B[1][:, 256:264].with_ap(B[1].t[:, 256:264].rearrange("p (h two) -> p h two", two=2)[:, :, 1]))
            yield
            for qi, q in enumerate((Bt, Kt, KKt, Rt)):
                for h in range(4):
                    idx = qi * 4 + h
                    bk = B[3] if idx < 8 else B[4]
                    kb.pe.transpose(out=bk[:, (idx % 8) * 64:(idx % 8 + 1) * 64], in_=q[p][:, h * 64:(h + 1) * 64], identity=cx.ident_f[0:64, 0:64])
            fm = FMq[p]
            kb.act.copy(out=fm[:, 0:8, :], in_=B[3][:, :].with_ap(B[3].t[:, :].rearrange("p (a b) -> p a b", b=64)))
            kb.dve.tensor_copy(out=fm[:, 8:16, :], in_=B[4][:, :].with_ap(B[4].t[:, :].rearrange("p (a b) -> p a b", b=64)))
            BT = lambda h: fm[:, 0 + h, :]
            KT = lambda h: fm[:, 4 + h, :]
            KKT = lambda h: fm[:, 8 + h, :]
            RT = lambda h: fm[:, 12 + h, :]
            fm4 = fm.t.rearrange("p (q h) t -> p q h t", q=4)
            for h in range(4):
                kkr = Ref(fm, None, fm4[:, 2:4, h, :])
                o5 = B[5][:, h * 128:(h + 1) * 128]
                o6 = B[6][:, h * 128:(h + 1) * 128]
                kb.pe.matmul(out=o5, lhsT=BT(h), rhs=kkr, start=True, stop=True)
                kb.pe.matmul(out=o6, lhsT=KT(h), rhs=kkr, start=True, stop=True)
                kb.pe.matmul(out=B[7][:, h * 64:(h + 1) * 64], lhsT=KKT(h), rhs=BT(h), start=True, stop=True)
            hw = lambda bk, w: Ref(bk, None, bk.t.rearrange("p (h w t) -> p h w t", h=4, w=2)[:, :, w, :])
            b3 = lambda bk, c0: bk[:, c0:c0 + 256].with_ap(bk.t[:, c0:c0 + 256].rearrange("p (h d) -> p h d", h=4))
            TAv = TA_[p].t.rearrange("p (h w) t -> p h w t", w=2)
            TA2 = TA_[p].t.rearrange("p a t -> p (a t)")
            Tv = Ref(TA_[p], None, TAv[:, :, 0, :])
            Av = Ref(TA_[p], None, TAv[:, :, 1, :])
            kb.dve.scalar_tensor_tensor(out=Av, in0=hw(B[5], 0), scalar=-1.0, in1=mst[:, :, :], op0=ALU.mult, op1=ALU.mult)
            kb.dve.scalar_tensor_tensor(out=NT_[p][:, :, :], in0=b3(B[7], 0), scalar=-1.0, in1=mstT[:, :, :], op0=ALU.mult, op1=ALU.mult)
            kb.dve.tensor_tensor(out=Mak[p][:, :, :], in0=hw(B[6], 0), in1=mst[:, :, :], op=ALU.mult)
            kb.dve.tensor_tensor(out=Abr[p][:, :, :], in0=hw(B[5], 1), in1=minc[:, :, :], op=ALU.mult)
            kb.dve.tensor_tensor(out=Akr[p][:, :, :], in0=hw(B[6], 1), in1=minc[:, :, :], op=ALU.mult)
            yield
            kb.pool.tensor_copy(out=Tv, in_=idb)
            AT_ = NT_[p]
            B5v = B[5].t.rearrange("p (h w t) -> p h w t", h=4, w=2)
            for j in range(6):
                last = (j == 5)
                for h in range(4):
                    if last:
                        kb.pe.matmul(out=B[5][:, h * 128:h * 128 + 64], lhsT=AT_[:, h, :], rhs=Ref(TA_[p], None, TAv[:, h, 0, :]), start=True, stop=True)
                    else:
                        kb.pe.matmul(out=B[5][:, h * 128:(h + 1) * 128], lhsT=AT_[:, h, :], rhs=Ref(TA_[p], None, TA2[:, h * 128:(h + 1) * 128]), start=True, stop=True)
                        kb.pe.matmul(out=B[6][:, h * 64:(h + 1) * 64], lhsT=Ref(TA_[p], None, TAv[:, h, 1, :]), rhs=AT_[:, h, :], start=True, stop=True)
                kb.dve.tensor_tensor(out=Tv, in0=Tv, in1=Ref(B[5], None, B5v[:, :, 0, :]), op=ALU.add)
                if not last:
                    kb.act.copy(out=Av, in_=Ref(B[5], None, B5v[:, :, 1, :]))
                    kb.dve.tensor_copy(out=AT_[:, :, :], in_=b3(B[6], 0))
                yield
            yield
            for h in range(4):
                kb.pe.matmul(out=B[7][:, 256 + h * 64:256 + (h + 1) * 64], lhsT=KKt[p][:, h * 64:(h + 1) * 64], rhs=Ref(TA_[p], None, TAv[:, h, 0, :]), start=True, stop=True)
                kb.pe.matmul(out=B[3][:, h * 64:(h + 1) * 64], lhsT=Mak[p][:, h, :], rhs=rkv[p][:, 512 + h * 64:512 + (h + 1) * 64], start=True, stop=True)
            kb.act.copy(out=Wsb[p][:, :, :], in_=b3(B[7], 256))
            kb.dve.tensor_copy(out=Xsb[p][:, :, :], in_=b3(B[3], 0))
            for h in range(4):
                kb.pe.matmul(out=B[3][:, 256 + h * 64:256 + (h + 1) * 64], lhsT=Ref(TA_[p], None, TAv[:, h, 0, :]), rhs=Xsb[p][:, h, :], start=True, stop=True)
            kb.act.activation(out=U0T[p][:, :, :], in_=b3(B[3], 256), func=AF.Copy, scale=-1.0)
            yield
            for h in range(4):
                vh = rkv[p][:, 512 + h * 64:512 + (h + 1) * 64]
                kb.pe.matmul(out=B[4][:, h * 64:(h + 1) * 64], lhsT=Wsb[p][:, h, :], rhs=ST[:, h, :], start=True, stop=True)
                kb.dve.tensor_tensor(out=UT[p][:, h, :], in0=U0T[p][:, h, :], in1=B[4][:, h * 64:(h + 1) * 64], op=ALU.subtract)
                yo = B[4][:, 256 + h * 64:256 + (h + 1) * 64]
                kb.pe.matmul(out=yo, lhsT=RT(h), rhs=ST[:, h, :], start=True, stop=False)
                kb.pe.matmul(out=yo, lhsT=Abr[p][:, h, :], rhs=UT[p][:, h, :], start=False, stop=False)
                kb.pe.matmul(out=yo, lhsT=Akr[p][:, h, :], rhs=vh, start=False, stop=True)
                kb.act.copy(out=yv[p][:, h * 64:(h + 1) * 64], in_=yo)
                so = B[2][:, h * 64:(h + 1) * 64]
                kb.pe.matmul(out=so, lhsT=Bh[p][:, h * 64:(h + 1) * 64], rhs=UT[p][:, h, :], start=True, stop=False)
                kb.pe.matmul(out=so, lhsT=Kh[p][:, h * 64:(h + 1) * 64], rhs=vh, start=False, stop=True)
                kb.dve.scalar_tensor_tensor(out=ST[:, h, :], in0=ST[:, h, :], scalar=pcs[p][:, h:h + 1], in1=so, op0=ALU.mult, op1=ALU.add)
                yield
            yield
            y = yv[p]
            kb.pool.tensor_tensor(out=tmp[p][:, :], in0=r_, in1=k2[p][:, :], op=ALU.mult)
            kb.pool.tensor_tensor(out=tmp[p][:, :], in0=tmp[p][:, :], in1=rk_bc[:, :], op=ALU.mult)
            kb.dve.tensor_reduce(out=small[p][:, 8:12], in_=v3(tmp[p], 0), axis=AX.X, op=ALU.add)
            kb.dve.tensor_tensor(out=v3(tmp2[p], 0), in0=v3(rkv[p], 512), in1=hb(small[p], 8), op=ALU.mult)
            kb.dve.tensor_tensor(out=y[:, :], in0=tmp2[p][:, :], in1=y[:, :], op=ALU.add)
            kb.dve.tensor_reduce(out=small[p][:, 0:4], in_=v3(y, 0), axis=AX.X, op=ALU.add)
            kb.dve.tensor_scalar(out=small[p][:, 0:4], in0=small[p][:, 0:4], scalar1=1.0 / 64, scalar2=None, op0=ALU.mult)
            kb.dve.tensor_tensor(out=v3(y, 0), in0=v3(y, 0), in1=hb(small[p], 0), op=ALU.subtract)
            kb.pool.tensor_tensor(out=tmp[p][:, :], in0=y[:, :], in1=y[:, :], op=ALU.mult)
            kb.dve.tensor_reduce(out=small[p][:, 4:8], in_=v3(tmp[p], 0), axis=AX.X, op=ALU.add)
            kb.dve.tensor_scalar(out=small[p][:, 4:8], in0=small[p][:, 4:8], scalar1=1.0 / 64, scalar2=64e-5, op0=ALU.mult, op1=ALU.add)
            kb.act.activation(out=small[p][:, 4:8], in_=small[p][:, 4:8], func=AF.Sqrt)
            kb.dve.reciprocal(out=small[p][:, 12:16], in_=small[p][:, 4:8])
            kb.dve.tensor_tensor(out=v3(y, 0), in0=v3(y, 0), in1=hb(small[p], 12), op=ALU.mult)
            kb.pool.tensor_tensor(out=y[:, :], in0=y[:, :], in1=lg_bc[:, :], op=ALU.mult)
            kb.pool.tensor_tensor(out=y[:, :], in0=y[:, :], in1=lb_bc[:, :], op=ALU.add)
            kb.dve.tensor_tensor(out=y[:, :], in0=y[:, :], in1=g_t[p][:, :], op=ALU.mult)
            kb.dma(out=scr.ymix.k(("c", c))[t0:t0 + CH, 512:768], in_=y[:, :])

        import os as _os
        nch = int(_os.environ.get('RWKV_NCH', S // CH))
        active = []
        nxt = 0
        while nxt < nch or active:
            if nxt < nch and len(active) < NBUF:
                active.append(chunk(nxt))
                nxt += 1
            for g in list(active):
                try:
                    next(g)
                except StopIteration:
                    active.remove(g)
        kb.barrier()
def build(depth=DEPTH, debug=None, stop_after=None, only=None):
    kb = KB()
    nc = kb.nc
    cx = Ctx()
    cx.P = {}
    x_in = kb.dram("x", [S, D], F32, kind="ExternalInput")
    for n, shp in PARAM_SHAPES.items():
        cx.P[n] = kb.dram(n, list(shp), F32, kind="ExternalInput")
    consts = make_consts()
    cx.C = {}
    for n, a in consts.items():
        cx.C[n] = kb.dram("c_" + n, list(a.shape), CONST_DT.get(n, F32), kind="ExternalInput")
    y_out = kb.dram("y", [S, D], F32, kind="ExternalOutput")
    scr = Ctx()
    cx.scr = scr
    dbg = debug or []

    def scratch(name, shape, dt):
        kind = "ExternalOutput" if name in dbg else "Internal"
        return kb.dram("scr_" + name, shape, dt, kind=kind)
    scr.qT = scratch("qT", [256, S], BF16)
    scr.kcvcT = scratch("kcvcT", [128, S], BF16)
    scr.ksT = scratch("ksT", [64, S], BF16)
    scr.kwT = scratch("kwT", [64, S], BF16)
    scr.dqT = scratch("dqT", [256, S], BF16)
    scr.dkT = scratch("dkT", [256, S], BF16)
    scr.loraT = scratch("loraT", [256, S], F32)
    scr.convT = scratch("convT", [512, S], F32)
    scr.tmA = scratch("tmA", [S, 204], F32)
    scr.tmB = scratch("tmB", [S, 1024], F32)
    scr.ymix = scratch("ymix", [S, D], F32)
    cx.xres = scratch("xres", [S, D], F32)
    outs = [y_out] + [getattr(scr, n) if hasattr(scr, n) else cx.xres for n in dbg]

    gst = ExitStack()
    cx.ident_bf = kb.sb(gst, "ident_bf", [128, 128], BF16)
    cx.ident_f = kb.sb(gst, "ident_f", [128, 128], F32)
    kb.dma(out=cx.ident_bf[:, :], in_=cx.C["ident_bf"][:, :])
    kb.dma(out=cx.ident_f[:, :], in_=cx.C["ident_f"][:, :])
    kb.dma(out=cx.xres[:, :], in_=x_in[:, :])

    for l in range(depth):
        phase_a(kb, cx, l)
        if stop_after == "a":
            break
        if only in (None, "conv"):
            phase_conv(kb, cx, l)
        if only in (None, "dil"):
            phase_dil(kb, cx, l)
        if only in (None, "nsa"):
            phase_nsa(kb, cx, l)
        if only in (None, "rwkv"):
            phase_rwkv(kb, cx, l)
        if stop_after == "mix":
            break
        phase_b(kb, cx, l)
        if stop_after == "b":
            break
        phase_c(kb, cx, l)
    if stop_after is None:
        phase_final(kb, cx, y_out)
    gst.close()
    kb.finish(outs)
    return kb, consts


_CACHE = {}


def kernel(**inputs):
    if "prog" not in _CACHE:
        _CACHE["prog"] = build()
    kb, consts = _CACHE["prog"]
    x = np.ascontiguousarray(inputs["x"], dtype=np.float32)
    in_maps = []
    for c in range(8):
        m = {"x": x[c]}
        for n in PARAM_SHAPES:
            m[n] = np.ascontiguousarray(inputs[n], dtype=np.float32)
        for n, a in consts.items():
            m["c_" + n] = a
        in_maps.append(m)
    res = run_bass_kernel_spmd(kb.nc, in_maps, core_ids=list(range(8)))
    return np.stack([res.results[c]["y"] for c in range(8)], axis=0)
```

```python
import numpy as np
import ml_dtypes
from contextlib import ExitStack
import concourse.bass as bass
import concourse.mybir as mybir
from concourse.bass_utils import run_bass_kernel_spmd

F32 = mybir.dt.float32
BF16 = mybir.dt.bfloat16
I32 = mybir.dt.int32
AF = mybir.ActivationFunctionType
ALU = mybir.AluOpType
AX = mybir.AxisListType

S = 4096
D = 1024
NT = S // 128
NBLK = S // 512
DEPTH = 4
DFF = 2816
IN_COLS = 2956
WRITE_NAMES = ("out", "accum_out", "ap")


class SemT:
    def __init__(self, handle):
        self.h = handle
        self.count = 0


class Rec:
    __slots__ = ("lw", "rd")

    def __init__(self):
        self.lw = None
        self.rd = []


class Tile:
    def __init__(self, t, name):
        self.t = t
        self.name = name
        self.regs = {None: Rec()}
        self.excl = False

    def recs_dep(self, key):
        if key is None:
            return list(self.regs.values())
        if key not in self.regs:
            self.regs[key] = Rec()
        return [self.regs[key], self.regs[None]]

    def recs_upd(self, key, is_write):
        if key is None:
            return list(self.regs.values()) if is_write else [self.regs[None]]
        if key not in self.regs:
            self.regs[key] = Rec()
        return [self.regs[key]]

    def __getitem__(self, idx):
        return Ref(self, None, self.t[idx])

    def k(self, key):
        return KeyView(self, key)


class KeyView:
    def __init__(self, tile, key):
        self.tile = tile
        self.key = key

    def __getitem__(self, idx):
        return Ref(self.tile, self.key, self.tile.t[idx])


class Ref:
    def __init__(self, tile, key, ap):
        self.tile = tile
        self.key = key
        self.ap = ap

    def with_ap(self, ap):
        return Ref(self.tile, self.key, ap)


class Eng:
    def __init__(self, kb, name, raw, sem, is_pe=False):
        self.kb = kb
        self.name = name
        self.raw = raw
        self.sem = sem
        self.waited = {}
        self.is_pe = is_pe

    def __getattr__(self, opname):
        def call(**kw):
            reads, writes = [], []
            kw2 = {}
            for n, v in kw.items():
                if isinstance(v, Ref):
                    (writes if n in WRITE_NAMES else reads).append(v)
                    kw2[n] = v.ap
                else:
                    kw2[n] = v
            return self.kb.emit(self, lambda: getattr(self.raw, opname)(**kw2), reads, writes)
        return call


class KB:
    def __init__(self):
        self.nc = bass.Bass("TRN2", target_bir_lowering=False)
        nc = self.nc
        self.es = ExitStack()
        mk = lambda n: SemT(self.es.enter_context(nc.semaphore(n)))
        self.pe = Eng(self, "pe", nc.tensor, mk("s_pe"), is_pe=True)
        self.act = Eng(self, "act", nc.scalar, mk("s_act"))
        self.dve = Eng(self, "dve", nc.vector, mk("s_dve"))
        self.pool = Eng(self, "pool", nc.gpsimd, mk("s_pool"))
        self.sp = Eng(self, "sp", nc.sync, mk("s_sp"))
        self.ring = [mk(f"s_dma{i}") for i in range(24)]
        self.ring_i = 0
        self.pring = [mk(f"s_pdma{i}") for i in range(8)]
        self.pring_i = 0
        self.n_inst = 0
        self.out_deps = []

    def dram(self, name, shape, dtype, kind="Internal"):
        return Tile(self.nc.dram_tensor(name, list(shape), dtype, kind=kind).ap(), name)

    def sb(self, stack, name, shape, dtype):
        self.n_alloc = getattr(self, "n_alloc", 0) + 1
        name = f"{name}_{self.n_alloc}"
        return Tile(stack.enter_context(self.nc.sbuf_tensor(name, list(shape), dtype)), name)

    def psum(self, stack, name, shape, dtype):
        self.n_alloc = getattr(self, "n_alloc", 0) + 1
        name = f"{name}_{self.n_alloc}"
        t = Tile(stack.enter_context(self.nc.psum_tensor(name, list(shape), dtype)), name)
        t.excl = True
        return t

    def _wait(self, eng, deps):
        best = {}
        for (st, v) in deps:
            if v is None:
                continue
            if id(st) not in best or best[id(st)][1] < v:
                best[id(st)] = (st, v)
        for st, v in best.values():
            if st is eng.sem and eng.is_pe:
                continue
            if eng.waited.get(id(st), 0) >= v:
                continue
            eng.raw.wait_ge(st.h, v)
            eng.waited[id(st)] = v

    def _collect(self, reads, writes):
        deps = []
        for r in reads:
            for rec in r.tile.recs_dep(r.key):
                if rec.lw is not None:
                    deps.append(rec.lw)
                if r.tile.excl:
                    deps.extend(rec.rd)
        for w in writes:
            for rec in w.tile.recs_dep(w.key):
                if rec.lw is not None:
                    deps.append(rec.lw)
                deps.extend(rec.rd)
        return deps

    def _update(self, reads, writes, tag):
        for r in reads:
            for rec in r.tile.recs_upd(r.key, False):
                rec.rd.append(tag)
                if len(rec.rd) > 48:
                    best = {}
                    for st, v in rec.rd:
                        if id(st) not in best or best[id(st)][1] < v:
                            best[id(st)] = (st, v)
                    rec.rd = list(best.values())
        for w in writes:
            for rec in w.tile.recs_upd(w.key, True):
                rec.lw = tag
                rec.rd = []

    def emit(self, eng, fn, reads, writes):
        deps = self._collect(reads, writes)
        self._wait(eng, deps)
        inst = fn()
        eng.sem.count += 1
        inst.then_inc(eng.sem.h, 1)
        self._update(reads, writes, (eng.sem, eng.sem.count))
        self.n_inst += 1
        return inst

    def dma(self, out, in_, via_pool=False, **kw):
        eng = self.pool if via_pool else self.sp
        if via_pool:
            st = self.pring[self.pring_i % len(self.pring)]
            self.pring_i += 1
        else:
            st = self.ring[self.ring_i % len(self.ring)]
            self.ring_i += 1
        deps = self._collect([in_], [out])
        deps.append((st, st.count))
        self._wait(eng, deps)
        inst = eng.raw.dma_start(out=out.ap, in_=in_.ap, **kw)
        st.count += 16
        inst.then_inc(st.h, 16)
        tag = (st, st.count)
        self._update([in_], [out], tag)
        self.n_inst += 1
        return tag

    def barrier(self):
        deps = [(st, st.count) for st in self.ring + self.pring]
        deps += [(e.sem, e.sem.count) for e in (self.pe, self.act, self.dve, self.pool)]
        deps = [d for d in deps if d[1] > 0]
        for e in (self.pe, self.act, self.dve, self.pool, self.sp):
            self._wait(e, [d for d in deps if not (d[0] is e.sem)])

    def finish(self, out_tiles):
        deps = [(st, st.count) for st in self.ring + self.pring]
        deps += [(e.sem, e.sem.count) for e in (self.pe, self.act, self.dve, self.pool)]
        self._wait(self.sp, [d for d in deps if d[1] > 0])
        self.es.close()


def _bf(a):
    return np.ascontiguousarray(a.astype(np.float32)).astype(ml_dtypes.bfloat16)


def make_consts():
    c = {}
    c["ident_bf"] = _bf(np.eye(128))
    c["ident_f"] = np.eye(128, dtype=np.float32)
    sp = np.arange(128)[:, None]
    tq = np.arange(512)[None, :]
    c["cms"] = _bf(np.stack([(128 * j + sp <= tq) for j in range(4)], axis=1))
    tq1 = np.arange(128)[None, :]
    c["cmw"] = _bf(np.stack([(sp <= tq1), (sp >= tq1)], axis=1))
    dm = []
    for delta in range(-3, 17):
        d = tq - sp + 128 * delta
        cnt = ((d >= 0) & (d <= 128)).astype(np.float32)
        cnt += ((d >= 0) & (d <= 512) & (d % 4 == 0))
        cnt += ((d >= 0) & (d <= 2048) & (d % 16 == 0))
        dm.append(cnt)
    c["dm"] = _bf(np.stack(dm, axis=1))
    j = np.arange(64)[:, None, None]
    kt = np.arange(32)[None, :, None]
    s = np.arange(128)[None, None, :]
    c["ek"] = _bf(((128 * kt + s) // 64 == j))
    mc = np.zeros((128, 16, 512), np.float32)
    for b in range(8):
        for ct in range(2):
            mc[:, b * 2 + ct, :] = (16 * (128 * ct + sp) + 31 <= 512 * b + tq)
    c["mc"] = _bf(mc)
    c["gc"] = (16 * np.arange(256)[None, :] + 31 - np.arange(128)[:, None]).astype(np.float32)
    c["d0"] = (64 * np.arange(64)[None, :] - np.arange(128)[:, None]).astype(np.float32)
    i = np.arange(128)[:, None]
    t = np.arange(128)[None, :]
    c["tri"] = ((i // 64 == t // 64) & (i <= t)).astype(np.float32)
    i6 = np.arange(64)[:, None]
    t6 = np.arange(64)[None, :]
    c["mstrict"] = np.tile((i6 < t6).astype(np.float32)[:, None, :], (1, 8, 1))
    c["mincl"] = np.tile((i6 <= t6).astype(np.float32)[:, None, :], (1, 8, 1))
    c["mstrictT"] = np.tile((i6 > t6).astype(np.float32)[:, None, :], (1, 8, 1))
    return c


CONST_DT = {"ident_bf": BF16, "cms": BF16, "cmw": BF16, "dm": BF16, "ek": BF16, "mc": BF16}

PARAM_SHAPES = {
    'norm_mix': (DEPTH, D), 'w_in': (DEPTH, D, IN_COLS), 'cmp_pos': (DEPTH, 32, 64),
    'cmp_k_w1': (DEPTH, 2048, 128), 'cmp_k_w2': (DEPTH, 128, 64), 'cmp_v_w1': (DEPTH, 2048, 128),
    'cmp_v_w2': (DEPTH, 128, 64), 'beta_nsa': (DEPTH, 256), 'beta_dil': (DEPTH, 256),
    'rwkv_mu': (DEPTH, 1024), 'rwkv_w0': (DEPTH, 256), 'rwkv_w_up': (DEPTH, 64, 256),
    'rwkv_a0': (DEPTH, 256), 'rwkv_a_up': (DEPTH, 64, 256), 'rwkv_g_up': (DEPTH, 128, 256),
    'rwkv_k_k': (DEPTH, 256), 'rwkv_k_a': (DEPTH, 256), 'rwkv_r_k': (DEPTH, 4, 64),
    'rwkv_ln_g': (DEPTH, 256), 'rwkv_ln_b': (DEPTH, 256), 'conv_dw': (DEPTH, 31, 256),
    'conv_dw_b': (DEPTH, 256), 'conv_ln_g': (DEPTH, 256), 'conv_ln_b': (DEPTH, 256),
    'w_out': (DEPTH, D, D), 'norm_ffn': (DEPTH, D), 'ffn_up': (DEPTH, D, 2 * DFF),
    'ffn_dw': (DEPTH, 3, 2 * DFF), 'ffn_dw_b': (DEPTH, 2 * DFF), 'ffn_down': (DEPTH, DFF, D),
    'norm_final': (D,),
}


class Ctx:
    pass


def bcast_rows(ap_1d_row, nparts):
    return ap_1d_row.partition_broadcast(nparts)


def load_bcast(kb, dst_tile, src_dram_tile, row_ap, n):
    kb.dma(out=dst_tile[:, 0:n], in_=Ref(src_dram_tile, None, row_ap.partition_broadcast(128)))


FM_CHUNKS = [
    (0, 128, "qT", 0), (128, 128, "qT", 128), (256, 128, "kcvcT", 0), (384, 64, "ksT", 0), (512, 64, "kwT", 0),
    (652, 128, "dqT", 0), (780, 128, "dqT", 128), (908, 128, "dkT", 0), (1036, 128, "dkT", 128),
    (2188, 128, "loraT", 0), (2316, 128, "loraT", 128),
    (2444, 128, "convT", 0), (2572, 128, "convT", 128), (2700, 128, "convT", 256), (2828, 128, "convT", 384),
]
TM_GROUPS = [(448, 204, "tmA", 0), (1164, 512, "tmB", 0), (1676, 512, "tmB", 512)]


def load_weight_bf16(kb, dst, dram_w, rows0, nk, cols0, ncols):
    for k in range(nk):
        c = 0
        while c < ncols:
            w = min(1024, ncols - c)
            kb.dma(out=dst[:, k, c:c + w],
                   in_=dram_w[rows0 + k * 128: rows0 + (k + 1) * 128, cols0 + c: cols0 + c + w], via_pool=True)
            c += w


def rms_rstd(kb, ssq_ref, out_ref, n, eps, tmp_ref):
    kb.dve.tensor_scalar(out=tmp_ref, in0=ssq_ref, scalar1=1.0 / n, scalar2=eps, op0=ALU.mult, op1=ALU.add)
    kb.act.activation(out=tmp_ref, in_=tmp_ref, func=AF.Sqrt)
    kb.dve.reciprocal(out=out_ref, in_=tmp_ref)


def phase_a(kb, cx, l):
    P = cx.P
    scr = cx.scr
    with ExitStack() as st:
        w_sb = kb.sb(st, "wA", [128, 8, IN_COLS], BF16)
        wblocks = [(b, b * 512, min(512, IN_COLS - b * 512)) for b in range(6)]
        for (key, c0, cw) in wblocks:
            src = P["w_in"].t[l, :, c0:c0 + cw].rearrange("(k p) c -> p k c", p=128)
            kb.dma(out=w_sb.k(key)[:, :, c0:c0 + cw], in_=Ref(P["w_in"], None, src), via_pool=True)

        def wkey(c0, cw):
            return w_sb.k(c0 // 512) if c0 // 512 == (c0 + cw - 1) // 512 else w_sb
        gbc = kb.sb(st, "gbcA", [128, D], F32)
        kb.dma(out=gbc[:, :], in_=Ref(P["norm_mix"], None, P["norm_mix"].t[l:l + 1, :].partition_broadcast(128)))
        xt = [kb.sb(st, f"xtA{i}", [128, 4, D], F32) for i in range(2)]
        hbf = [kb.sb(st, f"hbfA{i}", [128, D], BF16) for i in range(2)]
        junk = kb.sb(st, "junkA", [128, D], BF16)
        hT = [kb.sb(st, f"hTA{i}", [128, 8, 512], BF16) for i in range(2)]
        small = kb.sb(st, "smallA", [128, 16], F32)
        stg_bf = [kb.sb(st, f"stgbA{i}", [128, 512], BF16) for i in range(3)]
        stg_f = [kb.sb(st, f"stgfA{i}", [128, 512], F32) for i in range(3)]
        tp = [kb.psum(st, f"tpA{i}", [128, 8, 128], BF16) for i in range(2)]
        acc = [kb.psum(st, f"accA{i}", [128, 512], F32) for i in range(4)]
        n_acc = 0
        n_stg = 0
        def load_x(b):
            kb.dma(out=xt[b % 2][:, :, :], in_=cx.xres[b * 512:(b + 1) * 512, :].with_ap(
                cx.xres.t[b * 512:(b + 1) * 512, :].rearrange("(j p) d -> p j d", p=128)))
        load_x(0)
        for b in range(NBLK):
            x_t = xt[b % 2]
            if b + 1 < NBLK:
                load_x(b + 1)
            h_T = hT[b % 2]
            for j in range(4):
                hb = hbf[j % 2]
                ssq = small[:, j:j + 1]
                kb.act.activation(out=junk[:, :], in_=x_t[:, j, :], func=AF.Square, accum_out=ssq)
                rms_rstd(kb, ssq, small[:, 4 + j:5 + j], D, 1e-6, small[:, 8 + j:9 + j])
                kb.dve.scalar_tensor_tensor(out=hb[:, :], in0=x_t[:, j, :], scalar=small[:, 4 + j:5 + j], in1=gbc[:, :],
                                            op0=ALU.mult, op1=ALU.mult)
                t_p = tp[j % 2]
                for kc in range(8):
                    kb.pe.transpose(out=t_p[:, kc, :], in_=hb[:, kc * 128:(kc + 1) * 128], identity=cx.ident_bf[:, :])
                kb.act.copy(out=h_T[:, :, j * 128:(j + 1) * 128], in_=t_p[:, :, :])
            for (c0, cw, dst, r0) in FM_CHUNKS:
                a = acc[n_acc % 4]
                n_acc += 1
                for kc in range(8):
                    kb.pe.matmul(out=a[0:cw, :], lhsT=wkey(c0, cw)[:, kc, c0:c0 + cw], rhs=h_T[:, kc, :], start=(kc == 0), stop=(kc == 7))
                dt_f32 = dst in ("loraT", "convT")
                sg = (stg_f if dt_f32 else stg_bf)[n_stg % 3]
                if n_stg % 2 == 0:
                    kb.dve.tensor_copy(out=sg[0:cw, :], in_=a[0:cw, :])
                else:
                    kb.act.copy(out=sg[0:cw, :], in_=a[0:cw, :])
                n_stg += 1
                kb.dma(out=getattr(scr, dst).k(b)[r0:r0 + cw, b * 512:(b + 1) * 512], in_=sg[0:cw, :])
            for j in range(4):
                for (c0, cw, dst, d0) in TM_GROUPS:
                    a = acc[n_acc % 4]
                    n_acc += 1
                    for kc in range(8):
                        kb.pe.matmul(out=a[:, 0:cw], lhsT=h_T[:, kc, j * 128:(j + 1) * 128], rhs=wkey(c0, cw)[:, kc, c0:c0 + cw],
                                     start=(kc == 0), stop=(kc == 7))
                    sg = stg_f[n_stg % 3]
                    if n_stg % 2 == 0:
                        kb.dve.tensor_copy(out=sg[:, 0:cw], in_=a[:, 0:cw])
                    else:
                        kb.act.copy(out=sg[:, 0:cw], in_=a[:, 0:cw])
                    n_stg += 1
                    r = b * 512 + j * 128
                    kb.dma(out=getattr(scr, dst).k(b)[r:r + 128, d0:d0 + cw], in_=sg[:, 0:cw])
        kb.barrier()


def phase_conv(kb, cx, l):
    P = cx.P
    scr = cx.scr
    with ExitStack() as st:
        glu = [kb.sb(st, f"gluD{i}", [128, 30 + S], F32) for i in range(2)]
        accs = [kb.sb(st, f"accD{i}", [128, S], F32) for i in range(2)]
        bt = kb.sb(st, "btD", [128, S], F32)
        wdw = kb.sb(st, "wdwD", [128, 2, 32], F32)
        lng = kb.sb(st, "lngD", [128, 256], F32)
        lnb = kb.sb(st, "lnbD", [128, 256], F32)
        small = kb.sb(st, "smallD", [128, 16], F32)
        stats = kb.sb(st, "statsD", [128, 8], F32)
        xn = [kb.sb(st, f"xnD{i}", [128, 256], F32) for i in range(2)]
        tps = [kb.psum(st, f"tpD{i}", [128, 512], F32) for i in range(2)]
        kb.dma(out=lng[:, :], in_=Ref(P["conv_ln_g"], None, P["conv_ln_g"].t[l:l + 1, :].partition_broadcast(128)))
        kb.dma(out=lnb[:, :], in_=Ref(P["conv_ln_b"], None, P["conv_ln_b"].t[l:l + 1, :].partition_broadcast(128)))
        for ci in range(2):
            with kb.nc.allow_non_contiguous_dma(reason="tiny transposed conv weights"):
                kb.dma(out=wdw[:, ci, 0:31], in_=Ref(P["conv_dw"], None,
                       P["conv_dw"].t[l, :, ci * 128:(ci + 1) * 128].rearrange("k c -> c k")))
                kb.dma(out=wdw[:, ci, 31:32], in_=Ref(P["conv_dw_b"], None,
                       P["conv_dw_b"].t[l:l + 1, ci * 128:(ci + 1) * 128].rearrange("o c -> c o")))
            g = glu[ci]
            kb.pool.memset(ap=g[:, 0:30], constant=0.0)
            kb.dma(out=g[:, 30:30 + S], in_=scr.convT[ci * 128:(ci + 1) * 128, :])
            kb.dma(out=bt[:, :], in_=scr.convT[256 + ci * 128:256 + (ci + 1) * 128, :])
            kb.act.activation(out=bt[:, :], in_=bt[:, :], func=AF.Sigmoid)
            kb.pool.tensor_tensor(out=g[:, 30:30 + S], in0=g[:, 30:30 + S], in1=bt[:, :], op=ALU.mult)
            a = accs[ci]
            for h0 in range(0, S, 2048):
                kb.dve.tensor_scalar(out=a[:, h0:h0 + 2048], in0=g[:, 30 + h0:30 + h0 + 2048], scalar1=wdw[:, ci, 30:31],
                                     scalar2=wdw[:, ci, 31:32], op0=ALU.mult, op1=ALU.add)
                for j in range(30):
                    kb.dve.scalar_tensor_tensor(out=a[:, h0:h0 + 2048], in0=g[:, j + h0:j + h0 + 2048], scalar=wdw[:, ci, j:j + 1],
                                                in1=a[:, h0:h0 + 2048], op0=ALU.mult, op1=ALU.add)
        for i in range(NT):
            tp = tps[i % 2]
            for ci in range(2):
                kb.pe.transpose(out=tp[:, ci * 128:(ci + 1) * 128], in_=accs[ci][:, i * 128:(i + 1) * 128], identity=cx.ident_f[:, :])
            x_n = xn[i % 2]
            kb.dve.bn_stats(out=stats[:, 0:6], in_=tp[:, 0:256])
            kb.dve.bn_aggr(out=small[:, 0:2], in_=stats[:, 0:6])
            kb.dve.tensor_scalar(out=small[:, 2:3], in0=small[:, 1:2], scalar1=1e-5, scalar2=None, op0=ALU.add)
            kb.act.activation(out=small[:, 2:3], in_=small[:, 2:3], func=AF.Sqrt)
            kb.dve.reciprocal(out=small[:, 3:4], in_=small[:, 2:3])
            kb.dve.tensor_scalar(out=x_n[:, :], in0=tp[:, 0:256], scalar1=small[:, 0:1], scalar2=small[:, 3:4],
                                 op0=ALU.subtract, op1=ALU.mult)
            kb.pool.tensor_tensor(out=x_n[:, :], in0=x_n[:, :], in1=lng[:, :], op=ALU.mult)
            kb.pool.tensor_tensor(out=x_n[:, :], in0=x_n[:, :], in1=lnb[:, :], op=ALU.add)
            kb.act.activation(out=x_n[:, :], in_=x_n[:, :], func=AF.Silu)
            kb.dma(out=scr.ymix.k(("d", i))[i * 128:(i + 1) * 128, 768:1024], in_=x_n[:, :])
        kb.barrier()


def load_w_bf16(kb, dst, wtile, l, nk, ncols):
    for k in range(nk):
        c = 0
        while c < ncols:
            w = min(1024, ncols - c)
            kb.dma(out=dst[:, k, c:c + w], in_=wtile[l, k * 128:(k + 1) * 128, c:c + w], via_pool=True)
            c += w


def load_w_blocks(kb, dst, wtile, l, nk, blocks):
    for (key, c0, cw) in blocks:
        src = wtile.t[l, 0:nk * 128, c0:c0 + cw].rearrange("(k p) c -> p k c", p=128)
        kb.dma(out=dst.k(key)[:, 0:nk, c0:c0 + cw], in_=Ref(wtile, None, src), via_pool=True)


def phase_b(kb, cx, l):
    P = cx.P
    scr = cx.scr
    with ExitStack() as st:
        wo = kb.sb(st, "woB", [128, 8, D], BF16)
        load_w_bf16(kb, wo, P["w_out"], l, 8, D)
        yt = [kb.sb(st, f"ytB{i}", [128, D], F32) for i in range(2)]
        xt = [kb.sb(st, f"xtB{i}", [128, D], F32) for i in range(2)]
        ybf = [kb.sb(st, f"ybfB{i}", [128, D], BF16) for i in range(2)]
        yT = [kb.sb(st, f"yTB{i}", [128, 8, 128], BF16) for i in range(2)]
        tp = [kb.psum(st, f"tpB{i}", [128, 8, 128], BF16) for i in range(2)]
        acc = [kb.psum(st, f"accB{i}", [128, 512], F32) for i in range(4)]
        na = 0
        def load_b(i):
            kb.dma(out=yt[i % 2][:, :], in_=scr.ymix[i * 128:(i + 1) * 128, :])
            kb.dma(out=xt[i % 2][:, :], in_=cx.xres.k(i)[i * 128:(i + 1) * 128, :])
        load_b(0)
        for i in range(NT):
            y_t, x_t, y_b, y_T, t_p = yt[i % 2], xt[i % 2], ybf[i % 2], yT[i % 2], tp[i % 2]
            if i + 1 < NT:
                load_b(i + 1)
            kb.pool.tensor_copy(out=y_b[:, :], in_=y_t[:, :])
            for kc in range(8):
                kb.pe.transpose(out=t_p[:, kc, :], in_=y_b[:, kc * 128:(kc + 1) * 128], identity=cx.ident_bf[:, :])
            kb.act.copy(out=y_T[:, :, :], in_=t_p[:, :, :])
            for c0 in (0, 512):
                a = acc[na % 4]
                na += 1
                for kc in range(8):
                    kb.pe.matmul(out=a[:, :], lhsT=y_T[:, kc, :], rhs=wo[:, kc, c0:c0 + 512], start=(kc == 0), stop=(kc == 7))
                kb.dve.tensor_tensor(out=x_t[:, c0:c0 + 512], in0=x_t[:, c0:c0 + 512], in1=a[:, :], op=ALU.add)
            kb.dma(out=cx.xres.k(i)[i * 128:(i + 1) * 128, :], in_=x_t[:, :])
        kb.barrier()


def phase_c(kb, cx, l):
    P = cx.P
    TB = 256
    with ExitStack() as st:
        wu = kb.sb(st, "wuC", [128, 8, 2 * DFF], BF16)
        wd = kb.sb(st, "wdC", [128, 22, D], BF16)
        order = []
        for i in range(6):
            order += [i, i + 5] if i + 5 < 11 else [i]
        order = [b for b in dict.fromkeys(order) if b < 11]
        load_w_blocks(kb, wu, P["ffn_up"], l, 8, [(b, b * 512, 512) for b in order])
        for k in range(22):
            kb.dma(out=wd.k(k)[:, k, :], in_=P["ffn_down"][l, k * 128:(k + 1) * 128, :], via_pool=True)
        gbc = kb.sb(st, "gbcC", [128, D], F32)
        kb.dma(out=gbc[:, :], in_=Ref(P["norm_ffn"], None, P["norm_ffn"].t[l:l + 1, :].partition_broadcast(128)))
        cw = kb.sb(st, "cwC", [128, 44, 4], F32)
        with kb.nc.allow_non_contiguous_dma(reason="tiny transposed conv weights"):
            for j in range(3):
                kb.dma(out=cw[:, :, j:j + 1], in_=Ref(P["ffn_dw"], None,
                       P["ffn_dw"].t[l, j:j + 1, :].rearrange("o (c p) -> p c o", p=128)))
            kb.dma(out=cw[:, :, 3:4], in_=Ref(P["ffn_dw_b"], None,
                   P["ffn_dw_b"].t[l:l + 1, :].rearrange("o (c p) -> p c o", p=128)))
        carry = kb.sb(st, "carryC", [128, 44, 2], F32)
        kb.pool.memset(ap=carry[:, :, :], constant=0.0)
        xt = [kb.sb(st, f"xtC{i}", [128, 2, D], F32) for i in range(2)]
        hbf = [kb.sb(st, f"hbfC{i}", [128, D], BF16) for i in range(2)]
        junk = kb.sb(st, "junkC", [128, D], BF16)
        hT = [kb.sb(st, f"hTC{i}", [128, 8, TB], BF16) for i in range(2)]
        small = kb.sb(st, "smallC", [128, 16], F32)
        G = [kb.sb(st, f"GC{i}", [128, 22, TB], BF16) for i in range(2)]
        ub = [kb.sb(st, f"ubC{i}", [128, TB + 2], F32) for i in range(4)]
        ac = [kb.sb(st, f"acC{i}", [128, TB], F32) for i in range(4)]
        tp = [kb.psum(st, f"tpC{i}", [128, 8, 128], BF16) for i in range(2)]
        ups = [kb.psum(st, f"upC{i}", [128, 512], F32) for i in range(4)]
        dps = [kb.psum(st, f"dpC{i}", [128, 512], F32) for i in range(2)]
        nu = 0
        nd = 0
        def load_c(b):
            kb.dma(out=xt[b % 2][:, :, :], in_=cx.xres.k(b)[b * TB:(b + 1) * TB, :].with_ap(
                cx.xres.t[b * TB:(b + 1) * TB, :].rearrange("(j p) d -> p j d", p=128)))
        load_c(0)
        for b in range(S // TB):
            x_t, h_T, Gb = xt[b % 2], hT[b % 2], G[b % 2]
            if b + 1 < S // TB:
                load_c(b + 1)
            for j in range(2):
                hb = hbf[j]
                kb.act.activation(out=junk[:, :], in_=x_t[:, j, :], func=AF.Square, accum_out=small[:, j:j + 1])
                rms_rstd(kb, small[:, j:j + 1], small[:, 4 + j:5 + j], D, 1e-6, small[:, 8 + j:9 + j])
                kb.dve.scalar_tensor_tensor(out=hb[:, :], in0=x_t[:, j, :], scalar=small[:, 4 + j:5 + j], in1=gbc[:, :],
                                            op0=ALU.mult, op1=ALU.mult)
                t_p = tp[j]
                for kc in range(8):
                    kb.pe.transpose(out=t_p[:, kc, :], in_=hb[:, kc * 128:(kc + 1) * 128], identity=cx.ident_bf[:, :])
                kb.act.copy(out=h_T[:, :, j * 128:(j + 1) * 128], in_=t_p[:, :, :])
            for ci in range(22):
                res = []
                for half in range(2):
                    c = ci + 22 * half
                    u = ups[nu % 4]
                    u_b = ub[nu % 4]
                    a = ac[nu % 4]
                    nu += 1
                    for kc in range(8):
                        kb.pe.matmul(out=u[:, 0:TB], lhsT=wu.k(c // 4)[:, kc, c * 128:(c + 1) * 128], rhs=h_T[:, kc, :],
                                     start=(kc == 0), stop=(kc == 7))
                    kb.act.copy(out=u_b[:, 2:TB + 2], in_=u[:, 0:TB])
                    kb.pool.tensor_copy(out=u_b[:, 0:2], in_=carry[:, c, :])
                    kb.dve.tensor_scalar(out=a[:, :], in0=u[:, 0:TB], scalar1=cw[:, c, 2:3], scalar2=cw[:, c, 3:4],
                                         op0=ALU.mult, op1=ALU.add)
                    kb.dve.scalar_tensor_tensor(out=a[:, :], in0=u_b[:, 1:TB + 1], scalar=cw[:, c, 1:2], in1=a[:, :],
                                                op0=ALU.mult, op1=ALU.add)
                    kb.dve.scalar_tensor_tensor(out=a[:, :], in0=u_b[:, 0:TB], scalar=cw[:, c, 0:1], in1=a[:, :],
                                                op0=ALU.mult, op1=ALU.add)
                    kb.pool.tensor_copy(out=carry[:, c, :], in_=u_b[:, TB:TB + 2])
                    res.append(a)
                kb.act.activation(out=res[0][:, :], in_=res[0][:, :], func=AF.Silu)
                kb.pool.tensor_tensor(out=Gb[:, ci, :], in0=res[0][:, :], in1=res[1][:, :], op=ALU.mult)
            for j in range(2):
                for c0 in (0, 512):
                    d = dps[nd % 2]
                    nd += 1
                    for ci in range(22):
                        kb.pe.matmul(out=d[:, :], lhsT=Gb[:, ci, j * 128:(j + 1) * 128], rhs=wd.k(ci)[:, ci, c0:c0 + 512],
                                     start=(ci == 0), stop=(ci == 21))
                    kb.dve.tensor_tensor(out=x_t[:, j, c0:c0 + 512], in0=x_t[:, j, c0:c0 + 512], in1=d[:, :], op=ALU.add)
            kb.dma(out=cx.xres.k(b)[b * TB:(b + 1) * TB, :].with_ap(
                cx.xres.t[b * TB:(b + 1) * TB, :].rearrange("(j p) d -> p j d", p=128)), in_=x_t[:, :, :])
        kb.barrier()


def phase_final(kb, cx, y_out):
    P = cx.P
    with ExitStack() as st:
        gbc = kb.sb(st, "gbcF", [128, D], F32)
        kb.dma(out=gbc[:, :], in_=Ref(P["norm_final"], None, P["norm_final"].t.rearrange("(o d) -> o d", o=1).partition_broadcast(128)))
        xt = [kb.sb(st, f"xtF{i}", [128, D], F32) for i in range(2)]
        ot = [kb.sb(st, f"otF{i}", [128, D], F32) for i in range(2)]
        junk = kb.sb(st, "junkF", [128, D], BF16)
        small = kb.sb(st, "smallF", [128, 16], F32)
        kb.dma(out=xt[0][:, :], in_=cx.xres[0:128, :])
        for i in range(NT):
            x_t, o_t = xt[i % 2], ot[i % 2]
            if i + 1 < NT:
                kb.dma(out=xt[(i + 1) % 2][:, :], in_=cx.xres[(i + 1) * 128:(i + 2) * 128, :])
            kb.act.activation(out=junk[:, :], in_=x_t[:, :], func=AF.Square, accum_out=small[:, 0:1])
            rms_rstd(kb, small[:, 0:1], small[:, 1:2], D, 1e-6, small[:, 2:3])
            kb.dve.scalar_tensor_tensor(out=o_t[:, :], in0=x_t[:, :], scalar=small[:, 1:2], in1=gbc[:, :],
                                        op0=ALU.mult, op1=ALU.mult)
            kb.dma(out=y_out[i * 128:(i + 1) * 128, :], in_=o_t[:, :])
        kb.barrier()


class AttnBufs:
    def __init__(self, kb, st, tag):
        self.sps = [kb.psum(st, f"sps{tag}{i}", [128, 512], F32) for i in range(2)]
        self.acc = kb.psum(st, f"acc{tag}", [128, 4, 512], F32)
        self.pts = [kb.sb(st, f"pts{tag}{i}", [128, 512], BF16) for i in range(3)]
        self.ns = 0
        self.nm = 0


def attn_core(kb, A, q_rhs, kv_list, heads_view=False):
    n = len(kv_list)
    for idx, (kT, Vp, mask) in enumerate(kv_list):
        sp = A.sps[A.ns % 2]
        pt = A.pts[A.ns % 3]
        A.ns += 1
        if heads_view:
            spv = sp[:, :].with_ap(sp.t[:, :].rearrange("p (h q) -> p h q", h=4))
            ptv = pt[:, :].with_ap(pt.t[:, :].rearrange("p (h q) -> p h q", h=4))
        else:
            spv, ptv = sp[:, :], pt[:, :]
        kb.pe.matmul(out=sp[:, :], lhsT=kT, rhs=q_rhs, start=True, stop=True)
        kb.act.activation(out=pt[:, :], in_=sp[:, :], func=AF.Exp, scale=0.125)
        if mask is not None:
            eng = kb.dve if (A.nm % 3 != 2) else kb.pool
            A.nm += 1
            eng.tensor_tensor(out=ptv, in0=ptv, in1=mask, op=ALU.mult)
        for j in range(4):
            kb.pe.matmul(out=A.acc[:, j, 0:65], lhsT=pt[:, j * 128:(j + 1) * 128], rhs=Vp, start=(idx == 0), stop=(idx == n - 1))


def attn_evac(kb, A, small, dst, gate=None, first=True, tmp=None):
    kb.dve.tensor_scalar(out=small[:, 0:4], in0=A.acc[:, :, 64], scalar1=1e-30, scalar2=None, op0=ALU.max)
    kb.dve.reciprocal(out=small[:, 4:8], in_=small[:, 0:4])
    if gate is not None:
        kb.dve.tensor_tensor(out=small[:, 4:8], in0=small[:, 4:8], in1=gate, op=ALU.mult)
    sc = small[:, 4:8].with_ap(small.t[:, 4:8].unsqueeze(2).to_broadcast([128, 4, 64]))
    if first:
        kb.dve.tensor_tensor(out=dst, in0=A.acc[:, :, 0:64], in1=sc, op=ALU.mult)
    else:
        kb.dve.tensor_tensor(out=tmp, in0=A.acc[:, :, 0:64], in1=sc, op=ALU.mult)
        kb.pool.tensor_tensor(out=dst, in0=dst, in1=tmp, op=ALU.add)


def group_rmsnorm_store(kb, ob_ref, beta_bc, small, junk, stage_ref, dram_ref):
    kb.act.activation(out=junk, in_=ob_ref, func=AF.Square, accum_out=small[:, 8:9])
    rms_rstd(kb, small[:, 8:9], small[:, 9:10], 256, 1e-6, small[:, 10:11])
    kb.dve.scalar_tensor_tensor(out=stage_ref, in0=ob_ref, scalar=small[:, 9:10], in1=beta_bc, op0=ALU.mult, op1=ALU.mult)
    kb.dma(out=dram_ref, in_=stage_ref)


def build_vprime(kb, vp, src_dram_cols, ld, nh):
    kb.dma(out=ld[:, :, 0:nh * 64], in_=src_dram_cols.with_ap(src_dram_cols.ap.rearrange("(i p) c -> p i c", p=128)))
    kb.pool.memset(ap=vp[:, :, :, 64:65], constant=1.0)
    for h in range(nh):
        kb.dve.tensor_copy(out=vp[:, :, h, 0:64], in_=ld[:, :, h * 64:(h + 1) * 64])


def phase_dil(kb, cx, l):
    P, scr, C = cx.P, cx.scr, cx.C
    with ExitStack() as st:
        qT = kb.sb(st, "qTd", [64, 4, S], BF16)
        kT = kb.sb(st, "kTd", [64, 4, S], BF16)
        kb.dma(out=qT[:, :, :], in_=scr.dqT[:, :].with_ap(scr.dqT.t.rearrange("(h d) s -> d h s", d=64)))
        kb.dma(out=kT[:, :, :], in_=scr.dkT[:, :].with_ap(scr.dkT.t.rearrange("(h d) s -> d h s", d=64)))
        vp = kb.sb(st, "vpd", [128, NT, 4, 65], BF16)
        with ExitStack() as st2:
            ld = kb.sb(st2, "ldd", [128, NT, 256], F32)
            build_vprime(kb, vp, scr.tmB[:, 0:256], ld, 4)
            kb.barrier()
        dm = kb.sb(st, "dmd", [128, 20, 512], BF16)
        kb.dma(out=dm[:, :, :], in_=C["dm"][:, :, :])
        beta = kb.sb(st, "betad", [128, 256], F32)
        kb.dma(out=beta[:, :], in_=Ref(P["beta_dil"], None, P["beta_dil"].t[l:l + 1, :].partition_broadcast(128)))
        ob = [kb.sb(st, f"obd{i}", [128, 4, 256], F32) for i in range(2)]
        stage = [kb.sb(st, f"stgd{i}", [128, 256], F32) for i in range(2)]
        junk = kb.sb(st, "junkd", [128, 256], BF16)
        small = kb.sb(st, "smalld", [128, 16], F32)
        A = AttnBufs(kb, st, "d")
        for b in range(NBLK):
            o_b = ob[b % 2]
            for h in range(4):
                kv = []
                for kt in range(max(0, 4 * b - 16), 4 * b + 4):
                    delta = 4 * b - kt
                    kv.append((kT[:, h, kt * 128:(kt + 1) * 128], vp[:, kt, h, :], dm[:, delta + 3, :]))
                attn_core(kb, A, qT[:, h, b * 512:(b + 1) * 512], kv)
                attn_evac(kb, A, small, o_b[:, :, h * 64:(h + 1) * 64])
            for qt in range(4):
                i = b * 4 + qt
                group_rmsnorm_store(kb, o_b[:, qt, :], beta[:, :], small, junk[:, :], stage[qt % 2][:, :],
                                    scr.ymix.k(("b", i))[i * 128:(i + 1) * 128, 256:512])
        kb.barrier()
def phase_nsa(kb, cx, l):
    P, scr, C = cx.P, cx.scr, cx.C
    with ExitStack() as st:
        qT = kb.sb(st, "qTn", [64, 4, S], BF16)
        kb.dma(out=qT[:, :, :], in_=scr.qT[:, :].with_ap(scr.qT.t.rearrange("(h d) s -> d h s", d=64)))
        ksT = kb.sb(st, "ksTn", [64, S], BF16)
        kwT = kb.sb(st, "kwTn", [64, S], BF16)
        kb.dma(out=ksT[:, :], in_=scr.ksT[:, :])
        kb.dma(out=kwT[:, :], in_=scr.kwT[:, :])
        vps = kb.sb(st, "vpsn", [128, NT, 1, 65], BF16)
        vpw = kb.sb(st, "vpwn", [128, NT, 1, 65], BF16)
        gts = kb.sb(st, "gtsn", [128, NT, 12], F32)
        kcmpT = kb.sb(st, "kcmpTn", [64, 256], BF16)
        vpc = kb.sb(st, "vpcn", [128, 2, 65], BF16)
        selT = kb.sb(st, "selTn", [64, S], BF16)
        small = kb.sb(st, "smalln", [128, 16], F32)
        A = AttnBufs(kb, st, "n")
        x1 = kb.psum(st, "x1n", [128, 512], F32)
        x2 = kb.psum(st, "x2n", [128, 512], F32)
        with ExitStack() as st2:
            ld = kb.sb(st2, "ldn", [128, NT, 204], F32)
            kb.dma(out=ld[:, :, :], in_=scr.tmA[:, :].with_ap(scr.tmA.t.rearrange("(i p) c -> p i c", p=128)))
            kb.pool.memset(ap=vps[:, :, :, 64:65], constant=1.0)
            kb.pool.memset(ap=vpw[:, :, :, 64:65], constant=1.0)
            kb.dve.tensor_copy(out=vps[:, :, 0, 0:64], in_=ld[:, :, 0:64])
            kb.dve.tensor_copy(out=vpw[:, :, 0, 0:64], in_=ld[:, :, 128:192])
            kb.act.activation(out=gts[:, :, :], in_=ld[:, :, 192:204], func=AF.Sigmoid)
            x2t = kb.sb(st2, "x2n_", [128, S], BF16)
            pos2 = kb.sb(st2, "pos2n", [128, 16], F32)
            w1 = kb.sb(st2, "w1n", [128, 16, 128], BF16)
            w2 = kb.sb(st2, "w2n", [128, 64], BF16)
            am = kb.sb(st2, "amn", [128, 16, 256], BF16)
            hidT = kb.sb(st2, "hidTn", [128, 256], BF16)
            with kb.nc.allow_non_contiguous_dma(reason="tiny pos table"):
                kb.dma(out=pos2[:, :], in_=Ref(P["cmp_pos"], None, P["cmp_pos"].t[l].rearrange("(m i) d -> (i d) m", i=2)))
            for kind in ("k", "v"):
                r0 = 0 if kind == "k" else 64
                kb.pool.memset(ap=x2t[:, S - 1:S], constant=0.0)
                kb.dma(out=x2t[0:64, :], in_=scr.kcvcT[r0:r0 + 64, :])
                kb.dma(out=x2t[64:128, 0:S - 1], in_=scr.kcvcT[r0:r0 + 64, 1:S])
                wn1 = P["cmp_k_w1" if kind == "k" else "cmp_v_w1"]
                wn2 = P["cmp_k_w2" if kind == "k" else "cmp_v_w2"]
                kb.dma(out=w1[:, :, :], in_=wn1[l].with_ap(wn1.t[l].rearrange("(m p) h -> p m h", p=128)), via_pool=True)
                kb.dma(out=w2[:, :], in_=wn2[l, :, :], via_pool=True)
                kb.pool.memset(ap=am[:, :, 255:256], constant=0.0)
                xv = x2t.t[:, :].rearrange("p (c r) -> p c r", r=16)
                for m in range(16):
                    src = xv[:, 0:255, 2 * m] if m < 8 else xv[:, 1:256, 2 * m - 16]
                    kb.dve.tensor_scalar(out=am[:, m, 0:255], in0=Ref(x2t, None, src), scalar1=pos2[:, m:m + 1], scalar2=None, op0=ALU.add)
                for m in range(16):
                    kb.pe.matmul(out=x1[:, 0:256], lhsT=w1[:, m, :], rhs=am[:, m, :], start=(m == 0), stop=(m == 15))
                kb.act.activation(out=hidT[:, :], in_=x1[:, 0:256], func=AF.Silu)
                if kind == "k":
                    kb.pe.matmul(out=x2[0:64, 0:256], lhsT=w2[:, :], rhs=hidT[:, :], start=True, stop=True)
                    kb.dve.tensor_copy(out=kcmpT[:, :], in_=x2[0:64, 0:256])
                else:
                    kb.pool.memset(ap=vpc[:, :, 64:65], constant=1.0)
                    for ct in range(2):
                        kb.pe.matmul(out=x2[:, ct * 64:(ct + 1) * 64], lhsT=hidT[:, ct * 128:(ct + 1) * 128], rhs=w2[:, :], start=True, stop=True)
                    kb.dve.tensor_copy(out=vpc[:, :, 0:64], in_=x2[:, 0:128].with_ap(x2.t[:, 0:128].rearrange("p (c d) -> p c d", c=2)))
            kb.barrier()
        with ExitStack() as st3:
            gc = kb.sb(st3, "gcn", [128, 256], F32)
            d0 = kb.sb(st3, "d0n", [128, 64], F32)
            kb.dma(out=gc[:, :], in_=C["gc"][:, :])
            kb.dma(out=d0[:, :], in_=C["d0"][:, :])
            pex = [kb.sb(st3, f"pexn{i}", [128, 4, 256], F32) for i in range(2)]
            imp = [kb.sb(st3, f"impn{i}", [128, 258], F32) for i in range(2)]
            chk = kb.sb(st3, "chkn", [128, 256], F32)
            blk = kb.sb(st3, "blkn", [128, 64], F32)
            sc = kb.sb(st3, "scn", [128, 64], F32)
            sc2 = kb.sb(st3, "sc2n", [128, 64], F32)
            vm = kb.sb(st3, "vmn", [128, 64], F32)
            m8 = kb.sb(st3, "m8n", [128, 16], F32)
            sel = [kb.sb(st3, f"seln{i}", [128, 64], BF16) for i in range(2)]
            for i in range(2):
                kb.pool.memset(ap=imp[i][:, :], constant=0.0)
            scps = [A.acc.k(0), A.acc.k(1)]
            for i in range(NT):
                pe_, im = pex[i % 2], imp[i % 2]
                base = (i % 2) * 2
                scv = A.acc.k(i % 2)[:, base:base + 2, :].with_ap(
                    A.acc.t[:, base:base + 2, :].rearrange("p b (h c) -> p (b h) c", h=2))
                for h in range(4):
                    kb.pe.matmul(out=A.acc.k(i % 2)[:, base + h // 2, (h % 2) * 256:(h % 2) * 256 + 256],
                                 lhsT=qT[:, h, i * 128:(i + 1) * 128], rhs=kcmpT[:, :], start=True, stop=True)
                kb.act.activation(out=pe_[:, :, :], in_=scv, func=AF.Exp, scale=0.125)
                gcb = Ref(gc, None, gc.t[:, :].unsqueeze(1).to_broadcast([128, 4, 256]))
                kb.dve.scalar_tensor_tensor(out=pe_[:, :, :], in0=gcb, scalar=float(128 * i), in1=pe_[:, :, :],
                                            op0=ALU.is_le, op1=ALU.mult)
                kb.dve.tensor_reduce(out=small[:, 0:4], in_=pe_[:, :, :], axis=AX.X, op=ALU.add)
                kb.dve.tensor_scalar(out=small[:, 0:4], in0=small[:, 0:4], scalar1=1e-30, scalar2=None, op0=ALU.max)
                kb.dve.reciprocal(out=small[:, 4:8], in_=small[:, 0:4])
                kb.dve.tensor_scalar(out=im[:, 1:257], in0=pe_[:, 0, :], scalar1=small[:, 4:5], scalar2=None, op0=ALU.mult)
                for h in range(1, 4):
                    kb.dve.scalar_tensor_tensor(out=im[:, 1:257], in0=pe_[:, h, :], scalar=small[:, 4 + h:5 + h], in1=im[:, 1:257],
                                                op0=ALU.mult, op1=ALU.add)
                kb.dve.tensor_tensor(out=chk[:, :], in0=im[:, 0:256], in1=im[:, 1:257], op=ALU.add)
                kb.dve.tensor_reduce(out=blk[:, :], in_=chk[:, :].with_ap(chk.t[:, :].rearrange("p (b r) -> p b r", r=4)),
                                     axis=AX.X, op=ALU.add)
                kb.dve.tensor_scalar(out=sc[:, :], in0=d0[:, :], scalar1=float(128 * i - 127), scalar2=1e9, op0=ALU.is_ge, op1=ALU.mult)
                kb.dve.tensor_tensor(out=sc[:, :], in0=sc[:, :], in1=blk[:, :], op=ALU.max)
                kb.dve.memset(ap=sc[:, 0:1], constant=1e9)
                kb.dve.tensor_single_scalar(out=vm[:, :], in_=d0[:, :], scalar=float(128 * i), op=ALU.is_le)
                kb.dve.tensor_tensor(out=sc[:, :], in0=sc[:, :], in1=vm[:, :], op=ALU.mult)
                kb.dve.scalar_tensor_tensor(out=sc[:, :], in0=vm[:, :], scalar=-1.0, in1=sc[:, :], op0=ALU.add, op1=ALU.add)
                kb.dve.max(out=m8[:, 0:8], in_=sc[:, :])
                kb.dve.match_replace(out=sc2[:, :], in_to_replace=m8[:, 0:8], in_values=sc[:, :], imm_value=-3e38)
                kb.dve.max(out=m8[:, 8:16], in_=sc2[:, :])
                kb.dve.tensor_scalar(out=sel[i % 2][:, :], in0=sc[:, :], scalar1=m8[:, 15:16], scalar2=None, op0=ALU.is_ge)
                tpv = x1[0:64, 0:64].with_ap(x1.t[0:64, 0:64].bitcast(BF16))
                kb.pe.transpose(out=tpv, in_=sel[i % 2][:, :], identity=cx.ident_bf[:, :])
                kb.act.copy(out=selT[:, i * 128:(i + 1) * 128], in_=tpv)
            kb.barrier()
        with ExitStack() as st4:
            maskS = kb.sb(st4, "maskSn", [128, NT, 512], BF16)
            mc = kb.sb(st4, "mcn", [128, 16, 512], BF16)
            cms = kb.sb(st4, "cmsn", [128, 4, 512], BF16)
            cmw = kb.sb(st4, "cmwn", [128, 2, 128], BF16)
            ek = kb.sb(st4, "ekn", [64, 32, 128], BF16)
            kb.dma(out=mc[:, :, :], in_=C["mc"][:, :, :])
            kb.dma(out=cms[:, :, :], in_=C["cms"][:, :, :])
            kb.dma(out=cmw[:, :, :], in_=C["cmw"][:, :, :])
            kb.dma(out=ek[:, :, :], in_=C["ek"][:, :, :])
            beta = kb.sb(st4, "betan", [128, 256], F32)
            kb.dma(out=beta[:, :], in_=Ref(P["beta_nsa"], None, P["beta_nsa"].t[l:l + 1, :].partition_broadcast(128)))
            ob = [kb.sb(st4, f"obn{i}", [128, 4, 256], F32) for i in range(2)]
            tmp = kb.sb(st4, "tmpn", [128, 4, 64], F32)
            stage = [kb.sb(st4, f"stgn{i}", [128, 256], F32) for i in range(2)]
            junk = kb.sb(st4, "junkn", [128, 256], BF16)
            xs = [x1, x2]
            nx = 0
            for b in range(NBLK):
                o_b = ob[b % 2]
                for kt in range(4 * b + 4):
                    xp = xs[nx % 2]
                    nx += 1
                    kb.pe.matmul(out=xp[:, :], lhsT=ek[:, kt, :], rhs=selT[:, b * 512:(b + 1) * 512], start=True, stop=True)
                    if kt >= 4 * b:
                        kb.dve.tensor_tensor(out=maskS[:, kt, :], in0=xp[:, :], in1=cms[:, kt - 4 * b, :], op=ALU.mult)
                    else:
                        kb.act.copy(out=maskS[:, kt, :], in_=xp[:, :])
                for h in range(4):
                    dst = o_b[:, :, h * 64:(h + 1) * 64]
                    kv = [(kcmpT[:, 0:128], vpc[:, 0, :], None if b >= 5 else mc[:, 2 * b, :])]
                    if b >= 4:
                        kv.append((kcmpT[:, 128:256], vpc[:, 1, :], mc[:, 2 * b + 1, :]))
                    attn_core(kb, A, qT[:, h, b * 512:(b + 1) * 512], kv)
                    attn_evac(kb, A, small, dst, gate=gts[:, 4 * b:4 * b + 4, 3 * h], first=True)
                    kv = [(ksT[:, kt * 128:(kt + 1) * 128], vps[:, kt, 0, :], maskS[:, kt, :]) for kt in range(4 * b + 4)]
                    attn_core(kb, A, qT[:, h, b * 512:(b + 1) * 512], kv)
                    attn_evac(kb, A, small, dst, gate=gts[:, 4 * b:4 * b + 4, 3 * h + 1], first=False, tmp=tmp[:, :, :])
                for qt in range(4):
                    i = 4 * b + qt
                    kv = []
                    for kt in range(max(0, i - 4), i + 1):
                        mk = None
                        if kt == i:
                            mk = Ref(cmw, None, cmw.t[:, 0:1, :].to_broadcast([128, 4, 128]))
                        elif kt == i - 4:
                            mk = Ref(cmw, None, cmw.t[:, 1:2, :].to_broadcast([128, 4, 128]))
                        kv.append((kwT[:, kt * 128:(kt + 1) * 128], vpw[:, kt, 0, :], mk))
                    attn_core(kb, A, qT[:, :, i * 128:(i + 1) * 128], kv, heads_view=True)
                    dstw = o_b[:, qt, :].with_ap(o_b.t[:, qt, :].rearrange("p (h d) -> p h d", h=4))
                    gw = gts[:, i, :].with_ap(gts.t[:, i, :].rearrange("p (h r) -> p h r", r=3)[:, :, 2])
                    attn_evac(kb, A, small, dstw, gate=gw, first=False, tmp=tmp[:, :, :])
                for qt in range(4):
                    i = b * 4 + qt
                    group_rmsnorm_store(kb, o_b[:, qt, :], beta[:, :], small, junk[:, :], stage[qt % 2][:, :],
                                        scr.ymix.k(("a", i))[i * 128:(i + 1) * 128, 0:256])
            kb.barrier()
C0 = 0.6065306597126334
RWKV_STAGE = [99]


def phase_rwkv(kb, cx, l):
    P, scr, C = cx.P, cx.scr, cx.C
    CH = 64
    NBUF = 3
    with ExitStack() as st:
        def bc(name, src, c0, n, rows=64):
            t = kb.sb(st, name, [rows, n], F32)
            kb.dma(out=t[:, :], in_=Ref(P[src], None, P[src].t[l:l + 1, c0:c0 + n].partition_broadcast(rows)))
            return t
        mu_bc = bc("mu_r", "rwkv_mu", 0, 768)
        w0_bc = bc("w0_r", "rwkv_w0", 0, 256)
        a0_bc = bc("a0_r", "rwkv_a0", 0, 256)
        kk_bc = bc("kk_r", "rwkv_k_k", 0, 256)
        ka_bc = bc("ka_r", "rwkv_k_a", 0, 256)
        lg_bc = bc("lg_r", "rwkv_ln_g", 0, 256)
        lb_bc = bc("lb_r", "rwkv_ln_b", 0, 256)
        rk_bc = kb.sb(st, "rk_r", [64, 256], F32)
        kb.dma(out=rk_bc[:, :], in_=Ref(P["rwkv_r_k"], None,
               P["rwkv_r_k"].t[l:l + 1].rearrange("o h d -> o (h d)").partition_broadcast(64)))
        oka = kb.sb(st, "oka_r", [64, 256], F32)
        kb.dve.tensor_scalar(out=oka[:, :], in0=ka_bc[:, :], scalar1=-1.0, scalar2=1.0, op0=ALU.mult, op1=ALU.add)
        mu_lo = kb.sb(st, "mulo_r", [128, 2], F32)
        with kb.nc.allow_non_contiguous_dma(reason="tiny mu columns"):
            kb.dma(out=mu_lo[:, 0:1], in_=Ref(P["rwkv_mu"], None, P["rwkv_mu"].t[l:l + 1, 768:896].rearrange("o c -> c o")))
            kb.dma(out=mu_lo[:, 1:2], in_=Ref(P["rwkv_mu"], None, P["rwkv_mu"].t[l:l + 1, 896:1024].rearrange("o c -> c o")))
        wa_up = kb.sb(st, "waup_r", [64, 256], F32)
        a_up = kb.sb(st, "aup_r", [64, 256], F32)
        g_up = kb.sb(st, "gup_r", [128, 256], F32)
        mu_a = kb.sb(st, "mua_r", [64, 1], F32)
        with kb.nc.allow_non_contiguous_dma(reason="tiny mu columns"):
            kb.dma(out=mu_a[:, 0:1], in_=Ref(P["rwkv_mu"], None, P["rwkv_mu"].t[l:l + 1, 832:896].rearrange("o c -> c o")))
        kb.dma(out=wa_up[0:64, :], in_=P["rwkv_w_up"][l, :, :])
        kb.dma(out=a_up[0:64, :], in_=P["rwkv_a_up"][l, :, :])
        kb.dma(out=g_up[:, :], in_=P["rwkv_g_up"][l, :, :])
        tri = kb.sb(st, "tri_r", [64, 2, 64], F32)
        kb.dma(out=tri[:, 0, :], in_=C["tri"][0:64, 0:64])
        kb.dma(out=tri[:, 1, :], in_=C["mstrictT"][:, 0, :])
        mst = kb.sb(st, "mst_r", [64, 4, 64], F32)
        mstT = kb.sb(st, "mstT_r", [64, 4, 64], F32)
        minc = kb.sb(st, "minc_r", [64, 4, 64], F32)
        kb.dma(out=mst[:, :, :], in_=C["mstrict"][:, 0:4, :])
        kb.dma(out=mstT[:, :, :], in_=C["mstrictT"][:, 0:4, :])
        kb.dma(out=minc[:, :, :], in_=C["mincl"][:, 0:4, :])
        ST = kb.sb(st, "ST_r", [64, 4, 64], F32)
        kb.pool.memset(ap=ST[:, :, :], constant=0.0)
        idb = Ref(cx.ident_f, None, cx.ident_f.t[0:64, 0:64].unsqueeze(1).to_broadcast([64, 4, 64]))

        def T2(name, shape, n=3):
            return [kb.sb(st, f"{name}{i}_r", shape, F32) for i in range(n)]
        rkv, prv, lo, gd = T2("rkv", [64, 768]), T2("prv", [64, 768]), T2("lo", [64, 65]), T2("gd", [128, 65])
        los, gds = T2("los", [64, 64]), T2("gds", [128, 64])
        loa, loas = T2("loa", [64, 65]), T2("loas", [64, 64])
        sgt, a_t, g_t = T2("sgt", [64, 256]), T2("at", [64, 256]), T2("gt", [64, 256])
        kk, k2, bb = T2("kkt", [64, 256]), T2("k2t", [64, 256]), T2("bbt", [64, 256])
        tmp, tmp2 = T2("tmp", [64, 256]), T2("tmp2", [64, 256])
        Pm, iP, Pp, Pr = T2("Pm", [64, 256]), T2("iP", [64, 256]), T2("Pp", [64, 256]), T2("Pr", [64, 256])
        Kt, Bt, KKt, Rt, Kh, Bh = (T2("Kt", [64, 256]), T2("Bt", [64, 256]), T2("KKt", [64, 256]), T2("Rt", [64, 256]),
                                    T2("Kh", [64, 256]), T2("Bh", [64, 256]))
        FMq = T2("FMq", [64, 16, 64])
        Mak, Abr, Akr = (T2("Mak", [64, 4, 64]), T2("Abr", [64, 4, 64]), T2("Akr", [64, 4, 64]))
        NT_ = [kb.sb(st, f"NTb{i}_r", [64, 4, 64], BF16) for i in range(NBUF)]
        TA_ = [kb.sb(st, f"TAb{i}_r", [64, 8, 64], BF16) for i in range(NBUF)]
        Tf = T2("Tf", [64, 4, 64])
        Wsb, Xsb, U0T, UT = T2("Wsb", [64, 4, 64]), T2("Xsb", [64, 4, 64]), T2("U0T", [64, 4, 64]), T2("UT", [64, 4, 64])
        pcs = T2("pcs", [64, 4])
        yv = T2("yv", [64, 256])
        small = T2("small", [64, 16])
        B = [kb.psum(st, f"B{i}_r", [64, 512], F32) for i in range(8)]

        def v3(ref_tile, c0):
            return ref_tile[:, c0:c0 + 256].with_ap(ref_tile.t[:, c0:c0 + 256].rearrange("p (h d) -> p h d", h=4))

        def hb(t_small, c0):
            return t_small[:, c0:c0 + 4].with_ap(t_small.t[:, c0:c0 + 4].unsqueeze(2).to_broadcast([64, 4, 64]))

        def chunk(c):
            p = c % NBUF
            t0 = c * CH
            kb.dma(out=rkv[p][:, :], in_=scr.tmB[t0:t0 + CH, 256:1024])
            if c == 0:
                kb.pool.memset(ap=prv[p][0:1, :], constant=0.0)
                kb.dma(out=prv[p][1:CH, :], in_=scr.tmB[0:CH - 1, 256:1024])
                kb.pool.memset(ap=lo[p][:, 0:1], constant=0.0)
                kb.pool.memset(ap=gd[p][:, 0:1], constant=0.0)
                kb.dma(out=lo[p][:, 1:65], in_=scr.loraT[0:64, 0:CH])
                kb.pool.memset(ap=loa[p][:, 0:1], constant=0.0)
                kb.dma(out=loa[p][:, 1:65], in_=scr.loraT[64:128, 0:CH])
                kb.dma(out=gd[p][:, 1:65], in_=scr.loraT[128:256, 0:CH])
            else:
                kb.dma(out=prv[p][:, :], in_=scr.tmB[t0 - 1:t0 + CH - 1, 256:1024])
                kb.dma(out=lo[p][:, :], in_=scr.loraT[0:64, t0 - 1:t0 + CH])
                kb.dma(out=loa[p][:, :], in_=scr.loraT[64:128, t0 - 1:t0 + CH])
                kb.dma(out=gd[p][:, :], in_=scr.loraT[128:256, t0 - 1:t0 + CH])
            kb.dve.tensor_tensor(out=los[p][:, :], in0=lo[p][:, 0:64], in1=lo[p][:, 1:65], op=ALU.subtract)
            kb.dve.scalar_tensor_tensor(out=los[p][:, :], in0=los[p][:, :], scalar=mu_lo[0:64, 0:1], in1=lo[p][:, 1:65], op0=ALU.mult, op1=ALU.add)
            kb.dve.tensor_tensor(out=loas[p][:, :], in0=loa[p][:, 0:64], in1=loa[p][:, 1:65], op=ALU.subtract)
            kb.dve.scalar_tensor_tensor(out=loas[p][:, :], in0=loas[p][:, :], scalar=mu_a[:, 0:1], in1=loa[p][:, 1:65], op0=ALU.mult, op1=ALU.add)
            kb.dve.tensor_tensor(out=gds[p][:, :], in0=gd[p][:, 0:64], in1=gd[p][:, 1:65], op=ALU.subtract)
            kb.dve.scalar_tensor_tensor(out=gds[p][:, :], in0=gds[p][:, :], scalar=mu_lo[:, 1:2], in1=gd[p][:, 1:65], op0=ALU.mult, op1=ALU.add)
            kb.act.activation(out=los[p][0:64, :], in_=los[p][0:64, :], func=AF.Tanh)
            kb.act.activation(out=gds[p][:, :], in_=gds[p][:, :], func=AF.Sigmoid)
            kb.pe.matmul(out=B[0][:, 0:256], lhsT=los[p][0:64, :], rhs=wa_up[0:64, :], start=True, stop=True)
            kb.pe.matmul(out=B[0][:, 256:512], lhsT=loas[p][:, :], rhs=a_up[:, :], start=True, stop=True)
            kb.pe.matmul(out=B[1][:, 0:256], lhsT=gds[p][:, :], rhs=g_up[:, :], start=True, stop=True)
            kb.dve.tensor_tensor(out=sgt[p][:, :], in0=B[0][:, 0:256], in1=w0_bc[:, :], op=ALU.add)
            kb.act.activation(out=sgt[p][:, :], in_=sgt[p][:, :], func=AF.Sigmoid)
            kb.dve.tensor_tensor(out=a_t[p][:, :], in0=B[0][:, 256:512], in1=a0_bc[:, :], op=ALU.add)
            kb.act.activation(out=a_t[p][:, :], in_=a_t[p][:, :], func=AF.Sigmoid)
            kb.act.copy(out=g_t[p][:, :], in_=B[1][:, 0:256])
            kb.pool.tensor_tensor(out=prv[p][:, :], in0=prv[p][:, :], in1=rkv[p][:, :], op=ALU.subtract)
            kb.pool.tensor_tensor(out=prv[p][:, :], in0=prv[p][:, :], in1=mu_bc[:, :], op=ALU.mult)
            kb.dve.tensor_tensor(out=rkv[p][:, :], in0=rkv[p][:, :], in1=prv[p][:, :], op=ALU.add)
            yield
            r_, k_, v_ = rkv[p][:, 0:256], rkv[p][:, 256:512], rkv[p][:, 512:768]
            kb.dve.tensor_tensor(out=kk[p][:, :], in0=k_, in1=kk_bc[:, :], op=ALU.mult)
            kb.pool.tensor_tensor(out=tmp[p][:, :], in0=kk[p][:, :], in1=kk[p][:, :], op=ALU.mult)
            kb.dve.tensor_reduce(out=small[p][:, 0:4], in_=v3(tmp[p], 0), axis=AX.X, op=ALU.add)
            kb.dve.tensor_scalar(out=small[p][:, 0:4], in0=small[p][:, 0:4], scalar1=1e-12, scalar2=None, op0=ALU.add)
            kb.act.activation(out=small[p][:, 0:4], in_=small[p][:, 0:4], func=AF.Sqrt)
            kb.dve.reciprocal(out=small[p][:, 4:8], in_=small[p][:, 0:4])
            kb.dve.tensor_tensor(out=v3(kk[p], 0), in0=v3(kk[p], 0), in1=hb(small[p], 4), op=ALU.mult)
            kb.pool.tensor_tensor(out=tmp[p][:, :], in0=a_t[p][:, :], in1=ka_bc[:, :], op=ALU.mult)
            kb.pool.tensor_tensor(out=tmp[p][:, :], in0=tmp[p][:, :], in1=oka[:, :], op=ALU.add)
            kb.dve.tensor_tensor(out=k2[p][:, :], in0=k_, in1=tmp[p][:, :], op=ALU.mult)
            kb.pool.tensor_tensor(out=bb[p][:, :], in0=kk[p][:, :], in1=a_t[p][:, :], op=ALU.mult)
            yield
            kb.pe.matmul(out=B[2][:, 0:256], lhsT=tri[:, 0, :], rhs=sgt[p][:, :], start=True, stop=True)
            kb.pe.matmul(out=B[2][:, 256:512], lhsT=tri[:, 1, :], rhs=sgt[p][:, :], start=True, stop=True)
            kb.act.activation(out=Pm[p][:, :], in_=B[2][:, 0:256], func=AF.Exp, scale=-C0)
            kb.act.activation(out=iP[p][:, :], in_=B[2][:, 0:256], func=AF.Exp, scale=C0)
            kb.dve.tensor_tensor(out=tmp2[p][:, :], in0=B[2][:, 0:256], in1=sgt[p][:, :], op=ALU.subtract)
            kb.act.activation(out=Pp[p][:, :], in_=tmp2[p][:, :], func=AF.Exp, scale=-C0)
            kb.act.activation(out=Pr[p][:, :], in_=B[2][:, 256:512], func=AF.Exp, scale=-C0)
            kb.dve.tensor_tensor(out=Kt[p][:, :], in0=k2[p][:, :], in1=iP[p][:, :], op=ALU.mult)
            kb.pool.tensor_tensor(out=Bt[p][:, :], in0=bb[p][:, :], in1=iP[p][:, :], op=ALU.mult)
            kb.dve.tensor_tensor(out=KKt[p][:, :], in0=kk[p][:, :], in1=Pp[p][:, :], op=ALU.mult)
            kb.pool.tensor_tensor(out=Rt[p][:, :], in0=r_, in1=Pm[p][:, :], op=ALU.mult)
            kb.dve.tensor_tensor(out=Kh[p][:, :], in0=k2[p][:, :], in1=Pr[p][:, :], op=ALU.mult)
            kb.pool.tensor_tensor(out=Bh[p][:, :], in0=bb[p][:, :], in1=Pr[p][:, :], op=ALU.mult)
            yield
            for h in range(4):
                kb.pe.matmul(out=B[1][:, 256 + 2 * h:258 + 2 * h], lhsT=Pm[p][:, h * 64:(h + 1) * 64], rhs=cx.ident_f[0:64, 62:64], start=True, stop=True)
            kb.act.copy(out=pcs[p][:, :], in_=B[1][:, 256:264].with_ap(B[1].t[:, 256:264].rearrange("p (h two) -> p h two", two=2)[:, :, 1]))
            yield
            for qi, q in enumerate((Bt, Kt, KKt, Rt)):
                for h in range(4):
                    idx = qi * 4 + h
                    bk = B[3] if idx < 8 else B[4]
                    kb.pe.transpose(out=bk[:, (idx % 8) * 64:(idx % 8 + 1) * 64], in_=q[p][:, h * 64:(h + 1) * 64], identity=cx.ident_f[0:64, 0:64])
            fm = FMq[p]
            kb.act.copy(out=fm[:, 0:8, :], in_=B[3][:, :].with_ap(B[3].t[:, :].rearrange("p (a b) -> p a b", b=64)))
            kb.dve.tensor_copy(out=fm[:, 8:16, :], in_=B[4][:, :].with_ap(B[4].t[:, :].rearrange("p (a b) -> p a b", b=64)))
            BT = lambda h: fm[:, 0 + h, :]
            KT = lambda h: fm[:, 4 + h, :]
            KKT = lambda h: fm[:, 8 + h, :]
            RT = lambda h: fm[:, 12 + h, :]
            fm4 = fm.t.rearrange("p (q h) t -> p q h t", q=4)
            for h in range(4):
                kkr = Ref(fm, None, fm4[:, 2:4, h, :])
                o5 = B[5][:, h * 128:(h + 1) * 128]
                o6 = B[6][:, h * 128:(h + 1) * 128]
                kb.pe.matmul(out=o5, lhsT=BT(h), rhs=kkr, start=True, stop=True)
                kb.pe.matmul(out=o6, lhsT=KT(h), rhs=kkr, start=True, stop=True)
                kb.pe.matmul(out=B[7][:, h * 64:(h + 1) * 64], lhsT=KKT(h), rhs=BT(h), start=True, stop=True)
            hw = lambda bk, w: Ref(bk, None, bk.t.rearrange("p (h w t) -> p h w t", h=4, w=2)[:, :, w, :])
            b3 = lambda bk, c0: bk[:, c0:c0 + 256].with_ap(bk.t[:, c0:c0 + 256].rearrange("p (h d) -> p h d", h=4))
            TAv = TA_[p].t.rearrange("p (h w) t -> p h w t", w=2)
            TA2 = TA_[p].t.rearrange("p a t -> p (a t)")
            Tv = Ref(TA_[p], None, TAv[:, :, 0, :])
            Av = Ref(TA_[p], None, TAv[:, :, 1, :])
            kb.dve.scalar_tensor_tensor(out=Av, in0=hw(B[5], 0), scalar=-1.0, in1=mst[:, :, :], op0=ALU.mult, op1=ALU.mult)
            kb.dve.scalar_tensor_tensor(out=NT_[p][:, :, :], in0=b3(B[7], 0), scalar=-1.0, in1=mstT[:, :, :], op0=ALU.mult, op1=ALU.mult)
            kb.dve.tensor_tensor(out=Mak[p][:, :, :], in0=hw(B[6], 0), in1=mst[:, :, :], op=ALU.mult)
            kb.dve.tensor_tensor(out=Abr[p][:, :, :], in0=hw(B[5], 1), in1=minc[:, :, :], op=ALU.mult)
            kb.dve.tensor_tensor(out=Akr[p][:, :, :], in0=hw(B[6], 1), in1=minc[:, :, :], op=ALU.mult)
            yield
            kb.pool.tensor_copy(out=Tv, in_=idb)
            AT_ = NT_[p]
            B5v = B[5].t.rearrange("p (h w t) -> p h w t", h=4, w=2)
            for j in range(6):
                last = (j == 5)
                for h in range(4):
                    if last:
                        kb.pe.matmul(out=B[5][:, h * 128:h * 128 + 64], lhsT=AT_[:, h, :], rhs=Ref(TA_[p], None, TAv[:, h, 0, :]), start=True, stop=True)
                    else:
                        kb.pe.matmul(out=B[5][:, h * 128:(h + 1) * 128], lhsT=AT_[:, h, :], rhs=Ref(TA_[p], None, TA2[:, h * 128:(h + 1) * 128]), start=True, stop=True)
                        kb.pe.matmul(out=B[6][:, h * 64:(h + 1) * 64], lhsT=Ref(TA_[p], None, TAv[:, h, 1, :]), rhs=AT_[:, h, :], start=True, stop=True)
                kb.dve.tensor_tensor(out=Tv, in0=Tv, in1=Ref(B[5], None, B5v[:, :, 0, :]), op=ALU.add)
                if not last:
                    kb.act.copy(out=Av, in_=Ref(B[5], None, B5v[:, :, 1, :]))
                    kb.dve.tensor_copy(out=AT_[:, :, :], in_=b3(B[6], 0))
                yield
            kb.pool.tensor_copy(out=Tf[p][:, :, :], in_=Tv)
            yield
            for h in range(4):
                kb.pe.matmul(out=B[7][:, 256 + h * 64:256 + (h + 1) * 64], lhsT=KKt[p][:, h * 64:(h + 1) * 64], rhs=Tf[p][:, h, :], start=True, stop=True)
                kb.pe.matmul(out=B[3][:, h * 64:(h + 1) * 64], lhsT=Mak[p][:, h, :], rhs=rkv[p][:, 512 + h * 64:512 + (h + 1) * 64], start=True, stop=True)
            kb.act.copy(out=Wsb[p][:, :, :], in_=b3(B[7], 256))
            kb.dve.tensor_copy(out=Xsb[p][:, :, :], in_=b3(B[3], 0))
            for h in range(4):
                kb.pe.matmul(out=B[3][:, 256 + h * 64:256 + (h + 1) * 64], lhsT=Tf[p][:, h, :], rhs=Xsb[p][:, h, :], start=True, stop=True)
            kb.act.activation(out=U0T[p][:, :, :], in_=b3(B[3], 256), func=AF.Copy, scale=-1.0)
            yield
            for h in range(4):
                vh = rkv[p][:, 512 + h * 64:512 + (h + 1) * 64]
                kb.pe.matmul(out=B[4][:, h * 64:(h + 1) * 64], lhsT=Wsb[p][:, h, :], rhs=ST[:, h, :], start=True, stop=True)
                kb.dve.tensor_tensor(out=UT[p][:, h, :], in0=U0T[p][:, h, :], in1=B[4][:, h * 64:(h + 1) * 64], op=ALU.subtract)
                yo = B[4][:, 256 + h * 64:256 + (h + 1) * 64]
                kb.pe.matmul(out=yo, lhsT=RT(h), rhs=ST[:, h, :], start=True, stop=False)
                kb.pe.matmul(out=yo, lhsT=Abr[p][:, h, :], rhs=UT[p][:, h, :], start=False, stop=False)
                kb.pe.matmul(out=yo, lhsT=Akr[p][:, h, :], rhs=vh, start=False, stop=True)
                kb.act.copy(out=yv[p][:, h * 64:(h + 1) * 64], in_=yo)
                so = B[2][:, h * 64:(h + 1) * 64]
                kb.pe.matmul(out=so, lhsT=Bh[p][:, h * 64:(h + 1) * 64], rhs=UT[p][:, h, :], start=True, stop=False)
                kb.pe.matmul(out=so, lhsT=Kh[p][:, h * 64:(h + 1) * 64], rhs=vh, start=False, stop=True)
                kb.dve.scalar_tensor_tensor(out=ST[:, h, :], in0=ST[:, h, :], scalar=pcs[p][:, h:h + 1], in1=so, op0=ALU.mult, op1=ALU.add)
                yield
            yield
            y = yv[p]
            kb.pool.tensor_tensor(out=tmp[p][:, :], in0=r_, in1=k2[p][:, :], op=ALU.mult)
            kb.pool.tensor_tensor(out=tmp[p][:, :], in0=tmp[p][:, :], in1=rk_bc[:, :], op=ALU.mult)
            kb.dve.tensor_reduce(out=small[p][:, 8:12], in_=v3(tmp[p], 0), axis=AX.X, op=ALU.add)
            kb.dve.tensor_tensor(out=v3(tmp2[p], 0), in0=v3(rkv[p], 512), in1=hb(small[p], 8), op=ALU.mult)
            kb.dve.tensor_tensor(out=y[:, :], in0=tmp2[p][:, :], in1=y[:, :], op=ALU.add)
            kb.dve.tensor_reduce(out=small[p][:, 0:4], in_=v3(y, 0), axis=AX.X, op=ALU.add)
            kb.dve.tensor_scalar(out=small[p][:, 0:4], in0=small[p][:, 0:4], scalar1=1.0 / 64, scalar2=None, op0=ALU.mult)
            kb.dve.tensor_tensor(out=v3(y, 0), in0=v3(y, 0), in1=hb(small[p], 0), op=ALU.subtract)
            kb.pool.tensor_tensor(out=tmp[p][:, :], in0=y[:, :], in1=y[:, :], op=ALU.mult)
            kb.dve.tensor_reduce(out=small[p][:, 4:8], in_=v3(tmp[p], 0), axis=AX.X, op=ALU.add)
            kb.dve.tensor_scalar(out=small[p][:, 4:8], in0=small[p][:, 4:8], scalar1=1.0 / 64, scalar2=64e-5, op0=ALU.mult, op1=ALU.add)
            kb.act.activation(out=small[p][:, 4:8], in_=small[p][:, 4:8], func=AF.Sqrt)
            kb.dve.reciprocal(out=small[p][:, 12:16], in_=small[p][:, 4:8])
            kb.dve.tensor_tensor(out=v3(y, 0), in0=v3(y, 0), in1=hb(small[p], 12), op=ALU.mult)
            kb.pool.tensor_tensor(out=y[:, :], in0=y[:, :], in1=lg_bc[:, :], op=ALU.mult)
            kb.pool.tensor_tensor(out=y[:, :], in0=y[:, :], in1=lb_bc[:, :], op=ALU.add)
            kb.dve.tensor_tensor(out=y[:, :], in0=y[:, :], in1=g_t[p][:, :], op=ALU.mult)
            kb.dma(out=scr.ymix.k(("c", c))[t0:t0 + CH, 512:768], in_=y[:, :])

        import os as _os
        nch = int(_os.environ.get('RWKV_NCH', S // CH))
        active = []
        nxt = 0
        while nxt < nch or active:
            if nxt < nch and len(active) < NBUF:
                active.append(chunk(nxt))
                nxt += 1
            for g in list(active):
                try:
                    next(g)
                except StopIteration:
                    active.remove(g)
        kb.barrier()
def build(depth=DEPTH, debug=None, stop_after=None, only=None):
    kb = KB()
    nc = kb.nc
    cx = Ctx()
    cx.P = {}
    x_in = kb.dram("x", [S, D], F32, kind="ExternalInput")
    for n, shp in PARAM_SHAPES.items():
        cx.P[n] = kb.dram(n, list(shp), F32, kind="ExternalInput")
    consts = make_consts()
    cx.C = {}
    for n, a in consts.items():
        cx.C[n] = kb.dram("c_" + n, list(a.shape), CONST_DT.get(n, F32), kind="ExternalInput")
    y_out = kb.dram("y", [S, D], F32, kind="ExternalOutput")
    scr = Ctx()
    cx.scr = scr
    dbg = debug or []

    def scratch(name, shape, dt):
        kind = "ExternalOutput" if name in dbg else "Internal"
        return kb.dram("scr_" + name, shape, dt, kind=kind)
    scr.qT = scratch("qT", [256, S], BF16)
    scr.kcvcT = scratch("kcvcT", [128, S], BF16)
    scr.ksT = scratch("ksT", [64, S], BF16)
    scr.kwT = scratch("kwT", [64, S], BF16)
    scr.dqT = scratch("dqT", [256, S], BF16)
    scr.dkT = scratch("dkT", [256, S], BF16)
    scr.loraT = scratch("loraT", [256, S], F32)
    scr.convT = scratch("convT", [512, S], F32)
    scr.tmA = scratch("tmA", [S, 204], F32)
    scr.tmB = scratch("tmB", [S, 1024], F32)
    scr.ymix = scratch("ymix", [S, D], F32)
    cx.xres = scratch("xres", [S, D], F32)
    outs = [y_out] + [getattr(scr, n) if hasattr(scr, n) else cx.xres for n in dbg]

    gst = ExitStack()
    cx.ident_bf = kb.sb(gst, "ident_bf", [128, 128], BF16)
    cx.ident_f = kb.sb(gst, "ident_f", [128, 128], F32)
    kb.dma(out=cx.ident_bf[:, :], in_=cx.C["ident_bf"][:, :])
    kb.dma(out=cx.ident_f[:, :], in_=cx.C["ident_f"][:, :])
    kb.dma(out=cx.xres[:, :], in_=x_in[:, :])

    for l in range(depth):
        phase_a(kb, cx, l)
        if stop_after == "a":
            break
        if only in (None, "conv"):
            phase_conv(kb, cx, l)
        if only in (None, "dil"):
            phase_dil(kb, cx, l)
        if only in (None, "nsa"):
            phase_nsa(kb, cx, l)
        if only in (None, "rwkv"):
            phase_rwkv(kb, cx, l)
        if stop_after == "mix":
            break
        phase_b(kb, cx, l)
        if stop_after == "b":
            break
        phase_c(kb, cx, l)
    if stop_after is None:
        phase_final(kb, cx, y_out)
    gst.close()
    kb.finish(outs)
    return kb, consts


_CACHE = {}


def kernel(**inputs):
    if "prog" not in _CACHE:
        _CACHE["prog"] = build()
    kb, consts = _CACHE["prog"]
    x = np.ascontiguousarray(inputs["x"], dtype=np.float32)
    in_maps = []
    for c in range(8):
        m = {"x": x[c]}
        for n in PARAM_SHAPES:
            m[n] = np.ascontiguousarray(inputs[n], dtype=np.float32)
        for n, a in consts.items():
            m["c_" + n] = a
        in_maps.append(m)
    res = run_bass_kernel_spmd(kb.nc, in_maps, core_ids=list(range(8)))
    return np.stack([res.results[c]["y"] for c in range(8)], axis=0)
```

```python
import numpy as np
import ml_dtypes
from contextlib import ExitStack
import concourse.bass as bass
import concourse.mybir as mybir
from concourse.bass_utils import run_bass_kernel_spmd

F32 = mybir.dt.float32
BF16 = mybir.dt.bfloat16
I32 = mybir.dt.int32
AF = mybir.ActivationFunctionType
ALU = mybir.AluOpType
AX = mybir.AxisListType

S = 4096
D = 1024
NT = S // 128
NBLK = S // 512
DEPTH = 4
DFF = 2816
IN_COLS = 2956
WRITE_NAMES = ("out", "accum_out", "ap")


class SemT:
    def __init__(self, handle):
        self.h = handle
        self.count = 0


class Rec:
    __slots__ = ("lw", "rd")

    def __init__(self):
        self.lw = None
        self.rd = []


class Tile:
    def __init__(self, t, name):
        self.t = t
        self.name = name
        self.regs = {None: Rec()}
        self.excl = False

    def recs_dep(self, key):
        if key is None:
            return list(self.regs.values())
        if key not in self.regs:
            self.regs[key] = Rec()
        return [self.regs[key], self.regs[None]]

    def recs_upd(self, key, is_write):
        if key is None:
            return list(self.regs.values()) if is_write else [self.regs[None]]
        if key not in self.regs:
            self.regs[key] = Rec()
        return [self.regs[key]]

    def __getitem__(self, idx):
        return Ref(self, None, self.t[idx])

    def k(self, key):
        return KeyView(self, key)


class KeyView:
    def __init__(self, tile, key):
        self.tile = tile
        self.key = key

    def __getitem__(self, idx):
        return Ref(self.tile, self.key, self.tile.t[idx])


class Ref:
    def __init__(self, tile, key, ap):
        self.tile = tile
        self.key = key
        self.ap = ap

    def with_ap(self, ap):
        return Ref(self.tile, self.key, ap)


class Eng:
    def __init__(self, kb, name, raw, sem, is_pe=False):
        self.kb = kb
        self.name = name
        self.raw = raw
        self.sem = sem
        self.waited = {}
        self.is_pe = is_pe

    def __getattr__(self, opname):
        def call(**kw):
            reads, writes = [], []
            kw2 = {}
            for n, v in kw.items():
                if isinstance(v, Ref):
                    (writes if n in WRITE_NAMES else reads).append(v)
                    kw2[n] = v.ap
                else:
                    kw2[n] = v
            return self.kb.emit(self, lambda: getattr(self.raw, opname)(**kw2), reads, writes)
        return call


class KB:
    def __init__(self):
        self.nc = bass.Bass("TRN2", target_bir_lowering=False)
        nc = self.nc
        self.es = ExitStack()
        mk = lambda n: SemT(self.es.enter_context(nc.semaphore(n)))
        self.pe = Eng(self, "pe", nc.tensor, mk("s_pe"), is_pe=True)
        self.act = Eng(self, "act", nc.scalar, mk("s_act"))
        self.dve = Eng(self, "dve", nc.vector, mk("s_dve"))
        self.pool = Eng(self, "pool", nc.gpsimd, mk("s_pool"))
        self.sp = Eng(self, "sp", nc.sync, mk("s_sp"))
        self.ring = [mk(f"s_dma{i}") for i in range(24)]
        self.ring_i = 0
        self.pring = [mk(f"s_pdma{i}") for i in range(8)]
        self.pring_i = 0
        self.n_inst = 0
        self.out_deps = []

    def dram(self, name, shape, dtype, kind="Internal"):
        return Tile(self.nc.dram_tensor(name, list(shape), dtype, kind=kind).ap(), name)

    def sb(self, stack, name, shape, dtype):
        self.n_alloc = getattr(self, "n_alloc", 0) + 1
        name = f"{name}_{self.n_alloc}"
        return Tile(stack.enter_context(self.nc.sbuf_tensor(name, list(shape), dtype)), name)

    def psum(self, stack, name, shape, dtype):
        self.n_alloc = getattr(self, "n_alloc", 0) + 1
        name = f"{name}_{self.n_alloc}"
        t = Tile(stack.enter_context(self.nc.psum_tensor(name, list(shape), dtype)), name)
        t.excl = True
        return t

    def _wait(self, eng, deps):
        best = {}
        for (st, v) in deps:
            if v is None:
                continue
            if id(st) not in best or best[id(st)][1] < v:
                best[id(st)] = (st, v)
        for st, v in best.values():
            if st is eng.sem and eng.is_pe:
                continue
            if eng.waited.get(id(st), 0) >= v:
                continue
            eng.raw.wait_ge(st.h, v)
            eng.waited[id(st)] = v

    def _collect(self, reads, writes):
        deps = []
        for r in reads:
            for rec in r.tile.recs_dep(r.key):
                if rec.lw is not None:
                    deps.append(rec.lw)
                if r.tile.excl:
                    deps.extend(rec.rd)
        for w in writes:
            for rec in w.tile.recs_dep(w.key):
                if rec.lw is not None:
                    deps.append(rec.lw)
                deps.extend(rec.rd)
        return deps

    def _update(self, reads, writes, tag):
        for r in reads:
            for rec in r.tile.recs_upd(r.key, False):
                rec.rd.append(tag)
                if len(rec.rd) > 48:
                    best = {}
                    for st, v in rec.rd:
                        if id(st) not in best or best[id(st)][1] < v:
                            best[id(st)] = (st, v)
                    rec.rd = list(best.values())
        for w in writes:
            for rec in w.tile.recs_upd(w.key, True):
                rec.lw = tag
                rec.rd = []

    def emit(self, eng, fn, reads, writes):
        deps = self._collect(reads, writes)
        self._wait(eng, deps)
        inst = fn()
        eng.sem.count += 1
        inst.then_inc(eng.sem.h, 1)
        self._update(reads, writes, (eng.sem, eng.sem.count))
        self.n_inst += 1
        return inst

    def dma(self, out, in_, via_pool=False, **kw):
        eng = self.pool if via_pool else self.sp
        if via_pool:
            st = self.pring[self.pring_i % len(self.pring)]
            self.pring_i += 1
        else:
            st = self.ring[self.ring_i % len(self.ring)]
            self.ring_i += 1
        deps = self._collect([in_], [out])
        deps.append((st, st.count))
        self._wait(eng, deps)
        inst = eng.raw.dma_start(out=out.ap, in_=in_.ap, **kw)
        st.count += 16
        inst.then_inc(st.h, 16)
        tag = (st, st.count)
        self._update([in_], [out], tag)
        self.n_inst += 1
        return tag

    def barrier(self):
        deps = [(st, st.count) for st in self.ring + self.pring]
        deps += [(e.sem, e.sem.count) for e in (self.pe, self.act, self.dve, self.pool)]
        deps = [d for d in deps if d[1] > 0]
        for e in (self.pe, self.act, self.dve, self.pool, self.sp):
            self._wait(e, [d for d in deps if not (d[0] is e.sem)])

    def finish(self, out_tiles):
        deps = [(st, st.count) for st in self.ring + self.pring]
        deps += [(e.sem, e.sem.count) for e in (self.pe, self.act, self.dve, self.pool)]
        self._wait(self.sp, [d for d in deps if d[1] > 0])
        self.es.close()


def _bf(a):
    return np.ascontiguousarray(a.astype(np.float32)).astype(ml_dtypes.bfloat16)


def make_consts():
    c = {}
    c["ident_bf"] = _bf(np.eye(128))
    c["ident_f"] = np.eye(128, dtype=np.float32)
    sp = np.arange(128)[:, None]
    tq = np.arange(512)[None, :]
    c["cms"] = _bf(np.stack([(128 * j + sp <= tq) for j in range(4)], axis=1))
    tq1 = np.arange(128)[None, :]
    c["cmw"] = _bf(np.stack([(sp <= tq1), (sp >= tq1)], axis=1))
    dm = []
    for delta in range(-3, 17):
        d = tq - sp + 128 * delta
        cnt = ((d >= 0) & (d <= 128)).astype(np.float32)
        cnt += ((d >= 0) & (d <= 512) & (d % 4 == 0))
        cnt += ((d >= 0) & (d <= 2048) & (d % 16 == 0))
        dm.append(cnt)
    c["dm"] = _bf(np.stack(dm, axis=1))
    j = np.arange(64)[:, None, None]
    kt = np.arange(32)[None, :, None]
    s = np.arange(128)[None, None, :]
    c["ek"] = _bf(((128 * kt + s) // 64 == j))
    mc = np.zeros((128, 16, 512), np.float32)
    for b in range(8):
        for ct in range(2):
            mc[:, b * 2 + ct, :] = (16 * (128 * ct + sp) + 31 <= 512 * b + tq)
    c["mc"] = _bf(mc)
    c["gc"] = (16 * np.arange(256)[None, :] + 31 - np.arange(128)[:, None]).astype(np.float32)
    c["d0"] = (64 * np.arange(64)[None, :] - np.arange(128)[:, None]).astype(np.float32)
    i = np.arange(128)[:, None]
    t = np.arange(128)[None, :]
    c["tri"] = ((i // 64 == t // 64) & (i <= t)).astype(np.float32)
    i6 = np.arange(64)[:, None]
    t6 = np.arange(64)[None, :]
    c["mstrict"] = np.tile((i6 < t6).astype(np.float32)[:, None, :], (1, 8, 1))
    c["mincl"] = np.tile((i6 <= t6).astype(np.float32)[:, None, :], (1, 8, 1))
    c["mstrictT"] = np.tile((i6 > t6).astype(np.float32)[:, None, :], (1, 8, 1))
    return c


CONST_DT = {"ident_bf": BF16, "cms": BF16, "cmw": BF16, "dm": BF16, "ek": BF16, "mc": BF16}

PARAM_SHAPES = {
    'norm_mix': (DEPTH, D), 'w_in': (DEPTH, D, IN_COLS), 'cmp_pos': (DEPTH, 32, 64),
    'cmp_k_w1': (DEPTH, 2048, 128), 'cmp_k_w2': (DEPTH, 128, 64), 'cmp_v_w1': (DEPTH, 2048, 128),
    'cmp_v_w2': (DEPTH, 128, 64), 'beta_nsa': (DEPTH, 256), 'beta_dil': (DEPTH, 256),
    'rwkv_mu': (DEPTH, 1024), 'rwkv_w0': (DEPTH, 256), 'rwkv_w_up': (DEPTH, 64, 256),
    'rwkv_a0': (DEPTH, 256), 'rwkv_a_up': (DEPTH, 64, 256), 'rwkv_g_up': (DEPTH, 128, 256),
    'rwkv_k_k': (DEPTH, 256), 'rwkv_k_a': (DEPTH, 256), 'rwkv_r_k': (DEPTH, 4, 64),
    'rwkv_ln_g': (DEPTH, 256), 'rwkv_ln_b': (DEPTH, 256), 'conv_dw': (DEPTH, 31, 256),
    'conv_dw_b': (DEPTH, 256), 'conv_ln_g': (DEPTH, 256), 'conv_ln_b': (DEPTH, 256),
    'w_out': (DEPTH, D, D), 'norm_ffn': (DEPTH, D), 'ffn_up': (DEPTH, D, 2 * DFF),
    'ffn_dw': (DEPTH, 3, 2 * DFF), 'ffn_dw_b': (DEPTH, 2 * DFF), 'ffn_down': (DEPTH, DFF, D),
    'norm_final': (D,),
}


class Ctx:
    pass


def bcast_rows(ap_1d_row, nparts):
    return ap_1d_row.partition_broadcast(nparts)


def load_bcast(kb, dst_tile, src_dram_tile, row_ap, n):
    kb.dma(out=dst_tile[:, 0:n], in_=Ref(src_dram_tile, None, row_ap.partition_broadcast(128)))


FM_CHUNKS = [
    (0, 128, "qT", 0), (128, 128, "qT", 128), (256, 128, "kcvcT", 0), (384, 64, "ksT", 0), (512, 64, "kwT", 0),
    (652, 128, "dqT", 0), (780, 128, "dqT", 128), (908, 128, "dkT", 0), (1036, 128, "dkT", 128),
    (2188, 128, "loraT", 0), (2316, 128, "loraT", 128),
    (2444, 128, "convT", 0), (2572, 128, "convT", 128), (2700, 128, "convT", 256), (2828, 128, "convT", 384),
]
TM_GROUPS = [(448, 204, "tmA", 0), (1164, 512, "tmB", 0), (1676, 512, "tmB", 512)]


def load_weight_bf16(kb, dst, dram_w, rows0, nk, cols0, ncols):
    for k in range(nk):
        c = 0
        while c < ncols:
            w = min(1024, ncols - c)
            kb.dma(out=dst[:, k, c:c + w],
                   in_=dram_w[rows0 + k * 128: rows0 + (k + 1) * 128, cols0 + c: cols0 + c + w], via_pool=True)
            c += w


def rms_rstd(kb, ssq_ref, out_ref, n, eps, tmp_ref):
    kb.dve.tensor_scalar(out=tmp_ref, in0=ssq_ref, scalar1=1.0 / n, scalar2=eps, op0=ALU.mult, op1=ALU.add)
    kb.act.activation(out=tmp_ref, in_=tmp_ref, func=AF.Sqrt)
    kb.dve.reciprocal(out=out_ref, in_=tmp_ref)


def phase_a(kb, cx, l):
    P = cx.P
    scr = cx.scr
    with ExitStack() as st:
        w_sb = kb.sb(st, "wA", [128, 8, IN_COLS], BF16)
        wblocks = [(b, b * 512, min(512, IN_COLS - b * 512)) for b in range(6)]
        for (key, c0, cw) in wblocks:
            src = P["w_in"].t[l, :, c0:c0 + cw].rearrange("(k p) c -> p k c", p=128)
            kb.dma(out=w_sb.k(key)[:, :, c0:c0 + cw], in_=Ref(P["w_in"], None, src), via_pool=True)

        def wkey(c0, cw):
            return w_sb.k(c0 // 512) if c0 // 512 == (c0 + cw - 1) // 512 else w_sb
        gbc = kb.sb(st, "gbcA", [128, D], F32)
        kb.dma(out=gbc[:, :], in_=Ref(P["norm_mix"], None, P["norm_mix"].t[l:l + 1, :].partition_broadcast(128)))
        xt = [kb.sb(st, f"xtA{i}", [128, 4, D], F32) for i in range(2)]
        hbf = [kb.sb(st, f"hbfA{i}", [128, D], BF16) for i in range(2)]
        junk = kb.sb(st, "junkA", [128, D], BF16)
        hT = [kb.sb(st, f"hTA{i}", [128, 8, 512], BF16) for i in range(2)]
        small = kb.sb(st, "smallA", [128, 16], F32)
        stg_bf = [kb.sb(st, f"stgbA{i}", [128, 512], BF16) for i in range(3)]
        stg_f = [kb.sb(st, f"stgfA{i}", [128, 512], F32) for i in range(3)]
        tp = [kb.psum(st, f"tpA{i}", [128, 8, 128], BF16) for i in range(2)]
        acc = [kb.psum(st, f"accA{i}", [128, 512], F32) for i in range(4)]
        n_acc = 0
        n_stg = 0
        def load_x(b):
            kb.dma(out=xt[b % 2][:, :, :], in_=cx.xres[b * 512:(b + 1) * 512, :].with_ap(
                cx.xres.t[b * 512:(b + 1) * 512, :].rearrange("(j p) d -> p j d", p=128)))
        load_x(0)
        for b in range(NBLK):
            x_t = xt[b % 2]
            if b + 1 < NBLK:
                load_x(b + 1)
            h_T = hT[b % 2]
            for j in range(4):
                hb = hbf[j % 2]
                ssq = small[:, j:j + 1]
                kb.act.activation(out=junk[:, :], in_=x_t[:, j, :], func=AF.Square, accum_out=ssq)
                rms_rstd(kb, ssq, small[:, 4 + j:5 + j], D, 1e-6, small[:, 8 + j:9 + j])
                kb.dve.scalar_tensor_tensor(out=hb[:, :], in0=x_t[:, j, :], scalar=small[:, 4 + j:5 + j], in1=gbc[:, :],
                                            op0=ALU.mult, op1=ALU.mult)
                t_p = tp[j % 2]
                for kc in range(8):
                    kb.pe.transpose(out=t_p[:, kc, :], in_=hb[:, kc * 128:(kc + 1) * 128], identity=cx.ident_bf[:, :])
                kb.act.copy(out=h_T[:, :, j * 128:(j + 1) * 128], in_=t_p[:, :, :])
            for (c0, cw, dst, r0) in FM_CHUNKS:
                a = acc[n_acc % 4]
                n_acc += 1
                for kc in range(8):
                    kb.pe.matmul(out=a[0:cw, :], lhsT=wkey(c0, cw)[:, kc, c0:c0 + cw], rhs=h_T[:, kc, :], start=(kc == 0), stop=(kc == 7))
                dt_f32 = dst in ("loraT", "convT")
                sg = (stg_f if dt_f32 else stg_bf)[n_stg % 3]
                if n_stg % 2 == 0:
                    kb.dve.tensor_copy(out=sg[0:cw, :], in_=a[0:cw, :])
                else:
                    kb.act.copy(out=sg[0:cw, :], in_=a[0:cw, :])
                n_stg += 1
                kb.dma(out=getattr(scr, dst).k(b)[r0:r0 + cw, b * 512:(b + 1) * 512], in_=sg[0:cw, :])
            for j in range(4):
                for (c0, cw, dst, d0) in TM_GROUPS:
                    a = acc[n_acc % 4]
                    n_acc += 1
                    for kc in range(8):
                        kb.pe.matmul(out=a[:, 0:cw], lhsT=h_T[:, kc, j * 128:(j + 1) * 128], rhs=wkey(c0, cw)[:, kc, c0:c0 + cw],
                                     start=(kc == 0), stop=(kc == 7))
                    sg = stg_f[n_stg % 3]
                    if n_stg % 2 == 0:
                        kb.dve.tensor_copy(out=sg[:, 0:cw], in_=a[:, 0:cw])
                    else:
                        kb.act.copy(out=sg[:, 0:cw], in_=a[:, 0:cw])
                    n_stg += 1
                    r = b * 512 + j * 128
                    kb.dma(out=getattr(scr, dst).k(b)[r:r + 128, d0:d0 + cw], in_=sg[:, 0:cw])
        kb.barrier()


def phase_conv(kb, cx, l):
    P = cx.P
    scr = cx.scr
    with ExitStack() as st:
        glu = [kb.sb(st, f"gluD{i}", [128, 30 + S], F32) for i in range(2)]
        accs = [kb.sb(st, f"accD{i}", [128, S], F32) for i in range(2)]
        bt = kb.sb(st, "btD", [128, S], F32)
        wdw = kb.sb(st, "wdwD", [128, 2, 32], F32)
        lng = kb.sb(st, "lngD", [128, 256], F32)
        lnb = kb.sb(st, "lnbD", [128, 256], F32)
        small = kb.sb(st, "smallD", [128, 16], F32)
        stats = kb.sb(st, "statsD", [128, 8], F32)
        xn = [kb.sb(st, f"xnD{i}", [128, 256], F32) for i in range(2)]
        tps = [kb.psum(st, f"tpD{i}", [128, 512], F32) for i in range(2)]
        kb.dma(out=lng[:, :], in_=Ref(P["conv_ln_g"], None, P["conv_ln_g"].t[l:l + 1, :].partition_broadcast(128)))
        kb.dma(out=lnb[:, :], in_=Ref(P["conv_ln_b"], None, P["conv_ln_b"].t[l:l + 1, :].partition_broadcast(128)))
        for ci in range(2):
            with kb.nc.allow_non_contiguous_dma(reason="tiny transposed conv weights"):
                kb.dma(out=wdw[:, ci, 0:31], in_=Ref(P["conv_dw"], None,
                       P["conv_dw"].t[l, :, ci * 128:(ci + 1) * 128].rearrange("k c -> c k")))
                kb.dma(out=wdw[:, ci, 31:32], in_=Ref(P["conv_dw_b"], None,
                       P["conv_dw_b"].t[l:l + 1, ci * 128:(ci + 1) * 128].rearrange("o c -> c o")))
            g = glu[ci]
            kb.pool.memset(ap=g[:, 0:30], constant=0.0)
            kb.dma(out=g[:, 30:30 + S], in_=scr.convT[ci * 128:(ci + 1) * 128, :])
            kb.dma(out=bt[:, :], in_=scr.convT[256 + ci * 128:256 + (ci + 1) * 128, :])
            kb.act.activation(out=bt[:, :], in_=bt[:, :], func=AF.Sigmoid)
            kb.pool.tensor_tensor(out=g[:, 30:30 + S], in0=g[:, 30:30 + S], in1=bt[:, :], op=ALU.mult)
            a = accs[ci]
            for h0 in range(0, S, 2048):
                kb.dve.tensor_scalar(out=a[:, h0:h0 + 2048], in0=g[:, 30 + h0:30 + h0 + 2048], scalar1=wdw[:, ci, 30:31],
                                     scalar2=wdw[:, ci, 31:32], op0=ALU.mult, op1=ALU.add)
                for j in range(30):
                    kb.dve.scalar_tensor_tensor(out=a[:, h0:h0 + 2048], in0=g[:, j + h0:j + h0 + 2048], scalar=wdw[:, ci, j:j + 1],
                                                in1=a[:, h0:h0 + 2048], op0=ALU.mult, op1=ALU.add)
        for i in range(NT):
            tp = tps[i % 2]
            for ci in range(2):
                kb.pe.transpose(out=tp[:, ci * 128:(ci + 1) * 128], in_=accs[ci][:, i * 128:(i + 1) * 128], identity=cx.ident_f[:, :])
            x_n = xn[i % 2]
            kb.dve.bn_stats(out=stats[:, 0:6], in_=tp[:, 0:256])
            kb.dve.bn_aggr(out=small[:, 0:2], in_=stats[:, 0:6])
            kb.dve.tensor_scalar(out=small[:, 2:3], in0=small[:, 1:2], scalar1=1e-5, scalar2=None, op0=ALU.add)
            kb.act.activation(out=small[:, 2:3], in_=small[:, 2:3], func=AF.Sqrt)
            kb.dve.reciprocal(out=small[:, 3:4], in_=small[:, 2:3])
            kb.dve.tensor_scalar(out=x_n[:, :], in0=tp[:, 0:256], scalar1=small[:, 0:1], scalar2=small[:, 3:4],
                                 op0=ALU.subtract, op1=ALU.mult)
            kb.pool.tensor_tensor(out=x_n[:, :], in0=x_n[:, :], in1=lng[:, :], op=ALU.mult)
            kb.pool.tensor_tensor(out=x_n[:, :], in0=x_n[:, :], in1=lnb[:, :], op=ALU.add)
            kb.act.activation(out=x_n[:, :], in_=x_n[:, :], func=AF.Silu)
            kb.dma(out=scr.ymix.k(("d", i))[i * 128:(i + 1) * 128, 768:1024], in_=x_n[:, :])
        kb.barrier()


def load_w_bf16(kb, dst, wtile, l, nk, ncols):
    for k in range(nk):
        c = 0
        while c < ncols:
            w = min(1024, ncols - c)
            kb.dma(out=dst[:, k, c:c + w], in_=wtile[l, k * 128:(k + 1) * 128, c:c + w], via_pool=True)
            c += w


def load_w_blocks(kb, dst, wtile, l, nk, blocks):
    for (key, c0, cw) in blocks:
        src = wtile.t[l, 0:nk * 128, c0:c0 + cw].rearrange("(k p) c -> p k c", p=128)
        kb.dma(out=dst.k(key)[:, 0:nk, c0:c0 + cw], in_=Ref(wtile, None, src), via_pool=True)


def phase_b(kb, cx, l):
    P = cx.P
    scr = cx.scr
    with ExitStack() as st:
        wo = kb.sb(st, "woB", [128, 8, D], BF16)
        load_w_bf16(kb, wo, P["w_out"], l, 8, D)
        yt = [kb.sb(st, f"ytB{i}", [128, D], F32) for i in range(2)]
        xt = [kb.sb(st, f"xtB{i}", [128, D], F32) for i in range(2)]
        ybf = [kb.sb(st, f"ybfB{i}", [128, D], BF16) for i in range(2)]
        yT = [kb.sb(st, f"yTB{i}", [128, 8, 128], BF16) for i in range(2)]
        tp = [kb.psum(st, f"tpB{i}", [128, 8, 128], BF16) for i in range(2)]
        acc = [kb.psum(st, f"accB{i}", [128, 512], F32) for i in range(4)]
        na = 0
        def load_b(i):
            kb.dma(out=yt[i % 2][:, :], in_=scr.ymix[i * 128:(i + 1) * 128, :])
            kb.dma(out=xt[i % 2][:, :], in_=cx.xres.k(i)[i * 128:(i + 1) * 128, :])
        load_b(0)
        for i in range(NT):
            y_t, x_t, y_b, y_T, t_p = yt[i % 2], xt[i % 2], ybf[i % 2], yT[i % 2], tp[i % 2]
            if i + 1 < NT:
                load_b(i + 1)
            kb.pool.tensor_copy(out=y_b[:, :], in_=y_t[:, :])
            for kc in range(8):
                kb.pe.transpose(out=t_p[:, kc, :], in_=y_b[:, kc * 128:(kc + 1) * 128], identity=cx.ident_bf[:, :])
            kb.act.copy(out=y_T[:, :, :], in_=t_p[:, :, :])
            for c0 in (0, 512):
                a = acc[na % 4]
                na += 1
                for kc in range(8):
                    kb.pe.matmul(out=a[:, :], lhsT=y_T[:, kc, :], rhs=wo[:, kc, c0:c0 + 512], start=(kc == 0), stop=(kc == 7))
                kb.dve.tensor_tensor(out=x_t[:, c0:c0 + 512], in0=x_t[:, c0:c0 + 512], in1=a[:, :], op=ALU.add)
            kb.dma(out=cx.xres.k(i)[i * 128:(i + 1) * 128, :], in_=x_t[:, :])
        kb.barrier()


def phase_c(kb, cx, l):
    P = cx.P
    TB = 256
    with ExitStack() as st:
        wu = kb.sb(st, "wuC", [128, 8, 2 * DFF], BF16)
        wd = kb.sb(st, "wdC", [128, 22, D], BF16)
        order = []
        for i in range(6):
            order += [i, i + 5] if i + 5 < 11 else [i]
        order = [b for b in dict.fromkeys(order) if b < 11]
        load_w_blocks(kb, wu, P["ffn_up"], l, 8, [(b, b * 512, 512) for b in order])
        for k in range(22):
            kb.dma(out=wd.k(k)[:, k, :], in_=P["ffn_down"][l, k * 128:(k + 1) * 128, :], via_pool=True)
        gbc = kb.sb(st, "gbcC", [128, D], F32)
        kb.dma(out=gbc[:, :], in_=Ref(P["norm_ffn"], None, P["norm_ffn"].t[l:l + 1, :].partition_broadcast(128)))
        cw = kb.sb(st, "cwC", [128, 44, 4], F32)
        with kb.nc.allow_non_contiguous_dma(reason="tiny transposed conv weights"):
            for j in range(3):
                kb.dma(out=cw[:, :, j:j + 1], in_=Ref(P["ffn_dw"], None,
                       P["ffn_dw"].t[l, j:j + 1, :].rearrange("o (c p) -> p c o", p=128)))
            kb.dma(out=cw[:, :, 3:4], in_=Ref(P["ffn_dw_b"], None,
                   P["ffn_dw_b"].t[l:l + 1, :].rearrange("o (c p) -> p c o", p=128)))
        carry = kb.sb(st, "carryC", [128, 44, 2], F32)
        kb.pool.memset(ap=carry[:, :, :], constant=0.0)
        xt = [kb.sb(st, f"xtC{i}", [128, 2, D], F32) for i in range(2)]
        hbf = [kb.sb(st, f"hbfC{i}", [128, D], BF16) for i in range(2)]
        junk = kb.sb(st, "junkC", [128, D], BF16)
        hT = [kb.sb(st, f"hTC{i}", [128, 8, TB], BF16) for i in range(2)]
        small = kb.sb(st, "smallC", [128, 16], F32)
        G = [kb.sb(st, f"GC{i}", [128, 22, TB], BF16) for i in range(2)]
        ub = [kb.sb(st, f"ubC{i}", [128, TB + 2], F32) for i in range(4)]
        ac = [kb.sb(st, f"acC{i}", [128, TB], F32) for i in range(4)]
        tp = [kb.psum(st, f"tpC{i}", [128, 8, 128], BF16) for i in range(2)]
        ups = [kb.psum(st, f"upC{i}", [128, 512], F32) for i in range(4)]
        dps = [kb.psum(st, f"dpC{i}", [128, 512], F32) for i in range(2)]
        nu = 0
        nd = 0
        def load_c(b):
            kb.dma(out=xt[b % 2][:, :, :], in_=cx.xres.k(b)[b * TB:(b + 1) * TB, :].with_ap(
                cx.xres.t[b * TB:(b + 1) * TB, :].rearrange("(j p) d -> p j d", p=128)))
        load_c(0)
        for b in range(S // TB):
            x_t, h_T, Gb = xt[b % 2], hT[b % 2], G[b % 2]
            if b + 1 < S // TB:
                load_c(b + 1)
            for j in range(2):
                hb = hbf[j]
                kb.act.activation(out=junk[:, :], in_=x_t[:, j, :], func=AF.Square, accum_out=small[:, j:j + 1])
                rms_rstd(kb, small[:, j:j + 1], small[:, 4 + j:5 + j], D, 1e-6, small[:, 8 + j:9 + j])
                kb.dve.scalar_tensor_tensor(out=hb[:, :], in0=x_t[:, j, :], scalar=small[:, 4 + j:5 + j], in1=gbc[:, :],
                                            op0=ALU.mult, op1=ALU.mult)
                t_p = tp[j]
                for kc in range(8):
                    kb.pe.transpose(out=t_p[:, kc, :], in_=hb[:, kc * 128:(kc + 1) * 128], identity=cx.ident_bf[:, :])
                kb.act.copy(out=h_T[:, :, j * 128:(j + 1) * 128], in_=t_p[:, :, :])
            for ci in range(22):
                res = []
                for half in range(2):
                    c = ci + 22 * half
                    u = ups[nu % 4]
                    u_b = ub[nu % 4]
                    a = ac[nu % 4]
                    nu += 1
                    for kc in range(8):
                        kb.pe.matmul(out=u[:, 0:TB], lhsT=wu.k(c // 4)[:, kc, c * 128:(c + 1) * 128], rhs=h_T[:, kc, :],
                                     start=(kc == 0), stop=(kc == 7))
                    kb.act.copy(out=u_b[:, 2:TB + 2], in_=u[:, 0:TB])
                    kb.pool.tensor_copy(out=u_b[:, 0:2], in_=carry[:, c, :])
                    kb.dve.tensor_scalar(out=a[:, :], in0=u[:, 0:TB], scalar1=cw[:, c, 2:3], scalar2=cw[:, c, 3:4],
                                         op0=ALU.mult, op1=ALU.add)
                    kb.dve.scalar_tensor_tensor(out=a[:, :], in0=u_b[:, 1:TB + 1], scalar=cw[:, c, 1:2], in1=a[:, :],
                                                op0=ALU.mult, op1=ALU.add)
                    kb.dve.scalar_tensor_tensor(out=a[:, :], in0=u_b[:, 0:TB], scalar=cw[:, c, 0:1], in1=a[:, :],
                                                op0=ALU.mult, op1=ALU.add)
                    kb.pool.tensor_copy(out=carry[:, c, :], in_=u_b[:, TB:TB + 2])
                    res.append(a)
                kb.act.activation(out=res[0][:, :], in_=res[0][:, :], func=AF.Silu)
                kb.pool.tensor_tensor(out=Gb[:, ci, :], in0=res[0][:, :], in1=res[1][:, :], op=ALU.mult)
            for j in range(2):
                for c0 in (0, 512):
                    d = dps[nd % 2]
                    nd += 1
                    for ci in range(22):
                        kb.pe.matmul(out=d[:, :], lhsT=Gb[:, ci, j * 128:(j + 1) * 128], rhs=wd.k(ci)[:, ci, c0:c0 + 512],
                                     start=(ci == 0), stop=(ci == 21))
                    kb.dve.tensor_tensor(out=x_t[:, j, c0:c0 + 512], in0=x_t[:, j, c0:c0 + 512], in1=d[:, :], op=ALU.add)
            kb.dma(out=cx.xres.k(b)[b * TB:(b + 1) * TB, :].with_ap(
                cx.xres.t[b * TB:(b + 1) * TB, :].rearrange("(j p) d -> p j d", p=128)), in_=x_t[:, :, :])
        kb.barrier()


def phase_final(kb, cx, y_out):
    P = cx.P
    with ExitStack() as st:
        gbc = kb.sb(st, "gbcF", [128, D], F32)
        kb.dma(out=gbc[:, :], in_=Ref(P["norm_final"], None, P["norm_final"].t.rearrange("(o d) -> o d", o=1).partition_broadcast(128)))
        xt = [kb.sb(st, f"xtF{i}", [128, D], F32) for i in range(2)]
        ot = [kb.sb(st, f"otF{i}", [128, D], F32) for i in range(2)]
        junk = kb.sb(st, "junkF", [128, D], BF16)
        small = kb.sb(st, "smallF", [128, 16], F32)
        kb.dma(out=xt[0][:, :], in_=cx.xres[0:128, :])
        for i in range(NT):
            x_t, o_t = xt[i % 2], ot[i % 2]
            if i + 1 < NT:
                kb.dma(out=xt[(i + 1) % 2][:, :], in_=cx.xres[(i + 1) * 128:(i + 2) * 128, :])
            kb.act.activation(out=junk[:, :], in_=x_t[:, :], func=AF.Square, accum_out=small[:, 0:1])
            rms_rstd(kb, small[:, 0:1], small[:, 1:2], D, 1e-6, small[:, 2:3])
            kb.dve.scalar_tensor_tensor(out=o_t[:, :], in0=x_t[:, :], scalar=small[:, 1:2], in1=gbc[:, :],
                                        op0=ALU.mult, op1=ALU.mult)
            kb.dma(out=y_out[i * 128:(i + 1) * 128, :], in_=o_t[:, :])
        kb.barrier()


class AttnBufs:
    def __init__(self, kb, st, tag):
        self.sps = [kb.psum(st, f"sps{tag}{i}", [128, 512], F32) for i in range(2)]
        self.acc = kb.psum(st, f"acc{tag}", [128, 4, 512], F32)
        self.pts = [kb.sb(st, f"pts{tag}{i}", [128, 512], BF16) for i in range(3)]
        self.ns = 0
        self.nm = 0


def attn_core(kb, A, q_rhs, kv_list, heads_view=False):
    n = len(kv_list)
    for idx, (kT, Vp, mask) in enumerate(kv_list):
        sp = A.sps[A.ns % 2]
        pt = A.pts[A.ns % 3]
        A.ns += 1
        if heads_view:
            spv = sp[:, :].with_ap(sp.t[:, :].rearrange("p (h q) -> p h q", h=4))
            ptv = pt[:, :].with_ap(pt.t[:, :].rearrange("p (h q) -> p h q", h=4))
        else:
            spv, ptv = sp[:, :], pt[:, :]
        kb.pe.matmul(out=sp[:, :], lhsT=kT, rhs=q_rhs, start=True, stop=True)
        kb.act.activation(out=pt[:, :], in_=sp[:, :], func=AF.Exp, scale=0.125)
        if mask is not None:
            eng = kb.dve if (A.nm % 3 != 2) else kb.pool
            A.nm += 1
            eng.tensor_tensor(out=ptv, in0=ptv, in1=mask, op=ALU.mult)
        for j in range(4):
            kb.pe.matmul(out=A.acc[:, j, 0:65], lhsT=pt[:, j * 128:(j + 1) * 128], rhs=Vp, start=(idx == 0), stop=(idx == n - 1))


def attn_evac(kb, A, small, dst, gate=None, first=True, tmp=None):
    kb.dve.tensor_scalar(out=small[:, 0:4], in0=A.acc[:, :, 64], scalar1=1e-30, scalar2=None, op0=ALU.max)
    kb.dve.reciprocal(out=small[:, 4:8], in_=small[:, 0:4])
    if gate is not None:
        kb.dve.tensor_tensor(out=small[:, 4:8], in0=small[:, 4:8], in1=gate, op=ALU.mult)
    sc = small[:, 4:8].with_ap(small.t[:, 4:8].unsqueeze(2).to_broadcast([128, 4, 64]))
    if first:
        kb.dve.tensor_tensor(out=dst, in0=A.acc[:, :, 0:64], in1=sc, op=ALU.mult)
    else:
        kb.dve.tensor_tensor(out=tmp, in0=A.acc[:, :, 0:64], in1=sc, op=ALU.mult)
        kb.pool.tensor_tensor(out=dst, in0=dst, in1=tmp, op=ALU.add)


def group_rmsnorm_store(kb, ob_ref, beta_bc, small, junk, stage_ref, dram_ref):
    kb.act.activation(out=junk, in_=ob_ref, func=AF.Square, accum_out=small[:, 8:9])
    rms_rstd(kb, small[:, 8:9], small[:, 9:10], 256, 1e-6, small[:, 10:11])
    kb.dve.scalar_tensor_tensor(out=stage_ref, in0=ob_ref, scalar=small[:, 9:10], in1=beta_bc, op0=ALU.mult, op1=ALU.mult)
    kb.dma(out=dram_ref, in_=stage_ref)


def build_vprime(kb, vp, src_dram_cols, ld, nh):
    kb.dma(out=ld[:, :, 0:nh * 64], in_=src_dram_cols.with_ap(src_dram_cols.ap.rearrange("(i p) c -> p i c", p=128)))
    kb.pool.memset(ap=vp[:, :, :, 64:65], constant=1.0)
    for h in range(nh):
        kb.dve.tensor_copy(out=vp[:, :, h, 0:64], in_=ld[:, :, h * 64:(h + 1) * 64])


def phase_dil(kb, cx, l):
    P, scr, C = cx.P, cx.scr, cx.C
    with ExitStack() as st:
        qT = kb.sb(st, "qTd", [64, 4, S], BF16)
        kT = kb.sb(st, "kTd", [64, 4, S], BF16)
        kb.dma(out=qT[:, :, :], in_=scr.dqT[:, :].with_ap(scr.dqT.t.rearrange("(h d) s -> d h s", d=64)))
        kb.dma(out=kT[:, :, :], in_=scr.dkT[:, :].with_ap(scr.dkT.t.rearrange("(h d) s -> d h s", d=64)))
        vp = kb.sb(st, "vpd", [128, NT, 4, 65], BF16)
        with ExitStack() as st2:
            ld = kb.sb(st2, "ldd", [128, NT, 256], F32)
            build_vprime(kb, vp, scr.tmB[:, 0:256], ld, 4)
            kb.barrier()
        dm = kb.sb(st, "dmd", [128, 20, 512], BF16)
        kb.dma(out=dm[:, :, :], in_=C["dm"][:, :, :])
        beta = kb.sb(st, "betad", [128, 256], F32)
        kb.dma(out=beta[:, :], in_=Ref(P["beta_dil"], None, P["beta_dil"].t[l:l + 1, :].partition_broadcast(128)))
        ob = [kb.sb(st, f"obd{i}", [128, 4, 256], F32) for i in range(2)]
        stage = [kb.sb(st, f"stgd{i}", [128, 256], F32) for i in range(2)]
        junk = kb.sb(st, "junkd", [128, 256], BF16)
        small = kb.sb(st, "smalld", [128, 16], F32)
        A = AttnBufs(kb, st, "d")
        for b in range(NBLK):
            o_b = ob[b % 2]
            for h in range(4):
                kv = []
                for kt in range(max(0, 4 * b - 16), 4 * b + 4):
                    delta = 4 * b - kt
                    kv.append((kT[:, h, kt * 128:(kt + 1) * 128], vp[:, kt, h, :], dm[:, delta + 3, :]))
                attn_core(kb, A, qT[:, h, b * 512:(b + 1) * 512], kv)
                attn_evac(kb, A, small, o_b[:, :, h * 64:(h + 1) * 64])
            for qt in range(4):
                i = b * 4 + qt
                group_rmsnorm_store(kb, o_b[:, qt, :], beta[:, :], small, junk[:, :], stage[qt % 2][:, :],
                                    scr.ymix.k(("b", i))[i * 128:(i + 1) * 128, 256:512])
        kb.barrier()
def phase_nsa(kb, cx, l):
    P, scr, C = cx.P, cx.scr, cx.C
    with ExitStack() as st:
        qT = kb.sb(st, "qTn", [64, 4, S], BF16)
        kb.dma(out=qT[:, :, :], in_=scr.qT[:, :].with_ap(scr.qT.t.rearrange("(h d) s -> d h s", d=64)))
        ksT = kb.sb(st, "ksTn", [64, S], BF16)
        kwT = kb.sb(st, "kwTn", [64, S], BF16)
        kb.dma(out=ksT[:, :], in_=scr.ksT[:, :])
        kb.dma(out=kwT[:, :], in_=scr.kwT[:, :])
        vps = kb.sb(st, "vpsn", [128, NT, 1, 65], BF16)
        vpw = kb.sb(st, "vpwn", [128, NT, 1, 65], BF16)
        gts = kb.sb(st, "gtsn", [128, NT, 12], F32)
        kcmpT = kb.sb(st, "kcmpTn", [64, 256], BF16)
        vpc = kb.sb(st, "vpcn", [128, 2, 65], BF16)
        selT = kb.sb(st, "selTn", [64, S], BF16)
        small = kb.sb(st, "smalln", [128, 16], F32)
        A = AttnBufs(kb, st, "n")
        x1 = kb.psum(st, "x1n", [128, 512], F32)
        x2 = kb.psum(st, "x2n", [128, 512], F32)
        with ExitStack() as st2:
            ld = kb.sb(st2, "ldn", [128, NT, 204], F32)
            kb.dma(out=ld[:, :, :], in_=scr.tmA[:, :].with_ap(scr.tmA.t.rearrange("(i p) c -> p i c", p=128)))
            kb.pool.memset(ap=vps[:, :, :, 64:65], constant=1.0)
            kb.pool.memset(ap=vpw[:, :, :, 64:65], constant=1.0)
            kb.dve.tensor_copy(out=vps[:, :, 0, 0:64], in_=ld[:, :, 0:64])
            kb.dve.tensor_copy(out=vpw[:, :, 0, 0:64], in_=ld[:, :, 128:192])
            kb.act.activation(out=gts[:, :, :], in_=ld[:, :, 192:204], func=AF.Sigmoid)
            x2t = kb.sb(st2, "x2n_", [128, S], BF16)
            pos2 = kb.sb(st2, "pos2n", [128, 16], F32)
            w1 = kb.sb(st2, "w1n", [128, 16, 128], BF16)
            w2 = kb.sb(st2, "w2n", [128, 64], BF16)
            am = kb.sb(st2, "amn", [128, 16, 256], BF16)
            hidT = kb.sb(st2, "hidTn", [128, 256], BF16)
            with kb.nc.allow_non_contiguous_dma(reason="tiny pos table"):
                kb.dma(out=pos2[:, :], in_=Ref(P["cmp_pos"], None, P["cmp_pos"].t[l].rearrange("(m i) d -> (i d) m", i=2)))
            for kind in ("k", "v"):
                r0 = 0 if kind == "k" else 64
                kb.pool.memset(ap=x2t[:, S - 1:S], constant=0.0)
                kb.dma(out=x2t[0:64, :], in_=scr.kcvcT[r0:r0 + 64, :])
                kb.dma(out=x2t[64:128, 0:S - 1], in_=scr.kcvcT[r0:r0 + 64, 1:S])
                wn1 = P["cmp_k_w1" if kind == "k" else "cmp_v_w1"]
                wn2 = P["cmp_k_w2" if kind == "k" else "cmp_v_w2"]
                kb.dma(out=w1[:, :, :], in_=wn1[l].with_ap(wn1.t[l].rearrange("(m p) h -> p m h", p=128)), via_pool=True)
                kb.dma(out=w2[:, :], in_=wn2[l, :, :], via_pool=True)
                kb.pool.memset(ap=am[:, :, 255:256], constant=0.0)
                xv = x2t.t[:, :].rearrange("p (c r) -> p c r", r=16)
                for m in range(16):
                    src = xv[:, 0:255, 2 * m] if m < 8 else xv[:, 1:256, 2 * m - 16]
                    kb.dve.tensor_scalar(out=am[:, m, 0:255], in0=Ref(x2t, None, src), scalar1=pos2[:, m:m + 1], scalar2=None, op0=ALU.add)
                for m in range(16):
                    kb.pe.matmul(out=x1[:, 0:256], lhsT=w1[:, m, :], rhs=am[:, m, :], start=(m == 0), stop=(m == 15))
                kb.act.activation(out=hidT[:, :], in_=x1[:, 0:256], func=AF.Silu)
                if kind == "k":
                    kb.pe.matmul(out=x2[0:64, 0:256], lhsT=w2[:, :], rhs=hidT[:, :], start=True, stop=True)
                    kb.dve.tensor_copy(out=kcmpT[:, :], in_=x2[0:64, 0:256])
                else:
                    kb.pool.memset(ap=vpc[:, :, 64:65], constant=1.0)
                    for ct in range(2):
                        kb.pe.matmul(out=x2[:, ct * 64:(ct + 1) * 64], lhsT=hidT[:, ct * 128:(ct + 1) * 128], rhs=w2[:, :], start=True, stop=True)
                    kb.dve.tensor_copy(out=vpc[:, :, 0:64], in_=x2[:, 0:128].with_ap(x2.t[:, 0:128].rearrange("p (c d) -> p c d", c=2)))
            kb.barrier()
        with ExitStack() as st3:
            gc = kb.sb(st3, "gcn", [128, 256], F32)
            d0 = kb.sb(st3, "d0n", [128, 64], F32)
            kb.dma(out=gc[:, :], in_=C["gc"][:, :])
            kb.dma(out=d0[:, :], in_=C["d0"][:, :])
            pex = [kb.sb(st3, f"pexn{i}", [128, 4, 256], F32) for i in range(2)]
            imp = [kb.sb(st3, f"impn{i}", [128, 258], F32) for i in range(2)]
            chk = kb.sb(st3, "chkn", [128, 256], F32)
            blk = kb.sb(st3, "blkn", [128, 64], F32)
            sc = kb.sb(st3, "scn", [128, 64], F32)
            sc2 = kb.sb(st3, "sc2n", [128, 64], F32)
            vm = kb.sb(st3, "vmn", [128, 64], F32)
            m8 = kb.sb(st3, "m8n", [128, 16], F32)
            sel = [kb.sb(st3, f"seln{i}", [128, 64], BF16) for i in range(2)]
            for i in range(2):
                kb.pool.memset(ap=imp[i][:, :], constant=0.0)
            scps = [A.acc.k(0), A.acc.k(1)]
            for i in range(NT):
                pe_, im = pex[i % 2], imp[i % 2]
                base = (i % 2) * 2
                scv = A.acc.k(i % 2)[:, base:base + 2, :].with_ap(
                    A.acc.t[:, base:base + 2, :].rearrange("p b (h c) -> p (b h) c", h=2))
                for h in range(4):
                    kb.pe.matmul(out=A.acc.k(i % 2)[:, base + h // 2, (h % 2) * 256:(h % 2) * 256 + 256],
                                 lhsT=qT[:, h, i * 128:(i + 1) * 128], rhs=kcmpT[:, :], start=True, stop=True)
                kb.act.activation(out=pe_[:, :, :], in_=scv, func=AF.Exp, scale=0.125)
                gcb = Ref(gc, None, gc.t[:, :].unsqueeze(1).to_broadcast([128, 4, 256]))
                kb.dve.scalar_tensor_tensor(out=pe_[:, :, :], in0=gcb, scalar=float(128 * i), in1=pe_[:, :, :],
                                            op0=ALU.is_le, op1=ALU.mult)
                kb.dve.tensor_reduce(out=small[:, 0:4], in_=pe_[:, :, :], axis=AX.X, op=ALU.add)
                kb.dve.tensor_scalar(out=small[:, 0:4], in0=small[:, 0:4], scalar1=1e-30, scalar2=None, op0=ALU.max)
                kb.dve.reciprocal(out=small[:, 4:8], in_=small[:, 0:4])
                kb.dve.tensor_scalar(out=im[:, 1:257], in0=pe_[:, 0, :], scalar1=small[:, 4:5], scalar2=None, op0=ALU.mult)
                for h in range(1, 4):
                    kb.dve.scalar_tensor_tensor(out=im[:, 1:257], in0=pe_[:, h, :], scalar=small[:, 4 + h:5 + h], in1=im[:, 1:257],
                                                op0=ALU.mult, op1=ALU.add)
                kb.dve.tensor_tensor(out=chk[:, :], in0=im[:, 0:256], in1=im[:, 1:257], op=ALU.add)
                kb.dve.tensor_reduce(out=blk[:, :], in_=chk[:, :].with_ap(chk.t[:, :].rearrange("p (b r) -> p b r", r=4)),
                                     axis=AX.X, op=ALU.add)
                kb.dve.tensor_scalar(out=sc[:, :], in0=d0[:, :], scalar1=float(128 * i - 127), scalar2=1e9, op0=ALU.is_ge, op1=ALU.mult)
                kb.dve.tensor_tensor(out=sc[:, :], in0=sc[:, :], in1=blk[:, :], op=ALU.max)
                kb.dve.memset(ap=sc[:, 0:1], constant=1e9)
                kb.dve.tensor_single_scalar(out=vm[:, :], in_=d0[:, :], scalar=float(128 * i), op=ALU.is_le)
                kb.dve.tensor_tensor(out=sc[:, :], in0=sc[:, :], in1=vm[:, :], op=ALU.mult)
                kb.dve.scalar_tensor_tensor(out=sc[:, :], in0=vm[:, :], scalar=-1.0, in1=sc[:, :], op0=ALU.add, op1=ALU.add)
                kb.dve.max(out=m8[:, 0:8], in_=sc[:, :])
                kb.dve.match_replace(out=sc2[:, :], in_to_replace=m8[:, 0:8], in_values=sc[:, :], imm_value=-3e38)
                kb.dve.max(out=m8[:, 8:16], in_=sc2[:, :])
                kb.dve.tensor_scalar(out=sel[i % 2][:, :], in0=sc[:, :], scalar1=m8[:, 15:16], scalar2=None, op0=ALU.is_ge)
                tpv = x1[0:64, 0:64].with_ap(x1.t[0:64, 0:64].bitcast(BF16))
                kb.pe.transpose(out=tpv, in_=sel[i % 2][:, :], identity=cx.ident_bf[:, :])
                kb.act.copy(out=selT[:, i * 128:(i + 1) * 128], in_=tpv)
            kb.barrier()
        with ExitStack() as st4:
            maskS = kb.sb(st4, "maskSn", [128, NT, 512], BF16)
            mc = kb.sb(st4, "mcn", [128, 16, 512], BF16)
            cms = kb.sb(st4, "cmsn", [128, 4, 512], BF16)
            cmw = kb.sb(st4, "cmwn", [128, 2, 128], BF16)
            ek = kb.sb(st4, "ekn", [64, 32, 128], BF16)
            kb.dma(out=mc[:, :, :], in_=C["mc"][:, :, :])
            kb.dma(out=cms[:, :, :], in_=C["cms"][:, :, :])
            kb.dma(out=cmw[:, :, :], in_=C["cmw"][:, :, :])
            kb.dma(out=ek[:, :, :], in_=C["ek"][:, :, :])
            beta = kb.sb(st4, "betan", [128, 256], F32)
            kb.dma(out=beta[:, :], in_=Ref(P["beta_nsa"], None, P["beta_nsa"].t[l:l + 1, :].partition_broadcast(128)))
            ob = [kb.sb(st4, f"obn{i}", [128, 4, 256], F32) for i in range(2)]
            tmp = kb.sb(st4, "tmpn", [128, 4, 64], F32)
            stage = [kb.sb(st4, f"stgn{i}", [128, 256], F32) for i in range(2)]
            junk = kb.sb(st4, "junkn", [128, 256], BF16)
            xs = [x1, x2]
            nx = 0
            for b in range(NBLK):
                o_b = ob[b % 2]
                for kt in range(4 * b + 4):
                    xp = xs[nx % 2]
                    nx += 1
                    kb.pe.matmul(out=xp[:, :], lhsT=ek[:, kt, :], rhs=selT[:, b * 512:(b + 1) * 512], start=True, stop=True)
                    if kt >= 4 * b:
                        kb.dve.tensor_tensor(out=maskS[:, kt, :], in0=xp[:, :], in1=cms[:, kt - 4 * b, :], op=ALU.mult)
                    else:
                        kb.act.copy(out=maskS[:, kt, :], in_=xp[:, :])
                for h in range(4):
                    dst = o_b[:, :, h * 64:(h + 1) * 64]
                    kv = [(kcmpT[:, 0:128], vpc[:, 0, :], None if b >= 5 else mc[:, 2 * b, :])]
                    if b >= 4:
                        kv.append((kcmpT[:, 128:256], vpc[:, 1, :], mc[:, 2 * b + 1, :]))
                    attn_core(kb, A, qT[:, h, b * 512:(b + 1) * 512], kv)
                    attn_evac(kb, A, small, dst, gate=gts[:, 4 * b:4 * b + 4, 3 * h], first=True)
                    kv = [(ksT[:, kt * 128:(kt + 1) * 128], vps[:, kt, 0, :], maskS[:, kt, :]) for kt in range(4 * b + 4)]
                    attn_core(kb, A, qT[:, h, b * 512:(b + 1) * 512], kv)
                    attn_evac(kb, A, small, dst, gate=gts[:, 4 * b:4 * b + 4, 3 * h + 1], first=False, tmp=tmp[:, :, :])
                for qt in range(4):
                    i = 4 * b + qt
                    kv = []
                    for kt in range(max(0, i - 4), i + 1):
                        mk = None
                        if kt == i:
                            mk = Ref(cmw, None, cmw.t[:, 0:1, :].to_broadcast([128, 4, 128]))
                        elif kt == i - 4:
                            mk = Ref(cmw, None, cmw.t[:, 1:2, :].to_broadcast([128, 4, 128]))
                        kv.append((kwT[:, kt * 128:(kt + 1) * 128], vpw[:, kt, 0, :], mk))
                    attn_core(kb, A, qT[:, :, i * 128:(i + 1) * 128], kv, heads_view=True)
                    dstw = o_b[:, qt, :].with_ap(o_b.t[:, qt, :].rearrange("p (h d) -> p h d", h=4))
                    gw = gts[:, i, :].with_ap(gts.t[:, i, :].rearrange("p (h r) -> p h r", r=3)[:, :, 2])
                    attn_evac(kb, A, small, dstw, gate=gw, first=False, tmp=tmp[:, :, :])
                for qt in range(4):
                    i = b * 4 + qt
                    group_rmsnorm_store(kb, o_b[:, qt, :], beta[:, :], small, junk[:, :], stage[qt % 2][:, :],
                                        scr.ymix.k(("a", i))[i * 128:(i + 1) * 128, 0:256])
            kb.barrier()
C0 = 0.6065306597126334
RWKV_STAGE = [99]


def phase_rwkv(kb, cx, l):
    P, scr, C = cx.P, cx.scr, cx.C
    CH = 64
    NBUF = 3
    with ExitStack() as st:
        def bc(name, src, c0, n, rows=64):
            t = kb.sb(st, name, [rows, n], F32)
            kb.dma(out=t[:, :], in_=Ref(P[src], None, P[src].t[l:l + 1, c0:c0 + n].partition_broadcast(rows)))
            return t
        mu_bc = bc("mu_r", "rwkv_mu", 0, 768)
        w0_bc = bc("w0_r", "rwkv_w0", 0, 256)
        a0_bc = bc("a0_r", "rwkv_a0", 0, 256)
        kk_bc = bc("kk_r", "rwkv_k_k", 0, 256)
        ka_bc = bc("ka_r", "rwkv_k_a", 0, 256)
        lg_bc = bc("lg_r", "rwkv_ln_g", 0, 256)
        lb_bc = bc("lb_r", "rwkv_ln_b", 0, 256)
        rk_bc = kb.sb(st, "rk_r", [64, 256], F32)
        kb.dma(out=rk_bc[:, :], in_=Ref(P["rwkv_r_k"], None,
               P["rwkv_r_k"].t[l:l + 1].rearrange("o h d -> o (h d)").partition_broadcast(64)))
        oka = kb.sb(st, "oka_r", [64, 256], F32)
        kb.dve.tensor_scalar(out=oka[:, :], in0=ka_bc[:, :], scalar1=-1.0, scalar2=1.0, op0=ALU.mult, op1=ALU.add)
        mu_lo = kb.sb(st, "mulo_r", [128, 2], F32)
        with kb.nc.allow_non_contiguous_dma(reason="tiny mu columns"):
            kb.dma(out=mu_lo[:, 0:1], in_=Ref(P["rwkv_mu"], None, P["rwkv_mu"].t[l:l + 1, 768:896].rearrange("o c -> c o")))
            kb.dma(out=mu_lo[:, 1:2], in_=Ref(P["rwkv_mu"], None, P["rwkv_mu"].t[l:l + 1, 896:1024].rearrange("o c -> c o")))
        wa_up = kb.sb(st, "waup_r", [64, 256], F32)
        a_up = kb.sb(st, "aup_r", [64, 256], F32)
        g_up = kb.sb(st, "gup_r", [128, 256], F32)
        mu_a = kb.sb(st, "mua_r", [64, 1], F32)
        with kb.nc.allow_non_contiguous_dma(reason="tiny mu columns"):
            kb.dma(out=mu_a[:, 0:1], in_=Ref(P["rwkv_mu"], None, P["rwkv_mu"].t[l:l + 1, 832:896].rearrange("o c -> c o")))
        kb.dma(out=wa_up[0:64, :], in_=P["rwkv_w_up"][l, :, :])
        kb.dma(out=a_up[0:64, :], in_=P["rwkv_a_up"][l, :, :])
        kb.dma(out=g_up[:, :], in_=P["rwkv_g_up"][l, :, :])
        tri = kb.sb(st, "tri_r", [64, 2, 64], F32)
        kb.dma(out=tri[:, 0, :], in_=C["tri"][0:64, 0:64])
        kb.dma(out=tri[:, 1, :], in_=C["mstrictT"][:, 0, :])
        mst = kb.sb(st, "mst_r", [64, 4, 64], F32)
        mstT = kb.sb(st, "mstT_r", [64, 4, 64], F32)
        minc = kb.sb(st, "minc_r", [64, 4, 64], F32)
        kb.dma(out=mst[:, :, :], in_=C["mstrict"][:, 0:4, :])
        kb.dma(out=mstT[:, :, :], in_=C["mstrictT"][:, 0:4, :])
        kb.dma(out=minc[:, :, :], in_=C["mincl"][:, 0:4, :])
        ST = kb.sb(st, "ST_r", [64, 4, 64], F32)
        kb.pool.memset(ap=ST[:, :, :], constant=0.0)
        idb = Ref(cx.ident_f, None, cx.ident_f.t[0:64, 0:64].unsqueeze(1).to_broadcast([64, 4, 64]))

        def T2(name, shape, n=3):
            return [kb.sb(st, f"{name}{i}_r", shape, F32) for i in range(n)]
        rkv, prv, lo, gd = T2("rkv", [64, 768]), T2("prv", [64, 768]), T2("lo", [64, 65]), T2("gd", [128, 65])
        los, gds = T2("los", [64, 64]), T2("gds", [128, 64])
        loa, loas = T2("loa", [64, 65]), T2("loas", [64, 64])
        sgt, a_t, g_t = T2("sgt", [64, 256]), T2("at", [64, 256]), T2("gt", [64, 256])
        kk, k2, bb = T2("kkt", [64, 256]), T2("k2t", [64, 256]), T2("bbt", [64, 256])
        tmp, tmp2 = T2("tmp", [64, 256]), T2("tmp2", [64, 256])
        Pm, iP, Pp, Pr = T2("Pm", [64, 256]), T2("iP", [64, 256]), T2("Pp", [64, 256]), T2("Pr", [64, 256])
        def TB2(name, shape):
            return [kb.sb(st, f"{name}{i}_r", shape, BF16) for i in range(NBUF)]
        Kt, Bt, KKt, Rtb = TB2("Kt", [64, 256]), TB2("Bt", [64, 256]), TB2("KKt", [64, 256]), TB2("Rtb", [64, 256])
        Rt, Kh, Bh = T2("Rt", [64, 256]), T2("Kh", [64, 256]), T2("Bh", [64, 256])
        vb = TB2("vb", [64, 256])
        FMq = TB2("FMq", [64, 16, 64])
        FMr = T2("FMr", [64, 4, 64])
        Abr, Akr = T2("Abr", [64, 4, 64]), T2("Akr", [64, 4, 64])
        Mak = [kb.sb(st, f"Makb{i}_r", [64, 4, 64], BF16) for i in range(NBUF)]
        NT_ = [kb.sb(st, f"NTb{i}_r", [64, 4, 64], BF16) for i in range(NBUF)]
        TA_ = [kb.sb(st, f"TAb{i}_r", [64, 8, 64], BF16) for i in range(NBUF)]
        Tf = T2("Tf", [64, 4, 64])
        Wsb, U0T, UT = T2("Wsb", [64, 4, 64]), T2("U0T", [64, 4, 64]), T2("UT", [64, 4, 64])
        Xsb = [kb.sb(st, f"Xsbb{i}_r", [64, 4, 64], BF16) for i in range(NBUF)]
        pcs = T2("pcs", [64, 4])
        yv = T2("yv", [64, 256])
        small = T2("small", [64, 16])
        B = [kb.psum(st, f"B{i}_r", [64, 512], F32) for i in range(8)]

        def v3(ref_tile, c0):
            return ref_tile[:, c0:c0 + 256].with_ap(ref_tile.t[:, c0:c0 + 256].rearrange("p (h d) -> p h d", h=4))

        def hb(t_small, c0):
            return t_small[:, c0:c0 + 4].with_ap(t_small.t[:, c0:c0 + 4].unsqueeze(2).to_broadcast([64, 4, 64]))

        def chunk(c):
            p = c % NBUF
            t0 = c * CH
            kb.dma(out=rkv[p][:, :], in_=scr.tmB[t0:t0 + CH, 256:1024])
            if c == 0:
                kb.pool.memset(ap=prv[p][0:1, :], constant=0.0)
                kb.dma(out=prv[p][1:CH, :], in_=scr.tmB[0:CH - 1, 256:1024])
                kb.pool.memset(ap=lo[p][:, 0:1], constant=0.0)
                kb.pool.memset(ap=gd[p][:, 0:1], constant=0.0)
                kb.dma(out=lo[p][:, 1:65], in_=scr.loraT[0:64, 0:CH])
                kb.pool.memset(ap=loa[p][:, 0:1], constant=0.0)
                kb.dma(out=loa[p][:, 1:65], in_=scr.loraT[64:128, 0:CH])
                kb.dma(out=gd[p][:, 1:65], in_=scr.loraT[128:256, 0:CH])
            else:
                kb.dma(out=prv[p][:, :], in_=scr.tmB[t0 - 1:t0 + CH - 1, 256:1024])
                kb.dma(out=lo[p][:, :], in_=scr.loraT[0:64, t0 - 1:t0 + CH])
                kb.dma(out=loa[p][:, :], in_=scr.loraT[64:128, t0 - 1:t0 + CH])
                kb.dma(out=gd[p][:, :], in_=scr.loraT[128:256, t0 - 1:t0 + CH])
            kb.dve.tensor_tensor(out=los[p][:, :], in0=lo[p][:, 0:64], in1=lo[p][:, 1:65], op=ALU.subtract)
            kb.dve.scalar_tensor_tensor(out=los[p][:, :], in0=los[p][:, :], scalar=mu_lo[0:64, 0:1], in1=lo[p][:, 1:65], op0=ALU.mult, op1=ALU.add)
            kb.dve.tensor_tensor(out=loas[p][:, :], in0=loa[p][:, 0:64], in1=loa[p][:, 1:65], op=ALU.subtract)
            kb.dve.scalar_tensor_tensor(out=loas[p][:, :], in0=loas[p][:, :], scalar=mu_a[:, 0:1], in1=loa[p][:, 1:65], op0=ALU.mult, op1=ALU.add)
            kb.dve.tensor_tensor(out=gds[p][:, :], in0=gd[p][:, 0:64], in1=gd[p][:, 1:65], op=ALU.subtract)
            kb.dve.scalar_tensor_tensor(out=gds[p][:, :], in0=gds[p][:, :], scalar=mu_lo[:, 1:2], in1=gd[p][:, 1:65], op0=ALU.mult, op1=ALU.add)
            kb.act.activation(out=los[p][0:64, :], in_=los[p][0:64, :], func=AF.Tanh)
            kb.act.activation(out=gds[p][:, :], in_=gds[p][:, :], func=AF.Sigmoid)
            kb.pe.matmul(out=B[0][:, 0:256], lhsT=los[p][0:64, :], rhs=wa_up[0:64, :], start=True, stop=True)
            kb.pe.matmul(out=B[0][:, 256:512], lhsT=loas[p][:, :], rhs=a_up[:, :], start=True, stop=True)
            kb.pe.matmul(out=B[1][:, 0:256], lhsT=gds[p][:, :], rhs=g_up[:, :], start=True, stop=True)
            kb.dve.tensor_tensor(out=sgt[p][:, :], in0=B[0][:, 0:256], in1=w0_bc[:, :], op=ALU.add)
            kb.act.activation(out=sgt[p][:, :], in_=sgt[p][:, :], func=AF.Sigmoid)
            kb.dve.tensor_tensor(out=a_t[p][:, :], in0=B[0][:, 256:512], in1=a0_bc[:, :], op=ALU.add)
            kb.act.activation(out=a_t[p][:, :], in_=a_t[p][:, :], func=AF.Sigmoid)
            kb.act.copy(out=g_t[p][:, :], in_=B[1][:, 0:256])
            kb.pool.tensor_tensor(out=prv[p][:, :], in0=prv[p][:, :], in1=rkv[p][:, :], op=ALU.subtract)
            kb.pool.tensor_tensor(out=prv[p][:, :], in0=prv[p][:, :], in1=mu_bc[:, :], op=ALU.mult)
            kb.dve.tensor_tensor(out=rkv[p][:, :], in0=rkv[p][:, :], in1=prv[p][:, :], op=ALU.add)
            yield
            r_, k_, v_ = rkv[p][:, 0:256], rkv[p][:, 256:512], rkv[p][:, 512:768]
            kb.dve.tensor_tensor(out=kk[p][:, :], in0=k_, in1=kk_bc[:, :], op=ALU.mult)
            kb.pool.tensor_tensor(out=tmp[p][:, :], in0=kk[p][:, :], in1=kk[p][:, :], op=ALU.mult)
            kb.dve.tensor_reduce(out=small[p][:, 0:4], in_=v3(tmp[p], 0), axis=AX.X, op=ALU.add)
            kb.dve.tensor_scalar(out=small[p][:, 0:4], in0=small[p][:, 0:4], scalar1=1e-12, scalar2=None, op0=ALU.add)
            kb.act.activation(out=small[p][:, 0:4], in_=small[p][:, 0:4], func=AF.Sqrt)
            kb.dve.reciprocal(out=small[p][:, 4:8], in_=small[p][:, 0:4])
            kb.dve.tensor_tensor(out=v3(kk[p], 0), in0=v3(kk[p], 0), in1=hb(small[p], 4), op=ALU.mult)
            kb.pool.tensor_tensor(out=tmp[p][:, :], in0=a_t[p][:, :], in1=ka_bc[:, :], op=ALU.mult)
            kb.pool.tensor_tensor(out=tmp[p][:, :], in0=tmp[p][:, :], in1=oka[:, :], op=ALU.add)
            kb.dve.tensor_tensor(out=k2[p][:, :], in0=k_, in1=tmp[p][:, :], op=ALU.mult)
            kb.pool.tensor_tensor(out=bb[p][:, :], in0=kk[p][:, :], in1=a_t[p][:, :], op=ALU.mult)
            yield
            kb.pe.matmul(out=B[2][:, 0:256], lhsT=tri[:, 0, :], rhs=sgt[p][:, :], start=True, stop=True)
            kb.pe.matmul(out=B[2][:, 256:512], lhsT=tri[:, 1, :], rhs=sgt[p][:, :], start=True, stop=True)
            kb.act.activation(out=Pm[p][:, :], in_=B[2][:, 0:256], func=AF.Exp, scale=-C0)
            kb.act.activation(out=iP[p][:, :], in_=B[2][:, 0:256], func=AF.Exp, scale=C0)
            kb.dve.tensor_tensor(out=tmp2[p][:, :], in0=B[2][:, 0:256], in1=sgt[p][:, :], op=ALU.subtract)
            kb.act.activation(out=Pp[p][:, :], in_=tmp2[p][:, :], func=AF.Exp, scale=-C0)
            kb.act.activation(out=Pr[p][:, :], in_=B[2][:, 256:512], func=AF.Exp, scale=-C0)
            kb.dve.tensor_tensor(out=Kt[p][:, :], in0=k2[p][:, :], in1=iP[p][:, :], op=ALU.mult)
            kb.pool.tensor_tensor(out=Bt[p][:, :], in0=bb[p][:, :], in1=iP[p][:, :], op=ALU.mult)
            kb.dve.tensor_tensor(out=KKt[p][:, :], in0=kk[p][:, :], in1=Pp[p][:, :], op=ALU.mult)
            kb.pool.tensor_tensor(out=Rt[p][:, :], in0=r_, in1=Pm[p][:, :], op=ALU.mult)
            kb.act.copy(out=Rtb[p][:, :], in_=Rt[p][:, :])
            kb.act.copy(out=vb[p][:, :], in_=v_)
            kb.dve.tensor_tensor(out=Kh[p][:, :], in0=k2[p][:, :], in1=Pr[p][:, :], op=ALU.mult)
            kb.pool.tensor_tensor(out=Bh[p][:, :], in0=bb[p][:, :], in1=Pr[p][:, :], op=ALU.mult)
            yield
            for h in range(4):
                kb.pe.matmul(out=B[1][:, 256 + 2 * h:258 + 2 * h], lhsT=Pm[p][:, h * 64:(h + 1) * 64], rhs=cx.ident_f[0:64, 62:64], start=True, stop=True)
            kb.act.copy(out=pcs[p][:, :], in_=B[1][:, 256:264].with_ap(B[1].t[:, 256:264].rearrange("p (h two) -> p h two", two=2)[:, :, 1]))
            yield
            b3bf = B[3].t[:, :].bitcast(BF16)
            for qi, q in enumerate((Bt, Kt, KKt, Rtb)):
                for h in range(4):
                    idx = qi * 4 + h
                    kb.pe.transpose(out=Ref(B[3], None, b3bf[:, idx * 64:(idx + 1) * 64]), in_=q[p][:, h * 64:(h + 1) * 64], identity=cx.ident_bf[0:64, 0:64])
            for h in range(4):
                kb.pe.transpose(out=B[4][:, h * 64:(h + 1) * 64], in_=Rt[p][:, h * 64:(h + 1) * 64], identity=cx.ident_f[0:64, 0:64])
            fm = FMq[p]
            kb.act.copy(out=fm[:, :, :], in_=Ref(B[3], None, b3bf.rearrange("p (a b) -> p a b", b=64)))
            kb.dve.tensor_copy(out=FMr[p][:, :, :], in_=B[4][:, 0:256].with_ap(B[4].t[:, 0:256].rearrange("p (a b) -> p a b", b=64)))
            BT = lambda h: fm[:, 0 + h, :]
            KT = lambda h: fm[:, 4 + h, :]
            KKT = lambda h: fm[:, 8 + h, :]
            RT = lambda h: FMr[p][:, h, :]
            fm4 = fm.t.rearrange("p (q h) t -> p q h t", q=4)
            for h in range(4):
                kkr = Ref(fm, None, fm4[:, 2:4, h, :])
                o5 = B[5][:, h * 128:(h + 1) * 128]
                o6 = B[6][:, h * 128:(h + 1) * 128]
                kb.pe.matmul(out=o5, lhsT=BT(h), rhs=kkr, start=True, stop=True)
                kb.pe.matmul(out=o6, lhsT=KT(h), rhs=kkr, start=True, stop=True)
                kb.pe.matmul(out=B[7][:, h * 64:(h + 1) * 64], lhsT=KKT(h), rhs=BT(h), start=True, stop=True)
            hw = lambda bk, w: Ref(bk, None, bk.t.rearrange("p (h w t) -> p h w t", h=4, w=2)[:, :, w, :])
            b3 = lambda bk, c0: bk[:, c0:c0 + 256].with_ap(bk.t[:, c0:c0 + 256].rearrange("p (h d) -> p h d", h=4))
            TAv = TA_[p].t.rearrange("p (h w) t -> p h w t", w=2)
            TA2 = TA_[p].t.rearrange("p a t -> p (a t)")
            Tv = Ref(TA_[p], None, TAv[:, :, 0, :])
            Av = Ref(TA_[p], None, TAv[:, :, 1, :])
            kb.dve.scalar_tensor_tensor(out=Av, in0=hw(B[5], 0), scalar=-1.0, in1=mst[:, :, :], op0=ALU.mult, op1=ALU.mult)
            kb.dve.scalar_tensor_tensor(out=NT_[p][:, :, :], in0=b3(B[7], 0), scalar=-1.0, in1=mstT[:, :, :], op0=ALU.mult, op1=ALU.mult)
            kb.dve.tensor_tensor(out=Mak[p][:, :, :], in0=hw(B[6], 0), in1=mst[:, :, :], op=ALU.mult)
            kb.dve.tensor_tensor(out=Abr[p][:, :, :], in0=hw(B[5], 1), in1=minc[:, :, :], op=ALU.mult)
            kb.dve.tensor_tensor(out=Akr[p][:, :, :], in0=hw(B[6], 1), in1=minc[:, :, :], op=ALU.mult)
            yield
            kb.pool.tensor_copy(out=Tv, in_=idb)
            AT_ = NT_[p]
            B5v = B[5].t.rearrange("p (h w t) -> p h w t", h=4, w=2)
            for j in range(6):
                last = (j == 5)
                for h in range(4):
                    if last:
                        kb.pe.matmul(out=B[5][:, h * 128:h * 128 + 64], lhsT=AT_[:, h, :], rhs=Ref(TA_[p], None, TAv[:, h, 0, :]), start=True, stop=True)
                    else:
                        kb.pe.matmul(out=B[5][:, h * 128:(h + 1) * 128], lhsT=AT_[:, h, :], rhs=Ref(TA_[p], None, TA2[:, h * 128:(h + 1) * 128]), start=True, stop=True)
                        kb.pe.matmul(out=B[6][:, h * 64:(h + 1) * 64], lhsT=Ref(TA_[p], None, TAv[:, h, 1, :]), rhs=AT_[:, h, :], start=True, stop=True)
                kb.dve.tensor_tensor(out=Tv, in0=Tv, in1=Ref(B[5], None, B5v[:, :, 0, :]), op=ALU.add)
                if not last:
                    kb.act.copy(out=Av, in_=Ref(B[5], None, B5v[:, :, 1, :]))
                    kb.dve.tensor_copy(out=AT_[:, :, :], in_=b3(B[6], 0))
                yield
            yield
            for h in range(4):
                kb.pe.matmul(out=B[7][:, 256 + h * 64:256 + (h + 1) * 64], lhsT=KKt[p][:, h * 64:(h + 1) * 64], rhs=Ref(TA_[p], None, TAv[:, h, 0, :]), start=True, stop=True)
                kb.pe.matmul(out=B[3][:, h * 64:(h + 1) * 64], lhsT=Mak[p][:, h, :], rhs=vb[p][:, h * 64:(h + 1) * 64], start=True, stop=True)
            kb.act.copy(out=Wsb[p][:, :, :], in_=b3(B[7], 256))
            kb.dve.tensor_copy(out=Xsb[p][:, :, :], in_=b3(B[3], 0))
            for h in range(4):
                kb.pe.matmul(out=B[3][:, 256 + h * 64:256 + (h + 1) * 64], lhsT=Ref(TA_[p], None, TAv[:, h, 0, :]), rhs=Xsb[p][:, h, :], start=True, stop=True)
            kb.act.activation(out=U0T[p][:, :, :], in_=b3(B[3], 256), func=AF.Copy, scale=-1.0)
            yield
            for h in range(4):
                vh = rkv[p][:, 512 + h * 64:512 + (h + 1) * 64]
                kb.pe.matmul(out=B[4][:, h * 64:(h + 1) * 64], lhsT=Wsb[p][:, h, :], rhs=ST[:, h, :], start=True, stop=True)
                kb.dve.tensor_tensor(out=UT[p][:, h, :], in0=U0T[p][:, h, :], in1=B[4][:, h * 64:(h + 1) * 64], op=ALU.subtract)
                yo = B[4][:, 256 + h * 64:256 + (h + 1) * 64]
                kb.pe.matmul(out=yo, lhsT=RT(h), rhs=ST[:, h, :], start=True, stop=False)
                kb.pe.matmul(out=yo, lhsT=Abr[p][:, h, :], rhs=UT[p][:, h, :], start=False, stop=False)
                kb.pe.matmul(out=yo, lhsT=Akr[p][:, h, :], rhs=vh, start=False, stop=True)
                kb.act.copy(out=yv[p][:, h * 64:(h + 1) * 64], in_=yo)
                so = B[2][:, h * 64:(h + 1) * 64]
                kb.pe.matmul(out=so, lhsT=Bh[p][:, h * 64:(h + 1) * 64], rhs=UT[p][:, h, :], start=True, stop=False)
                kb.pe.matmul(out=so, lhsT=Kh[p][:, h * 64:(h + 1) * 64], rhs=vh, start=False, stop=True)
                kb.dve.scalar_tensor_tensor(out=ST[:, h, :], in0=ST[:, h, :], scalar=pcs[p][:, h:h + 1], in1=so, op0=ALU.mult, op1=ALU.add)
                yield
            yield
            y = yv[p]
            kb.pool.tensor_tensor(out=tmp[p][:, :], in0=r_, in1=k2[p][:, :], op=ALU.mult)
            kb.pool.tensor_tensor(out=tmp[p][:, :], in0=tmp[p][:, :], in1=rk_bc[:, :], op=ALU.mult)
            kb.dve.tensor_reduce(out=small[p][:, 8:12], in_=v3(tmp[p], 0), axis=AX.X, op=ALU.add)
            kb.dve.tensor_tensor(out=v3(tmp2[p], 0), in0=v3(rkv[p], 512), in1=hb(small[p], 8), op=ALU.mult)
            kb.dve.tensor_tensor(out=y[:, :], in0=tmp2[p][:, :], in1=y[:, :], op=ALU.add)
            kb.dve.tensor_reduce(out=small[p][:, 0:4], in_=v3(y, 0), axis=AX.X, op=ALU.add)
            kb.dve.tensor_scalar(out=small[p][:, 0:4], in0=small[p][:, 0:4], scalar1=1.0 / 64, scalar2=None, op0=ALU.mult)
            kb.dve.tensor_tensor(out=v3(y, 0), in0=v3(y, 0), in1=hb(small[p], 0), op=ALU.subtract)
            kb.pool.tensor_tensor(out=tmp[p][:, :], in0=y[:, :], in1=y[:, :], op=ALU.mult)
            kb.dve.tensor_reduce(out=small[p][:, 4:8], in_=v3(tmp[p], 0), axis=AX.X, op=ALU.add)
            kb.dve.tensor_scalar(out=small[p][:, 4:8], in0=small[p][:, 4:8], scalar1=1.0 / 64, scalar2=64e-5, op0=ALU.mult, op1=ALU.add)
            kb.act.activation(out=small[p][:, 4:8], in_=small[p][:, 4:8], func=AF.Sqrt)
            kb.dve.reciprocal(out=small[p][:, 12:16], in_=small[p][:, 4:8])
            kb.dve.tensor_tensor(out=v3(y, 0), in0=v3(y, 0), in1=hb(small[p], 12), op=ALU.mult)
            kb.pool.tensor_tensor(out=y[:, :], in0=y[:, :], in1=lg_bc[:, :], op=ALU.mult)
            kb.pool.tensor_tensor(out=y[:, :], in0=y[:, :], in1=lb_bc[:, :], op=ALU.add)
            kb.dve.tensor_tensor(out=y[:, :], in0=y[:, :], in1=g_t[p][:, :], op=ALU.mult)
            kb.dma(out=scr.ymix.k(("c", c))[t0:t0 + CH, 512:768], in_=y[:, :])

        import os as _os
        nch = int(_os.environ.get('RWKV_NCH', S // CH))
        active = []
        nxt = 0
        while nxt < nch or active:
            if nxt < nch and len(active) < NBUF:
                active.append(chunk(nxt))
                nxt += 1
            for g in list(active):
                try:
                    next(g)
                except StopIteration:
                    active.remove(g)
        kb.barrier()
def build(depth=DEPTH, debug=None, stop_after=None, only=None):
    kb = KB()
    nc = kb.nc
    cx = Ctx()
    cx.P = {}
    x_in = kb.dram("x", [S, D], F32, kind="ExternalInput")
    for n, shp in PARAM_SHAPES.items():
        cx.P[n] = kb.dram(n, list(shp), F32, kind="ExternalInput")
    consts = make_consts()
    cx.C = {}
    for n, a in consts.items():
        cx.C[n] = kb.dram("c_" + n, list(a.shape), CONST_DT.get(n, F32), kind="ExternalInput")
    y_out = kb.dram("y", [S, D], F32, kind="ExternalOutput")
    scr = Ctx()
    cx.scr = scr
    dbg = debug or []

    def scratch(name, shape, dt):
        kind = "ExternalOutput" if name in dbg else "Internal"
        return kb.dram("scr_" + name, shape, dt, kind=kind)
    scr.qT = scratch("qT", [256, S], BF16)
    scr.kcvcT = scratch("kcvcT", [128, S], BF16)
    scr.ksT = scratch("ksT", [64, S], BF16)
    scr.kwT = scratch("kwT", [64, S], BF16)
    scr.dqT = scratch("dqT", [256, S], BF16)
    scr.dkT = scratch("dkT", [256, S], BF16)
    scr.loraT = scratch("loraT", [256, S], F32)
    scr.convT = scratch("convT", [512, S], F32)
    scr.tmA = scratch("tmA", [S, 204], F32)
    scr.tmB = scratch("tmB", [S, 1024], F32)
    scr.ymix = scratch("ymix", [S, D], F32)
    cx.xres = scratch("xres", [S, D], F32)
    outs = [y_out] + [getattr(scr, n) if hasattr(scr, n) else cx.xres for n in dbg]

    gst = ExitStack()
    cx.ident_bf = kb.sb(gst, "ident_bf", [128, 128], BF16)
    cx.ident_f = kb.sb(gst, "ident_f", [128, 128], F32)
    kb.dma(out=cx.ident_bf[:, :], in_=cx.C["ident_bf"][:, :])
    kb.dma(out=cx.ident_f[:, :], in_=cx.C["ident_f"][:, :])
    kb.dma(out=cx.xres[:, :], in_=x_in[:, :])

    for l in range(depth):
        phase_a(kb, cx, l)
        if stop_after == "a":
            break
        if only in (None, "conv"):
            phase_conv(kb, cx, l)
        if only in (None, "dil"):
            phase_dil(kb, cx, l)
        if only in (None, "nsa"):
            phase_nsa(kb, cx, l)
        if only in (None, "rwkv"):
            phase_rwkv(kb, cx, l)
        if stop_after == "mix":
            break
        phase_b(kb, cx, l)
        if stop_after == "b":
            break
        phase_c(kb, cx, l)
    if stop_after is None:
        phase_final(kb, cx, y_out)
    gst.close()
    kb.finish(outs)
    return kb, consts


_CACHE = {}


def kernel(**inputs):
    if "prog" not in _CACHE:
        _CACHE["prog"] = build()
    kb, consts = _CACHE["prog"]
    x = np.ascontiguousarray(inputs["x"], dtype=np.float32)
    in_maps = []
    for c in range(8):
        m = {"x": x[c]}
        for n in PARAM_SHAPES:
            m[n] = np.ascontiguousarray(inputs[n], dtype=np.float32)
        for n, a in consts.items():
            m["c_" + n] = a
        in_maps.append(m)
    res = run_bass_kernel_spmd(kb.nc, in_maps, core_ids=list(range(8)))
    return np.stack([res.results[c]["y"] for c in range(8)], axis=0)
```
